# Optimizing a Trainium2 kernel written in Bass

```python
import math
import jax, jax.numpy as jnp
from jax import lax
import numpy as np

D_MODEL = 1024
BATCH = 2
SEQ = 16384
DEPTH = 2

CTX_LEN = 256
GRID_W = 64
HEAD_DIM = 64
GROUP_WIDTH = D_MODEL // 4
QB = 128
ROPE_THETA = 10000.0
EPS = 1e-6
NEG = -1e30
F32 = jnp.float32

A_HEADS = GROUP_WIDTH // HEAD_DIM
A_QK = HEAD_DIM // 2
A_V = HEAD_DIM
B_HEADS = GROUP_WIDTH // HEAD_DIM
B_KV_HEADS = B_HEADS // 2
C_HEADS = GROUP_WIDTH // HEAD_DIM
C_Q_RANK = (3 * D_MODEL) // 16
C_KV_RANK = D_MODEL // 8
C_NOPE = HEAD_DIM
C_ROPE = HEAD_DIM // 2
C_V = HEAD_DIM
D_HEADS = GROUP_WIDTH // HEAD_DIM
D_KV_HEADS = D_HEADS // 2
WINDOW = 128
N_EXPERTS = 16
N_EXPERT_GROUPS = 4
EXPERTS_PER_GROUP = N_EXPERTS // N_EXPERT_GROUPS
TOP_K = 2
D_EXPERT = D_MODEL // 4
ALPHA = (2 * DEPTH) ** 0.25
BETA = (8 * DEPTH) ** -0.25

IN_SPLITS = (A_HEADS * 2 * A_QK, A_HEADS * 2 * A_QK, A_HEADS * A_V,
             B_HEADS * HEAD_DIM, B_KV_HEADS * HEAD_DIM, B_KV_HEADS * HEAD_DIM,
             C_Q_RANK, C_KV_RANK, C_ROPE,
             D_HEADS * HEAD_DIM, D_KV_HEADS * HEAD_DIM, D_KV_HEADS * HEAD_DIM)
IN_COLS = sum(IN_SPLITS)

kernel_name = 'hybrid_parallel_head_groups_dit_moe'


def layer_norm(x, g=None, b=None):
    xf = x.astype(F32)
    xc = xf - jnp.mean(xf, -1, keepdims=True)
    y = xc * lax.rsqrt(jnp.mean(xc * xc, -1, keepdims=True) + EPS)
    if g is not None:
        y = y * g.astype(F32) + b.astype(F32)
    return y.astype(x.dtype)


def rms_norm(x, g):
    xf = x.astype(F32)
    y = xf * lax.rsqrt(jnp.mean(xf * xf, -1, keepdims=True) + EPS) * g.astype(F32)
    return y.astype(x.dtype)


def modulate(x, shift, scale):
    return layer_norm(x) * (1 + scale) + shift


def axial_angles(row, col, rot_dim):
    n = rot_dim // 4
    inv = ROPE_THETA ** (-jnp.arange(n, dtype=F32) / n)
    return row.astype(F32)[:, None] * inv, col.astype(F32)[:, None] * inv


def rope_1d(x, ang):
    x1, x2 = jnp.split(x, 2, axis=-1)
    cos = jnp.cos(ang)[None, :, None, :]
    sin = jnp.sin(ang)[None, :, None, :]
    return jnp.concatenate([x1 * cos - x2 * sin, x2 * cos + x1 * sin], -1)


def rope_2d(x, angs):
    ang_r, ang_c = angs
    xf = x.astype(F32)
    half = x.shape[-1] // 2
    return jnp.concatenate([rope_1d(xf[..., :half], ang_r), rope_1d(xf[..., half:], ang_c)], -1).astype(x.dtype)


def sweep_query_blocks(block_fn, qs):
    b, n = qs[0].shape[:2]
    nb = n // QB
    blocks = tuple(jnp.moveaxis(q.reshape((b, nb, QB) + q.shape[2:]), 1, 0) for q in qs)
    out = jnp.moveaxis(lax.map(block_fn, blocks), 0, 1)
    return out.reshape((b, n) + out.shape[3:])


def gqa_core(q, k, v, scale, sink=None):
    s = jnp.einsum('bqgrd,bkgd->bgrqk', q.astype(F32), k.astype(F32)) * scale
    if sink is not None:
        sk = jnp.broadcast_to(sink.astype(F32)[None, :, :, None, None], s.shape[:-1] + (1,))
        p = jax.nn.softmax(jnp.concatenate([s, sk], -1), axis=-1)[..., :-1]
    else:
        p = jax.nn.softmax(s, axis=-1)
    return jnp.einsum('bgrqk,bkgd->bqgrd', p.astype(v.dtype), v)


def diff_core(q1, q2, k1, k2, v, lam, scale):
    p1 = jax.nn.softmax(jnp.einsum('bqhd,bkhd->bhqk', q1.astype(F32), k1.astype(F32)) * scale, axis=-1)
    p2 = jax.nn.softmax(jnp.einsum('bqhd,bkhd->bhqk', q2.astype(F32), k2.astype(F32)) * scale, axis=-1)
    return jnp.einsum('bhqk,bkhd->bqhd', (p1 - lam * p2).astype(v.dtype), v)


def mixer_diff(p_lat, p_ctx, angs, lq1, lk1, lq2, lk2, subln_g, layer_idx, need_ctx):
    lam_init = 0.8 - 0.6 * math.exp(-0.3 * layer_idx)
    lam = (jnp.exp(jnp.sum(lq1.astype(F32) * lk1.astype(F32)))
           - jnp.exp(jnp.sum(lq2.astype(F32) * lk2.astype(F32))) + lam_init)
    scale = A_QK ** -0.5

    def heads(q, k, v):
        b, n = q.shape[:2]
        q = q.reshape(b, n, A_HEADS, 2, A_QK)
        k = k.reshape(b, n, A_HEADS, 2, A_QK)
        return q[..., 0, :], q[..., 1, :], k[..., 0, :], k[..., 1, :], v.reshape(b, n, A_HEADS, A_V)

    def post(o):
        o = rms_norm(o, subln_g) * (1.0 - lam_init)
        return o.reshape(o.shape[0], o.shape[1], GROUP_WIDTH)

    q1, q2, k1, k2, v = heads(*p_lat)
    q1, q2, k1, k2 = [rope_2d(t, angs) for t in (q1, q2, k1, k2)]
    cq1, cq2, ck1, ck2, cv = heads(*p_ctx)
    k1a = jnp.concatenate([ck1, k1], axis=1)
    k2a = jnp.concatenate([ck2, k2], axis=1)
    va = jnp.concatenate([cv, v], axis=1)
    o_lat = sweep_query_blocks(lambda qb: diff_core(qb[0], qb[1], k1a, k2a, va, lam, scale), (q1, q2))
    o_ctx = post(diff_core(cq1, cq2, ck1, ck2, cv, lam, scale)) if need_ctx else None
    return post(o_lat), o_ctx


def mixer_gqa(p_lat, p_ctx, angs, qn_g, kn_g, need_ctx):
    rep = B_HEADS // B_KV_HEADS
    scale = HEAD_DIM ** -0.5

    def heads(q, k, v):
        b, n = q.shape[:2]
        q = rms_norm(q.reshape(b, n, B_HEADS, HEAD_DIM), qn_g)
        k = rms_norm(k.reshape(b, n, B_KV_HEADS, HEAD_DIM), kn_g)
        return q, k, v.reshape(b, n, B_KV_HEADS, HEAD_DIM)

    def group(q):
        return q.reshape(q.shape[0], q.shape[1], B_KV_HEADS, rep, HEAD_DIM)

    def flat(o):
        return o.reshape(o.shape[0], o.shape[1], GROUP_WIDTH)

    q, k, v = heads(*p_lat)
    q, k = rope_2d(q, angs), rope_2d(k, angs)
    cq, ck, cv = heads(*p_ctx)
    ka = jnp.concatenate([ck, k], axis=1)
    va = jnp.concatenate([cv, v], axis=1)
    o_lat = sweep_query_blocks(lambda qb: gqa_core(qb[0], ka, va, scale), (group(q),))
    o_ctx = flat(gqa_core(group(cq), ck, cv, scale)) if need_ctx else None
    return flat(o_lat), o_ctx


def mixer_mla(p_lat, p_ctx, angs, qn_g, kvn_g, w_uq, w_ukv, need_ctx):
    scale = (C_NOPE + C_ROPE) ** -0.5

    def heads(cq, ckv, kr, rotate):
        b, n = cq.shape[:2]
        q = (rms_norm(cq, qn_g) @ w_uq).reshape(b, n, C_HEADS, C_NOPE + C_ROPE)
        kv = (rms_norm(ckv, kvn_g) @ w_ukv).reshape(b, n, C_HEADS, C_NOPE + C_V)
        q_nope, q_rope = q[..., :C_NOPE], q[..., C_NOPE:]
        k_nope, v = kv[..., :C_NOPE], kv[..., C_NOPE:]
        k_rope = kr[:, :, None, :]
        if rotate:
            q_rope, k_rope = rope_2d(q_rope, angs), rope_2d(k_rope, angs)
        q = jnp.concatenate([q_nope, q_rope], -1)[:, :, :, None, :]
        k = jnp.concatenate([k_nope, jnp.broadcast_to(k_rope, k_nope.shape[:-1] + (C_ROPE,))], -1)
        return q, k, v

    def flat(o):
        return o.reshape(o.shape[0], o.shape[1], GROUP_WIDTH)

    q, k, v = heads(*p_lat, True)
    cq, ck, cv = heads(*p_ctx, False)
    ka = jnp.concatenate([ck, k], axis=1)
    va = jnp.concatenate([cv, v], axis=1)
    o_lat = sweep_query_blocks(lambda qb: gqa_core(qb[0], ka, va, scale), (q,))
    o_ctx = flat(gqa_core(cq, ck, cv, scale)) if need_ctx else None
    return flat(o_lat), o_ctx


def mixer_swa(p_lat, p_ctx, angs, sink, need_ctx):
    rep = D_HEADS // D_KV_HEADS
    scale = HEAD_DIM ** -0.5
    sink_gr = sink.reshape(D_KV_HEADS, rep)

    def heads(q, k, v):
        b, n = q.shape[:2]
        return (q.reshape(b, n, D_HEADS, HEAD_DIM), k.reshape(b, n, D_KV_HEADS, HEAD_DIM),
                v.reshape(b, n, D_KV_HEADS, HEAD_DIM))

    q, k, v = heads(*p_lat)
    q, k = rope_2d(q, angs), rope_2d(k, angs)
    cq, ck, cv = heads(*p_ctx)
    b, n = q.shape[:2]
    nb = n // QB
    qb = q.reshape(b, nb, QB, D_KV_HEADS, rep, HEAD_DIM)

    def band(t):
        tp = jnp.pad(t, ((0, 0), (WINDOW, WINDOW), (0, 0), (0, 0)))
        tb = tp.reshape(b, nb + 2, QB, D_KV_HEADS, t.shape[-1])
        return jnp.concatenate([tb[:, :-2], tb[:, 1:-1], tb[:, 2:]], axis=2)

    kband, vband = band(k), band(v)
    blk = jnp.arange(nb, dtype=jnp.int32)[:, None]
    tq = blk * QB + jnp.arange(QB, dtype=jnp.int32)[None, :]
    tk = (blk - 1) * QB + jnp.arange(3 * QB, dtype=jnp.int32)[None, :]
    valid = ((jnp.abs(tq[:, :, None] - tk[:, None, :]) <= WINDOW)
             & (tk[:, None, :] >= 0) & (tk[:, None, :] < n))
    s_band = jnp.einsum('bnqgrd,bnkgd->bngrqk', qb.astype(F32), kband.astype(F32)) * scale
    s_band = jnp.where(valid[None, :, None, None, :, :], s_band, NEG)
    s_ctx = jnp.einsum('bnqgrd,bcgd->bngrqc', qb.astype(F32), ck.astype(F32)) * scale
    s_sink = jnp.broadcast_to(sink_gr.astype(F32)[None, None, :, :, None, None], s_band.shape[:-1] + (1,))
    p = jax.nn.softmax(jnp.concatenate([s_ctx, s_band, s_sink], -1), axis=-1)
    c_len = ck.shape[1]
    o = (jnp.einsum('bngrqc,bcgd->bnqgrd', p[..., :c_len].astype(v.dtype), cv)
         + jnp.einsum('bngrqk,bnkgd->bnqgrd', p[..., c_len:c_len + 3 * QB].astype(v.dtype), vband))
    o_lat = o.reshape(b, n, GROUP_WIDTH)
    o_ctx = None
    if need_ctx:
        cqg = cq.reshape(cq.shape[0], cq.shape[1], D_KV_HEADS, rep, HEAD_DIM)
        o_ctx = gqa_core(cqg, ck, cv, scale, sink=sink_gr).reshape(cq.shape[0], cq.shape[1], GROUP_WIDTH)
    return o_lat, o_ctx


def moe(h, router_w, router_bias, w_gate, w_up, w_down):
    s = jax.nn.sigmoid(jnp.einsum('btd,de->bte', h.astype(F32), router_w.astype(F32)))
    sel = s + router_bias.astype(F32)
    grp = sel.reshape(sel.shape[:-1] + (N_EXPERT_GROUPS, EXPERTS_PER_GROUP))
    g_best = jnp.argmax(lax.top_k(grp, TOP_K)[0].sum(-1), axis=-1)
    expert_group = jnp.arange(N_EXPERTS, dtype=jnp.int32) // EXPERTS_PER_GROUP
    masked = jnp.where(expert_group == g_best[..., None], sel, -jnp.inf)
    _, idx = lax.top_k(masked, TOP_K)
    w = jnp.take_along_axis(s, idx, axis=-1)
    w = w / jnp.sum(w, -1, keepdims=True)
    gates = jnp.sum(jax.nn.one_hot(idx, N_EXPERTS, dtype=F32) * w[..., None], axis=-2)
    hg = jnp.einsum('btd,edf->btef', h, w_gate)
    hu = jnp.einsum('btd,edf->btef', h, w_up)
    act = jax.nn.silu(hg) * hu * gates[..., None].astype(h.dtype)
    return jnp.einsum('btef,efd->btd', act, w_down)


def setup_inputs(seed: int = 0) -> dict:
    key = jax.random.key(seed)
    ks = jax.random.split(key, 29)

    def nrm(k, shape, scale):
        return jax.random.normal(k, shape, F32) * scale

    def gain(k, shape):
        return 1.0 + 0.02 * jax.random.normal(k, shape, F32)

    return {
        'x': nrm(ks[0], (BATCH, SEQ, D_MODEL), 1.0),
        'c': nrm(ks[1], (BATCH, D_MODEL), 1.0),
        'ctx': nrm(ks[2], (BATCH, CTX_LEN, D_MODEL), 1.0),
        'c_ctx': nrm(ks[3], (D_MODEL,), 1.0),
        'w_ada': nrm(ks[4], (DEPTH, D_MODEL, 6 * D_MODEL), 0.5 * D_MODEL ** -0.5),
        'b_ada': nrm(ks[5], (DEPTH, 6 * D_MODEL), 0.02),
        'w_in': nrm(ks[6], (DEPTH, D_MODEL, IN_COLS), D_MODEL ** -0.5),
        'w_out': nrm(ks[7], (DEPTH, D_MODEL, D_MODEL), BETA * D_MODEL ** -0.5),
        'diff_lambda_q1': nrm(ks[8], (DEPTH, A_QK), 0.1),
        'diff_lambda_k1': nrm(ks[9], (DEPTH, A_QK), 0.1),
        'diff_lambda_q2': nrm(ks[10], (DEPTH, A_QK), 0.1),
        'diff_lambda_k2': nrm(ks[11], (DEPTH, A_QK), 0.1),
        'diff_subln_g': gain(ks[12], (DEPTH, A_V)),
        'gqa_q_norm_g': gain(ks[13], (DEPTH, HEAD_DIM)),
        'gqa_k_norm_g': gain(ks[14], (DEPTH, HEAD_DIM)),
        'mla_q_norm_g': gain(ks[15], (DEPTH, C_Q_RANK)),
        'mla_kv_norm_g': gain(ks[16], (DEPTH, C_KV_RANK)),
        'mla_w_uq': nrm(ks[17], (DEPTH, C_Q_RANK, C_HEADS * (C_NOPE + C_ROPE)), C_Q_RANK ** -0.5),
        'mla_w_ukv': nrm(ks[18], (DEPTH, C_KV_RANK, C_HEADS * (C_NOPE + C_V)), C_KV_RANK ** -0.5),
        'swa_sink': nrm(ks[19], (DEPTH, D_HEADS), 0.5),
        'ln1_g': gain(ks[20], (DEPTH, D_MODEL)),
        'ln1_b': nrm(ks[21], (DEPTH, D_MODEL), 0.02),
        'ln2_g': gain(ks[22], (DEPTH, D_MODEL)),
        'ln2_b': nrm(ks[23], (DEPTH, D_MODEL), 0.02),
        'router_w': nrm(ks[24], (D_MODEL, N_EXPERTS), D_MODEL ** -0.5),
        'router_bias': nrm(ks[25], (N_EXPERTS,), 0.01),
        'exp_w_gate': nrm(ks[26], (DEPTH, N_EXPERTS, D_MODEL, D_EXPERT), D_MODEL ** -0.5),
        'exp_w_up': nrm(ks[27], (DEPTH, N_EXPERTS, D_MODEL, D_EXPERT), D_MODEL ** -0.5),
        'exp_w_down': nrm(ks[28], (DEPTH, N_EXPERTS, D_EXPERT, D_MODEL), BETA * D_EXPERT ** -0.5),
    }


def reference(x, c, ctx, c_ctx, w_ada, b_ada, w_in, w_out,
              diff_lambda_q1, diff_lambda_k1, diff_lambda_q2, diff_lambda_k2, diff_subln_g,
              gqa_q_norm_g, gqa_k_norm_g, mla_q_norm_g, mla_kv_norm_g, mla_w_uq, mla_w_ukv,
              swa_sink, ln1_g, ln1_b, ln2_g, ln2_b, router_w, router_bias,
              exp_w_gate, exp_w_up, exp_w_down):
    n = x.shape[1]
    ROWS = n // GRID_W
    row = jnp.repeat(jnp.arange(ROWS, dtype=jnp.int32), GRID_W)
    col = jnp.tile(jnp.arange(GRID_W, dtype=jnp.int32), ROWS)
    angs32 = axial_angles(row, col, A_QK)
    angs64 = axial_angles(row, col, HEAD_DIM)
    split_idx = tuple(int(i) for i in np.cumsum(IN_SPLITS)[:-1])

    xl, xc = x, ctx
    for l in range(DEPTH):
        need_ctx = l < DEPTH - 1
        sh1, sc1, g1, sh2, sc2, g2 = [m[:, None, :] for m in
                                      jnp.split(jax.nn.silu(c) @ w_ada[l] + b_ada[l], 6, axis=-1)]
        csh1, csc1, cg1, csh2, csc2, cg2 = jnp.split(jax.nn.silu(c_ctx) @ w_ada[l] + b_ada[l], 6, axis=-1)

        pl = jnp.split(modulate(xl, sh1, sc1) @ w_in[l], split_idx, axis=-1)
        pc = jnp.split(modulate(xc, csh1, csc1) @ w_in[l], split_idx, axis=-1)
        oa_l, oa_c = mixer_diff(pl[0:3], pc[0:3], angs32, diff_lambda_q1[l], diff_lambda_k1[l],
                                diff_lambda_q2[l], diff_lambda_k2[l], diff_subln_g[l], l, need_ctx)
        ob_l, ob_c = mixer_gqa(pl[3:6], pc[3:6], angs64, gqa_q_norm_g[l], gqa_k_norm_g[l], need_ctx)
        oc_l, oc_c = mixer_mla(pl[6:9], pc[6:9], angs32, mla_q_norm_g[l], mla_kv_norm_g[l],
                               mla_w_uq[l], mla_w_ukv[l], need_ctx)
        od_l, od_c = mixer_swa(pl[9:12], pc[9:12], angs64, swa_sink[l], need_ctx)
        yl = jnp.concatenate([oa_l, ob_l, oc_l, od_l], axis=-1) @ w_out[l]
        xl = layer_norm(ALPHA * xl + g1 * yl, ln1_g[l], ln1_b[l])

        yl = moe(modulate(xl, sh2, sc2), router_w, router_bias, exp_w_gate[l], exp_w_up[l], exp_w_down[l])
        xl = layer_norm(ALPHA * xl + g2 * yl, ln2_g[l], ln2_b[l])

        if need_ctx:
            yc = jnp.concatenate([oa_c, ob_c, oc_c, od_c], axis=-1) @ w_out[l]
            xc = layer_norm(ALPHA * xc + cg1 * yc, ln1_g[l], ln1_b[l])
            yc = moe(modulate(xc, csh2, csc2), router_w, router_bias, exp_w_gate[l], exp_w_up[l], exp_w_down[l])
            xc = layer_norm(ALPHA * xc + cg2 * yc, ln2_g[l], ln2_b[l])
    return xl
```

```python
import numpy as np
from contextlib import ExitStack
import concourse.bass as bass
import concourse.mybir as mybir
from concourse.bass_utils import run_bass_kernel_spmd

F32 = mybir.dt.float32
BF16 = mybir.dt.bfloat16
AF = mybir.ActivationFunctionType
ALU = mybir.AluOpType
AX = mybir.AxisListType

D = 1024
BATCH = 2
SEQ = 16384
DEPTH = 2
CTX = 256
NCORES = 8
TOK = SEQ // 4
NT = TOK // 128
NE = 16
DE = 256
EPS = 1e-6
ALPHA = (2 * DEPTH) ** 0.25
IN_COLS = 2144


class Buf:
    __slots__ = ("name", "t", "w", "r")

    _uid = [0]

    def __init__(self, name, t):
        Buf._uid[0] += 1
        self.name = "%s.%d" % (name, Buf._uid[0])
        self.t = t
        self.w = {}
        self.r = {}

    def __getitem__(self, idx):
        return self.t[idx]


class Rec:
    ENGS = ("pe", "act", "dve", "pool", "sp")

    def __init__(self, nc, stack):
        self.nc = nc
        self.stack = stack
        self.ops = {e: [] for e in self.ENGS}
        self.sems = {}
        self.cnt = {}
        self.seen = {e: {} for e in self.ENGS}
        self.nbuf = 0
        for e in self.ENGS:
            self._sem("E_" + e)

    def _sem(self, key):
        if key not in self.sems:
            if key.startswith("D_H_") and getattr(self, "free", None):
                sem, c0 = self.free.pop()
                self.sems[key] = sem
                self.cnt[key] = c0
            else:
                self.nsem = getattr(self, "nsem", 0) + 1
                self.sems[key] = self.stack.enter_context(self.nc.semaphore("s%d" % self.nsem))
                self.cnt[key] = 0
        return self.sems[key]

    def release_dma_sems(self):
        if not hasattr(self, "free"):
            self.free = []
        for key in [k for k in self.sems if k.startswith("D_")]:
            sem, c = self.sems.pop(key), self.cnt.pop(key)
            if key.startswith("D_H_"):
                self.free.append((sem, c))
            for e in self.ENGS:
                self.seen[e].pop(key, None)

    def collective_piece(self, fn, out_buf):
        key = "E_cc"
        self._sem(key)
        self.cnt[key] += 1
        self.ops["pool"].append(([], fn, self.sems[key], 1))
        out_buf.w = {key: (self.cnt[key], "cc")}
        out_buf.r = {}

    def collective(self, fn):
        self.barrier()
        key = "E_cc"
        self._sem(key)
        self.cnt[key] += 1
        self.ops["pool"].append(([], fn, self.sems[key], 1))
        self.barrier()

    def buf(self, name, t):
        self.nbuf += 1
        return Buf("%s#%d" % (name, self.nbuf), t)

    def sb(self, name, shape, dtype, stack=None):
        st = stack if stack is not None else self.stack
        self.nbuf += 1
        t = st.enter_context(self.nc.sbuf_tensor("%s_%d" % (name, self.nbuf), list(shape), dtype))
        return Buf(name, t)

    def ps(self, name, shape, dtype, stack=None):
        st = stack if stack is not None else self.stack
        self.nbuf += 1
        t = st.enter_context(self.nc.psum_tensor("%s_%d" % (name, self.nbuf), list(shape), dtype))
        return Buf(name, t)

    def _collect(self, eng, reads, writes, is_dma):
        need = {}

        def add(d):
            for k, (v, pe) in d.items():
                if k not in self.sems:
                    continue
                if (not is_dma) and eng == "pe" and pe == "pe" and k == "E_pe":
                    continue
                if need.get(k, 0) < v:
                    need[k] = v
        for b in reads:
            add(b.w)
        for b in writes:
            add(b.w)
            add(b.r)
        waits = []
        seen = self.seen[eng]
        for k, v in need.items():
            if seen.get(k, 0) < v:
                seen[k] = v
                waits.append((self.sems[k], v))
        return waits

    def op(self, eng, fn, reads=(), writes=()):
        waits = self._collect(eng, reads, writes, False)
        key = "E_" + eng
        self.cnt[key] += 1
        tk = (self.cnt[key], eng)
        self.ops[eng].append((waits, fn, self.sems[key], 1))
        for b in reads:
            b.r[key] = tk
        for b in writes:
            b.w = {key: tk}
            b.r = {}

    def dma(self, eng, out_ap, in_ap, reads=(), writes=(), key=None, cast=False):
        if key is None:
            b0 = (list(writes) + list(reads))[0]
            key = b0.name + ("_w" if writes else "_r")
        key = ("D_P_" if eng == "pool" else "D_H_") + key
        self._sem(key)
        waits = self._collect(eng, reads, writes, True)
        self.cnt[key] += 16
        tk = (self.cnt[key], "dma")
        if cast:
            self.ops[eng].append((waits, lambda e, o=out_ap, i=in_ap: e.dma_start(out=o, in_=i, max_dma_last_dim=4096), self.sems[key], 16))
        else:
            self.ops[eng].append((waits, lambda e, o=out_ap, i=in_ap: e.dma_start(out=o, in_=i), self.sems[key], 16))
        for b in reads:
            b.r[key] = tk
        for b in writes:
            b.w = {key: tk}
            b.r = {}

    def barrier(self):
        for e in self.ENGS:
            waits = []
            for k, v in self.cnt.items():
                if v > 0 and self.seen[e].get(k, 0) < v:
                    self.seen[e][k] = v
                    waits.append((self.sems[k], v))
            if waits:
                self.ops[e].append((waits, None, None, 0))

    def wait_all_on(self, eng):
        waits = []
        for k, v in self.cnt.items():
            if v > 0 and self.seen[eng].get(k, 0) < v:
                self.seen[eng][k] = v
                waits.append((self.sems[k], v))
        if waits:
            self.ops[eng].append((waits, None, None, 0))

    def emit(self):
        nc = self.nc
        block = self.stack.enter_context(nc.Block())

        def run(e, ops):
            for waits, fn, sem, inc in ops:
                for s, v in waits:
                    e.wait_ge(s, v)
                if fn is not None:
                    fn(e).then_inc(sem, inc)

        @block.tensor
        def _(e):
            run(e, self.ops["pe"])

        @block.scalar
        def _(e):
            run(e, self.ops["act"])

        @block.vector
        def _(e):
            run(e, self.ops["dve"])

        @block.gpsimd
        def _(e):
            run(e, self.ops["pool"])

        @block.sync
        def _(e):
            run(e, self.ops["sp"])


def _bc(ap, shape):
    return ap.to_broadcast(list(shape))


class Ctx:
    def __init__(self):
        self.nc = bass.Bass("TRN2", target_bir_lowering=False)
        self.stack = ExitStack()
        self.rec = Rec(self.nc, self.stack)
        self.dram = {}

    sfx = ""
    shared = ("c_ident", "cT", "rope_tab", "c_masks", "router_w", "router_bias", "x_own", "xc", "onehot")

    def din(self, name, shape, dtype=F32):
        if name not in self.shared:
            name = name + self.sfx
        if name in self.dram:
            return self.dram[name]
        t = self.nc.dram_tensor(name, list(shape), dtype, kind="ExternalInput")
        b = Buf(name, t.ap())
        self.dram[name] = b
        return b

    def dout(self, name, shape, dtype=F32):
        t = self.nc.dram_tensor(name, list(shape), dtype, kind="ExternalOutput")
        b = Buf(name, t.ap())
        self.dram[name] = b
        return b

    def dint(self, name, shape, dtype=F32):
        t = self.nc.dram_tensor(name, list(shape), dtype)
        b = Buf(name, t.ap())
        self.dram[name] = b
        return b


def _in_perm():
    o = {}
    off = 0
    for name, n in (("Aq", 256), ("Ak", 256), ("Av", 256), ("Bq", 256), ("Bk", 128), ("Bv", 128),
                    ("Ccq", 192), ("Cckv", 128), ("Ckr", 32), ("Dq", 256), ("Dk", 128), ("Dv", 128)):
        o[name] = np.arange(off, off + n)
        off += n
    order = ["Aq", "Ak", "Bq", "Bk", "Dk", "Dq", "Ckr", "Av", "Bv", "Dv", "Ccq", "Cckv"]
    return np.concatenate([o[k] for k in order])


GOFF = (0, 512, 1024, 1312, 1824)
GW = (512, 512, 288, 512, 320)


def rope_tables(tok_idx):
    row = (tok_idx // 64).astype(np.float64)
    col = (tok_idx % 64).astype(np.float64)

    def tab(n):
        inv = 10000.0 ** (-np.arange(n, dtype=np.float32).astype(np.float64) / n)
        inv = (np.float32(10000.0) ** (-(np.arange(n, dtype=np.float32) / np.float32(n)))).astype(np.float32)
        ar = (row.astype(np.float32)[:, None] * inv).astype(np.float32)
        ac = (col.astype(np.float32)[:, None] * inv).astype(np.float32)
        cr, sr, cc, sc = np.cos(ar), np.sin(ar), np.cos(ac), np.sin(ac)
        cos = np.concatenate([cr, cr, cc, cc], 1)
        sin = np.concatenate([-sr, sr, -sc, sc], 1)
        return cos.astype(np.float32), sin.astype(np.float32)
    c32, s32 = tab(8)
    c64, s64 = tab(16)
    return np.ascontiguousarray(np.concatenate([c32, s32, c64, s64], 1), dtype=np.float32)


def setup_consts(cx, L):
    rec, nc = cx.rec, cx.nc
    L.ident_f = rec.sb("identf", [128, 128], F32)
    L.ident_b = rec.sb("identb", [128, 128], BF16)
    L.eps = rec.sb("eps", [128, 1], F32)
    L.ones64 = rec.sb("ones64", [64, 64], F32)
    idn = cx.din("c_ident", [128, 128], F32)
    rec.dma("sp", L.ident_f[:, :], idn[:, :], writes=[L.ident_f])
    rec.op("dve", lambda e: e.tensor_copy(out=L.ident_b[:, :], in_=L.ident_f[:, :]), reads=[L.ident_f], writes=[L.ident_b])
    rec.op("dve", lambda e: e.memset(L.eps[:, :], EPS), writes=[L.eps])
    rec.op("dve", lambda e: e.memset(L.ones64[:, :], 1.0 / 64.0), writes=[L.ones64])


class NS:
    pass


DBG = {"stop": 99, "att": 9, "kinds": "ABCD"}


class Ring:
    def __init__(self, bufs):
        self.bufs = bufs
        self.i = 0

    def next(self):
        b = self.bufs[self.i % len(self.bufs)]
        self.i += 1
        return b


def rope_ops(rec, eng, src_buf, src, nv, R, tab_buf, cos, sin, t1b, t2b):
    h = R // 4
    n = nv * R
    t1 = t1b[:, 0:n]
    t2 = t2b[:, 0:n]
    rec.op(eng, lambda e: e.tensor_tensor(out=t1.rearrange("p (v r) -> p v r", v=nv), in0=src.rearrange("p (v r) -> p v r", v=nv),
                                          in1=cos.unsqueeze(1).to_broadcast([128, nv, R]), op=ALU.mult),
           reads=[src_buf, tab_buf], writes=[t1b])
    s5 = src.rearrange("p (v a b h) -> p v a b h", v=nv, a=2, b=2, h=h)
    t5 = t2.rearrange("p (v a b h) -> p v a b h", v=nv, a=2, b=2, h=h)
    sn = sin.rearrange("p (a b h) -> p a b h", a=2, b=2, h=h)
    for b in (0, 1):
        rec.op(eng, lambda e, b=b: e.tensor_tensor(out=t5[:, :, :, b, :], in0=s5[:, :, :, 1 - b, :],
                                                   in1=sn[:, :, b, :].unsqueeze(1).to_broadcast([128, nv, 2, h]), op=ALU.mult),
               reads=[src_buf, tab_buf], writes=[t2b])
    rec.op(eng, lambda e: e.tensor_tensor(out=t1, in0=t1, in1=t2, op=ALU.add), reads=[t2b], writes=[t1b])
    return t1


def layer_setup(cx, L, st, pst=None):
    rec = cx.rec
    cT = cx.din("cT", [128, 8, 2])
    wada = cx.din("w_ada", [128, 8, 6144])
    badaT = cx.din("b_adaT", [128, 48])
    bada = cx.din("b_ada", [6144])
    if pst != "prealloc":
        L.modT = rec.sb("modT", [128, 48, 2], F32, pst)
        L.Gb = [[rec.sb("Gb", [128, 1024], F32, pst) for _ in range(2)] for _ in range(2)]
    sl0 = rec.sb("sl0", [128, 8, 2], F32, st)
    sl = rec.sb("sl", [128, 8, 2], F32, st)
    slrep = rec.sb("slrep", [128, 8, 2, 128], F32, st)
    bT = rec.sb("bT", [128, 48], F32, st)
    was = Ring([rec.sb("wa", [128, 8, 512], F32, st) for _ in range(2)])
    bbs = Ring([rec.sb("bb", [128, 512], F32, st) for _ in range(2)])
    psA = rec.ps("psA", [128, 512], F32, st)
    psB = Ring([rec.ps("psB", [128, 512], F32, st) for _ in range(2)])
    rec.op("dve", lambda e: e.memset(L.modT[:, :, :], 0.0), writes=[L.modT])
    rec.dma("sp", sl0[:, :, :], cT[:, :, :], writes=[sl0])
    rec.dma("sp", bT[:, :], badaT[:, :], writes=[bT])
    rec.op("act", lambda e: e.activation(out=sl[:, :, :], in_=sl0[:, :, :], func=AF.Silu), reads=[sl0], writes=[sl])
    rec.op("dve", lambda e: e.tensor_copy(out=slrep[:, :, :, :], in_=sl[:, :, :].unsqueeze(3).to_broadcast([128, 8, 2, 128])),
           reads=[sl], writes=[slrep])
    for gi in range(12):
        wa = was.next()
        rec.dma("sp", wa[:, :, :], wada[:, :, gi * 512:(gi + 1) * 512], writes=[wa])
        v = gi // 2
        if v in (2, 5):
            bb = bbs.next()
            rec.dma("pool", bb[:, :], bada[gi * 512:(gi + 1) * 512].partition_broadcast(128), writes=[bb])
            for cond in range(2):
                ps = psB.next()
                for kc in range(8):
                    rec.op("pe", lambda e, kc=kc, cond=cond, ps=ps, wa=wa: e.matmul(ps[:, :], lhsT=slrep[:, kc, cond, :], rhs=wa[:, kc, :],
                                                                                 start=(kc == 0), stop=(kc == 7)),
                           reads=[slrep, wa], writes=[ps])
                gb = L.Gb[0 if v == 2 else 1][cond]
                rec.op("dve", lambda e, ps=ps, gb=gb, bb=bb, gi=gi: e.tensor_tensor(out=gb[:, (gi % 2) * 512:(gi % 2 + 1) * 512], in0=ps[:, :],
                                                                                 in1=bb[:, :], op=ALU.add),
                       reads=[ps, bb], writes=[gb])
        else:
            for jj in range(4):
                for kc in range(8):
                    rec.op("pe", lambda e, kc=kc, jj=jj, wa=wa: e.matmul(psA[:, jj * 2:jj * 2 + 2], lhsT=wa[:, kc, jj * 128:(jj + 1) * 128],
                                                                       rhs=sl[:, kc, :], start=(kc == 0), stop=(kc == 7)),
                           reads=[sl, wa], writes=[psA])
            rec.op("dve", lambda e, gi=gi: e.tensor_tensor(out=L.modT[:, gi * 4:gi * 4 + 4, :],
                                                           in0=psA[:, 0:8].rearrange("p (j c) -> p j c", c=2),
                                                           in1=bT[:, gi * 4:gi * 4 + 4].unsqueeze(2).to_broadcast([128, 4, 2]), op=ALU.add),
                   reads=[psA, bT], writes=[L.modT])
    for lo in (8, 32):
        rec.op("dve", lambda e, lo=lo: e.tensor_scalar(out=L.modT[:, lo:lo + 8, :], in0=L.modT[:, lo:lo + 8, :], scalar1=1.0, scalar2=None,
                                                       op0=ALU.add), reads=[L.modT], writes=[L.modT])


def phase1(cx, L, st, x_src, xc_src, tab_src, outs, n_lat_tiles=NT, do_ctx=True):
    rec = cx.rec
    win_d = cx.din("w_in", [128, 8, IN_COLS])
    wuq_d = cx.din("w_uq", [128, 2, 384])
    wukv_d = cx.din("w_ukv", [128, 512])
    g1_d = cx.din("gain_g1", [512])
    gc_d = cx.din("gain_c", [320])
    win = rec.sb("win", [128, 8, IN_COLS], BF16, st)
    wuq = rec.sb("wuq", [128, 2, 384], BF16, st)
    wukv = rec.sb("wukv", [128, 512], BF16, st)
    for kc in range(8):
        rec.dma("pool", win[:, kc, :], win_d[:, kc, :], writes=[win], cast=True)
    rec.dma("pool", wuq[:, :, :], wuq_d[:, :, :], writes=[wuq], cast=True)
    rec.dma("pool", wukv[:, :], wukv_d[:, :], writes=[wukv], cast=True)
    G1t = rec.sb("G1t", [128, 512], F32, st)
    Gct = rec.sb("Gct", [128, 320], F32, st)
    rec.dma("sp", G1t[:, :], g1_d[0:512].partition_broadcast(128), writes=[G1t])
    rec.dma("sp", Gct[:, :], gc_d[0:320].partition_broadcast(128), writes=[Gct])
    xts = Ring([rec.sb("xt", [128, 1024], F32, st) for _ in range(2)])
    tabs = Ring([rec.sb("tab", [128, 192], F32, st) for _ in range(2)])
    st6 = rec.sb("st6", [128, 2, 6], F32, st)
    mv = rec.sb("mv", [128, 2], F32, st)
    sq1 = rec.sb("sq1", [128, 1], F32, st)
    rstd = rec.sb("rstd", [128, 1], F32, st)
    xn = rec.sb("xn", [128, 1024], BF16, st)
    hT = rec.sb("hT", [128, 8, 128], BF16, st)
    t1b = rec.sb("t1b", [128, 512], F32, st)
    t2b = rec.sb("t2b", [128, 512], F32, st)
    xg = rec.sb("xg", [128, 512], F32, st)
    sqt = rec.sb("sqt", [128, 512], F32, st)
    ss8 = rec.sb("ss8", [128, 8], F32, st)
    rs8 = rec.sb("rs8", [128, 8], F32, st)
    ssc = rec.sb("ssc", [128, 2], F32, st)
    rsc = rec.sb("rsc", [128, 2], F32, st)
    cn = rec.sb("cn", [128, 320], BF16, st)
    cnT = rec.sb("cnT", [128, 3, 128], BF16, st)
    qk = rec.sb("qk", [128, 2, 1280], BF16, st)
    vts = Ring([rec.sb("vt", [128, 12, 128], BF16, st) for _ in range(2)])
    qkT = Ring([rec.sb("qkT", [128, 2, 10, 512], BF16, st) for _ in range(2)])
    psT = rec.ps("psT", [128, 1024], BF16, st)
    psG = [rec.ps("psG", [128, 512], F32, st) for _ in range(5)]
    psU = [rec.ps("psU", [128, 512], F32, st) for _ in range(2)]
    for vt in vts.bufs:
        rec.op("pool", lambda e, vt=vt: e.memset(vt[:, :, 64:128], 1.0), writes=[vt])
    rec.op("pool", lambda e: e.memset(qk[:, :, :], 0.0), writes=[qk])

    def tile(src_ap, src_buf, tab_ap, cond, qkt, col, vdst_ap, vdst_buf):
        xt = xts.next()
        tab = tabs.next()
        vt = vts.next()
        rec.dma("sp", xt[:, :], src_ap, writes=[xt])
        rec.dma("sp", tab[:, :], tab_ap, writes=[tab])
        c32, s32, c64, s64 = tab[:, 0:32], tab[:, 32:64], tab[:, 64:128], tab[:, 128:192]
        for i in range(2):
            rec.op("dve", lambda e, i=i: e.bn_stats(out=st6[:, i, :], in_=xt[:, i * 512:(i + 1) * 512]), reads=[xt], writes=[st6])
        rec.op("dve", lambda e: e.bn_aggr(out=mv[:, :], in_=st6[:, :, :].rearrange("p a b -> p (a b)")), reads=[st6], writes=[mv])
        rec.op("act", lambda e: e.activation(out=sq1[:, :], in_=mv[:, 1:2], func=AF.Sqrt, bias=L.eps[:, 0:1], scale=1.0),
               reads=[mv, L.eps], writes=[sq1])
        rec.op("dve", lambda e: e.reciprocal(out=rstd[:, :], in_=sq1[:, :]), reads=[sq1], writes=[rstd])
        rec.op("dve", lambda e: e.tensor_scalar(out=xn[:, :], in0=xt[:, :], scalar1=mv[:, 0:1], scalar2=rstd[:, 0:1],
                                                op0=ALU.subtract, op1=ALU.mult), reads=[xt, mv, rstd], writes=[xn])
        if DBG["stop"] <= 1:
            return
        for kc in range(8):
            rec.op("pe", lambda e, kc=kc: e.transpose(out=psT[:, kc * 128:(kc + 1) * 128], in_=xn[:, kc * 128:(kc + 1) * 128], identity=L.ident_b[:, :]),
                   reads=[xn, L.ident_b], writes=[psT])
        for kc in range(8):
            rec.op("dve", lambda e, kc=kc: e.tensor_scalar(out=hT[:, kc, :], in0=psT[:, kc * 128:(kc + 1) * 128],
                                                           scalar1=L.modT[:, 8 + kc, cond:cond + 1], scalar2=L.modT[:, kc, cond:cond + 1],
                                                           op0=ALU.mult, op1=ALU.add), reads=[psT, L.modT], writes=[hT])
        if DBG["stop"] <= 2:
            return
        for g in range(5):
            for kc in range(8):
                rec.op("pe", lambda e, g=g, kc=kc: e.matmul(psG[g][:, 0:GW[g]], lhsT=hT[:, kc, :], rhs=win[:, kc, GOFF[g]:GOFF[g] + GW[g]],
                                                            start=(kc == 0), stop=(kc == 7)), reads=[hT, win], writes=[psG[g]])
        if DBG["stop"] <= 3:
            return
        r = rope_ops(rec, "dve", psG[0], psG[0][:, 0:512], 16, 32, tab, c32, s32, t1b, t2b)
        rec.op("act", lambda e, r=r: e.activation(out=qk[:, :, 0:256], in_=r.rearrange("p (a c) -> p a c", a=2), func=AF.Copy),
               reads=[t1b], writes=[qk])
        if DBG["stop"] <= 4:
            return
        rec.op("act", lambda e: e.activation(out=sqt[:, :], in_=psG[1][:, :], func=AF.Square), reads=[psG[1]], writes=[sqt])
        rec.op("dve", lambda e: e.tensor_reduce(out=ss8[:, :], in_=sqt[:, :].rearrange("p (v r) -> p v r", v=8), axis=AX.X, op=ALU.add),
               reads=[sqt], writes=[ss8])
        rec.op("act", lambda e: e.activation(out=ss8[:, :], in_=ss8[:, :], func=AF.Sqrt, bias=L.eps[:, 0:1], scale=1.0 / 64.0),
               reads=[ss8, L.eps], writes=[ss8])
        rec.op("dve", lambda e: e.reciprocal(out=rs8[:, :], in_=ss8[:, :]), reads=[ss8], writes=[rs8])
        rec.op("dve", lambda e: e.memset(rs8[:, 6:8], 1.0), writes=[rs8])
        rec.op("dve", lambda e: e.tensor_tensor(out=xg[:, :], in0=psG[1][:, :], in1=G1t[:, :], op=ALU.mult), reads=[psG[1], G1t], writes=[xg])
        r = rope_ops(rec, "dve", xg, xg[:, 0:512], 8, 64, tab, c64, s64, t1b, t2b)
        r3 = r.rearrange("p (v r) -> p v r", v=8)
        rec.op("dve", lambda e, r3=r3: e.tensor_tensor(out=qk[:, 0, 256:512].rearrange("p (v r) -> p v r", v=4), in0=r3[:, 0:4, :],
                                                in1=rs8[:, 0:4].unsqueeze(2).to_broadcast([128, 4, 64]), op=ALU.mult),
               reads=[t1b, rs8], writes=[qk])
        for (lo, dst0) in ((4, 256), (6, 1024)):
            rec.op("dve", lambda e, lo=lo, dst0=dst0, r3=r3: e.tensor_tensor(
                out=qk[:, 1, dst0:dst0 + 256].rearrange("p (g d r) -> p g d r", g=2, d=2),
                in0=r3[:, lo:lo + 2, :].unsqueeze(2).to_broadcast([128, 2, 2, 64]),
                in1=rs8[:, lo:lo + 2].unsqueeze(2).unsqueeze(3).to_broadcast([128, 2, 2, 64]), op=ALU.mult),
                reads=[t1b, rs8], writes=[qk])
        if DBG["stop"] <= 5:
            return
        r = rope_ops(rec, "dve", psG[2], psG[2][:, 0:256], 4, 64, tab, c64, s64, t1b, t2b)
        rec.op("act", lambda e, r=r: e.activation(out=qk[:, 0, 1024:1280], in_=r, func=AF.Copy), reads=[t1b], writes=[qk])
        r = rope_ops(rec, "dve", psG[2], psG[2][:, 256:288], 1, 32, tab, c32, s32, t1b, t2b)
        rec.op("dve", lambda e, r=r: e.tensor_copy(out=qk[:, 1, 512:1024].rearrange("p (h c) -> p h c", h=4)[:, :, 64:96],
                                              in_=r.unsqueeze(1).to_broadcast([128, 4, 32])), reads=[t1b], writes=[qk])
        rec.op("act", lambda e: e.activation(out=vt[:, 0:4, 0:64], in_=psG[3][:, 0:256].rearrange("p (h d) -> p h d", h=4), func=AF.Copy),
               reads=[psG[3]], writes=[vt])
        rec.op("act", lambda e: e.activation(out=vt[:, 4:6, 0:64], in_=psG[3][:, 256:384].rearrange("p (h d) -> p h d", h=2), func=AF.Copy),
               reads=[psG[3]], writes=[vt])
        rec.op("act", lambda e: e.activation(out=vt[:, 10:12, 0:64], in_=psG[3][:, 384:512].rearrange("p (h d) -> p h d", h=2), func=AF.Copy),
               reads=[psG[3]], writes=[vt])
        if DBG["stop"] <= 6:
            return
        rec.op("act", lambda e: e.activation(out=sqt[:, 0:320], in_=psG[4][:, 0:320], func=AF.Square), reads=[psG[4]], writes=[sqt])
        rec.op("dve", lambda e: e.tensor_reduce(out=ssc[:, 0:1], in_=sqt[:, 0:192], axis=AX.X, op=ALU.add), reads=[sqt], writes=[ssc])
        rec.op("dve", lambda e: e.tensor_reduce(out=ssc[:, 1:2], in_=sqt[:, 192:320], axis=AX.X, op=ALU.add), reads=[sqt], writes=[ssc])
        rec.op("act", lambda e: e.activation(out=ssc[:, 0:1], in_=ssc[:, 0:1], func=AF.Sqrt, bias=L.eps[:, 0:1], scale=1.0 / 192.0),
               reads=[ssc, L.eps], writes=[ssc])
        rec.op("act", lambda e: e.activation(out=ssc[:, 1:2], in_=ssc[:, 1:2], func=AF.Sqrt, bias=L.eps[:, 0:1], scale=1.0 / 128.0),
               reads=[ssc, L.eps], writes=[ssc])
        rec.op("dve", lambda e: e.reciprocal(out=rsc[:, :], in_=ssc[:, :]), reads=[ssc], writes=[rsc])
        for (lo, hi, j) in ((0, 192, 0), (192, 320, 1)):
            rec.op("dve", lambda e, lo=lo, hi=hi, j=j: e.scalar_tensor_tensor(out=cn[:, lo:hi], in0=psG[4][:, lo:hi], scalar=rsc[:, j:j + 1],
                                                                             in1=Gct[:, lo:hi], op0=ALU.mult, op1=ALU.mult),
                   reads=[psG[4], rsc, Gct], writes=[cn])
        if DBG["stop"] <= 6.2:
            return
        for j, (lo, hi) in enumerate(((0, 128), (128, 192), (192, 320))):
            rec.op("pe", lambda e, j=j, lo=lo, hi=hi: e.transpose(out=psT[0:hi - lo, j * 128:(j + 1) * 128], in_=cn[:, lo:hi], identity=L.ident_b[:, :]),
                   reads=[cn, L.ident_b], writes=[psT])
        rec.op("dve", lambda e: e.tensor_copy(out=cnT[:, 0, :], in_=psT[:, 0:128]), reads=[psT], writes=[cnT])
        rec.op("dve", lambda e: e.tensor_copy(out=cnT[0:64, 1, :], in_=psT[0:64, 128:256]), reads=[psT], writes=[cnT])
        rec.op("dve", lambda e: e.tensor_copy(out=cnT[:, 2, :], in_=psT[:, 256:384]), reads=[psT], writes=[cnT])
        if DBG["stop"] <= 6.4:
            return
        rec.op("pe", lambda e: e.matmul(psU[0][:, 0:384], lhsT=cnT[:, 0, :], rhs=wuq[:, 0, :], start=True, stop=False), reads=[cnT, wuq], writes=[psU[0]])
        rec.op("pe", lambda e: e.matmul(psU[0][:, 0:384], lhsT=cnT[0:64, 1, :], rhs=wuq[0:64, 1, :], start=False, stop=True), reads=[cnT, wuq], writes=[psU[0]])
        rec.op("pe", lambda e: e.matmul(psU[1][:, 0:512], lhsT=cnT[:, 2, :], rhs=wukv[:, :], start=True, stop=True), reads=[cnT, wukv], writes=[psU[1]])
        if DBG["stop"] <= 6.6:
            return
        qc = psU[0][:, 0:384].rearrange("p (h c) -> p h c", h=4)
        kvc = psU[1][:, 0:512].rearrange("p (h c) -> p h c", h=4)
        qdst = qk[:, 0, 512:1024].rearrange("p (h c) -> p h c", h=4)
        kdst = qk[:, 1, 512:1024].rearrange("p (h c) -> p h c", h=4)
        rec.op("act", lambda e: e.activation(out=qdst[:, :, 0:64], in_=qc[:, :, 0:64], func=AF.Copy), reads=[psU[0]], writes=[qk])
        rec.op("act", lambda e: e.activation(out=kdst[:, :, 0:64], in_=kvc[:, :, 0:64], func=AF.Copy), reads=[psU[1]], writes=[qk])
        rec.op("act", lambda e: e.activation(out=vt[:, 6:10, 0:64], in_=kvc[:, :, 64:128], func=AF.Copy), reads=[psU[1]], writes=[vt])
        if DBG["stop"] <= 6.8:
            return
        rec.op("act", lambda e: e.activation(out=xg[:, 0:128].rearrange("p (h c) -> p h c", h=4), in_=qc[:, :, 64:96], func=AF.Copy), reads=[psU[0]], writes=[xg])
        if DBG["stop"] <= 6.85:
            return
        r = rope_ops(rec, "dve", xg, xg[:, 0:128], 4, 32, tab, c32, s32, t1b, t2b)
        if DBG["stop"] <= 6.9:
            return
        rec.op("dve", lambda e, r=r: e.tensor_copy(out=qdst[:, :, 64:96], in_=r.rearrange("p (h c) -> p h c", h=4)), reads=[t1b], writes=[qk])
        if DBG["stop"] <= 7:
            return
        for a in range(2):
            for (c0, c1) in ((0, 8), (8, 10)):
                for c in range(c0, c1):
                    rec.op("pe", lambda e, a=a, c=c, c0=c0: e.transpose(out=psT[:, (c - c0) * 128:(c - c0 + 1) * 128], in_=qk[:, a, c * 128:(c + 1) * 128],
                                                                        identity=L.ident_b[:, :]), reads=[qk, L.ident_b], writes=[psT])
                eng = "act" if a == 0 else "dve"
                if eng == "act":
                    rec.op("act", lambda e, a=a, c0=c0, c1=c1: e.activation(out=qkt[:, a, c0:c1, col:col + 128],
                                                                          in_=psT[:, 0:(c1 - c0) * 128].rearrange("p (c t) -> p c t", t=128), func=AF.Copy),
                           reads=[psT], writes=[qkt])
                else:
                    rec.op("dve", lambda e, a=a, c0=c0, c1=c1: e.tensor_copy(out=qkt[:, a, c0:c1, col:col + 128],
                                                                           in_=psT[:, 0:(c1 - c0) * 128].rearrange("p (c t) -> p c t", t=128)),
                           reads=[psT], writes=[qkt])
        if DBG["stop"] <= 8:
            return
        rec.dma("sp", vdst_ap, vt[:, :, :], reads=[vt])

    if DBG["stop"] <= 0:
        return
    for blk in range(n_lat_tiles // 4):
        qkt = qkT.next()
        for j in range(4):
            t = blk * 4 + j
            tile(x_src[t * 128:(t + 1) * 128, :], x_src, tab_src[t * 128:(t + 1) * 128, :], 0, qkt, j * 128,
                 outs["v"][:, :, t, :].rearrange("s p d -> p s d"), outs["v"])
        rec.dma("sp", outs["qT"][:, :, blk * 512:(blk + 1) * 512].rearrange("c p t -> p c t"), qkt[:, 0, :, :], reads=[qkt])
        rec.dma("sp", outs["kT"][:, :, blk * 512:(blk + 1) * 512].rearrange("c p t -> p c t"), qkt[:, 1, :, :], reads=[qkt])
    if do_ctx:
        qkt = qkT.next()
        for t in range(2):
            tile(xc_src[t * 128:(t + 1) * 128, :], xc_src, tab_src[TOK + t * 128:TOK + (t + 1) * 128, :], 1, qkt, t * 128,
                 outs["vc"][:, :, t, :].rearrange("s p d -> p s d"), outs["vc"])
        rec.dma("sp", outs["qTc"][:, :, :].rearrange("c p t -> p c t"), qkt[:, 0, :, 0:256], reads=[qkt])
        rec.dma("sp", outs["kTc"][:, :, :].rearrange("c p t -> p c t"), qkt[:, 1, :, 0:256], reads=[qkt])


def _pm(w, kc):
    k, n = w.shape
    return np.ascontiguousarray(w.reshape(kc, 128, n).transpose(1, 0, 2))


def prep_layer(inp, l):
    f = np.float32
    d = {}
    d["w_ada"] = _pm(inp["w_ada"][l], 8)
    d["b_adaT"] = np.ascontiguousarray(inp["b_ada"][l].reshape(48, 128).T)
    d["b_ada"] = np.ascontiguousarray(inp["b_ada"][l])
    d["w_in"] = _pm(inp["w_in"][l][:, _in_perm()], 8)
    wuq = np.zeros((256, 384), f)
    wuq[:192] = inp["mla_w_uq"][l]
    d["w_uq"] = _pm(wuq, 2)
    d["w_ukv"] = np.ascontiguousarray(inp["mla_w_ukv"][l])
    d["gain_g1"] = np.concatenate([np.tile(inp["gqa_q_norm_g"][l], 4), np.tile(inp["gqa_k_norm_g"][l], 2), np.ones(128, f)]).astype(f)
    d["gain_c"] = np.concatenate([inp["mla_q_norm_g"][l], inp["mla_kv_norm_g"][l]]).astype(f)
    return d


def prep_core_common(inp, core):
    b = core // 4
    r = core % 4
    d = {}
    cc = np.stack([inp["c"][b], inp["c_ctx"]], 1)
    d["cT"] = _pm(cc, 8)
    tok = np.arange(r * TOK, (r + 1) * TOK)
    tab = rope_tables(tok)
    ctab = np.zeros((CTX, 192), np.float32)
    ctab[:, 0:32] = 1.0
    ctab[:, 64:128] = 1.0
    d["rope_tab"] = np.ascontiguousarray(np.concatenate([tab, ctab], 0))
    d["c_ident"] = np.eye(128, dtype=np.float32)
    return d


NKT = 2 + 4 * NT
NWIN = NT + 2


def attention(cx, L, st, l, src, need_ctx, n_qb=TOK // 512, kt_limit=None):
    rec = cx.rec
    lam_init = 0.8 - 0.6 * float(np.exp(-0.3 * l))
    lamv_d = cx.din("lamv", [4, 32])
    subg_d = cx.din("subln_g", [64, 1])
    sink_d = cx.din("swa_sink", [4])
    mask_d = cx.din("c_masks", [128, 4, 128])
    lam = rec.sb("lam", [128, 1], F32, st)
    gsub = rec.sb("gsub", [64, 1], F32, st)
    esink = rec.sb("esink", [128, 4], F32, st)
    masks = rec.sb("masks", [128, 4, 128], BF16, st)
    lv = rec.sb("lv", [128, 4, 32], F32, st)
    lp = rec.sb("lp", [128, 2, 32], F32, st)
    ls = rec.sb("ls", [128, 2], F32, st)
    kts = Ring([rec.sb("ktb", [128, NKT * 128], BF16, st) for _ in range(2)])
    vtsr = Ring([rec.sb("vtb", [128, NKT, 128], BF16, st) for _ in range(2)])
    qts = Ring([rec.sb("qtb", [128, TOK], BF16, st) for _ in range(2)])
    qtc = rec.sb("qtc", [128, 10, 256], BF16, st)
    pTs = Ring([rec.sb("pT", [128, 1024], BF16, st) for _ in range(4)])
    zss = Ring([rec.sb("zs", [64, 512], F32, st) for _ in range(2)])
    rzs = Ring([rec.sb("rz", [64, 512], F32, st) for _ in range(2)])
    fa = rec.sb("fa", [64, 512], F32, st)
    fb = rec.sb("fb", [64, 512], F32, st)
    fc = rec.sb("fc", [64, 512], F32, st)
    ots = Ring([rec.sb("ot", [64, 512], BF16, st) for _ in range(2)])
    qmask = [[Ring([rec.sb("qm", [128, 512], BF16, st) for _ in range(2)]) for _ in range(2)] for _ in range(2)]
    for hp in range(2):
        for cp in range(2):
            for b_ in qmask[hp][cp].bufs:
                rec.op("pool", lambda e, b_=b_: e.memset(b_[:, :], 0.0), writes=[b_])
    if "kTwin" not in src:
        oh_d = cx.din("onehot", [128, 8])
        onehot = rec.sb("onehot", [128, 8], F32, st)
        hck = rec.sb("hck", [128, 4, 128], BF16, st)
        hcv = rec.sb("hcv", [128, 4, 128], BF16, st)
        hacc = rec.sb("hacc", [128, 128], F32, st)
        rec.dma("sp", onehot[:, :], oh_d[:, :], writes=[onehot])
    Sr = Ring([rec.ps("S", [128, 1024], F32, st) for _ in range(3)])
    accs = Ring([rec.ps("acc", [128, 512], F32, st) for _ in range(2)])
    dummy = None
    ndummy = 0
    rec.dma("sp", lv[:, :, :].rearrange("p a b -> p (a b)"), lamv_d[:, :].rearrange("a b -> (a b)").partition_broadcast(128), writes=[lv])
    rec.dma("sp", gsub[:, :], subg_d[:, :], writes=[gsub])
    rec.dma("sp", esink[:, :], sink_d[0:4].partition_broadcast(128), writes=[esink])
    rec.dma("pool", masks[:, :, :], mask_d[:, :, :], writes=[masks], cast=True)
    rec.op("dve", lambda e: e.tensor_tensor(out=lp[:, :, :], in0=lv[:, 0:4:2, :], in1=lv[:, 1:4:2, :], op=ALU.mult), reads=[lv], writes=[lp])
    rec.op("dve", lambda e: e.tensor_reduce(out=ls[:, :], in_=lp[:, :, :], axis=AX.X, op=ALU.add), reads=[lp], writes=[ls])
    rec.op("act", lambda e: e.activation(out=ls[:, :], in_=ls[:, :], func=AF.Exp), reads=[ls], writes=[ls])
    rec.op("act", lambda e: e.activation(out=esink[:, :], in_=esink[:, :], func=AF.Exp), reads=[esink], writes=[esink])
    rec.op("dve", lambda e: e.tensor_tensor(out=lam[:, :], in0=ls[:, 0:1], in1=ls[:, 1:2], op=ALU.subtract), reads=[ls], writes=[lam])
    rec.op("dve", lambda e: e.tensor_scalar(out=lam[:, :], in0=lam[:, :], scalar1=lam_init, scalar2=None, op0=ALU.add), reads=[lam], writes=[lam])
    rec.op("dve", lambda e: e.tensor_scalar(out=gsub[:, :], in0=gsub[:, :], scalar1=1.0 - lam_init, scalar2=None, op0=ALU.mult), reads=[gsub], writes=[gsub])
    if need_ctx:
        rec.dma("sp", qtc[:, :, :], src["qTc"][:, :, :].rearrange("c p t -> p c t"), writes=[qtc])

    def mm(out_ap, lhsT, rhs, start, stop, base, reads, writes):
        kw = {}
        if base == 96:
            kw["tile_position"] = (96, 0)
        rec.op("pe", lambda e: e.matmul(out_ap, lhsT=lhsT, rhs=rhs, start=start, stop=stop, skip_group_check=True, **kw), reads=reads, writes=writes)

    def finalize(accl, W, kind, h, dst_ap, sink_h=None):
        rzl = []
        zsl = []
        osl = []
        for a in accl:
            zs = zss.next()
            if sink_h is None:
                rec.op("dve", lambda e, a=a, zs=zs: e.tensor_scalar(out=zs[:, 0:W], in0=a[64:128, 0:W], scalar1=1.0, scalar2=None, op0=ALU.mult),
                       reads=[a], writes=[zs])
            else:
                rec.op("dve", lambda e, a=a, zs=zs: e.tensor_scalar(out=zs[:, 0:W], in0=a[64:128, 0:W], scalar1=esink[0:64, sink_h:sink_h + 1],
                                                                  scalar2=None, op0=ALU.add), reads=[a, esink], writes=[zs])
            zsl.append(zs)
            if kind == "A":
                ob = (fa, fb)[len(osl)]
                rec.op("dve", lambda e, a=a, ob=ob: e.tensor_scalar(out=ob[:, 0:W], in0=a[0:64, 0:W], scalar1=1.0, scalar2=None, op0=ALU.mult),
                       reads=[a], writes=[ob])
                osl.append(ob)
        for zs in zsl:
            rz = rzs.next()
            rec.op("dve", lambda e, zs=zs, rz=rz: e.reciprocal(out=rz[:, 0:W], in_=zs[:, 0:W]), reads=[zs], writes=[rz])
            rzl.append(rz)
        if kind == "A":
            accl = osl
        ot = ots.next()
        if kind != "A":
            a, rz = accl[0], rzl[0]
            rec.op("dve", lambda e: e.tensor_tensor(out=ot[:, 0:W], in0=a[0:64, 0:W], in1=rz[:, 0:W], op=ALU.mult), reads=[a, rz], writes=[ot])
        else:
            a1, a2 = accl
            r1, r2 = rzl
            rec.op("dve", lambda e: e.tensor_tensor(out=fa[:, 0:W], in0=fa[:, 0:W], in1=r1[:, 0:W], op=ALU.mult), reads=[r1], writes=[fa])
            rec.op("dve", lambda e: e.scalar_tensor_tensor(out=fb[:, 0:W], in0=fb[:, 0:W], scalar=lam[0:64, 0:1], in1=r2[:, 0:W],
                                                           op0=ALU.mult, op1=ALU.mult), reads=[r2, lam], writes=[fb])
            rec.op("dve", lambda e: e.tensor_tensor(out=fa[:, 0:W], in0=fa[:, 0:W], in1=fb[:, 0:W], op=ALU.subtract), reads=[fb], writes=[fa])
            rec.op("pool", lambda e: e.tensor_tensor(out=fc[:, 0:W], in0=fa[:, 0:W], in1=fa[:, 0:W], op=ALU.mult), reads=[fa], writes=[fc])
            pm = Sr.next()
            rec.op("pe", lambda e: e.matmul(pm[0:64, 0:W], lhsT=L.ones64[:, :], rhs=fc[:, 0:W], start=True, stop=True), reads=[fc, L.ones64], writes=[pm])
            rec.op("act", lambda e: e.activation(out=fb[:, 0:W], in_=pm[0:64, 0:W], func=AF.Sqrt, bias=L.eps[0:64, 0:1], scale=1.0), reads=[pm, L.eps], writes=[fb])
            rec.op("dve", lambda e: e.reciprocal(out=fc[:, 0:W], in_=fb[:, 0:W]), reads=[fb], writes=[fc])
            rec.op("dve", lambda e: e.scalar_tensor_tensor(out=ot[:, 0:W], in0=fa[:, 0:W], scalar=gsub[:, 0:1], in1=fc[:, 0:W],
                                                           op0=ALU.mult, op1=ALU.mult), reads=[fa, fc, gsub], writes=[ot])
        rec.dma("sp", dst_ap, ot[:, 0:W], reads=[ot])

    def attend(ktb, vtb, q_ap_fn, qbuf, comps, scale, ktiles, W, kind, h, dst_ap, sink_h=None):
        nu = len(comps)
        accl = [accs.next() for _ in range(nu)]
        if nu == 2:
            groups = [[(kt, 0), (kt, 1)] for kt in ktiles]
        else:
            groups = [[(kt, 0) for kt in ktiles[i:i + 2]] for i in range(0, len(ktiles), 2)]
        started = [False] * nu

        def qk(grp):
            S = Sr.next()
            for j, (kt, u) in enumerate(grp):
                base, K = comps[u]
                qap, qb_ = q_ap_fn(u, base, K)
                mm(S[:, j * W:(j + 1) * W], ktb[base:base + K, kt * 128:(kt + 1) * 128], qap, True, True, base, [ktb, qb_], [S])
            return S
        PD = DBG.get("pd", 2)
        Sq = [qk(groups[i]) for i in range(min(PD, len(groups)))]
        for gi, grp in enumerate(groups):
            S = Sq.pop(0)
            P = pTs.next()
            n = len(grp) * W
            if DBG["att"] <= 1:
                continue
            rec.op("act", lambda e, S=S, P=P, n=n: e.activation(out=P[:, 0:n], in_=S[:, 0:n], func=AF.Exp, scale=scale), reads=[S], writes=[P])
            if DBG["att"] <= 2:
                continue
            for _ in range(ndummy if W == 512 else 0):
                rec.op("pe", lambda e: e.matmul(dummy[:, 0:128 * DBG.get("dumw", 2)], lhsT=L.ident_b[:, :], rhs=masks[:, 0:DBG.get("dumw", 2), :].rearrange("p a b -> p (a b)"), start=True, stop=True,
                                                skip_group_check=True), reads=[], writes=[])
            for j, (kt, u) in enumerate(grp):
                a = accl[u]
                last = (gi == len(groups) - 1) and (nu == 2 or j == len(grp) - 1)
                mm(a[:, 0:W], vtb[:, kt, :], P[:, j * W:(j + 1) * W], not started[u], last, 0, [vtb, P], [a])
                started[u] = True
            if gi + PD < len(groups):
                Sq.append(qk(groups[gi + PD]))
        if DBG["att"] >= 4:
            finalize(accl, W, kind, h, dst_ap, sink_h)

    def kall(r, c):
        if "ga" in src:
            b_ = src["ga"][c]
            return b_[r * 128:(r + 1) * 128, :], [b_]
        return src["kTall"][r, c, :, :], []

    def vall(r, slot):
        if "ga" in src:
            b_ = src["ga"][10 + slot]
            return b_[r * 128:(r + 1) * 128, :].rearrange("p (t d) -> p t d", d=128), [b_]
        return src["vall"][r, slot, :, :, :], []

    def load_kv(c, slot):
        ktb = kts.next()
        vtb = vtsr.next()
        rec.dma("sp", ktb[:, 0:256], src["kTc"][c, :, :], writes=[ktb])
        rec.dma("sp", vtb[:, 0:2, :], src["vc"][slot, :, :, :], writes=[vtb])
        for r in range(4):
            ap_, rd = kall(r, c)
            rec.dma("sp", ktb[:, 256 + r * TOK:256 + (r + 1) * TOK], ap_, reads=rd, writes=[ktb])
            ap_, rd = vall(r, slot)
            rec.dma("sp", vtb[:, 2 + r * NT:2 + (r + 1) * NT, :], ap_, reads=rd, writes=[vtb])
        return ktb, vtb

    def load_v(slot):
        vtb = vtsr.next()
        rec.dma("sp", vtb[:, 0:2, :], src["vc"][slot, :, :, :], writes=[vtb])
        for r in range(4):
            ap_, rd = vall(r, slot)
            rec.dma("sp", vtb[:, 2 + r * NT:2 + (r + 1) * NT, :], ap_, reads=rd, writes=[vtb])
        return vtb

    ktiles_all = list(range(NKT)) if kt_limit is None else list(range(kt_limit))
    jobs = []
    for i in range(2):
        jobs.append((i, [(h, [((h % 2) * 64, 32), ((h % 2) * 64 + 32, 32)], h, h // 2, (h % 2) * 64) for h in (2 * i, 2 * i + 1)], 32 ** -0.5, "A"))
    for g in range(2):
        jobs.append((2 + g, [(h, [((h % 2) * 64, 64)], 4 + g, 2 + h // 2, (h % 2) * 64) for h in (2 * g, 2 * g + 1)], 64 ** -0.5, "B"))
    for h in range(4):
        jobs.append((4 + h, [(h, [(0, 96)], 6 + h, 4 + h // 2, (h % 2) * 64)], 96 ** -0.5, "C"))
    if DBG["att"] <= 0:
        return
    hjobs = []
    for (c, heads, scale, kind) in jobs:
        if kind not in DBG["kinds"]:
            continue
        prev_slot = None
        for hi, (h, comps, slot, oc, orow) in enumerate(heads):
            hjobs.append(dict(c=c, h=h, comps=comps, slot=slot, oc=oc, orow=orow, scale=scale, kind=kind,
                              ldk=(hi == 0), ldv=(slot != prev_slot)))
            prev_slot = slot
    state = {"ktb": None, "vtb": None, "qtb": None}

    def issue_loads(j):
        if j["ldk"]:
            qtb = qts.next()
            rec.dma("sp", qtb[:, :], src["qT"][j["c"], :, :], writes=[qtb])
            ktb = kts.next()
            rec.dma("sp", ktb[:, 0:256], src["kTc"][j["c"], :, :], writes=[ktb])
            for r in range(4):
                ap_, rd = kall(r, j["c"])
                rec.dma("sp", ktb[:, 256 + r * TOK:256 + (r + 1) * TOK], ap_, reads=rd, writes=[ktb])
            j["ktb"], j["qtb"] = ktb, qtb
        if j["ldv"]:
            j["vtb"] = load_v(j["slot"])

    if hjobs:
        issue_loads(hjobs[0])
    for ji, j in enumerate(hjobs):
        for k_ in ("ktb", "vtb", "qtb"):
            if k_ in j:
                state[k_] = j[k_]
        ktb, vtb, qtb = state["ktb"], state["vtb"], state["qtb"]
        if ji + 1 < len(hjobs):
            issue_loads(hjobs[ji + 1])
        c, h, comps, oc, orow, scale, kind = j["c"], j["h"], j["comps"], j["oc"], j["orow"], j["scale"], j["kind"]

        def masked_q(src_fn, src_buf, W):
            bl = []
            for cp in range(2):
                mb = qmask[h % 2][cp].next()
                rows = (h % 2) * 64 + cp * 32
                rec.op("pool", lambda e, mb=mb, rows=rows: e.tensor_copy(out=mb[rows:rows + 32, 0:W], in_=src_fn(rows)), reads=[src_buf], writes=[mb])
                bl.append(mb)
            return bl
        for qb in range(n_qb):
            if kind == "A":
                bl = masked_q(lambda rows, qb=qb, qtb=qtb: qtb[rows:rows + 32, qb * 512:(qb + 1) * 512], qtb, 512)
                attend(ktb, vtb, lambda u, base, K, bl=bl: (bl[u][:, 0:512], bl[u]), None, [(0, 128), (0, 128)], scale, ktiles_all, 512,
                       kind, h, src["OT"][oc, orow:orow + 64, qb * 512:(qb + 1) * 512])
            else:
                attend(ktb, vtb, lambda u, base, K, qb=qb, qtb=qtb: (qtb[base:base + K, qb * 512:(qb + 1) * 512], qtb), None, comps, scale, ktiles_all, 512,
                       kind, h, src["OT"][oc, orow:orow + 64, qb * 512:(qb + 1) * 512])
        if need_ctx:
            if kind == "A":
                bl = masked_q(lambda rows, c=c: qtc[rows:rows + 32, c, :], qtc, 256)
                attend(ktb, vtb, lambda u, base, K, bl=bl: (bl[u][:, 0:256], bl[u]), None, [(0, 128), (0, 128)], scale, [0, 1], 256, kind, h,
                       src["OTc"][oc, orow:orow + 64, :])
            else:
                attend(ktb, vtb, lambda u, base, K, c=c: (qtc[base:base + K, c, :], qtc), None, comps, scale, [0, 1], 256, kind, h,
                       src["OTc"][oc, orow:orow + 64, :])
    for g in range(2 if "D" in DBG["kinds"] else 0):
        c = 8 + g
        slot = 10 + g
        qtb = qts.next()
        rec.dma("sp", qtb[:, :], src["qT"][c, :, :], writes=[qtb])
        ktb = kts.next()
        vtb = vtsr.next()
        rec.dma("sp", ktb[:, 0:256], src["kTc"][c, :, :], writes=[ktb])
        rec.dma("sp", vtb[:, 0:2, :], src["vc"][slot, :, :, :], writes=[vtb])
        if "kTwin" in src:
            rec.dma("sp", ktb[:, 256:256 + NWIN * 128], src["kTwin"][g, :, :], writes=[ktb])
            rec.dma("sp", vtb[:, 2:2 + NWIN, :], src["vwin"][g, :, :, :], writes=[vtb])
        else:
            rec.dma("sp", ktb[:, 384:384 + TOK], src["kTown"][c, :, :], writes=[ktb])
            rec.dma("sp", vtb[:, 3:3 + NT, :], src["vown"][slot, :, :, :], writes=[vtb])
            for side in range(2):
                kcol = (TOK - 128) if side == 0 else 0
                vt_i = (NT - 1) if side == 0 else 0
                for r_ in range(4):
                    ap_, rd = kall(r_, c)
                    rec.dma("sp", hck[:, r_, :], ap_[:, kcol:kcol + 128], reads=rd, writes=[hck])
                    ap_, rd = vall(r_, slot)
                    rec.dma("sp", hcv[:, r_, :], ap_[:, vt_i, :], reads=rd, writes=[hcv])
                kd = ktb[:, 256:384] if side == 0 else ktb[:, 384 + TOK:384 + TOK + 128]
                vd = vtb[:, 2, :] if side == 0 else vtb[:, 3 + NT, :]
                for (cand, cb, dst_ap, dst_b) in ((hck, hck, kd, ktb), (hcv, hcv, vd, vtb)):
                    rec.op("dve", lambda e, cand=cand, side=side: e.tensor_scalar(out=hacc[:, :], in0=cand[:, 0, :], scalar1=onehot[:, side * 4:side * 4 + 1],
                                                                               scalar2=None, op0=ALU.mult), reads=[cb, onehot], writes=[hacc])
                    for r_ in range(1, 4):
                        last = r_ == 3
                        rec.op("dve", lambda e, cand=cand, side=side, r_=r_, last=last, dst_ap=dst_ap: e.scalar_tensor_tensor(
                            out=dst_ap if last else hacc[:, :], in0=cand[:, r_, :], scalar=onehot[:, side * 4 + r_:side * 4 + r_ + 1], in1=hacc[:, :],
                            op0=ALU.mult, op1=ALU.add), reads=[cb, onehot, hacc], writes=[dst_b] if last else [hacc])
        for h in (2 * g, 2 * g + 1):
            base = (h % 2) * 64
            for qb in range(n_qb):
                acc = accs.next()
                for s in range(4):
                    j = qb * 4 + s
                    tiles = [0, 1, 2 + j, 3 + j, 4 + j]
                    S = Sr.next()
                    P = pTs.next()
                    for i, kt in enumerate(tiles):
                        mm(S[:, i * 128:(i + 1) * 128], ktb[base:base + 64, kt * 128:(kt + 1) * 128], qtb[base:base + 64, j * 128:(j + 1) * 128],
                           True, True, base, [ktb, qtb], [S])
                    rec.op("act", lambda e, S=S, P=P: e.activation(out=P[:, 0:640], in_=S[:, 0:640], func=AF.Exp, scale=64 ** -0.5), reads=[S], writes=[P])
                    mp = 2 if j == 0 else 0
                    mn = 3 if j == NT - 1 else 1
                    rec.op("pool", lambda e, P=P, mp=mp: e.tensor_tensor(out=P[:, 256:384], in0=P[:, 256:384], in1=masks[:, mp, :], op=ALU.mult),
                           reads=[masks], writes=[P])
                    rec.op("pool", lambda e, P=P, mn=mn: e.tensor_tensor(out=P[:, 512:640], in0=P[:, 512:640], in1=masks[:, mn, :], op=ALU.mult),
                           reads=[masks], writes=[P])
                    for i, kt in enumerate(tiles):
                        mm(acc[:, s * 128:(s + 1) * 128], vtb[:, kt, :], P[:, i * 128:(i + 1) * 128], i == 0, i == 4, 0, [vtb, P], [acc])
                finalize([acc], 512, "D", h, src["OT"][6 + h // 2, base:base + 64, qb * 512:(qb + 1) * 512], sink_h=h)
            if need_ctx:
                attend(ktb, vtb, lambda u, b_, K, c=c: (qtc[b_:b_ + K, c, :], qtc), None, [(base, 64)], 64 ** -0.5, [0, 1], 256, "D", h,
                       src["OTc"][6 + h // 2, base:base + 64, :], sink_h=h)


def phase3(cx, L, st, x_srcs, wts):
    rec = cx.rec
    wout = rec.sb("wout", [128, 8, 1024], BF16, st)
    rw = rec.sb("rw", [128, 8, 16], F32, st)
    rbias = rec.sb("rbias", [128, 16], F32, st)
    lnt = [rec.sb("lnt", [128, 1024], F32, st) for _ in range(4)]
    pre = wts.get("bf16", False)
    wq = "sp" if pre else "pool"
    for kc in range(8):
        rec.dma(wq, wout[:, kc, :], wts["w_out"][:, kc, :], writes=[wout], cast=not pre)
    rec.dma("sp", rw[:, :, :], wts["router_w"][:, :, :], writes=[rw])
    rec.dma("sp", rbias[:, :], wts["router_bias"][0:16].partition_broadcast(128), writes=[rbias])
    for i in range(4):
        rec.dma("sp", lnt[i][:, :], wts["ln"][i, :].partition_broadcast(128), writes=[lnt[i]])
    x1s = [rec.sb("x1s", [128, 1024], F32, st) for _ in range(4)]
    xts = Ring([rec.sb("xt3", [128, 1024], F32, st) for _ in range(2)])
    u = rec.sb("u", [128, 1024], F32, st)
    tmp = rec.sb("tmp", [128, 1024], F32, st)
    h2Tf = rec.sb("h2Tf", [128, 8, 128], F32, st)
    h2T = rec.sb("h2T", [128, 8, 512], BF16, st)
    otin = rec.sb("otin", [128, 8, 512], BF16, st)
    actT = rec.sb("actT", [128, 16, 2, 512], BF16, st)
    wgus = Ring([rec.sb("wgu", [128, 8, 512], BF16, st) for _ in range(2)])
    wds = Ring([rec.sb("wd", [128, 2, 1024], BF16, st) for _ in range(2)])
    gates = rec.sb("gates", [128, 4, 16], F32, st)
    st6 = rec.sb("st6b", [128, 2, 6], F32, st)
    mv = rec.sb("mvb", [128, 2], F32, st)
    sq1 = rec.sb("sq1b", [128, 1], F32, st)
    rstd = rec.sb("rstdb", [128, 1], F32, st)
    s16 = rec.sb("s16", [128, 16], F32, st)
    sel = rec.sb("sel", [128, 16], F32, st)
    sel2 = rec.sb("sel2", [128, 16], F32, st)
    eq = rec.sb("eq", [128, 16], F32, st)
    m1 = rec.sb("m1", [128, 4], F32, st)
    m2 = rec.sb("m2", [128, 4], F32, st)
    gs = rec.sb("gs", [128, 4], F32, st)
    gm = rec.sb("gm", [128, 1], F32, st)
    sil = Ring([rec.sb("sil", [128, 256], F32, st) for _ in range(2)])
    actb = Ring([rec.sb("actb", [128, 256], BF16, st) for _ in range(2)])
    B = [rec.ps("B", [128, 512], F32, st) for _ in range(8)]
    psTb = rec.buf("psTb", B[7].t[:, :].bitcast(BF16))

    def ln_stats(src):
        for i in range(2):
            rec.op("dve", lambda e, i=i: e.bn_stats(out=st6[:, i, :], in_=src[:, i * 512:(i + 1) * 512]), reads=[src], writes=[st6])
        rec.op("dve", lambda e: e.bn_aggr(out=mv[:, :], in_=st6[:, :, :].rearrange("p a b -> p (a b)")), reads=[st6], writes=[mv])
        rec.op("act", lambda e: e.activation(out=sq1[:, :], in_=mv[:, 1:2], func=AF.Sqrt, bias=L.eps[:, 0:1], scale=1.0), reads=[mv, L.eps], writes=[sq1])
        rec.op("dve", lambda e: e.reciprocal(out=rstd[:, :], in_=sq1[:, :]), reads=[sq1], writes=[rstd])

    def gated_ln(x_in, ybanks, Gt, gt, bt, dst):
        for hh in range(2):
            rec.op("dve", lambda e, hh=hh: e.tensor_tensor(out=tmp[:, hh * 512:(hh + 1) * 512], in0=ybanks[hh][:, :], in1=Gt[:, hh * 512:(hh + 1) * 512],
                                                           op=ALU.mult), reads=[ybanks[hh], Gt], writes=[tmp])
        rec.op("dve", lambda e: e.scalar_tensor_tensor(out=u[:, :], in0=x_in[:, :], scalar=ALPHA, in1=tmp[:, :], op0=ALU.mult, op1=ALU.add),
               reads=[x_in, tmp], writes=[u])
        ln_stats(u)
        rec.op("dve", lambda e: e.tensor_scalar(out=u[:, :], in0=u[:, :], scalar1=mv[:, 0:1], scalar2=rstd[:, 0:1], op0=ALU.subtract, op1=ALU.mult),
               reads=[mv, rstd], writes=[u])
        rec.op("dve", lambda e: e.tensor_tensor(out=u[:, :], in0=u[:, :], in1=gt[:, :], op=ALU.mult), reads=[gt], writes=[u])
        rec.op("dve", lambda e: e.tensor_tensor(out=dst[:, :], in0=u[:, :], in1=bt[:, :], op=ALU.add), reads=[u, bt], writes=[dst])

    for (x_src, OT, x_dst, cond, ntok) in x_srcs:
        nblk = (ntok + 511) // 512
        for blk in range(nblk):
            nt = min(4, (ntok - blk * 512) // 128)
            ncol = nt * 128
            rec.dma("sp", otin[:, :, 0:ncol], OT[:, :, blk * 512:blk * 512 + ncol].rearrange("c p t -> p c t"), writes=[otin])
            for t in range(nt):
                xt = xts.next()
                r0 = blk * 512 + t * 128
                rec.dma("sp", xt[:, :], x_src[r0:r0 + 128, :], writes=[xt])
                for hh in range(2):
                    for kc in range(8):
                        rec.op("pe", lambda e, hh=hh, kc=kc, t=t: e.matmul(B[hh][:, :], lhsT=otin[:, kc, t * 128:(t + 1) * 128],
                                                                       rhs=wout[:, kc, hh * 512:(hh + 1) * 512], start=(kc == 0), stop=(kc == 7)),
                               reads=[otin, wout], writes=[B[hh]])
                x1 = x1s[t]
                gated_ln(xt, B[0:2], L.Gb[0][cond], lnt[0], lnt[1], x1)
                ln_stats(x1)
                rec.op("dve", lambda e, x1=x1: e.tensor_scalar(out=tmp[:, :], in0=x1[:, :], scalar1=mv[:, 0:1], scalar2=rstd[:, 0:1],
                                                               op0=ALU.subtract, op1=ALU.mult), reads=[x1, mv, rstd], writes=[tmp])
                for kc in range(8):
                    bk = B[2 + kc // 4]
                    rec.op("pe", lambda e, kc=kc, bk=bk: e.transpose(out=bk[:, (kc % 4) * 128:(kc % 4 + 1) * 128], in_=tmp[:, kc * 128:(kc + 1) * 128],
                                                                     identity=L.ident_f[:, :]), reads=[tmp, L.ident_f], writes=[bk])
                for kc in range(8):
                    bk = B[2 + kc // 4]
                    rec.op("dve", lambda e, kc=kc, bk=bk, cond=cond: e.tensor_scalar(out=h2Tf[:, kc, :], in0=bk[:, (kc % 4) * 128:(kc % 4 + 1) * 128],
                                                                          scalar1=L.modT[:, 32 + kc, cond:cond + 1], scalar2=L.modT[:, 24 + kc, cond:cond + 1],
                                                                          op0=ALU.mult, op1=ALU.add), reads=[bk, L.modT], writes=[h2Tf])
                rec.op("pool", lambda e, t=t: e.tensor_copy(out=h2T[:, :, t * 128:(t + 1) * 128], in_=h2Tf[:, :, :]), reads=[h2Tf], writes=[h2T])
                for kc in range(8):
                    rec.op("pe", lambda e, kc=kc: e.matmul(B[4][:, 0:16], lhsT=h2Tf[:, kc, :], rhs=rw[:, kc, :], start=(kc == 0), stop=(kc == 7)),
                           reads=[h2Tf, rw], writes=[B[4]])
                rec.op("act", lambda e: e.activation(out=s16[:, :], in_=B[4][:, 0:16], func=AF.Sigmoid), reads=[B[4]], writes=[s16])
                v44 = lambda b_: b_[:, :].rearrange("p (g k) -> p g k", g=4)
                bc4 = lambda b_: b_[:, :].unsqueeze(2).to_broadcast([128, 4, 4])
                rec.op("dve", lambda e: e.tensor_tensor(out=sel[:, :], in0=s16[:, :], in1=rbias[:, :], op=ALU.add), reads=[s16, rbias], writes=[sel])
                rec.op("dve", lambda e: e.tensor_reduce(out=m1[:, :], in_=v44(sel), axis=AX.X, op=ALU.max), reads=[sel], writes=[m1])
                rec.op("dve", lambda e: e.tensor_tensor(out=v44(eq), in0=v44(sel), in1=bc4(m1), op=ALU.is_equal), reads=[sel, m1], writes=[eq])
                rec.op("dve", lambda e: e.scalar_tensor_tensor(out=sel2[:, :], in0=eq[:, :], scalar=-1e9, in1=sel[:, :], op0=ALU.mult, op1=ALU.add),
                       reads=[eq, sel], writes=[sel2])
                rec.op("dve", lambda e: e.tensor_reduce(out=m2[:, :], in_=v44(sel2), axis=AX.X, op=ALU.max), reads=[sel2], writes=[m2])
                rec.op("dve", lambda e: e.tensor_tensor(out=gs[:, :], in0=m1[:, :], in1=m2[:, :], op=ALU.add), reads=[m1, m2], writes=[gs])
                rec.op("dve", lambda e: e.tensor_reduce(out=gm[:, :], in_=gs[:, :], axis=AX.X, op=ALU.max), reads=[gs], writes=[gm])
                rec.op("dve", lambda e: e.tensor_scalar(out=gs[:, :], in0=gs[:, :], scalar1=gm[:, 0:1], scalar2=None, op0=ALU.is_equal), reads=[gm], writes=[gs])
                rec.op("dve", lambda e: e.tensor_tensor(out=v44(eq), in0=v44(sel), in1=bc4(m2), op=ALU.is_ge), reads=[sel, m2], writes=[eq])
                rec.op("dve", lambda e: e.tensor_tensor(out=v44(eq), in0=v44(eq), in1=bc4(gs), op=ALU.mult), reads=[gs], writes=[eq])
                rec.op("dve", lambda e: e.tensor_tensor(out=eq[:, :], in0=eq[:, :], in1=s16[:, :], op=ALU.mult), reads=[s16], writes=[eq])
                rec.op("dve", lambda e: e.tensor_reduce(out=gm[:, :], in_=eq[:, :], axis=AX.X, op=ALU.add), reads=[eq], writes=[gm])
                rec.op("dve", lambda e: e.reciprocal(out=gm[:, :], in_=gm[:, :]), reads=[gm], writes=[gm])
                rec.op("dve", lambda e, t=t: e.tensor_scalar(out=gates[:, t, :], in0=eq[:, :], scalar1=gm[:, 0:1], scalar2=None, op0=ALU.mult),
                       reads=[eq, gm], writes=[gates])
            items = [(ex, t) for ex in range(NE) for t in range(nt)]
            cur_w = [None]

            def mm_part(ex, t):
                if t == 0:
                    cur_w[0] = wgus.next()
                    rec.dma(wq, cur_w[0][:, :, :], wts["wgu"][ex, :, :, :], writes=[cur_w[0]], cast=not pre)
                wgu = cur_w[0]
                bk = B[(ex * nt + t) % 4]
                for kc in range(8):
                    rec.op("pe", lambda e, kc=kc, t=t, bk=bk, wgu=wgu: e.matmul(bk[:, :], lhsT=h2T[:, kc, t * 128:(t + 1) * 128], rhs=wgu[:, kc, :],
                                                                             start=(kc == 0), stop=(kc == 7)), reads=[h2T, wgu], writes=[bk])
                return bk

            def post_part(ex, t, bk):
                sl_ = sil.next()
                ab = actb.next()
                rec.op("act", lambda e, bk=bk, sl_=sl_: e.activation(out=sl_[:, :], in_=bk[:, 0:256], func=AF.Silu), reads=[bk], writes=[sl_])
                rec.op("dve", lambda e, bk=bk, sl_=sl_, ab=ab, t=t, ex=ex: e.scalar_tensor_tensor(out=ab[:, :], in0=sl_[:, :], scalar=gates[:, t, ex:ex + 1],
                                                                                                in1=bk[:, 256:512], op0=ALU.mult, op1=ALU.mult),
                       reads=[sl_, gates, bk], writes=[ab])
                for fc in range(2):
                    rec.op("pe", lambda e, fc=fc, ab=ab: e.transpose(out=psTb[:, fc * 128:(fc + 1) * 128], in_=ab[:, fc * 128:(fc + 1) * 128],
                                                                     identity=L.ident_b[:, :]), reads=[ab, L.ident_b], writes=[psTb, B[7]])
                rec.op("act", lambda e, ex=ex, t=t: e.activation(out=actT[:, ex, :, t * 128:(t + 1) * 128],
                                                                 in_=psTb[:, 0:256].rearrange("p (f c) -> p f c", f=2), func=AF.Copy),
                       reads=[psTb, B[7]], writes=[actT])

            bk_cur = mm_part(*items[0])
            for ii, (ex, t) in enumerate(items):
                bk_next = mm_part(*items[ii + 1]) if ii + 1 < len(items) else None
                post_part(ex, t, bk_cur)
                bk_cur = bk_next
            for ex in range(NE):
                wd = wds.next()
                rec.dma(wq, wd[:, :, :], wts["wd"][ex, :, :, :], writes=[wd], cast=not pre)
                for t in range(nt):
                    for hh in range(2):
                        for fc in range(2):
                            rec.op("pe", lambda e, ex=ex, t=t, hh=hh, fc=fc, wd=wd: e.matmul(
                                B[t * 2 + hh][:, :], lhsT=actT[:, ex, fc, t * 128:(t + 1) * 128], rhs=wd[:, fc, hh * 512:(hh + 1) * 512],
                                start=(ex == 0 and fc == 0), stop=(ex == NE - 1 and fc == 1)), reads=[actT, wd], writes=[B[t * 2 + hh], psTb] if t == 3 and hh == 1 else [B[t * 2 + hh]])
            for t in range(nt):
                r0 = blk * 512 + t * 128
                xo = xts.next()
                gated_ln(x1s[t], B[2 * t:2 * t + 2], L.Gb[1][cond], lnt[2], lnt[3], xo)
                rec.dma("sp", x_dst[r0:r0 + 128, :], xo[:, :], reads=[xo])


def build_A(n_lat_tiles=NT, do_ctx=True):
    cx = Ctx()
    L = NS()
    rec = cx.rec
    setup_consts(cx, L)
    st = ExitStack()
    layer_setup(cx, L, st)
    st.close()
    rec.barrier()
    x_src = cx.din("x_own", [TOK, 1024])
    xc_src = cx.din("xc", [CTX, 1024])
    tab_src = cx.din("rope_tab", [TOK + CTX, 192])
    outs = {"qT": cx.dout("qT", [10, 128, TOK], BF16), "kT": cx.dout("kT", [10, 128, TOK], BF16), "v": cx.dout("v", [12, 128, NT, 128], BF16),
            "qTc": cx.dout("qTc", [10, 128, CTX], BF16), "kTc": cx.dout("kTc", [10, 128, CTX], BF16), "vc": cx.dout("vc", [12, 128, 2, 128], BF16)}
    st = ExitStack()
    phase1(cx, L, st, x_src, xc_src, tab_src, outs, n_lat_tiles=n_lat_tiles, do_ctx=do_ctx)
    rec.wait_all_on("sp")
    rec.emit()
    return cx


def build_B(l, need_ctx):
    cx = Ctx()
    L = NS()
    rec = cx.rec
    setup_consts(cx, L)
    st = ExitStack()
    layer_setup(cx, L, st)
    st.close()
    rec.barrier()
    src = {"qT": cx.din("qT", [10, 128, TOK], BF16), "kTall": cx.din("kTall", [4, 10, 128, TOK], BF16),
           "vall": cx.din("vall", [4, 12, 128, NT, 128], BF16), "kTc": cx.din("kTc", [10, 128, CTX], BF16),
           "vc": cx.din("vc", [12, 128, 2, 128], BF16), "qTc": cx.din("qTc", [10, 128, CTX], BF16),
           "kTwin": cx.din("kTwin", [2, 128, NWIN * 128], BF16), "vwin": cx.din("vwin", [2, 128, NWIN, 128], BF16),
           "OT": (cx.dout if DBG.get("export") else cx.dint)("OT", [8, 128, TOK], BF16),
           "OTc": (cx.dout if DBG.get("export") else cx.dint)("OTc", [8, 128, CTX], BF16)}
    st = ExitStack()
    attention(cx, L, st, l, src, need_ctx)
    st.close()
    rec.barrier()
    wts = {"w_out": cx.din("w_out", [128, 8, 1024]), "wgu": cx.din("wgu", [NE, 128, 8, 512]), "wd": cx.din("wd", [NE, 128, 2, 1024]),
           "router_w": cx.din("router_w", [128, 8, 16]), "router_bias": cx.din("router_bias", [16]), "ln": cx.din("ln", [4, 1024])}
    x_src = cx.din("x_own", [TOK, 1024])
    x_dst = cx.dout("x_next", [TOK, 1024])
    xs = [(x_src, src["OT"], x_dst, 0, TOK)]
    if need_ctx:
        xc_src = cx.din("xc", [CTX, 1024])
        xc_dst = cx.dout("xc_next", [CTX, 1024])
        xs.append((xc_src, src["OTc"], xc_dst, 1, CTX))
    st = ExitStack()
    if DBG.get("p3", True):
        phase3(cx, L, st, xs, wts)
    else:
        rec.dma("sp", x_dst[0:128, :], x_src[0:128, :], key="D_dbg")
    rec.wait_all_on("sp")
    rec.emit()
    return cx


def prep_B_weights(inp, l):
    d = {}
    d["w_out"] = _pm(inp["w_out"][l], 8)
    d["wgu"] = np.ascontiguousarray(np.stack([_pm(np.concatenate([inp["exp_w_gate"][l, e], inp["exp_w_up"][l, e]], 1), 8) for e in range(NE)]))
    d["wd"] = np.ascontiguousarray(np.stack([_pm(inp["exp_w_down"][l, e], 2) for e in range(NE)]))
    d["router_w"] = _pm(inp["router_w"], 8)
    d["router_bias"] = np.ascontiguousarray(inp["router_bias"])
    d["ln"] = np.ascontiguousarray(np.stack([inp["ln1_g"][l], inp["ln1_b"][l], inp["ln2_g"][l], inp["ln2_b"][l]]))
    d["lamv"] = np.ascontiguousarray(np.stack([inp["diff_lambda_q1"][l], inp["diff_lambda_k1"][l], inp["diff_lambda_q2"][l], inp["diff_lambda_k2"][l]]))
    d["subln_g"] = np.ascontiguousarray(inp["diff_subln_g"][l].reshape(64, 1))
    d["swa_sink"] = np.ascontiguousarray(inp["swa_sink"][l])
    return d


def band_masks(r):
    ki = np.arange(128)[:, None]
    qi = np.arange(128)[None, :]
    mprev = (qi <= ki).astype(np.float32)
    mnext = (ki <= qi).astype(np.float32)
    m = np.stack([mprev, mnext, mprev * (0.0 if r == 0 else 1.0), mnext * (0.0 if r == 3 else 1.0)], 1)
    return np.ascontiguousarray(m.astype(np.float32))


KVR = 1280 + 1536


def build_fused():
    cx = Ctx()
    L = NS()
    rec = cx.rec
    nc = cx.nc
    setup_consts(cx, L)
    x_in = cx.din("x_own", [TOK, 1024])
    xc_in = cx.din("xc", [CTX, 1024])
    tab_src = cx.din("rope_tab", [TOK + CTX, 192])
    out = cx.dout("out", [TOK, 1024])
    x1 = cx.dint("x1", [TOK, 1024])
    xc1 = cx.dint("xc1", [CTX, 1024])
    groups = [[0, 1, 2, 3], [4, 5, 6, 7]]
    for l in range(DEPTH):
        cx.sfx = "_l%d" % l
        need_ctx = l < DEPTH - 1
        lst = ExitStack()
        st = ExitStack()
        L.modT = rec.sb("modT", [128, 48, 2], F32, lst)
        L.Gb = [[rec.sb("Gb", [128, 1024], F32, lst) for _ in range(2)] for _ in range(2)]
        wsrc = {"w_out": cx.din("w_out", [128, 8, 1024]), "wgu": cx.din("wgu", [NE, 128, 8, 512]), "wd": cx.din("wd", [NE, 128, 2, 1024])}
        wbf = {"w_out": cx.dint("w_out_bf%d" % l, [128, 8, 1024], BF16), "wgu": cx.dint("wgu_bf%d" % l, [NE, 128, 8, 512], BF16),
               "wd": cx.dint("wd_bf%d" % l, [NE, 128, 2, 1024], BF16)}
        layer_setup(cx, L, st, pst="prealloc")
        st.close()
        rec.barrier()
        rec.release_dma_sems()
        for ex in range(NE):
            rec.dma("pool", wbf["wgu"][ex, :, :, :].rearrange("p a b -> p (a b)"), wsrc["wgu"][ex, :, :, :].rearrange("p a b -> p (a b)"), key="D_wcast", cast=True)
            rec.dma("pool", wbf["wd"][ex, :, :, :].rearrange("p a b -> p (a b)"), wsrc["wd"][ex, :, :, :].rearrange("p a b -> p (a b)"), key="D_wcast", cast=True)
        rec.dma("pool", wbf["w_out"][:, :, :].rearrange("p a b -> p (a b)"), wsrc["w_out"][:, :, :].rearrange("p a b -> p (a b)"), key="D_wcast", cast=True)
        kv_own = nc.dram_tensor("kv_own%d" % l, [KVR, TOK], BF16).ap()
        ga = [Buf("ga", nc.dram_tensor("ga%d_%d" % (l, p_), [4 * 128, TOK], BF16).ap()) for p_ in range(22)]
        qT = cx.dint("qT%d" % l, [10, 128, TOK], BF16)
        qTc = cx.dint("qTc%d" % l, [10, 128, CTX], BF16)
        kTc = cx.dint("kTc%d" % l, [10, 128, CTX], BF16)
        vc = cx.dint("vc%d" % l, [12, 128, 2, 128], BF16)
        OT = cx.dint("OT%d" % l, [8, 128, TOK], BF16)
        OTc = cx.dint("OTc%d" % l, [8, 128, CTX], BF16)
        kT_own = Buf("kTown", kv_own[0:1280, :].rearrange("(c p) t -> c p t", p=128))
        v_own = Buf("vown", kv_own[1280:KVR, :].rearrange("(s p) (t d) -> s p t d", p=128, d=128))
        outs = {"qT": qT, "kT": kT_own, "v": v_own, "qTc": qTc, "kTc": kTc, "vc": vc}
        st = ExitStack()
        phase1(cx, L, st, x_in if l == 0 else x1, xc_in if l == 0 else xc1, tab_src, outs)
        st.close()
        rec.barrier()
        rec.release_dma_sems()
        order = [0, 10, 11, 1, 12, 13, 2, 14, 3, 15, 4, 16, 5, 17, 6, 18, 7, 19, 8, 20, 9, 21]
        for p_ in order:
            rec.collective_piece(lambda e, a=kv_own[p_ * 128:(p_ + 1) * 128, :], b=ga[p_]: e.collective_compute(
                "AllGather", ALU.bypass, replica_groups=groups, ins=[a], outs=[b[:, :]]), ga[p_])
        src = {"qT": qT, "ga": ga, "kTc": kTc, "vc": vc, "qTc": qTc, "kTown": kT_own, "vown": v_own, "OT": OT, "OTc": OTc}
        st = ExitStack()
        attention(cx, L, st, l, src, need_ctx)
        st.close()
        rec.barrier()
        rec.release_dma_sems()
        wts = {"w_out": wbf["w_out"], "wgu": wbf["wgu"], "wd": wbf["wd"], "bf16": True,
               "router_w": cx.din("router_w", [128, 8, 16]), "router_bias": cx.din("router_bias", [16]), "ln": cx.din("ln", [4, 1024])}
        xs = [(x_in if l == 0 else x1, OT, x1 if l == 0 else out, 0, TOK)]
        if need_ctx:
            xs.append((xc_in, OTc, xc1, 1, CTX))
        st = ExitStack()
        phase3(cx, L, st, xs, wts)
        st.close()
        rec.barrier()
        rec.release_dma_sems()
        lst.close()
    rec.wait_all_on("sp")
    rec.emit()
    return cx


def kernel_fused(inp):
    cores = list(range(NCORES))
    lws = [prep_layer(inp, l) for l in range(DEPTH)]
    bws = [prep_B_weights(inp, l) for l in range(DEPTH)]
    shared_w = {"router_w": bws[0]["router_w"], "router_bias": bws[0]["router_bias"]}
    in_maps = []
    for c in cores:
        b, r = c // 4, c % 4
        m = dict(prep_core_common(inp, c))
        m.update(shared_w)
        for l in range(DEPTH):
            for k, v in list(lws[l].items()) + list(bws[l].items()):
                if k not in shared_w:
                    m["%s_l%d" % (k, l)] = v
        m["x_own"] = np.ascontiguousarray(inp["x"][b, r * TOK:(r + 1) * TOK])
        m["xc"] = np.ascontiguousarray(inp["ctx"][b])
        m["c_masks"] = band_masks(r)
        oh = np.zeros((128, 8), np.float32)
        if r > 0:
            oh[:, r - 1] = 1.0
        if r < 3:
            oh[:, 4 + r + 1] = 1.0
        m["onehot"] = oh
        in_maps.append(m)
    prog = _prog("fused", build_fused)
    names = set(prog.dram.keys())
    in_maps = [{k: v for k, v in m.items() if k in names} for m in in_maps]
    res = run_bass_kernel_spmd(prog.nc, in_maps, core_ids=cores).results
    out = np.zeros((BATCH, SEQ, D), np.float32)
    for c in cores:
        out[c // 4, (c % 4) * TOK:(c % 4 + 1) * TOK] = res[c]["out"]
    return out


_PROGS = {}


def _prog(key, fn):
    if key not in _PROGS:
        _PROGS[key] = fn()
    return _PROGS[key]


def kernel(**inputs):
    inp = {k: np.asarray(v) for k, v in inputs.items()}
    return kernel_fused(inp)


def kernel_unfused(**inputs):
    inp = {k: np.asarray(v) for k, v in inputs.items()}
    cores = list(range(NCORES))
    common = [prep_core_common(inp, c) for c in cores]
    x_cur = [np.ascontiguousarray(inp["x"][c // 4, (c % 4) * TOK:(c % 4 + 1) * TOK]) for c in cores]
    xc_cur = [np.ascontiguousarray(inp["ctx"][c // 4]) for c in cores]
    for l in range(DEPTH):
        need_ctx = l < DEPTH - 1
        lw = prep_layer(inp, l)
        pa = _prog("A", build_A)
        in_maps = []
        for c in cores:
            m = dict(lw)
            m.update(common[c])
            m["x_own"] = x_cur[c]
            m["xc"] = xc_cur[c]
            in_maps.append(m)
        ra = run_bass_kernel_spmd(pa.nc, in_maps, core_ids=cores).results
        bw = prep_B_weights(inp, l)
        in_maps = []
        for c in cores:
            b, r = c // 4, c % 4
            grp = [ra[b * 4 + i] for i in range(4)]
            m = {}
            for k in ("w_ada", "b_adaT", "b_ada"):
                m[k] = lw[k]
            m["cT"] = common[c]["cT"]
            m["c_ident"] = common[c]["c_ident"]
            m.update(bw)
            m["qT"] = ra[c]["qT"]
            m["kTc"] = ra[c]["kTc"]
            m["vc"] = ra[c]["vc"]
            m["qTc"] = ra[c]["qTc"]
            m["kTall"] = np.ascontiguousarray(np.stack([g["kT"] for g in grp]))
            m["vall"] = np.ascontiguousarray(np.stack([g["v"] for g in grp]))
            kfull = np.concatenate([g["kT"][8:10] for g in grp], axis=2)
            kpad = np.zeros((2, 128, SEQ + 256), kfull.dtype)
            kpad[:, :, 128:128 + SEQ] = kfull
            m["kTwin"] = np.ascontiguousarray(kpad[:, :, r * TOK:r * TOK + NWIN * 128])
            vfull = np.concatenate([g["v"][10:12] for g in grp], axis=2)
            vpad = np.zeros((2, 128, 4 * NT + 2, 128), vfull.dtype)
            vpad[:, :, 1:1 + 4 * NT] = vfull
            m["vwin"] = np.ascontiguousarray(vpad[:, :, r * NT:r * NT + NWIN])
            m["c_masks"] = band_masks(r)
            m["x_own"] = x_cur[c]
            if need_ctx:
                m["xc"] = xc_cur[c]
            in_maps.append(m)
        pb = _prog(("B", l), lambda: build_B(l, need_ctx))
        rb = run_bass_kernel_spmd(pb.nc, in_maps, core_ids=cores).results
        x_cur = [np.ascontiguousarray(rb[c]["x_next"]) for c in cores]
        if need_ctx:
            xc_cur = [np.ascontiguousarray(rb[c]["xc_next"]) for c in cores]
    out = np.zeros((BATCH, SEQ, D), np.float32)
    for c in cores:
        out[c // 4, (c % 4) * TOK:(c % 4 + 1) * TOK] = x_cur[c]
    return out
```

```python
import numpy as np
from contextlib import ExitStack
import concourse.bass as bass
import concourse.mybir as mybir
from concourse.bass_utils import run_bass_kernel_spmd

F32 = mybir.dt.float32
BF16 = mybir.dt.bfloat16
AF = mybir.ActivationFunctionType
ALU = mybir.AluOpType
AX = mybir.AxisListType

D = 1024
BATCH = 2
SEQ = 16384
DEPTH = 2
CTX = 256
NCORES = 8
TOK = SEQ // 4
NT = TOK // 128
NE = 16
DE = 256
EPS = 1e-6
ALPHA = (2 * DEPTH) ** 0.25
IN_COLS = 2144


class Buf:
    __slots__ = ("name", "t", "w", "r")

    _uid = [0]

    def __init__(self, name, t):
        Buf._uid[0] += 1
        self.name = "%s.%d" % (name, Buf._uid[0])
        self.t = t
        self.w = {}
        self.r = {}

    def __getitem__(self, idx):
        return self.t[idx]


class Rec:
    ENGS = ("pe", "act", "dve", "pool", "sp")

    def __init__(self, nc, stack):
        self.nc = nc
        self.stack = stack
        self.ops = {e: [] for e in self.ENGS}
        self.sems = {}
        self.cnt = {}
        self.seen = {e: {} for e in self.ENGS}
        self.nbuf = 0
        for e in self.ENGS:
            self._sem("E_" + e)

    def _sem(self, key):
        if key not in self.sems:
            if key.startswith("D_H_") and getattr(self, "free", None):
                sem, c0 = self.free.pop()
                self.sems[key] = sem
                self.cnt[key] = c0
            else:
                self.nsem = getattr(self, "nsem", 0) + 1
                self.sems[key] = self.stack.enter_context(self.nc.semaphore("s%d" % self.nsem))
                self.cnt[key] = 0
        return self.sems[key]

    def release_dma_sems(self):
        if not hasattr(self, "free"):
            self.free = []
        for key in [k for k in self.sems if k.startswith("D_")]:
            sem, c = self.sems.pop(key), self.cnt.pop(key)
            if key.startswith("D_H_"):
                self.free.append((sem, c))
            for e in self.ENGS:
                self.seen[e].pop(key, None)

    def collective_piece(self, fn, out_buf):
        key = "E_cc"
        self._sem(key)
        self.cnt[key] += 1
        self.ops["pool"].append(([], fn, self.sems[key], 1))
        out_buf.w = {key: (self.cnt[key], "cc")}
        out_buf.r = {}

    def collective(self, fn):
        self.barrier()
        key = "E_cc"
        self._sem(key)
        self.cnt[key] += 1
        self.ops["pool"].append(([], fn, self.sems[key], 1))
        self.barrier()

    def buf(self, name, t):
        self.nbuf += 1
        return Buf("%s#%d" % (name, self.nbuf), t)

    def sb(self, name, shape, dtype, stack=None):
        st = stack if stack is not None else self.stack
        self.nbuf += 1
        t = st.enter_context(self.nc.sbuf_tensor("%s_%d" % (name, self.nbuf), list(shape), dtype))
        return Buf(name, t)

    def ps(self, name, shape, dtype, stack=None):
        st = stack if stack is not None else self.stack
        self.nbuf += 1
        t = st.enter_context(self.nc.psum_tensor("%s_%d" % (name, self.nbuf), list(shape), dtype))
        return Buf(name, t)

    def _collect(self, eng, reads, writes, is_dma):
        need = {}

        def add(d):
            for k, (v, pe) in d.items():
                if k not in self.sems:
                    continue
                if (not is_dma) and eng == "pe" and pe == "pe" and k == "E_pe":
                    continue
                if need.get(k, 0) < v:
                    need[k] = v
        for b in reads:
            add(b.w)
        for b in writes:
            add(b.w)
            add(b.r)
        waits = []
        seen = self.seen[eng]
        for k, v in need.items():
            if seen.get(k, 0) < v:
                seen[k] = v
                waits.append((self.sems[k], v))
        return waits

    def op(self, eng, fn, reads=(), writes=()):
        waits = self._collect(eng, reads, writes, False)
        key = "E_" + eng
        self.cnt[key] += 1
        tk = (self.cnt[key], eng)
        self.ops[eng].append((waits, fn, self.sems[key], 1))
        for b in reads:
            b.r[key] = tk
        for b in writes:
            b.w = {key: tk}
            b.r = {}

    def dma(self, eng, out_ap, in_ap, reads=(), writes=(), key=None, cast=False):
        if key is None:
            b0 = (list(writes) + list(reads))[0]
            key = b0.name + ("_w" if writes else "_r")
        key = ("D_P_" if eng == "pool" else "D_H_") + key
        self._sem(key)
        waits = self._collect(eng, reads, writes, True)
        self.cnt[key] += 16
        tk = (self.cnt[key], "dma")
        if cast:
            self.ops[eng].append((waits, lambda e, o=out_ap, i=in_ap: e.dma_start(out=o, in_=i, max_dma_last_dim=4096), self.sems[key], 16))
        else:
            self.ops[eng].append((waits, lambda e, o=out_ap, i=in_ap: e.dma_start(out=o, in_=i), self.sems[key], 16))
        for b in reads:
            b.r[key] = tk
        for b in writes:
            b.w = {key: tk}
            b.r = {}

    def barrier(self):
        for e in self.ENGS:
            waits = []
            for k, v in self.cnt.items():
                if v > 0 and self.seen[e].get(k, 0) < v:
                    self.seen[e][k] = v
                    waits.append((self.sems[k], v))
            if waits:
                self.ops[e].append((waits, None, None, 0))

    def wait_all_on(self, eng):
        waits = []
        for k, v in self.cnt.items():
            if v > 0 and self.seen[eng].get(k, 0) < v:
                self.seen[eng][k] = v
                waits.append((self.sems[k], v))
        if waits:
            self.ops[eng].append((waits, None, None, 0))

    def emit(self):
        nc = self.nc
        block = self.stack.enter_context(nc.Block())

        def run(e, ops):
            for waits, fn, sem, inc in ops:
                for s, v in waits:
                    e.wait_ge(s, v)
                if fn is not None:
                    fn(e).then_inc(sem, inc)

        @block.tensor
        def _(e):
            run(e, self.ops["pe"])

        @block.scalar
        def _(e):
            run(e, self.ops["act"])

        @block.vector
        def _(e):
            run(e, self.ops["dve"])

        @block.gpsimd
        def _(e):
            run(e, self.ops["pool"])

        @block.sync
        def _(e):
            run(e, self.ops["sp"])


def _bc(ap, shape):
    return ap.to_broadcast(list(shape))


class Ctx:
    def __init__(self):
        self.nc = bass.Bass("TRN2", target_bir_lowering=False)
        self.stack = ExitStack()
        self.rec = Rec(self.nc, self.stack)
        self.dram = {}

    sfx = ""
    shared = ("c_ident", "cT", "rope_tab", "c_masks", "router_w", "router_bias", "x_own", "xc", "onehot")

    def din(self, name, shape, dtype=F32):
        if name not in self.shared:
            name = name + self.sfx
        if name in self.dram:
            return self.dram[name]
        t = self.nc.dram_tensor(name, list(shape), dtype, kind="ExternalInput")
        b = Buf(name, t.ap())
        self.dram[name] = b
        return b

    def dout(self, name, shape, dtype=F32):
        t = self.nc.dram_tensor(name, list(shape), dtype, kind="ExternalOutput")
        b = Buf(name, t.ap())
        self.dram[name] = b
        return b

    def dint(self, name, shape, dtype=F32):
        t = self.nc.dram_tensor(name, list(shape), dtype)
        b = Buf(name, t.ap())
        self.dram[name] = b
        return b


def _in_perm():
    o = {}
    off = 0
    for name, n in (("Aq", 256), ("Ak", 256), ("Av", 256), ("Bq", 256), ("Bk", 128), ("Bv", 128),
                    ("Ccq", 192), ("Cckv", 128), ("Ckr", 32), ("Dq", 256), ("Dk", 128), ("Dv", 128)):
        o[name] = np.arange(off, off + n)
        off += n
    order = ["Aq", "Ak", "Bq", "Bk", "Dk", "Dq", "Ckr", "Av", "Bv", "Dv", "Ccq", "Cckv"]
    return np.concatenate([o[k] for k in order])


GOFF = (0, 512, 1024, 1312, 1824)
GW = (512, 512, 288, 512, 320)


def rope_tables(tok_idx):
    row = (tok_idx // 64).astype(np.float64)
    col = (tok_idx % 64).astype(np.float64)

    def tab(n):
        inv = 10000.0 ** (-np.arange(n, dtype=np.float32).astype(np.float64) / n)
        inv = (np.float32(10000.0) ** (-(np.arange(n, dtype=np.float32) / np.float32(n)))).astype(np.float32)
        ar = (row.astype(np.float32)[:, None] * inv).astype(np.float32)
        ac = (col.astype(np.float32)[:, None] * inv).astype(np.float32)
        cr, sr, cc, sc = np.cos(ar), np.sin(ar), np.cos(ac), np.sin(ac)
        cos = np.concatenate([cr, cr, cc, cc], 1)
        sin = np.concatenate([-sr, sr, -sc, sc], 1)
        return cos.astype(np.float32), sin.astype(np.float32)
    c32, s32 = tab(8)
    c64, s64 = tab(16)
    return np.ascontiguousarray(np.concatenate([c32, s32, c64, s64], 1), dtype=np.float32)


def setup_consts(cx, L):
    rec, nc = cx.rec, cx.nc
    L.ident_f = rec.sb("identf", [128, 128], F32)
    L.ident_b = rec.sb("identb", [128, 128], BF16)
    L.eps = rec.sb("eps", [128, 1], F32)
    L.ones64 = rec.sb("ones64", [64, 64], F32)
    idn = cx.din("c_ident", [128, 128], F32)
    rec.dma("sp", L.ident_f[:, :], idn[:, :], writes=[L.ident_f])
    rec.op("dve", lambda e: e.tensor_copy(out=L.ident_b[:, :], in_=L.ident_f[:, :]), reads=[L.ident_f], writes=[L.ident_b])
    rec.op("dve", lambda e: e.memset(L.eps[:, :], EPS), writes=[L.eps])
    rec.op("dve", lambda e: e.memset(L.ones64[:, :], 1.0 / 64.0), writes=[L.ones64])


class NS:
    pass


DBG = {"stop": 99, "att": 9, "kinds": "ABCD"}


class Ring:
    def __init__(self, bufs):
        self.bufs = bufs
        self.i = 0

    def next(self):
        b = self.bufs[self.i % len(self.bufs)]
        self.i += 1
        return b


def rope_ops(rec, eng, src_buf, src, nv, R, tab_buf, cos, sin, t1b, t2b):
    h = R // 4
    n = nv * R
    t1 = t1b[:, 0:n]
    t2 = t2b[:, 0:n]
    rec.op(eng, lambda e: e.tensor_tensor(out=t1.rearrange("p (v r) -> p v r", v=nv), in0=src.rearrange("p (v r) -> p v r", v=nv),
                                          in1=cos.unsqueeze(1).to_broadcast([128, nv, R]), op=ALU.mult),
           reads=[src_buf, tab_buf], writes=[t1b])
    s5 = src.rearrange("p (v a b h) -> p v a b h", v=nv, a=2, b=2, h=h)
    t5 = t2.rearrange("p (v a b h) -> p v a b h", v=nv, a=2, b=2, h=h)
    sn = sin.rearrange("p (a b h) -> p a b h", a=2, b=2, h=h)
    for b in (0, 1):
        rec.op(eng, lambda e, b=b: e.tensor_tensor(out=t5[:, :, :, b, :], in0=s5[:, :, :, 1 - b, :],
                                                   in1=sn[:, :, b, :].unsqueeze(1).to_broadcast([128, nv, 2, h]), op=ALU.mult),
               reads=[src_buf, tab_buf], writes=[t2b])
    rec.op(eng, lambda e: e.tensor_tensor(out=t1, in0=t1, in1=t2, op=ALU.add), reads=[t2b], writes=[t1b])
    return t1


def layer_setup(cx, L, st, pst=None):
    rec = cx.rec
    cT = cx.din("cT", [128, 8, 2])
    wada = cx.din("w_ada", [128, 8, 6144])
    badaT = cx.din("b_adaT", [128, 48])
    bada = cx.din("b_ada", [6144])
    if pst != "prealloc":
        L.modT = rec.sb("modT", [128, 48, 2], F32, pst)
        L.Gb = [[rec.sb("Gb", [128, 1024], F32, pst) for _ in range(2)] for _ in range(2)]
    sl0 = rec.sb("sl0", [128, 8, 2], F32, st)
    sl = rec.sb("sl", [128, 8, 2], F32, st)
    slrep = rec.sb("slrep", [128, 8, 2, 128], F32, st)
    bT = rec.sb("bT", [128, 48], F32, st)
    was = Ring([rec.sb("wa", [128, 8, 512], F32, st) for _ in range(2)])
    bbs = Ring([rec.sb("bb", [128, 512], F32, st) for _ in range(2)])
    psA = rec.ps("psA", [128, 512], F32, st)
    psB = Ring([rec.ps("psB", [128, 512], F32, st) for _ in range(2)])
    rec.op("dve", lambda e: e.memset(L.modT[:, :, :], 0.0), writes=[L.modT])
    rec.dma("sp", sl0[:, :, :], cT[:, :, :], writes=[sl0])
    rec.dma("sp", bT[:, :], badaT[:, :], writes=[bT])
    rec.op("act", lambda e: e.activation(out=sl[:, :, :], in_=sl0[:, :, :], func=AF.Silu), reads=[sl0], writes=[sl])
    rec.op("dve", lambda e: e.tensor_copy(out=slrep[:, :, :, :], in_=sl[:, :, :].unsqueeze(3).to_broadcast([128, 8, 2, 128])),
           reads=[sl], writes=[slrep])
    for gi in range(12):
        wa = was.next()
        rec.dma("sp", wa[:, :, :], wada[:, :, gi * 512:(gi + 1) * 512], writes=[wa])
        v = gi // 2
        if v in (2, 5):
            bb = bbs.next()
            rec.dma("pool", bb[:, :], bada[gi * 512:(gi + 1) * 512].partition_broadcast(128), writes=[bb])
            for cond in range(2):
                ps = psB.next()
                for kc in range(8):
                    rec.op("pe", lambda e, kc=kc, cond=cond, ps=ps, wa=wa: e.matmul(ps[:, :], lhsT=slrep[:, kc, cond, :], rhs=wa[:, kc, :],
                                                                                 start=(kc == 0), stop=(kc == 7)),
                           reads=[slrep, wa], writes=[ps])
                gb = L.Gb[0 if v == 2 else 1][cond]
                rec.op("dve", lambda e, ps=ps, gb=gb, bb=bb, gi=gi: e.tensor_tensor(out=gb[:, (gi % 2) * 512:(gi % 2 + 1) * 512], in0=ps[:, :],
                                                                                 in1=bb[:, :], op=ALU.add),
                       reads=[ps, bb], writes=[gb])
        else:
            for jj in range(4):
                for kc in range(8):
                    rec.op("pe", lambda e, kc=kc, jj=jj, wa=wa: e.matmul(psA[:, jj * 2:jj * 2 + 2], lhsT=wa[:, kc, jj * 128:(jj + 1) * 128],
                                                                       rhs=sl[:, kc, :], start=(kc == 0), stop=(kc == 7)),
                           reads=[sl, wa], writes=[psA])
            rec.op("dve", lambda e, gi=gi: e.tensor_tensor(out=L.modT[:, gi * 4:gi * 4 + 4, :],
                                                           in0=psA[:, 0:8].rearrange("p (j c) -> p j c", c=2),
                                                           in1=bT[:, gi * 4:gi * 4 + 4].unsqueeze(2).to_broadcast([128, 4, 2]), op=ALU.add),
                   reads=[psA, bT], writes=[L.modT])
    for lo in (8, 32):
        rec.op("dve", lambda e, lo=lo: e.tensor_scalar(out=L.modT[:, lo:lo + 8, :], in0=L.modT[:, lo:lo + 8, :], scalar1=1.0, scalar2=None,
                                                       op0=ALU.add), reads=[L.modT], writes=[L.modT])


def phase1(cx, L, st, x_src, xc_src, tab_src, outs, n_lat_tiles=NT, do_ctx=True):
    rec = cx.rec
    win_d = cx.din("w_in", [128, 8, IN_COLS])
    wuq_d = cx.din("w_uq", [128, 2, 384])
    wukv_d = cx.din("w_ukv", [128, 512])
    g1_d = cx.din("gain_g1", [512])
    gc_d = cx.din("gain_c", [320])
    win = rec.sb("win", [128, 8, IN_COLS], BF16, st)
    wuq = rec.sb("wuq", [128, 2, 384], BF16, st)
    wukv = rec.sb("wukv", [128, 512], BF16, st)
    for kc in range(8):
        rec.dma("pool", win[:, kc, :], win_d[:, kc, :], writes=[win], cast=True)
    rec.dma("pool", wuq[:, :, :], wuq_d[:, :, :], writes=[wuq], cast=True)
    rec.dma("pool", wukv[:, :], wukv_d[:, :], writes=[wukv], cast=True)
    G1t = rec.sb("G1t", [128, 512], F32, st)
    Gct = rec.sb("Gct", [128, 320], F32, st)
    rec.dma("sp", G1t[:, :], g1_d[0:512].partition_broadcast(128), writes=[G1t])
    rec.dma("sp", Gct[:, :], gc_d[0:320].partition_broadcast(128), writes=[Gct])
    xts = Ring([rec.sb("xt", [128, 1024], F32, st) for _ in range(2)])
    tabs = Ring([rec.sb("tab", [128, 192], F32, st) for _ in range(2)])
    st6 = rec.sb("st6", [128, 2, 6], F32, st)
    mv = rec.sb("mv", [128, 2], F32, st)
    sq1 = rec.sb("sq1", [128, 1], F32, st)
    rstd = rec.sb("rstd", [128, 1], F32, st)
    xn = rec.sb("xn", [128, 1024], BF16, st)
    hT = rec.sb("hT", [128, 8, 128], BF16, st)
    t1b = rec.sb("t1b", [128, 512], F32, st)
    t2b = rec.sb("t2b", [128, 512], F32, st)
    xg = rec.sb("xg", [128, 512], F32, st)
    sqt = rec.sb("sqt", [128, 512], F32, st)
    ss8 = rec.sb("ss8", [128, 8], F32, st)
    rs8 = rec.sb("rs8", [128, 8], F32, st)
    ssc = rec.sb("ssc", [128, 2], F32, st)
    rsc = rec.sb("rsc", [128, 2], F32, st)
    cn = rec.sb("cn", [128, 320], BF16, st)
    cnT = rec.sb("cnT", [128, 3, 128], BF16, st)
    qk = rec.sb("qk", [128, 2, 1280], BF16, st)
    vts = Ring([rec.sb("vt", [128, 12, 128], BF16, st) for _ in range(2)])
    qkT = Ring([rec.sb("qkT", [128, 2, 10, 512], BF16, st) for _ in range(2)])
    psT = rec.ps("psT", [128, 1024], BF16, st)
    psG = [rec.ps("psG", [128, 512], F32, st) for _ in range(5)]
    psU = [rec.ps("psU", [128, 512], F32, st) for _ in range(2)]
    for vt in vts.bufs:
        rec.op("pool", lambda e, vt=vt: e.memset(vt[:, :, 64:128], 1.0), writes=[vt])
    rec.op("pool", lambda e: e.memset(qk[:, :, :], 0.0), writes=[qk])

    def tile(src_ap, src_buf, tab_ap, cond, qkt, col, vdst_ap, vdst_buf):
        xt = xts.next()
        tab = tabs.next()
        vt = vts.next()
        rec.dma("sp", xt[:, :], src_ap, writes=[xt])
        rec.dma("sp", tab[:, :], tab_ap, writes=[tab])
        c32, s32, c64, s64 = tab[:, 0:32], tab[:, 32:64], tab[:, 64:128], tab[:, 128:192]
        for i in range(2):
            rec.op("dve", lambda e, i=i: e.bn_stats(out=st6[:, i, :], in_=xt[:, i * 512:(i + 1) * 512]), reads=[xt], writes=[st6])
        rec.op("dve", lambda e: e.bn_aggr(out=mv[:, :], in_=st6[:, :, :].rearrange("p a b -> p (a b)")), reads=[st6], writes=[mv])
        rec.op("act", lambda e: e.activation(out=sq1[:, :], in_=mv[:, 1:2], func=AF.Sqrt, bias=L.eps[:, 0:1], scale=1.0),
               reads=[mv, L.eps], writes=[sq1])
        rec.op("dve", lambda e: e.reciprocal(out=rstd[:, :], in_=sq1[:, :]), reads=[sq1], writes=[rstd])
        rec.op("dve", lambda e: e.tensor_scalar(out=xn[:, :], in0=xt[:, :], scalar1=mv[:, 0:1], scalar2=rstd[:, 0:1],
                                                op0=ALU.subtract, op1=ALU.mult), reads=[xt, mv, rstd], writes=[xn])
        if DBG["stop"] <= 1:
            return
        for kc in range(8):
            rec.op("pe", lambda e, kc=kc: e.transpose(out=psT[:, kc * 128:(kc + 1) * 128], in_=xn[:, kc * 128:(kc + 1) * 128], identity=L.ident_b[:, :]),
                   reads=[xn, L.ident_b], writes=[psT])
        for kc in range(8):
            rec.op("dve", lambda e, kc=kc: e.tensor_scalar(out=hT[:, kc, :], in0=psT[:, kc * 128:(kc + 1) * 128],
                                                           scalar1=L.modT[:, 8 + kc, cond:cond + 1], scalar2=L.modT[:, kc, cond:cond + 1],
                                                           op0=ALU.mult, op1=ALU.add), reads=[psT, L.modT], writes=[hT])
        if DBG["stop"] <= 2:
            return
        for g in range(5):
            for kc in range(8):
                rec.op("pe", lambda e, g=g, kc=kc: e.matmul(psG[g][:, 0:GW[g]], lhsT=hT[:, kc, :], rhs=win[:, kc, GOFF[g]:GOFF[g] + GW[g]],
                                                            start=(kc == 0), stop=(kc == 7)), reads=[hT, win], writes=[psG[g]])
        if DBG["stop"] <= 3:
            return
        r = rope_ops(rec, "dve", psG[0], psG[0][:, 0:512], 16, 32, tab, c32, s32, t1b, t2b)
        rec.op("act", lambda e, r=r: e.activation(out=qk[:, :, 0:256], in_=r.rearrange("p (a c) -> p a c", a=2), func=AF.Copy),
               reads=[t1b], writes=[qk])
        if DBG["stop"] <= 4:
            return
        rec.op("act", lambda e: e.activation(out=sqt[:, :], in_=psG[1][:, :], func=AF.Square), reads=[psG[1]], writes=[sqt])
        rec.op("dve", lambda e: e.tensor_reduce(out=ss8[:, :], in_=sqt[:, :].rearrange("p (v r) -> p v r", v=8), axis=AX.X, op=ALU.add),
               reads=[sqt], writes=[ss8])
        rec.op("act", lambda e: e.activation(out=ss8[:, :], in_=ss8[:, :], func=AF.Sqrt, bias=L.eps[:, 0:1], scale=1.0 / 64.0),
               reads=[ss8, L.eps], writes=[ss8])
        rec.op("dve", lambda e: e.reciprocal(out=rs8[:, :], in_=ss8[:, :]), reads=[ss8], writes=[rs8])
        rec.op("dve", lambda e: e.memset(rs8[:, 6:8], 1.0), writes=[rs8])
        rec.op("dve", lambda e: e.tensor_tensor(out=xg[:, :], in0=psG[1][:, :], in1=G1t[:, :], op=ALU.mult), reads=[psG[1], G1t], writes=[xg])
        r = rope_ops(rec, "dve", xg, xg[:, 0:512], 8, 64, tab, c64, s64, t1b, t2b)
        r3 = r.rearrange("p (v r) -> p v r", v=8)
        rec.op("dve", lambda e, r3=r3: e.tensor_tensor(out=qk[:, 0, 256:512].rearrange("p (v r) -> p v r", v=4), in0=r3[:, 0:4, :],
                                                in1=rs8[:, 0:4].unsqueeze(2).to_broadcast([128, 4, 64]), op=ALU.mult),
               reads=[t1b, rs8], writes=[qk])
        for (lo, dst0) in ((4, 256), (6, 1024)):
            rec.op("dve", lambda e, lo=lo, dst0=dst0, r3=r3: e.tensor_tensor(
                out=qk[:, 1, dst0:dst0 + 256].rearrange("p (g d r) -> p g d r", g=2, d=2),
                in0=r3[:, lo:lo + 2, :].unsqueeze(2).to_broadcast([128, 2, 2, 64]),
                in1=rs8[:, lo:lo + 2].unsqueeze(2).unsqueeze(3).to_broadcast([128, 2, 2, 64]), op=ALU.mult),
                reads=[t1b, rs8], writes=[qk])
        if DBG["stop"] <= 5:
            return
        r = rope_ops(rec, "dve", psG[2], psG[2][:, 0:256], 4, 64, tab, c64, s64, t1b, t2b)
        rec.op("act", lambda e, r=r: e.activation(out=qk[:, 0, 1024:1280], in_=r, func=AF.Copy), reads=[t1b], writes=[qk])
        r = rope_ops(rec, "dve", psG[2], psG[2][:, 256:288], 1, 32, tab, c32, s32, t1b, t2b)
        rec.op("dve", lambda e, r=r: e.tensor_copy(out=qk[:, 1, 512:1024].rearrange("p (h c) -> p h c", h=4)[:, :, 64:96],
                                              in_=r.unsqueeze(1).to_broadcast([128, 4, 32])), reads=[t1b], writes=[qk])
        rec.op("act", lambda e: e.activation(out=vt[:, 0:4, 0:64], in_=psG[3][:, 0:256].rearrange("p (h d) -> p h d", h=4), func=AF.Copy),
               reads=[psG[3]], writes=[vt])
        rec.op("act", lambda e: e.activation(out=vt[:, 4:6, 0:64], in_=psG[3][:, 256:384].rearrange("p (h d) -> p h d", h=2), func=AF.Copy),
               reads=[psG[3]], writes=[vt])
        rec.op("act", lambda e: e.activation(out=vt[:, 10:12, 0:64], in_=psG[3][:, 384:512].rearrange("p (h d) -> p h d", h=2), func=AF.Copy),
               reads=[psG[3]], writes=[vt])
        if DBG["stop"] <= 6:
            return
        rec.op("act", lambda e: e.activation(out=sqt[:, 0:320], in_=psG[4][:, 0:320], func=AF.Square), reads=[psG[4]], writes=[sqt])
        rec.op("dve", lambda e: e.tensor_reduce(out=ssc[:, 0:1], in_=sqt[:, 0:192], axis=AX.X, op=ALU.add), reads=[sqt], writes=[ssc])
        rec.op("dve", lambda e: e.tensor_reduce(out=ssc[:, 1:2], in_=sqt[:, 192:320], axis=AX.X, op=ALU.add), reads=[sqt], writes=[ssc])
        rec.op("act", lambda e: e.activation(out=ssc[:, 0:1], in_=ssc[:, 0:1], func=AF.Sqrt, bias=L.eps[:, 0:1], scale=1.0 / 192.0),
               reads=[ssc, L.eps], writes=[ssc])
        rec.op("act", lambda e: e.activation(out=ssc[:, 1:2], in_=ssc[:, 1:2], func=AF.Sqrt, bias=L.eps[:, 0:1], scale=1.0 / 128.0),
               reads=[ssc, L.eps], writes=[ssc])
        rec.op("dve", lambda e: e.reciprocal(out=rsc[:, :], in_=ssc[:, :]), reads=[ssc], writes=[rsc])
        for (lo, hi, j) in ((0, 192, 0), (192, 320, 1)):
            rec.op("dve", lambda e, lo=lo, hi=hi, j=j: e.scalar_tensor_tensor(out=cn[:, lo:hi], in0=psG[4][:, lo:hi], scalar=rsc[:, j:j + 1],
                                                                             in1=Gct[:, lo:hi], op0=ALU.mult, op1=ALU.mult),
                   reads=[psG[4], rsc, Gct], writes=[cn])
        if DBG["stop"] <= 6.2:
            return
        for j, (lo, hi) in enumerate(((0, 128), (128, 192), (192, 320))):
            rec.op("pe", lambda e, j=j, lo=lo, hi=hi: e.transpose(out=psT[0:hi - lo, j * 128:(j + 1) * 128], in_=cn[:, lo:hi], identity=L.ident_b[:, :]),
                   reads=[cn, L.ident_b], writes=[psT])
        rec.op("dve", lambda e: e.tensor_copy(out=cnT[:, 0, :], in_=psT[:, 0:128]), reads=[psT], writes=[cnT])
        rec.op("dve", lambda e: e.tensor_copy(out=cnT[0:64, 1, :], in_=psT[0:64, 128:256]), reads=[psT], writes=[cnT])
        rec.op("dve", lambda e: e.tensor_copy(out=cnT[:, 2, :], in_=psT[:, 256:384]), reads=[psT], writes=[cnT])
        if DBG["stop"] <= 6.4:
            return
        rec.op("pe", lambda e: e.matmul(psU[0][:, 0:384], lhsT=cnT[:, 0, :], rhs=wuq[:, 0, :], start=True, stop=False), reads=[cnT, wuq], writes=[psU[0]])
        rec.op("pe", lambda e: e.matmul(psU[0][:, 0:384], lhsT=cnT[0:64, 1, :], rhs=wuq[0:64, 1, :], start=False, stop=True), reads=[cnT, wuq], writes=[psU[0]])
        rec.op("pe", lambda e: e.matmul(psU[1][:, 0:512], lhsT=cnT[:, 2, :], rhs=wukv[:, :], start=True, stop=True), reads=[cnT, wukv], writes=[psU[1]])
        if DBG["stop"] <= 6.6:
            return
        qc = psU[0][:, 0:384].rearrange("p (h c) -> p h c", h=4)
        kvc = psU[1][:, 0:512].rearrange("p (h c) -> p h c", h=4)
        qdst = qk[:, 0, 512:1024].rearrange("p (h c) -> p h c", h=4)
        kdst = qk[:, 1, 512:1024].rearrange("p (h c) -> p h c", h=4)
        rec.op("act", lambda e: e.activation(out=qdst[:, :, 0:64], in_=qc[:, :, 0:64], func=AF.Copy), reads=[psU[0]], writes=[qk])
        rec.op("act", lambda e: e.activation(out=kdst[:, :, 0:64], in_=kvc[:, :, 0:64], func=AF.Copy), reads=[psU[1]], writes=[qk])
        rec.op("act", lambda e: e.activation(out=vt[:, 6:10, 0:64], in_=kvc[:, :, 64:128], func=AF.Copy), reads=[psU[1]], writes=[vt])
        if DBG["stop"] <= 6.8:
            return
        rec.op("act", lambda e: e.activation(out=xg[:, 0:128].rearrange("p (h c) -> p h c", h=4), in_=qc[:, :, 64:96], func=AF.Copy), reads=[psU[0]], writes=[xg])
        if DBG["stop"] <= 6.85:
            return
        r = rope_ops(rec, "dve", xg, xg[:, 0:128], 4, 32, tab, c32, s32, t1b, t2b)
        if DBG["stop"] <= 6.9:
            return
        rec.op("dve", lambda e, r=r: e.tensor_copy(out=qdst[:, :, 64:96], in_=r.rearrange("p (h c) -> p h c", h=4)), reads=[t1b], writes=[qk])
        if DBG["stop"] <= 7:
            return
        for a in range(2):
            for (c0, c1) in ((0, 8), (8, 10)):
                for c in range(c0, c1):
                    rec.op("pe", lambda e, a=a, c=c, c0=c0: e.transpose(out=psT[:, (c - c0) * 128:(c - c0 + 1) * 128], in_=qk[:, a, c * 128:(c + 1) * 128],
                                                                        identity=L.ident_b[:, :]), reads=[qk, L.ident_b], writes=[psT])
                eng = "act" if a == 0 else "dve"
                if eng == "act":
                    rec.op("act", lambda e, a=a, c0=c0, c1=c1: e.activation(out=qkt[:, a, c0:c1, col:col + 128],
                                                                          in_=psT[:, 0:(c1 - c0) * 128].rearrange("p (c t) -> p c t", t=128), func=AF.Copy),
                           reads=[psT], writes=[qkt])
                else:
                    rec.op("dve", lambda e, a=a, c0=c0, c1=c1: e.tensor_copy(out=qkt[:, a, c0:c1, col:col + 128],
                                                                           in_=psT[:, 0:(c1 - c0) * 128].rearrange("p (c t) -> p c t", t=128)),
                           reads=[psT], writes=[qkt])
        if DBG["stop"] <= 8:
            return
        rec.dma("sp", vdst_ap, vt[:, :, :], reads=[vt])

    if DBG["stop"] <= 0:
        return
    for blk in range(n_lat_tiles // 4):
        qkt = qkT.next()
        for j in range(4):
            t = blk * 4 + j
            tile(x_src[t * 128:(t + 1) * 128, :], x_src, tab_src[t * 128:(t + 1) * 128, :], 0, qkt, j * 128,
                 outs["v"][:, :, t, :].rearrange("s p d -> p s d"), outs["v"])
        rec.dma("sp", outs["qT"][:, :, blk * 512:(blk + 1) * 512].rearrange("c p t -> p c t"), qkt[:, 0, :, :], reads=[qkt])
        rec.dma("sp", outs["kT"][:, :, blk * 512:(blk + 1) * 512].rearrange("c p t -> p c t"), qkt[:, 1, :, :], reads=[qkt])
    if do_ctx:
        qkt = qkT.next()
        for t in range(2):
            tile(xc_src[t * 128:(t + 1) * 128, :], xc_src, tab_src[TOK + t * 128:TOK + (t + 1) * 128, :], 1, qkt, t * 128,
                 outs["vc"][:, :, t, :].rearrange("s p d -> p s d"), outs["vc"])
        rec.dma("sp", outs["qTc"][:, :, :].rearrange("c p t -> p c t"), qkt[:, 0, :, 0:256], reads=[qkt])
        rec.dma("sp", outs["kTc"][:, :, :].rearrange("c p t -> p c t"), qkt[:, 1, :, 0:256], reads=[qkt])


def _pm(w, kc):
    k, n = w.shape
    return np.ascontiguousarray(w.reshape(kc, 128, n).transpose(1, 0, 2))


def prep_layer(inp, l):
    f = np.float32
    d = {}
    d["w_ada"] = _pm(inp["w_ada"][l], 8)
    d["b_adaT"] = np.ascontiguousarray(inp["b_ada"][l].reshape(48, 128).T)
    d["b_ada"] = np.ascontiguousarray(inp["b_ada"][l])
    d["w_in"] = _pm(inp["w_in"][l][:, _in_perm()], 8)
    wuq = np.zeros((256, 384), f)
    wuq[:192] = inp["mla_w_uq"][l]
    d["w_uq"] = _pm(wuq, 2)
    d["w_ukv"] = np.ascontiguousarray(inp["mla_w_ukv"][l])
    d["gain_g1"] = np.concatenate([np.tile(inp["gqa_q_norm_g"][l], 4), np.tile(inp["gqa_k_norm_g"][l], 2), np.ones(128, f)]).astype(f)
    d["gain_c"] = np.concatenate([inp["mla_q_norm_g"][l], inp["mla_kv_norm_g"][l]]).astype(f)
    return d


def prep_core_common(inp, core):
    b = core // 4
    r = core % 4
    d = {}
    cc = np.stack([inp["c"][b], inp["c_ctx"]], 1)
    d["cT"] = _pm(cc, 8)
    tok = np.arange(r * TOK, (r + 1) * TOK)
    tab = rope_tables(tok)
    ctab = np.zeros((CTX, 192), np.float32)
    ctab[:, 0:32] = 1.0
    ctab[:, 64:128] = 1.0
    d["rope_tab"] = np.ascontiguousarray(np.concatenate([tab, ctab], 0))
    d["c_ident"] = np.eye(128, dtype=np.float32)
    return d


NKT = 2 + 4 * NT
NWIN = NT + 2


def attention(cx, L, st, l, src, need_ctx, n_qb=TOK // 512, kt_limit=None):
    rec = cx.rec
    lam_init = 0.8 - 0.6 * float(np.exp(-0.3 * l))
    lamv_d = cx.din("lamv", [4, 32])
    subg_d = cx.din("subln_g", [64, 1])
    sink_d = cx.din("swa_sink", [4])
    mask_d = cx.din("c_masks", [128, 4, 128])
    lam = rec.sb("lam", [128, 1], F32, st)
    gsub = rec.sb("gsub", [64, 1], F32, st)
    esink = rec.sb("esink", [128, 4], F32, st)
    masks = rec.sb("masks", [128, 4, 128], BF16, st)
    lv = rec.sb("lv", [128, 4, 32], F32, st)
    lp = rec.sb("lp", [128, 2, 32], F32, st)
    ls = rec.sb("ls", [128, 2], F32, st)
    kts = Ring([rec.sb("ktb", [128, NKT * 128], BF16, st) for _ in range(2)])
    vtsr = Ring([rec.sb("vtb", [128, NKT, 128], BF16, st) for _ in range(2)])
    qts = Ring([rec.sb("qtb", [128, TOK], BF16, st) for _ in range(2)])
    qtc = rec.sb("qtc", [128, 10, 256], BF16, st)
    pTs = Ring([rec.sb("pT", [128, 1024], BF16, st) for _ in range(4)])
    zss = Ring([rec.sb("zs", [64, 512], F32, st) for _ in range(2)])
    rzs = Ring([rec.sb("rz", [64, 512], F32, st) for _ in range(2)])
    fa = rec.sb("fa", [64, 512], F32, st)
    fb = rec.sb("fb", [64, 512], F32, st)
    fc = rec.sb("fc", [64, 512], F32, st)
    ots = Ring([rec.sb("ot", [64, 512], BF16, st) for _ in range(2)])
    qmask = [[Ring([rec.sb("qm", [128, 512], BF16, st) for _ in range(2)]) for _ in range(2)] for _ in range(2)]
    for hp in range(2):
        for cp in range(2):
            for b_ in qmask[hp][cp].bufs:
                rec.op("pool", lambda e, b_=b_: e.memset(b_[:, :], 0.0), writes=[b_])
    if "kTwin" not in src:
        oh_d = cx.din("onehot", [128, 8])
        onehot = rec.sb("onehot", [128, 8], F32, st)
        hck = rec.sb("hck", [128, 4, 128], BF16, st)
        hcv = rec.sb("hcv", [128, 4, 128], BF16, st)
        hacc = rec.sb("hacc", [128, 128], F32, st)
        rec.dma("sp", onehot[:, :], oh_d[:, :], writes=[onehot])
    Sr = Ring([rec.ps("S", [128, 1024], F32, st) for _ in range(3)])
    accs = Ring([rec.ps("acc", [128, 512], F32, st) for _ in range(2)])
    dummy = None
    ndummy = 0
    rec.dma("sp", lv[:, :, :].rearrange("p a b -> p (a b)"), lamv_d[:, :].rearrange("a b -> (a b)").partition_broadcast(128), writes=[lv])
    rec.dma("sp", gsub[:, :], subg_d[:, :], writes=[gsub])
    rec.dma("sp", esink[:, :], sink_d[0:4].partition_broadcast(128), writes=[esink])
    rec.dma("pool", masks[:, :, :], mask_d[:, :, :], writes=[masks], cast=True)
    rec.op("dve", lambda e: e.tensor_tensor(out=lp[:, :, :], in0=lv[:, 0:4:2, :], in1=lv[:, 1:4:2, :], op=ALU.mult), reads=[lv], writes=[lp])
    rec.op("dve", lambda e: e.tensor_reduce(out=ls[:, :], in_=lp[:, :, :], axis=AX.X, op=ALU.add), reads=[lp], writes=[ls])
    rec.op("act", lambda e: e.activation(out=ls[:, :], in_=ls[:, :], func=AF.Exp), reads=[ls], writes=[ls])
    rec.op("act", lambda e: e.activation(out=esink[:, :], in_=esink[:, :], func=AF.Exp), reads=[esink], writes=[esink])
    rec.op("dve", lambda e: e.tensor_tensor(out=lam[:, :], in0=ls[:, 0:1], in1=ls[:, 1:2], op=ALU.subtract), reads=[ls], writes=[lam])
    rec.op("dve", lambda e: e.tensor_scalar(out=lam[:, :], in0=lam[:, :], scalar1=lam_init, scalar2=None, op0=ALU.add), reads=[lam], writes=[lam])
    rec.op("dve", lambda e: e.tensor_scalar(out=gsub[:, :], in0=gsub[:, :], scalar1=1.0 - lam_init, scalar2=None, op0=ALU.mult), reads=[gsub], writes=[gsub])
    if need_ctx:
        rec.dma("sp", qtc[:, :, :], src["qTc"][:, :, :].rearrange("c p t -> p c t"), writes=[qtc])

    def mm(out_ap, lhsT, rhs, start, stop, base, reads, writes):
        kw = {}
        if base == 96:
            kw["tile_position"] = (96, 0)
        rec.op("pe", lambda e: e.matmul(out_ap, lhsT=lhsT, rhs=rhs, start=start, stop=stop, skip_group_check=True, **kw), reads=reads, writes=writes)

    def finalize(accl, W, kind, h, dst_ap, sink_h=None):
        rzl = []
        zsl = []
        osl = []
        for a in accl:
            zs = zss.next()
            if sink_h is None:
                rec.op("dve", lambda e, a=a, zs=zs: e.tensor_scalar(out=zs[:, 0:W], in0=a[64:128, 0:W], scalar1=1.0, scalar2=None, op0=ALU.mult),
                       reads=[a], writes=[zs])
            else:
                rec.op("dve", lambda e, a=a, zs=zs: e.tensor_scalar(out=zs[:, 0:W], in0=a[64:128, 0:W], scalar1=esink[0:64, sink_h:sink_h + 1],
                                                                  scalar2=None, op0=ALU.add), reads=[a, esink], writes=[zs])
            zsl.append(zs)
            if kind == "A":
                ob = (fa, fb)[len(osl)]
                rec.op("dve", lambda e, a=a, ob=ob: e.tensor_scalar(out=ob[:, 0:W], in0=a[0:64, 0:W], scalar1=1.0, scalar2=None, op0=ALU.mult),
                       reads=[a], writes=[ob])
                osl.append(ob)
        for zs in zsl:
            rz = rzs.next()
            rec.op("dve", lambda e, zs=zs, rz=rz: e.reciprocal(out=rz[:, 0:W], in_=zs[:, 0:W]), reads=[zs], writes=[rz])
            rzl.append(rz)
        if kind == "A":
            accl = osl
        ot = ots.next()
        if kind != "A":
            a, rz = accl[0], rzl[0]
            rec.op("dve", lambda e: e.tensor_tensor(out=ot[:, 0:W], in0=a[0:64, 0:W], in1=rz[:, 0:W], op=ALU.mult), reads=[a, rz], writes=[ot])
        else:
            a1, a2 = accl
            r1, r2 = rzl
            rec.op("dve", lambda e: e.tensor_tensor(out=fa[:, 0:W], in0=fa[:, 0:W], in1=r1[:, 0:W], op=ALU.mult), reads=[r1], writes=[fa])
            rec.op("dve", lambda e: e.scalar_tensor_tensor(out=fb[:, 0:W], in0=fb[:, 0:W], scalar=lam[0:64, 0:1], in1=r2[:, 0:W],
                                                           op0=ALU.mult, op1=ALU.mult), reads=[r2, lam], writes=[fb])
            rec.op("dve", lambda e: e.tensor_tensor(out=fa[:, 0:W], in0=fa[:, 0:W], in1=fb[:, 0:W], op=ALU.subtract), reads=[fb], writes=[fa])
            rec.op("pool", lambda e: e.tensor_tensor(out=fc[:, 0:W], in0=fa[:, 0:W], in1=fa[:, 0:W], op=ALU.mult), reads=[fa], writes=[fc])
            pm = Sr.next()
            rec.op("pe", lambda e: e.matmul(pm[0:64, 0:W], lhsT=L.ones64[:, :], rhs=fc[:, 0:W], start=True, stop=True), reads=[fc, L.ones64], writes=[pm])
            rec.op("act", lambda e: e.activation(out=fb[:, 0:W], in_=pm[0:64, 0:W], func=AF.Sqrt, bias=L.eps[0:64, 0:1], scale=1.0), reads=[pm, L.eps], writes=[fb])
            rec.op("dve", lambda e: e.reciprocal(out=fc[:, 0:W], in_=fb[:, 0:W]), reads=[fb], writes=[fc])
            rec.op("dve", lambda e: e.scalar_tensor_tensor(out=ot[:, 0:W], in0=fa[:, 0:W], scalar=gsub[:, 0:1], in1=fc[:, 0:W],
                                                           op0=ALU.mult, op1=ALU.mult), reads=[fa, fc, gsub], writes=[ot])
        rec.dma("sp", dst_ap, ot[:, 0:W], reads=[ot])

    def attend(ktb, vtb, q_ap_fn, qbuf, comps, scale, ktiles, W, kind, h, dst_ap, sink_h=None):
        nu = len(comps)
        accl = [accs.next() for _ in range(nu)]
        if nu == 2:
            groups = [[(kt, 0), (kt, 1)] for kt in ktiles]
        else:
            groups = [[(kt, 0) for kt in ktiles[i:i + 2]] for i in range(0, len(ktiles), 2)]
        started = [False] * nu

        def qk(grp):
            S = Sr.next()
            for j, (kt, u) in enumerate(grp):
                base, K = comps[u]
                qap, qb_ = q_ap_fn(u, base, K)
                mm(S[:, j * W:(j + 1) * W], ktb[base:base + K, kt * 128:(kt + 1) * 128], qap, True, True, base, [ktb, qb_], [S])
            return S
        PD = DBG.get("pd", 2)
        Sq = [qk(groups[i]) for i in range(min(PD, len(groups)))]
        for gi, grp in enumerate(groups):
            if gi + PD < len(groups):
                Sq.append(qk(groups[gi + PD]))
            S = Sq.pop(0)
            P = pTs.next()
            n = len(grp) * W
            if DBG["att"] <= 1:
                continue
            rec.op("act", lambda e, S=S, P=P, n=n: e.activation(out=P[:, 0:n], in_=S[:, 0:n], func=AF.Exp, scale=scale), reads=[S], writes=[P])
            if DBG["att"] <= 2:
                continue
            for _ in range(ndummy if W == 512 else 0):
                rec.op("pe", lambda e: e.matmul(dummy[:, 0:128 * DBG.get("dumw", 2)], lhsT=L.ident_b[:, :], rhs=masks[:, 0:DBG.get("dumw", 2), :].rearrange("p a b -> p (a b)"), start=True, stop=True,
                                                skip_group_check=True), reads=[], writes=[])
            for j, (kt, u) in enumerate(grp):
                a = accl[u]
                last = (gi == len(groups) - 1) and (nu == 2 or j == len(grp) - 1)
                mm(a[:, 0:W], vtb[:, kt, :], P[:, j * W:(j + 1) * W], not started[u], last, 0, [vtb, P], [a])
                started[u] = True
        if DBG["att"] >= 4:
            finalize(accl, W, kind, h, dst_ap, sink_h)

    def kall(r, c):
        if "ga" in src:
            b_ = src["ga"][c]
            return b_[r * 128:(r + 1) * 128, :], [b_]
        return src["kTall"][r, c, :, :], []

    def vall(r, slot):
        if "ga" in src:
            b_ = src["ga"][10 + slot]
            return b_[r * 128:(r + 1) * 128, :].rearrange("p (t d) -> p t d", d=128), [b_]
        return src["vall"][r, slot, :, :, :], []

    def load_kv(c, slot):
        ktb = kts.next()
        vtb = vtsr.next()
        rec.dma("sp", ktb[:, 0:256], src["kTc"][c, :, :], writes=[ktb])
        rec.dma("sp", vtb[:, 0:2, :], src["vc"][slot, :, :, :], writes=[vtb])
        for r in range(4):
            ap_, rd = kall(r, c)
            rec.dma("sp", ktb[:, 256 + r * TOK:256 + (r + 1) * TOK], ap_, reads=rd, writes=[ktb])
            ap_, rd = vall(r, slot)
            rec.dma("sp", vtb[:, 2 + r * NT:2 + (r + 1) * NT, :], ap_, reads=rd, writes=[vtb])
        return ktb, vtb

    def load_v(slot):
        vtb = vtsr.next()
        rec.dma("sp", vtb[:, 0:2, :], src["vc"][slot, :, :, :], writes=[vtb])
        for r in range(4):
            ap_, rd = vall(r, slot)
            rec.dma("sp", vtb[:, 2 + r * NT:2 + (r + 1) * NT, :], ap_, reads=rd, writes=[vtb])
        return vtb

    ktiles_all = list(range(NKT)) if kt_limit is None else list(range(kt_limit))
    jobs = []
    for i in range(2):
        jobs.append((i, [(h, [((h % 2) * 64, 32), ((h % 2) * 64 + 32, 32)], h, h // 2, (h % 2) * 64) for h in (2 * i, 2 * i + 1)], 32 ** -0.5, "A"))
    for g in range(2):
        jobs.append((2 + g, [(h, [((h % 2) * 64, 64)], 4 + g, 2 + h // 2, (h % 2) * 64) for h in (2 * g, 2 * g + 1)], 64 ** -0.5, "B"))
    for h in range(4):
        jobs.append((4 + h, [(h, [(0, 96)], 6 + h, 4 + h // 2, (h % 2) * 64)], 96 ** -0.5, "C"))
    if DBG["att"] <= 0:
        return
    hjobs = []
    for (c, heads, scale, kind) in jobs:
        if kind not in DBG["kinds"]:
            continue
        prev_slot = None
        for hi, (h, comps, slot, oc, orow) in enumerate(heads):
            hjobs.append(dict(c=c, h=h, comps=comps, slot=slot, oc=oc, orow=orow, scale=scale, kind=kind,
                              ldk=(hi == 0), ldv=(slot != prev_slot)))
            prev_slot = slot
    state = {"ktb": None, "vtb": None, "qtb": None}

    def issue_loads(j):
        if j["ldk"]:
            qtb = qts.next()
            rec.dma("sp", qtb[:, :], src["qT"][j["c"], :, :], writes=[qtb])
            ktb = kts.next()
            rec.dma("sp", ktb[:, 0:256], src["kTc"][j["c"], :, :], writes=[ktb])
            for r in range(4):
                ap_, rd = kall(r, j["c"])
                rec.dma("sp", ktb[:, 256 + r * TOK:256 + (r + 1) * TOK], ap_, reads=rd, writes=[ktb])
            j["ktb"], j["qtb"] = ktb, qtb
        if j["ldv"]:
            j["vtb"] = load_v(j["slot"])

    if hjobs:
        issue_loads(hjobs[0])
    for ji, j in enumerate(hjobs):
        for k_ in ("ktb", "vtb", "qtb"):
            if k_ in j:
                state[k_] = j[k_]
        ktb, vtb, qtb = state["ktb"], state["vtb"], state["qtb"]
        if ji + 1 < len(hjobs):
            issue_loads(hjobs[ji + 1])
        c, h, comps, oc, orow, scale, kind = j["c"], j["h"], j["comps"], j["oc"], j["orow"], j["scale"], j["kind"]

        def masked_q(src_fn, src_buf, W):
            bl = []
            for cp in range(2):
                mb = qmask[h % 2][cp].next()
                rows = (h % 2) * 64 + cp * 32
                rec.op("pool", lambda e, mb=mb, rows=rows: e.tensor_copy(out=mb[rows:rows + 32, 0:W], in_=src_fn(rows)), reads=[src_buf], writes=[mb])
                bl.append(mb)
            return bl
        for qb in range(n_qb):
            if kind == "A":
                bl = masked_q(lambda rows, qb=qb, qtb=qtb: qtb[rows:rows + 32, qb * 512:(qb + 1) * 512], qtb, 512)
                attend(ktb, vtb, lambda u, base, K, bl=bl: (bl[u][:, 0:512], bl[u]), None, [(0, 128), (0, 128)], scale, ktiles_all, 512,
                       kind, h, src["OT"][oc, orow:orow + 64, qb * 512:(qb + 1) * 512])
            else:
                attend(ktb, vtb, lambda u, base, K, qb=qb, qtb=qtb: (qtb[base:base + K, qb * 512:(qb + 1) * 512], qtb), None, comps, scale, ktiles_all, 512,
                       kind, h, src["OT"][oc, orow:orow + 64, qb * 512:(qb + 1) * 512])
        if need_ctx:
            if kind == "A":
                bl = masked_q(lambda rows, c=c: qtc[rows:rows + 32, c, :], qtc, 256)
                attend(ktb, vtb, lambda u, base, K, bl=bl: (bl[u][:, 0:256], bl[u]), None, [(0, 128), (0, 128)], scale, [0, 1], 256, kind, h,
                       src["OTc"][oc, orow:orow + 64, :])
            else:
                attend(ktb, vtb, lambda u, base, K, c=c: (qtc[base:base + K, c, :], qtc), None, comps, scale, [0, 1], 256, kind, h,
                       src["OTc"][oc, orow:orow + 64, :])
    for g in range(2 if "D" in DBG["kinds"] else 0):
        c = 8 + g
        slot = 10 + g
        qtb = qts.next()
        rec.dma("sp", qtb[:, :], src["qT"][c, :, :], writes=[qtb])
        ktb = kts.next()
        vtb = vtsr.next()
        rec.dma("sp", ktb[:, 0:256], src["kTc"][c, :, :], writes=[ktb])
        rec.dma("sp", vtb[:, 0:2, :], src["vc"][slot, :, :, :], writes=[vtb])
        if "kTwin" in src:
            rec.dma("sp", ktb[:, 256:256 + NWIN * 128], src["kTwin"][g, :, :], writes=[ktb])
            rec.dma("sp", vtb[:, 2:2 + NWIN, :], src["vwin"][g, :, :, :], writes=[vtb])
        else:
            rec.dma("sp", ktb[:, 384:384 + TOK], src["kTown"][c, :, :], writes=[ktb])
            rec.dma("sp", vtb[:, 3:3 + NT, :], src["vown"][slot, :, :, :], writes=[vtb])
            for side in range(2):
                kcol = (TOK - 128) if side == 0 else 0
                vt_i = (NT - 1) if side == 0 else 0
                for r_ in range(4):
                    ap_, rd = kall(r_, c)
                    rec.dma("sp", hck[:, r_, :], ap_[:, kcol:kcol + 128], reads=rd, writes=[hck])
                    ap_, rd = vall(r_, slot)
                    rec.dma("sp", hcv[:, r_, :], ap_[:, vt_i, :], reads=rd, writes=[hcv])
                kd = ktb[:, 256:384] if side == 0 else ktb[:, 384 + TOK:384 + TOK + 128]
                vd = vtb[:, 2, :] if side == 0 else vtb[:, 3 + NT, :]
                for (cand, cb, dst_ap, dst_b) in ((hck, hck, kd, ktb), (hcv, hcv, vd, vtb)):
                    rec.op("dve", lambda e, cand=cand, side=side: e.tensor_scalar(out=hacc[:, :], in0=cand[:, 0, :], scalar1=onehot[:, side * 4:side * 4 + 1],
                                                                               scalar2=None, op0=ALU.mult), reads=[cb, onehot], writes=[hacc])
                    for r_ in range(1, 4):
                        last = r_ == 3
                        rec.op("dve", lambda e, cand=cand, side=side, r_=r_, last=last, dst_ap=dst_ap: e.scalar_tensor_tensor(
                            out=dst_ap if last else hacc[:, :], in0=cand[:, r_, :], scalar=onehot[:, side * 4 + r_:side * 4 + r_ + 1], in1=hacc[:, :],
                            op0=ALU.mult, op1=ALU.add), reads=[cb, onehot, hacc], writes=[dst_b] if last else [hacc])
        for h in (2 * g, 2 * g + 1):
            base = (h % 2) * 64
            for qb in range(n_qb):
                acc = accs.next()
                for s in range(4):
                    j = qb * 4 + s
                    tiles = [0, 1, 2 + j, 3 + j, 4 + j]
                    S = Sr.next()
                    P = pTs.next()
                    for i, kt in enumerate(tiles):
                        mm(S[:, i * 128:(i + 1) * 128], ktb[base:base + 64, kt * 128:(kt + 1) * 128], qtb[base:base + 64, j * 128:(j + 1) * 128],
                           True, True, base, [ktb, qtb], [S])
                    rec.op("act", lambda e, S=S, P=P: e.activation(out=P[:, 0:640], in_=S[:, 0:640], func=AF.Exp, scale=64 ** -0.5), reads=[S], writes=[P])
                    mp = 2 if j == 0 else 0
                    mn = 3 if j == NT - 1 else 1
                    rec.op("pool", lambda e, P=P, mp=mp: e.tensor_tensor(out=P[:, 256:384], in0=P[:, 256:384], in1=masks[:, mp, :], op=ALU.mult),
                           reads=[masks], writes=[P])
                    rec.op("pool", lambda e, P=P, mn=mn: e.tensor_tensor(out=P[:, 512:640], in0=P[:, 512:640], in1=masks[:, mn, :], op=ALU.mult),
                           reads=[masks], writes=[P])
                    for i, kt in enumerate(tiles):
                        mm(acc[:, s * 128:(s + 1) * 128], vtb[:, kt, :], P[:, i * 128:(i + 1) * 128], i == 0, i == 4, 0, [vtb, P], [acc])
                finalize([acc], 512, "D", h, src["OT"][6 + h // 2, base:base + 64, qb * 512:(qb + 1) * 512], sink_h=h)
            if need_ctx:
                attend(ktb, vtb, lambda u, b_, K, c=c: (qtc[b_:b_ + K, c, :], qtc), None, [(base, 64)], 64 ** -0.5, [0, 1], 256, "D", h,
                       src["OTc"][6 + h // 2, base:base + 64, :], sink_h=h)


def phase3(cx, L, st, x_srcs, wts):
    rec = cx.rec
    wout = rec.sb("wout", [128, 8, 1024], BF16, st)
    rw = rec.sb("rw", [128, 8, 16], F32, st)
    rbias = rec.sb("rbias", [128, 16], F32, st)
    lnt = [rec.sb("lnt", [128, 1024], F32, st) for _ in range(4)]
    pre = wts.get("bf16", False)
    wq = "sp" if pre else "pool"
    for kc in range(8):
        rec.dma(wq, wout[:, kc, :], wts["w_out"][:, kc, :], writes=[wout], cast=not pre)
    rec.dma("sp", rw[:, :, :], wts["router_w"][:, :, :], writes=[rw])
    rec.dma("sp", rbias[:, :], wts["router_bias"][0:16].partition_broadcast(128), writes=[rbias])
    for i in range(4):
        rec.dma("sp", lnt[i][:, :], wts["ln"][i, :].partition_broadcast(128), writes=[lnt[i]])
    x1s = [rec.sb("x1s", [128, 1024], F32, st) for _ in range(4)]
    xts = Ring([rec.sb("xt3", [128, 1024], F32, st) for _ in range(2)])
    u = rec.sb("u", [128, 1024], F32, st)
    tmp = rec.sb("tmp", [128, 1024], F32, st)
    h2Tf = rec.sb("h2Tf", [128, 8, 128], F32, st)
    h2T = rec.sb("h2T", [128, 8, 512], BF16, st)
    otin = rec.sb("otin", [128, 8, 512], BF16, st)
    actT = rec.sb("actT", [128, 16, 2, 512], BF16, st)
    wgus = Ring([rec.sb("wgu", [128, 8, 512], BF16, st) for _ in range(3)])
    wds = Ring([rec.sb("wd", [128, 2, 1024], BF16, st) for _ in range(4)])
    gates = rec.sb("gates", [128, 4, 16], F32, st)
    st6 = rec.sb("st6b", [128, 2, 6], F32, st)
    mv = rec.sb("mvb", [128, 2], F32, st)
    sq1 = rec.sb("sq1b", [128, 1], F32, st)
    rstd = rec.sb("rstdb", [128, 1], F32, st)
    s16 = rec.sb("s16", [128, 16], F32, st)
    sel = rec.sb("sel", [128, 16], F32, st)
    sel2 = rec.sb("sel2", [128, 16], F32, st)
    eq = rec.sb("eq", [128, 16], F32, st)
    m1 = rec.sb("m1", [128, 4], F32, st)
    m2 = rec.sb("m2", [128, 4], F32, st)
    gs = rec.sb("gs", [128, 4], F32, st)
    gm = rec.sb("gm", [128, 1], F32, st)
    sil = Ring([rec.sb("sil", [128, 256], F32, st) for _ in range(2)])
    actb = Ring([rec.sb("actb", [128, 256], BF16, st) for _ in range(2)])
    B = [rec.ps("B", [128, 512], F32, st) for _ in range(8)]
    psTb = rec.buf("psTb", B[7].t[:, :].bitcast(BF16))

    def ln_stats(src):
        for i in range(2):
            rec.op("dve", lambda e, i=i: e.bn_stats(out=st6[:, i, :], in_=src[:, i * 512:(i + 1) * 512]), reads=[src], writes=[st6])
        rec.op("dve", lambda e: e.bn_aggr(out=mv[:, :], in_=st6[:, :, :].rearrange("p a b -> p (a b)")), reads=[st6], writes=[mv])
        rec.op("act", lambda e: e.activation(out=sq1[:, :], in_=mv[:, 1:2], func=AF.Sqrt, bias=L.eps[:, 0:1], scale=1.0), reads=[mv, L.eps], writes=[sq1])
        rec.op("dve", lambda e: e.reciprocal(out=rstd[:, :], in_=sq1[:, :]), reads=[sq1], writes=[rstd])

    def gated_ln(x_in, ybanks, Gt, gt, bt, dst):
        for hh in range(2):
            rec.op("dve", lambda e, hh=hh: e.tensor_tensor(out=tmp[:, hh * 512:(hh + 1) * 512], in0=ybanks[hh][:, :], in1=Gt[:, hh * 512:(hh + 1) * 512],
                                                           op=ALU.mult), reads=[ybanks[hh], Gt], writes=[tmp])
        rec.op("dve", lambda e: e.scalar_tensor_tensor(out=u[:, :], in0=x_in[:, :], scalar=ALPHA, in1=tmp[:, :], op0=ALU.mult, op1=ALU.add),
               reads=[x_in, tmp], writes=[u])
        ln_stats(u)
        rec.op("dve", lambda e: e.tensor_scalar(out=u[:, :], in0=u[:, :], scalar1=mv[:, 0:1], scalar2=rstd[:, 0:1], op0=ALU.subtract, op1=ALU.mult),
               reads=[mv, rstd], writes=[u])
        rec.op("dve", lambda e: e.tensor_tensor(out=u[:, :], in0=u[:, :], in1=gt[:, :], op=ALU.mult), reads=[gt], writes=[u])
        rec.op("dve", lambda e: e.tensor_tensor(out=dst[:, :], in0=u[:, :], in1=bt[:, :], op=ALU.add), reads=[u, bt], writes=[dst])

    for (x_src, OT, x_dst, cond, ntok) in x_srcs:
        nblk = (ntok + 511) // 512
        for blk in range(nblk):
            nt = min(4, (ntok - blk * 512) // 128)
            ncol = nt * 128
            rec.dma("sp", otin[:, :, 0:ncol], OT[:, :, blk * 512:blk * 512 + ncol].rearrange("c p t -> p c t"), writes=[otin])
            for t in range(nt):
                xt = xts.next()
                r0 = blk * 512 + t * 128
                rec.dma("sp", xt[:, :], x_src[r0:r0 + 128, :], writes=[xt])
                for hh in range(2):
                    for kc in range(8):
                        rec.op("pe", lambda e, hh=hh, kc=kc, t=t: e.matmul(B[hh][:, :], lhsT=otin[:, kc, t * 128:(t + 1) * 128],
                                                                       rhs=wout[:, kc, hh * 512:(hh + 1) * 512], start=(kc == 0), stop=(kc == 7)),
                               reads=[otin, wout], writes=[B[hh]])
                x1 = x1s[t]
                gated_ln(xt, B[0:2], L.Gb[0][cond], lnt[0], lnt[1], x1)
                ln_stats(x1)
                rec.op("dve", lambda e, x1=x1: e.tensor_scalar(out=tmp[:, :], in0=x1[:, :], scalar1=mv[:, 0:1], scalar2=rstd[:, 0:1],
                                                               op0=ALU.subtract, op1=ALU.mult), reads=[x1, mv, rstd], writes=[tmp])
                for kc in range(8):
                    bk = B[2 + kc // 4]
                    rec.op("pe", lambda e, kc=kc, bk=bk: e.transpose(out=bk[:, (kc % 4) * 128:(kc % 4 + 1) * 128], in_=tmp[:, kc * 128:(kc + 1) * 128],
                                                                     identity=L.ident_f[:, :]), reads=[tmp, L.ident_f], writes=[bk])
                for kc in range(8):
                    bk = B[2 + kc // 4]
                    rec.op("dve", lambda e, kc=kc, bk=bk, cond=cond: e.tensor_scalar(out=h2Tf[:, kc, :], in0=bk[:, (kc % 4) * 128:(kc % 4 + 1) * 128],
                                                                          scalar1=L.modT[:, 32 + kc, cond:cond + 1], scalar2=L.modT[:, 24 + kc, cond:cond + 1],
                                                                          op0=ALU.mult, op1=ALU.add), reads=[bk, L.modT], writes=[h2Tf])
                rec.op("pool", lambda e, t=t: e.tensor_copy(out=h2T[:, :, t * 128:(t + 1) * 128], in_=h2Tf[:, :, :]), reads=[h2Tf], writes=[h2T])
                for kc in range(8):
                    rec.op("pe", lambda e, kc=kc: e.matmul(B[4][:, 0:16], lhsT=h2Tf[:, kc, :], rhs=rw[:, kc, :], start=(kc == 0), stop=(kc == 7)),
                           reads=[h2Tf, rw], writes=[B[4]])
                rec.op("act", lambda e: e.activation(out=s16[:, :], in_=B[4][:, 0:16], func=AF.Sigmoid), reads=[B[4]], writes=[s16])
                v44 = lambda b_: b_[:, :].rearrange("p (g k) -> p g k", g=4)
                bc4 = lambda b_: b_[:, :].unsqueeze(2).to_broadcast([128, 4, 4])
                rec.op("dve", lambda e: e.tensor_tensor(out=sel[:, :], in0=s16[:, :], in1=rbias[:, :], op=ALU.add), reads=[s16, rbias], writes=[sel])
                rec.op("dve", lambda e: e.tensor_reduce(out=m1[:, :], in_=v44(sel), axis=AX.X, op=ALU.max), reads=[sel], writes=[m1])
                rec.op("dve", lambda e: e.tensor_tensor(out=v44(eq), in0=v44(sel), in1=bc4(m1), op=ALU.is_equal), reads=[sel, m1], writes=[eq])
                rec.op("dve", lambda e: e.scalar_tensor_tensor(out=sel2[:, :], in0=eq[:, :], scalar=-1e9, in1=sel[:, :], op0=ALU.mult, op1=ALU.add),
                       reads=[eq, sel], writes=[sel2])
                rec.op("dve", lambda e: e.tensor_reduce(out=m2[:, :], in_=v44(sel2), axis=AX.X, op=ALU.max), reads=[sel2], writes=[m2])
                rec.op("dve", lambda e: e.tensor_tensor(out=gs[:, :], in0=m1[:, :], in1=m2[:, :], op=ALU.add), reads=[m1, m2], writes=[gs])
                rec.op("dve", lambda e: e.tensor_reduce(out=gm[:, :], in_=gs[:, :], axis=AX.X, op=ALU.max), reads=[gs], writes=[gm])
                rec.op("dve", lambda e: e.tensor_scalar(out=gs[:, :], in0=gs[:, :], scalar1=gm[:, 0:1], scalar2=None, op0=ALU.is_equal), reads=[gm], writes=[gs])
                rec.op("dve", lambda e: e.tensor_tensor(out=v44(eq), in0=v44(sel), in1=bc4(m2), op=ALU.is_ge), reads=[sel, m2], writes=[eq])
                rec.op("dve", lambda e: e.tensor_tensor(out=v44(eq), in0=v44(eq), in1=bc4(gs), op=ALU.mult), reads=[gs], writes=[eq])
                rec.op("dve", lambda e: e.tensor_tensor(out=eq[:, :], in0=eq[:, :], in1=s16[:, :], op=ALU.mult), reads=[s16], writes=[eq])
                rec.op("dve", lambda e: e.tensor_reduce(out=gm[:, :], in_=eq[:, :], axis=AX.X, op=ALU.add), reads=[eq], writes=[gm])
                rec.op("dve", lambda e: e.reciprocal(out=gm[:, :], in_=gm[:, :]), reads=[gm], writes=[gm])
                rec.op("dve", lambda e, t=t: e.tensor_scalar(out=gates[:, t, :], in0=eq[:, :], scalar1=gm[:, 0:1], scalar2=None, op0=ALU.mult),
                       reads=[eq, gm], writes=[gates])
            items = [(ex, t) for ex in range(NE) for t in range(nt)]
            cur_w = [None]

            def mm_part(ex, t):
                if t == 0:
                    cur_w[0] = wgus.next()
                    rec.dma(wq, cur_w[0][:, :, :], wts["wgu"][ex, :, :, :], writes=[cur_w[0]], cast=not pre)
                wgu = cur_w[0]
                bk = B[(ex * nt + t) % 4]
                for kc in range(8):
                    rec.op("pe", lambda e, kc=kc, t=t, bk=bk, wgu=wgu: e.matmul(bk[:, :], lhsT=h2T[:, kc, t * 128:(t + 1) * 128], rhs=wgu[:, kc, :],
                                                                             start=(kc == 0), stop=(kc == 7)), reads=[h2T, wgu], writes=[bk])
                return bk

            def post_part(ex, t, bk):
                sl_ = sil.next()
                ab = actb.next()
                rec.op("act", lambda e, bk=bk, sl_=sl_: e.activation(out=sl_[:, :], in_=bk[:, 0:256], func=AF.Silu), reads=[bk], writes=[sl_])
                rec.op("dve", lambda e, bk=bk, sl_=sl_, ab=ab, t=t, ex=ex: e.scalar_tensor_tensor(out=ab[:, :], in0=sl_[:, :], scalar=gates[:, t, ex:ex + 1],
                                                                                                in1=bk[:, 256:512], op0=ALU.mult, op1=ALU.mult),
                       reads=[sl_, gates, bk], writes=[ab])
                for fc in range(2):
                    rec.op("pe", lambda e, fc=fc, ab=ab: e.transpose(out=psTb[:, fc * 128:(fc + 1) * 128], in_=ab[:, fc * 128:(fc + 1) * 128],
                                                                     identity=L.ident_b[:, :]), reads=[ab, L.ident_b], writes=[psTb, B[7]])
                rec.op("act", lambda e, ex=ex, t=t: e.activation(out=actT[:, ex, :, t * 128:(t + 1) * 128],
                                                                 in_=psTb[:, 0:256].rearrange("p (f c) -> p f c", f=2), func=AF.Copy),
                       reads=[psTb, B[7]], writes=[actT])

            bk_cur = mm_part(*items[0])
            for ii, (ex, t) in enumerate(items):
                bk_next = mm_part(*items[ii + 1]) if ii + 1 < len(items) else None
                post_part(ex, t, bk_cur)
                bk_cur = bk_next
            for ex in range(NE):
                wd = wds.next()
                rec.dma(wq, wd[:, :, :], wts["wd"][ex, :, :, :], writes=[wd], cast=not pre)
                for t in range(nt):
                    for hh in range(2):
                        for fc in range(2):
                            rec.op("pe", lambda e, ex=ex, t=t, hh=hh, fc=fc, wd=wd: e.matmul(
                                B[t * 2 + hh][:, :], lhsT=actT[:, ex, fc, t * 128:(t + 1) * 128], rhs=wd[:, fc, hh * 512:(hh + 1) * 512],
                                start=(ex == 0 and fc == 0), stop=(ex == NE - 1 and fc == 1)), reads=[actT, wd], writes=[B[t * 2 + hh], psTb] if t == 3 and hh == 1 else [B[t * 2 + hh]])
            for t in range(nt):
                r0 = blk * 512 + t * 128
                xo = xts.next()
                gated_ln(x1s[t], B[2 * t:2 * t + 2], L.Gb[1][cond], lnt[2], lnt[3], xo)
                rec.dma("sp", x_dst[r0:r0 + 128, :], xo[:, :], reads=[xo])


def build_A(n_lat_tiles=NT, do_ctx=True):
    cx = Ctx()
    L = NS()
    rec = cx.rec
    setup_consts(cx, L)
    st = ExitStack()
    layer_setup(cx, L, st)
    st.close()
    rec.barrier()
    x_src = cx.din("x_own", [TOK, 1024])
    xc_src = cx.din("xc", [CTX, 1024])
    tab_src = cx.din("rope_tab", [TOK + CTX, 192])
    outs = {"qT": cx.dout("qT", [10, 128, TOK], BF16), "kT": cx.dout("kT", [10, 128, TOK], BF16), "v": cx.dout("v", [12, 128, NT, 128], BF16),
            "qTc": cx.dout("qTc", [10, 128, CTX], BF16), "kTc": cx.dout("kTc", [10, 128, CTX], BF16), "vc": cx.dout("vc", [12, 128, 2, 128], BF16)}
    st = ExitStack()
    phase1(cx, L, st, x_src, xc_src, tab_src, outs, n_lat_tiles=n_lat_tiles, do_ctx=do_ctx)
    rec.wait_all_on("sp")
    rec.emit()
    return cx


def build_B(l, need_ctx):
    cx = Ctx()
    L = NS()
    rec = cx.rec
    setup_consts(cx, L)
    st = ExitStack()
    layer_setup(cx, L, st)
    st.close()
    rec.barrier()
    src = {"qT": cx.din("qT", [10, 128, TOK], BF16), "kTall": cx.din("kTall", [4, 10, 128, TOK], BF16),
           "vall": cx.din("vall", [4, 12, 128, NT, 128], BF16), "kTc": cx.din("kTc", [10, 128, CTX], BF16),
           "vc": cx.din("vc", [12, 128, 2, 128], BF16), "qTc": cx.din("qTc", [10, 128, CTX], BF16),
           "kTwin": cx.din("kTwin", [2, 128, NWIN * 128], BF16), "vwin": cx.din("vwin", [2, 128, NWIN, 128], BF16),
           "OT": (cx.dout if DBG.get("export") else cx.dint)("OT", [8, 128, TOK], BF16),
           "OTc": (cx.dout if DBG.get("export") else cx.dint)("OTc", [8, 128, CTX], BF16)}
    st = ExitStack()
    attention(cx, L, st, l, src, need_ctx)
    st.close()
    rec.barrier()
    wts = {"w_out": cx.din("w_out", [128, 8, 1024]), "wgu": cx.din("wgu", [NE, 128, 8, 512]), "wd": cx.din("wd", [NE, 128, 2, 1024]),
           "router_w": cx.din("router_w", [128, 8, 16]), "router_bias": cx.din("router_bias", [16]), "ln": cx.din("ln", [4, 1024])}
    x_src = cx.din("x_own", [TOK, 1024])
    x_dst = cx.dout("x_next", [TOK, 1024])
    xs = [(x_src, src["OT"], x_dst, 0, TOK)]
    if need_ctx:
        xc_src = cx.din("xc", [CTX, 1024])
        xc_dst = cx.dout("xc_next", [CTX, 1024])
        xs.append((xc_src, src["OTc"], xc_dst, 1, CTX))
    st = ExitStack()
    if DBG.get("p3", True):
        phase3(cx, L, st, xs, wts)
    else:
        rec.dma("sp", x_dst[0:128, :], x_src[0:128, :], key="D_dbg")
    rec.wait_all_on("sp")
    rec.emit()
    return cx


def prep_B_weights(inp, l):
    d = {}
    d["w_out"] = _pm(inp["w_out"][l], 8)
    d["wgu"] = np.ascontiguousarray(np.stack([_pm(np.concatenate([inp["exp_w_gate"][l, e], inp["exp_w_up"][l, e]], 1), 8) for e in range(NE)]))
    d["wd"] = np.ascontiguousarray(np.stack([_pm(inp["exp_w_down"][l, e], 2) for e in range(NE)]))
    d["router_w"] = _pm(inp["router_w"], 8)
    d["router_bias"] = np.ascontiguousarray(inp["router_bias"])
    d["ln"] = np.ascontiguousarray(np.stack([inp["ln1_g"][l], inp["ln1_b"][l], inp["ln2_g"][l], inp["ln2_b"][l]]))
    d["lamv"] = np.ascontiguousarray(np.stack([inp["diff_lambda_q1"][l], inp["diff_lambda_k1"][l], inp["diff_lambda_q2"][l], inp["diff_lambda_k2"][l]]))
    d["subln_g"] = np.ascontiguousarray(inp["diff_subln_g"][l].reshape(64, 1))
    d["swa_sink"] = np.ascontiguousarray(inp["swa_sink"][l])
    return d


def band_masks(r):
    ki = np.arange(128)[:, None]
    qi = np.arange(128)[None, :]
    mprev = (qi <= ki).astype(np.float32)
    mnext = (ki <= qi).astype(np.float32)
    m = np.stack([mprev, mnext, mprev * (0.0 if r == 0 else 1.0), mnext * (0.0 if r == 3 else 1.0)], 1)
    return np.ascontiguousarray(m.astype(np.float32))


KVR = 1280 + 1536


def build_fused():
    cx = Ctx()
    L = NS()
    rec = cx.rec
    nc = cx.nc
    setup_consts(cx, L)
    x_in = cx.din("x_own", [TOK, 1024])
    xc_in = cx.din("xc", [CTX, 1024])
    tab_src = cx.din("rope_tab", [TOK + CTX, 192])
    out = cx.dout("out", [TOK, 1024])
    x1 = cx.dint("x1", [TOK, 1024])
    xc1 = cx.dint("xc1", [CTX, 1024])
    groups = [[0, 1, 2, 3], [4, 5, 6, 7]]
    for l in range(DEPTH):
        cx.sfx = "_l%d" % l
        need_ctx = l < DEPTH - 1
        lst = ExitStack()
        st = ExitStack()
        L.modT = rec.sb("modT", [128, 48, 2], F32, lst)
        L.Gb = [[rec.sb("Gb", [128, 1024], F32, lst) for _ in range(2)] for _ in range(2)]
        wsrc = {"w_out": cx.din("w_out", [128, 8, 1024]), "wgu": cx.din("wgu", [NE, 128, 8, 512]), "wd": cx.din("wd", [NE, 128, 2, 1024])}
        wbf = {"w_out": cx.dint("w_out_bf%d" % l, [128, 8, 1024], BF16), "wgu": cx.dint("wgu_bf%d" % l, [NE, 128, 8, 512], BF16),
               "wd": cx.dint("wd_bf%d" % l, [NE, 128, 2, 1024], BF16)}
        layer_setup(cx, L, st, pst="prealloc")
        st.close()
        rec.barrier()
        rec.release_dma_sems()
        for ex in range(NE):
            rec.dma("pool", wbf["wgu"][ex, :, :, :].rearrange("p a b -> p (a b)"), wsrc["wgu"][ex, :, :, :].rearrange("p a b -> p (a b)"), key="D_wcast", cast=True)
            rec.dma("pool", wbf["wd"][ex, :, :, :].rearrange("p a b -> p (a b)"), wsrc["wd"][ex, :, :, :].rearrange("p a b -> p (a b)"), key="D_wcast", cast=True)
        rec.dma("pool", wbf["w_out"][:, :, :].rearrange("p a b -> p (a b)"), wsrc["w_out"][:, :, :].rearrange("p a b -> p (a b)"), key="D_wcast", cast=True)
        kv_own = nc.dram_tensor("kv_own%d" % l, [KVR, TOK], BF16).ap()
        ga = [Buf("ga", nc.dram_tensor("ga%d_%d" % (l, p_), [4 * 128, TOK], BF16).ap()) for p_ in range(22)]
        qT = cx.dint("qT%d" % l, [10, 128, TOK], BF16)
        qTc = cx.dint("qTc%d" % l, [10, 128, CTX], BF16)
        kTc = cx.dint("kTc%d" % l, [10, 128, CTX], BF16)
        vc = cx.dint("vc%d" % l, [12, 128, 2, 128], BF16)
        OT = cx.dint("OT%d" % l, [8, 128, TOK], BF16)
        OTc = cx.dint("OTc%d" % l, [8, 128, CTX], BF16)
        kT_own = Buf("kTown", kv_own[0:1280, :].rearrange("(c p) t -> c p t", p=128))
        v_own = Buf("vown", kv_own[1280:KVR, :].rearrange("(s p) (t d) -> s p t d", p=128, d=128))
        outs = {"qT": qT, "kT": kT_own, "v": v_own, "qTc": qTc, "kTc": kTc, "vc": vc}
        st = ExitStack()
        phase1(cx, L, st, x_in if l == 0 else x1, xc_in if l == 0 else xc1, tab_src, outs)
        st.close()
        rec.barrier()
        rec.release_dma_sems()
        order = [0, 10, 11, 1, 12, 13, 2, 14, 3, 15, 4, 16, 5, 17, 6, 18, 7, 19, 8, 20, 9, 21]
        for p_ in order:
            rec.collective_piece(lambda e, a=kv_own[p_ * 128:(p_ + 1) * 128, :], b=ga[p_]: e.collective_compute(
                "AllGather", ALU.bypass, replica_groups=groups, ins=[a], outs=[b[:, :]]), ga[p_])
        src = {"qT": qT, "ga": ga, "kTc": kTc, "vc": vc, "qTc": qTc, "kTown": kT_own, "vown": v_own, "OT": OT, "OTc": OTc}
        st = ExitStack()
        attention(cx, L, st, l, src, need_ctx)
        st.close()
        rec.barrier()
        rec.release_dma_sems()
        wts = {"w_out": wbf["w_out"], "wgu": wbf["wgu"], "wd": wbf["wd"], "bf16": True,
               "router_w": cx.din("router_w", [128, 8, 16]), "router_bias": cx.din("router_bias", [16]), "ln": cx.din("ln", [4, 1024])}
        xs = [(x_in if l == 0 else x1, OT, x1 if l == 0 else out, 0, TOK)]
        if need_ctx:
            xs.append((xc_in, OTc, xc1, 1, CTX))
        st = ExitStack()
        phase3(cx, L, st, xs, wts)
        st.close()
        rec.barrier()
        rec.release_dma_sems()
        lst.close()
    rec.wait_all_on("sp")
    rec.emit()
    return cx


def kernel_fused(inp):
    cores = list(range(NCORES))
    lws = [prep_layer(inp, l) for l in range(DEPTH)]
    bws = [prep_B_weights(inp, l) for l in range(DEPTH)]
    shared_w = {"router_w": bws[0]["router_w"], "router_bias": bws[0]["router_bias"]}
    in_maps = []
    for c in cores:
        b, r = c // 4, c % 4
        m = dict(prep_core_common(inp, c))
        m.update(shared_w)
        for l in range(DEPTH):
            for k, v in list(lws[l].items()) + list(bws[l].items()):
                if k not in shared_w:
                    m["%s_l%d" % (k, l)] = v
        m["x_own"] = np.ascontiguousarray(inp["x"][b, r * TOK:(r + 1) * TOK])
        m["xc"] = np.ascontiguousarray(inp["ctx"][b])
        m["c_masks"] = band_masks(r)
        oh = np.zeros((128, 8), np.float32)
        if r > 0:
            oh[:, r - 1] = 1.0
        if r < 3:
            oh[:, 4 + r + 1] = 1.0
        m["onehot"] = oh
        in_maps.append(m)
    prog = _prog("fused", build_fused)
    names = set(prog.dram.keys())
    in_maps = [{k: v for k, v in m.items() if k in names} for m in in_maps]
    res = run_bass_kernel_spmd(prog.nc, in_maps, core_ids=cores).results
    out = np.zeros((BATCH, SEQ, D), np.float32)
    for c in cores:
        out[c // 4, (c % 4) * TOK:(c % 4 + 1) * TOK] = res[c]["out"]
    return out


_PROGS = {}


def _prog(key, fn):
    if key not in _PROGS:
        _PROGS[key] = fn()
    return _PROGS[key]


def kernel(**inputs):
    inp = {k: np.asarray(v) for k, v in inputs.items()}
    return kernel_fused(inp)


def kernel_unfused(**inputs):
    inp = {k: np.asarray(v) for k, v in inputs.items()}
    cores = list(range(NCORES))
    common = [prep_core_common(inp, c) for c in cores]
    x_cur = [np.ascontiguousarray(inp["x"][c // 4, (c % 4) * TOK:(c % 4 + 1) * TOK]) for c in cores]
    xc_cur = [np.ascontiguousarray(inp["ctx"][c // 4]) for c in cores]
    for l in range(DEPTH):
        need_ctx = l < DEPTH - 1
        lw = prep_layer(inp, l)
        pa = _prog("A", build_A)
        in_maps = []
        for c in cores:
            m = dict(lw)
            m.update(common[c])
            m["x_own"] = x_cur[c]
            m["xc"] = xc_cur[c]
            in_maps.append(m)
        ra = run_bass_kernel_spmd(pa.nc, in_maps, core_ids=cores).results
        bw = prep_B_weights(inp, l)
        in_maps = []
        for c in cores:
            b, r = c // 4, c % 4
            grp = [ra[b * 4 + i] for i in range(4)]
            m = {}
            for k in ("w_ada", "b_adaT", "b_ada"):
                m[k] = lw[k]
            m["cT"] = common[c]["cT"]
            m["c_ident"] = common[c]["c_ident"]
            m.update(bw)
            m["qT"] = ra[c]["qT"]
            m["kTc"] = ra[c]["kTc"]
            m["vc"] = ra[c]["vc"]
            m["qTc"] = ra[c]["qTc"]
            m["kTall"] = np.ascontiguousarray(np.stack([g["kT"] for g in grp]))
            m["vall"] = np.ascontiguousarray(np.stack([g["v"] for g in grp]))
            kfull = np.concatenate([g["kT"][8:10] for g in grp], axis=2)
            kpad = np.zeros((2, 128, SEQ + 256), kfull.dtype)
            kpad[:, :, 128:128 + SEQ] = kfull
            m["kTwin"] = np.ascontiguousarray(kpad[:, :, r * TOK:r * TOK + NWIN * 128])
            vfull = np.concatenate([g["v"][10:12] for g in grp], axis=2)
            vpad = np.zeros((2, 128, 4 * NT + 2, 128), vfull.dtype)
            vpad[:, :, 1:1 + 4 * NT] = vfull
            m["vwin"] = np.ascontiguousarray(vpad[:, :, r * NT:r * NT + NWIN])
            m["c_masks"] = band_masks(r)
            m["x_own"] = x_cur[c]
            if need_ctx:
                m["xc"] = xc_cur[c]
            in_maps.append(m)
        pb = _prog(("B", l), lambda: build_B(l, need_ctx))
        rb = run_bass_kernel_spmd(pb.nc, in_maps, core_ids=cores).results
        x_cur = [np.ascontiguousarray(rb[c]["x_next"]) for c in cores]
        if need_ctx:
            xc_cur = [np.ascontiguousarray(rb[c]["xc_next"]) for c in cores]
    out = np.zeros((BATCH, SEQ, D), np.float32)
    for c in cores:
        out[c // 4, (c % 4) * TOK:(c % 4 + 1) * TOK] = x_cur[c]
    return out
```

```python
import numpy as np
from contextlib import ExitStack
import concourse.bass as bass
import concourse.mybir as mybir
from concourse.bass_utils import run_bass_kernel_spmd

F32 = mybir.dt.float32
BF16 = mybir.dt.bfloat16
AF = mybir.ActivationFunctionType
ALU = mybir.AluOpType
AX = mybir.AxisListType

D = 1024
BATCH = 2
SEQ = 16384
DEPTH = 2
CTX = 256
NCORES = 8
TOK = SEQ // 4
NT = TOK // 128
NE = 16
DE = 256
EPS = 1e-6
ALPHA = (2 * DEPTH) ** 0.25
IN_COLS = 2144


class Buf:
    __slots__ = ("name", "t", "w", "r")

    _uid = [0]

    def __init__(self, name, t):
        Buf._uid[0] += 1
        self.name = "%s.%d" % (name, Buf._uid[0])
        self.t = t
        self.w = {}
        self.r = {}

    def __getitem__(self, idx):
        return self.t[idx]


class Rec:
    ENGS = ("pe", "act", "dve", "pool", "sp")

    def __init__(self, nc, stack):
        self.nc = nc
        self.stack = stack
        self.ops = {e: [] for e in self.ENGS}
        self.sems = {}
        self.cnt = {}
        self.seen = {e: {} for e in self.ENGS}
        self.nbuf = 0
        for e in self.ENGS:
            self._sem("E_" + e)

    def _sem(self, key):
        if key not in self.sems:
            if key.startswith("D_H_") and getattr(self, "free", None):
                sem, c0 = self.free.pop()
                self.sems[key] = sem
                self.cnt[key] = c0
            else:
                self.nsem = getattr(self, "nsem", 0) + 1
                self.sems[key] = self.stack.enter_context(self.nc.semaphore("s%d" % self.nsem))
                self.cnt[key] = 0
        return self.sems[key]

    def release_dma_sems(self):
        if not hasattr(self, "free"):
            self.free = []
        for key in [k for k in self.sems if k.startswith("D_")]:
            sem, c = self.sems.pop(key), self.cnt.pop(key)
            if key.startswith("D_H_"):
                self.free.append((sem, c))
            for e in self.ENGS:
                self.seen[e].pop(key, None)

    def collective_piece(self, fn, out_buf):
        key = "E_cc"
        self._sem(key)
        self.cnt[key] += 1
        self.ops["pool"].append(([], fn, self.sems[key], 1))
        out_buf.w = {key: (self.cnt[key], "cc")}
        out_buf.r = {}

    def collective(self, fn):
        self.barrier()
        key = "E_cc"
        self._sem(key)
        self.cnt[key] += 1
        self.ops["pool"].append(([], fn, self.sems[key], 1))
        self.barrier()

    def buf(self, name, t):
        self.nbuf += 1
        return Buf("%s#%d" % (name, self.nbuf), t)

    def sb(self, name, shape, dtype, stack=None):
        st = stack if stack is not None else self.stack
        self.nbuf += 1
        t = st.enter_context(self.nc.sbuf_tensor("%s_%d" % (name, self.nbuf), list(shape), dtype))
        return Buf(name, t)

    def ps(self, name, shape, dtype, stack=None):
        st = stack if stack is not None else self.stack
        self.nbuf += 1
        t = st.enter_context(self.nc.psum_tensor("%s_%d" % (name, self.nbuf), list(shape), dtype))
        return Buf(name, t)

    def _collect(self, eng, reads, writes, is_dma):
        need = {}

        def add(d):
            for k, (v, pe) in d.items():
                if k not in self.sems:
                    continue
                if (not is_dma) and eng == "pe" and pe == "pe" and k == "E_pe":
                    continue
                if need.get(k, 0) < v:
                    need[k] = v
        for b in reads:
            add(b.w)
        for b in writes:
            add(b.w)
            add(b.r)
        waits = []
        seen = self.seen[eng]
        for k, v in need.items():
            if seen.get(k, 0) < v:
                seen[k] = v
                waits.append((self.sems[k], v))
        return waits

    def op(self, eng, fn, reads=(), writes=()):
        waits = self._collect(eng, reads, writes, False)
        key = "E_" + eng
        self.cnt[key] += 1
        tk = (self.cnt[key], eng)
        self.ops[eng].append((waits, fn, self.sems[key], 1))
        for b in reads:
            b.r[key] = tk
        for b in writes:
            b.w = {key: tk}
            b.r = {}

    def dma(self, eng, out_ap, in_ap, reads=(), writes=(), key=None, cast=False):
        if key is None:
            b0 = (list(writes) + list(reads))[0]
            key = b0.name + ("_w" if writes else "_r")
        key = ("D_P_" if eng == "pool" else "D_H_") + key
        self._sem(key)
        waits = self._collect(eng, reads, writes, True)
        self.cnt[key] += 16
        tk = (self.cnt[key], "dma")
        if cast:
            self.ops[eng].append((waits, lambda e, o=out_ap, i=in_ap: e.dma_start(out=o, in_=i, max_dma_last_dim=4096), self.sems[key], 16))
        else:
            self.ops[eng].append((waits, lambda e, o=out_ap, i=in_ap: e.dma_start(out=o, in_=i), self.sems[key], 16))
        for b in reads:
            b.r[key] = tk
        for b in writes:
            b.w = {key: tk}
            b.r = {}

    def barrier(self):
        for e in self.ENGS:
            waits = []
            for k, v in self.cnt.items():
                if v > 0 and self.seen[e].get(k, 0) < v:
                    self.seen[e][k] = v
                    waits.append((self.sems[k], v))
            if waits:
                self.ops[e].append((waits, None, None, 0))

    def wait_all_on(self, eng):
        waits = []
        for k, v in self.cnt.items():
            if v > 0 and self.seen[eng].get(k, 0) < v:
                self.seen[eng][k] = v
                waits.append((self.sems[k], v))
        if waits:
            self.ops[eng].append((waits, None, None, 0))

    def emit(self):
        nc = self.nc
        block = self.stack.enter_context(nc.Block())

        def run(e, ops):
            for waits, fn, sem, inc in ops:
                for s, v in waits:
                    e.wait_ge(s, v)
                if fn is not None:
                    fn(e).then_inc(sem, inc)

        @block.tensor
        def _(e):
            run(e, self.ops["pe"])

        @block.scalar
        def _(e):
            run(e, self.ops["act"])

        @block.vector
        def _(e):
            run(e, self.ops["dve"])

        @block.gpsimd
        def _(e):
            run(e, self.ops["pool"])

        @block.sync
        def _(e):
            run(e, self.ops["sp"])


def _bc(ap, shape):
    return ap.to_broadcast(list(shape))


class Ctx:
    def __init__(self):
        self.nc = bass.Bass("TRN2", target_bir_lowering=False)
        self.stack = ExitStack()
        self.rec = Rec(self.nc, self.stack)
        self.dram = {}

    sfx = ""
    shared = ("c_ident", "cT", "rope_tab", "c_masks", "router_w", "router_bias", "x_own", "xc", "onehot")

    def din(self, name, shape, dtype=F32):
        if name not in self.shared:
            name = name + self.sfx
        if name in self.dram:
            return self.dram[name]
        t = self.nc.dram_tensor(name, list(shape), dtype, kind="ExternalInput")
        b = Buf(name, t.ap())
        self.dram[name] = b
        return b

    def dout(self, name, shape, dtype=F32):
        t = self.nc.dram_tensor(name, list(shape), dtype, kind="ExternalOutput")
        b = Buf(name, t.ap())
        self.dram[name] = b
        return b

    def dint(self, name, shape, dtype=F32):
        t = self.nc.dram_tensor(name, list(shape), dtype)
        b = Buf(name, t.ap())
        self.dram[name] = b
        return b


def _in_perm():
    o = {}
    off = 0
    for name, n in (("Aq", 256), ("Ak", 256), ("Av", 256), ("Bq", 256), ("Bk", 128), ("Bv", 128),
                    ("Ccq", 192), ("Cckv", 128), ("Ckr", 32), ("Dq", 256), ("Dk", 128), ("Dv", 128)):
        o[name] = np.arange(off, off + n)
        off += n
    order = ["Aq", "Ak", "Bq", "Bk", "Dk", "Dq", "Ckr", "Av", "Bv", "Dv", "Ccq", "Cckv"]
    return np.concatenate([o[k] for k in order])


GOFF = (0, 512, 1024, 1312, 1824)
GW = (512, 512, 288, 512, 320)


def rope_tables(tok_idx):
    row = (tok_idx // 64).astype(np.float64)
    col = (tok_idx % 64).astype(np.float64)

    def tab(n):
        inv = 10000.0 ** (-np.arange(n, dtype=np.float32).astype(np.float64) / n)
        inv = (np.float32(10000.0) ** (-(np.arange(n, dtype=np.float32) / np.float32(n)))).astype(np.float32)
        ar = (row.astype(np.float32)[:, None] * inv).astype(np.float32)
        ac = (col.astype(np.float32)[:, None] * inv).astype(np.float32)
        cr, sr, cc, sc = np.cos(ar), np.sin(ar), np.cos(ac), np.sin(ac)
        cos = np.concatenate([cr, cr, cc, cc], 1)
        sin = np.concatenate([-sr, sr, -sc, sc], 1)
        return cos.astype(np.float32), sin.astype(np.float32)
    c32, s32 = tab(8)
    c64, s64 = tab(16)
    return np.ascontiguousarray(np.concatenate([c32, s32, c64, s64], 1), dtype=np.float32)


def setup_consts(cx, L):
    rec, nc = cx.rec, cx.nc
    L.ident_f = rec.sb("identf", [128, 128], F32)
    L.ident_b = rec.sb("identb", [128, 128], BF16)
    L.eps = rec.sb("eps", [128, 1], F32)
    L.ones64 = rec.sb("ones64", [64, 64], F32)
    idn = cx.din("c_ident", [128, 128], F32)
    rec.dma("sp", L.ident_f[:, :], idn[:, :], writes=[L.ident_f])
    rec.op("dve", lambda e: e.tensor_copy(out=L.ident_b[:, :], in_=L.ident_f[:, :]), reads=[L.ident_f], writes=[L.ident_b])
    rec.op("dve", lambda e: e.memset(L.eps[:, :], EPS), writes=[L.eps])
    rec.op("dve", lambda e: e.memset(L.ones64[:, :], 1.0 / 64.0), writes=[L.ones64])


class NS:
    pass


DBG = {"stop": 99, "att": 9, "kinds": "ABCD"}


class Ring:
    def __init__(self, bufs):
        self.bufs = bufs
        self.i = 0

    def next(self):
        b = self.bufs[self.i % len(self.bufs)]
        self.i += 1
        return b


def rope_ops(rec, eng, src_buf, src, nv, R, tab_buf, cos, sin, t1b, t2b):
    h = R // 4
    n = nv * R
    t1 = t1b[:, 0:n]
    t2 = t2b[:, 0:n]
    rec.op(eng, lambda e: e.tensor_tensor(out=t1.rearrange("p (v r) -> p v r", v=nv), in0=src.rearrange("p (v r) -> p v r", v=nv),
                                          in1=cos.unsqueeze(1).to_broadcast([128, nv, R]), op=ALU.mult),
           reads=[src_buf, tab_buf], writes=[t1b])
    s5 = src.rearrange("p (v a b h) -> p v a b h", v=nv, a=2, b=2, h=h)
    t5 = t2.rearrange("p (v a b h) -> p v a b h", v=nv, a=2, b=2, h=h)
    sn = sin.rearrange("p (a b h) -> p a b h", a=2, b=2, h=h)
    for b in (0, 1):
        rec.op(eng, lambda e, b=b: e.tensor_tensor(out=t5[:, :, :, b, :], in0=s5[:, :, :, 1 - b, :],
                                                   in1=sn[:, :, b, :].unsqueeze(1).to_broadcast([128, nv, 2, h]), op=ALU.mult),
               reads=[src_buf, tab_buf], writes=[t2b])
    rec.op(eng, lambda e: e.tensor_tensor(out=t1, in0=t1, in1=t2, op=ALU.add), reads=[t2b], writes=[t1b])
    return t1


def layer_setup(cx, L, st, pst=None):
    rec = cx.rec
    cT = cx.din("cT", [128, 8, 2])
    wada = cx.din("w_ada", [128, 8, 6144])
    badaT = cx.din("b_adaT", [128, 48])
    bada = cx.din("b_ada", [6144])
    if pst != "prealloc":
        L.modT = rec.sb("modT", [128, 48, 2], F32, pst)
        L.Gb = [[rec.sb("Gb", [128, 1024], F32, pst) for _ in range(2)] for _ in range(2)]
    sl0 = rec.sb("sl0", [128, 8, 2], F32, st)
    sl = rec.sb("sl", [128, 8, 2], F32, st)
    slrep = rec.sb("slrep", [128, 8, 2, 128], F32, st)
    bT = rec.sb("bT", [128, 48], F32, st)
    was = Ring([rec.sb("wa", [128, 8, 512], F32, st) for _ in range(2)])
    bbs = Ring([rec.sb("bb", [128, 512], F32, st) for _ in range(2)])
    psA = rec.ps("psA", [128, 512], F32, st)
    psB = Ring([rec.ps("psB", [128, 512], F32, st) for _ in range(2)])
    rec.op("dve", lambda e: e.memset(L.modT[:, :, :], 0.0), writes=[L.modT])
    rec.dma("sp", sl0[:, :, :], cT[:, :, :], writes=[sl0])
    rec.dma("sp", bT[:, :], badaT[:, :], writes=[bT])
    rec.op("act", lambda e: e.activation(out=sl[:, :, :], in_=sl0[:, :, :], func=AF.Silu), reads=[sl0], writes=[sl])
    rec.op("dve", lambda e: e.tensor_copy(out=slrep[:, :, :, :], in_=sl[:, :, :].unsqueeze(3).to_broadcast([128, 8, 2, 128])),
           reads=[sl], writes=[slrep])
    for gi in range(12):
        wa = was.next()
        rec.dma("sp", wa[:, :, :], wada[:, :, gi * 512:(gi + 1) * 512], writes=[wa])
        v = gi // 2
        if v in (2, 5):
            bb = bbs.next()
            rec.dma("pool", bb[:, :], bada[gi * 512:(gi + 1) * 512].partition_broadcast(128), writes=[bb])
            for cond in range(2):
                ps = psB.next()
                for kc in range(8):
                    rec.op("pe", lambda e, kc=kc, cond=cond, ps=ps, wa=wa: e.matmul(ps[:, :], lhsT=slrep[:, kc, cond, :], rhs=wa[:, kc, :],
                                                                                 start=(kc == 0), stop=(kc == 7)),
                           reads=[slrep, wa], writes=[ps])
                gb = L.Gb[0 if v == 2 else 1][cond]
                rec.op("dve", lambda e, ps=ps, gb=gb, bb=bb, gi=gi: e.tensor_tensor(out=gb[:, (gi % 2) * 512:(gi % 2 + 1) * 512], in0=ps[:, :],
                                                                                 in1=bb[:, :], op=ALU.add),
                       reads=[ps, bb], writes=[gb])
        else:
            for jj in range(4):
                for kc in range(8):
                    rec.op("pe", lambda e, kc=kc, jj=jj, wa=wa: e.matmul(psA[:, jj * 2:jj * 2 + 2], lhsT=wa[:, kc, jj * 128:(jj + 1) * 128],
                                                                       rhs=sl[:, kc, :], start=(kc == 0), stop=(kc == 7)),
                           reads=[sl, wa], writes=[psA])
            rec.op("dve", lambda e, gi=gi: e.tensor_tensor(out=L.modT[:, gi * 4:gi * 4 + 4, :],
                                                           in0=psA[:, 0:8].rearrange("p (j c) -> p j c", c=2),
                                                           in1=bT[:, gi * 4:gi * 4 + 4].unsqueeze(2).to_broadcast([128, 4, 2]), op=ALU.add),
                   reads=[psA, bT], writes=[L.modT])
    for lo in (8, 32):
        rec.op("dve", lambda e, lo=lo: e.tensor_scalar(out=L.modT[:, lo:lo + 8, :], in0=L.modT[:, lo:lo + 8, :], scalar1=1.0, scalar2=None,
                                                       op0=ALU.add), reads=[L.modT], writes=[L.modT])


def phase1(cx, L, st, x_src, xc_src, tab_src, outs, n_lat_tiles=NT, do_ctx=True):
    rec = cx.rec
    win_d = cx.din("w_in", [128, 8, IN_COLS])
    wuq_d = cx.din("w_uq", [128, 2, 384])
    wukv_d = cx.din("w_ukv", [128, 512])
    g1_d = cx.din("gain_g1", [512])
    gc_d = cx.din("gain_c", [320])
    win = rec.sb("win", [128, 8, IN_COLS], BF16, st)
    wuq = rec.sb("wuq", [128, 2, 384], BF16, st)
    wukv = rec.sb("wukv", [128, 512], BF16, st)
    for kc in range(8):
        rec.dma("pool", win[:, kc, :], win_d[:, kc, :], writes=[win], cast=True)
    rec.dma("pool", wuq[:, :, :], wuq_d[:, :, :], writes=[wuq], cast=True)
    rec.dma("pool", wukv[:, :], wukv_d[:, :], writes=[wukv], cast=True)
    G1t = rec.sb("G1t", [128, 512], F32, st)
    Gct = rec.sb("Gct", [128, 320], F32, st)
    rec.dma("sp", G1t[:, :], g1_d[0:512].partition_broadcast(128), writes=[G1t])
    rec.dma("sp", Gct[:, :], gc_d[0:320].partition_broadcast(128), writes=[Gct])
    xts = Ring([rec.sb("xt", [128, 1024], F32, st) for _ in range(2)])
    tabs = Ring([rec.sb("tab", [128, 192], F32, st) for _ in range(2)])
    st6 = rec.sb("st6", [128, 2, 6], F32, st)
    mv = rec.sb("mv", [128, 2], F32, st)
    sq1 = rec.sb("sq1", [128, 1], F32, st)
    rstd = rec.sb("rstd", [128, 1], F32, st)
    xn = rec.sb("xn", [128, 1024], BF16, st)
    hT = rec.sb("hT", [128, 8, 128], BF16, st)
    t1b = rec.sb("t1b", [128, 512], F32, st)
    t2b = rec.sb("t2b", [128, 512], F32, st)
    xg = rec.sb("xg", [128, 512], F32, st)
    sqt = rec.sb("sqt", [128, 512], F32, st)
    ss8 = rec.sb("ss8", [128, 8], F32, st)
    rs8 = rec.sb("rs8", [128, 8], F32, st)
    ssc = rec.sb("ssc", [128, 2], F32, st)
    rsc = rec.sb("rsc", [128, 2], F32, st)
    cn = rec.sb("cn", [128, 320], BF16, st)
    cnT = rec.sb("cnT", [128, 3, 128], BF16, st)
    qk = rec.sb("qk", [128, 2, 1280], BF16, st)
    vts = Ring([rec.sb("vt", [128, 12, 128], BF16, st) for _ in range(2)])
    qkT = Ring([rec.sb("qkT", [128, 2, 10, 512], BF16, st) for _ in range(2)])
    psT = rec.ps("psT", [128, 1024], BF16, st)
    psG = [rec.ps("psG", [128, 512], F32, st) for _ in range(5)]
    psU = [rec.ps("psU", [128, 512], F32, st) for _ in range(2)]
    for vt in vts.bufs:
        rec.op("pool", lambda e, vt=vt: e.memset(vt[:, :, 64:128], 1.0), writes=[vt])
    rec.op("pool", lambda e: e.memset(qk[:, :, :], 0.0), writes=[qk])

    def tile(src_ap, src_buf, tab_ap, cond, qkt, col, vdst_ap, vdst_buf):
        xt = xts.next()
        tab = tabs.next()
        vt = vts.next()
        rec.dma("sp", xt[:, :], src_ap, writes=[xt])
        rec.dma("sp", tab[:, :], tab_ap, writes=[tab])
        c32, s32, c64, s64 = tab[:, 0:32], tab[:, 32:64], tab[:, 64:128], tab[:, 128:192]
        for i in range(2):
            rec.op("dve", lambda e, i=i: e.bn_stats(out=st6[:, i, :], in_=xt[:, i * 512:(i + 1) * 512]), reads=[xt], writes=[st6])
        rec.op("dve", lambda e: e.bn_aggr(out=mv[:, :], in_=st6[:, :, :].rearrange("p a b -> p (a b)")), reads=[st6], writes=[mv])
        rec.op("act", lambda e: e.activation(out=sq1[:, :], in_=mv[:, 1:2], func=AF.Sqrt, bias=L.eps[:, 0:1], scale=1.0),
               reads=[mv, L.eps], writes=[sq1])
        rec.op("dve", lambda e: e.reciprocal(out=rstd[:, :], in_=sq1[:, :]), reads=[sq1], writes=[rstd])
        rec.op("dve", lambda e: e.tensor_scalar(out=xn[:, :], in0=xt[:, :], scalar1=mv[:, 0:1], scalar2=rstd[:, 0:1],
                                                op0=ALU.subtract, op1=ALU.mult), reads=[xt, mv, rstd], writes=[xn])
        if DBG["stop"] <= 1:
            return
        for kc in range(8):
            rec.op("pe", lambda e, kc=kc: e.transpose(out=psT[:, kc * 128:(kc + 1) * 128], in_=xn[:, kc * 128:(kc + 1) * 128], identity=L.ident_b[:, :]),
                   reads=[xn, L.ident_b], writes=[psT])
        for kc in range(8):
            rec.op("dve", lambda e, kc=kc: e.tensor_scalar(out=hT[:, kc, :], in0=psT[:, kc * 128:(kc + 1) * 128],
                                                           scalar1=L.modT[:, 8 + kc, cond:cond + 1], scalar2=L.modT[:, kc, cond:cond + 1],
                                                           op0=ALU.mult, op1=ALU.add), reads=[psT, L.modT], writes=[hT])
        if DBG["stop"] <= 2:
            return
        for g in range(5):
            for kc in range(8):
                rec.op("pe", lambda e, g=g, kc=kc: e.matmul(psG[g][:, 0:GW[g]], lhsT=hT[:, kc, :], rhs=win[:, kc, GOFF[g]:GOFF[g] + GW[g]],
                                                            start=(kc == 0), stop=(kc == 7)), reads=[hT, win], writes=[psG[g]])
        if DBG["stop"] <= 3:
            return
        r = rope_ops(rec, "dve", psG[0], psG[0][:, 0:512], 16, 32, tab, c32, s32, t1b, t2b)
        rec.op("act", lambda e, r=r: e.activation(out=qk[:, :, 0:256], in_=r.rearrange("p (a c) -> p a c", a=2), func=AF.Copy),
               reads=[t1b], writes=[qk])
        if DBG["stop"] <= 4:
            return
        rec.op("act", lambda e: e.activation(out=sqt[:, :], in_=psG[1][:, :], func=AF.Square), reads=[psG[1]], writes=[sqt])
        rec.op("dve", lambda e: e.tensor_reduce(out=ss8[:, :], in_=sqt[:, :].rearrange("p (v r) -> p v r", v=8), axis=AX.X, op=ALU.add),
               reads=[sqt], writes=[ss8])
        rec.op("act", lambda e: e.activation(out=ss8[:, :], in_=ss8[:, :], func=AF.Sqrt, bias=L.eps[:, 0:1], scale=1.0 / 64.0),
               reads=[ss8, L.eps], writes=[ss8])
        rec.op("dve", lambda e: e.reciprocal(out=rs8[:, :], in_=ss8[:, :]), reads=[ss8], writes=[rs8])
        rec.op("dve", lambda e: e.memset(rs8[:, 6:8], 1.0), writes=[rs8])
        rec.op("dve", lambda e: e.tensor_tensor(out=xg[:, :], in0=psG[1][:, :], in1=G1t[:, :], op=ALU.mult), reads=[psG[1], G1t], writes=[xg])
        r = rope_ops(rec, "dve", xg, xg[:, 0:512], 8, 64, tab, c64, s64, t1b, t2b)
        r3 = r.rearrange("p (v r) -> p v r", v=8)
        rec.op("dve", lambda e, r3=r3: e.tensor_tensor(out=qk[:, 0, 256:512].rearrange("p (v r) -> p v r", v=4), in0=r3[:, 0:4, :],
                                                in1=rs8[:, 0:4].unsqueeze(2).to_broadcast([128, 4, 64]), op=ALU.mult),
               reads=[t1b, rs8], writes=[qk])
        for (lo, dst0) in ((4, 256), (6, 1024)):
            rec.op("dve", lambda e, lo=lo, dst0=dst0, r3=r3: e.tensor_tensor(
                out=qk[:, 1, dst0:dst0 + 256].rearrange("p (g d r) -> p g d r", g=2, d=2),
                in0=r3[:, lo:lo + 2, :].unsqueeze(2).to_broadcast([128, 2, 2, 64]),
                in1=rs8[:, lo:lo + 2].unsqueeze(2).unsqueeze(3).to_broadcast([128, 2, 2, 64]), op=ALU.mult),
                reads=[t1b, rs8], writes=[qk])
        if DBG["stop"] <= 5:
            return
        r = rope_ops(rec, "dve", psG[2], psG[2][:, 0:256], 4, 64, tab, c64, s64, t1b, t2b)
        rec.op("act", lambda e, r=r: e.activation(out=qk[:, 0, 1024:1280], in_=r, func=AF.Copy), reads=[t1b], writes=[qk])
        r = rope_ops(rec, "dve", psG[2], psG[2][:, 256:288], 1, 32, tab, c32, s32, t1b, t2b)
        rec.op("dve", lambda e, r=r: e.tensor_copy(out=qk[:, 1, 512:1024].rearrange("p (h c) -> p h c", h=4)[:, :, 64:96],
                                              in_=r.unsqueeze(1).to_broadcast([128, 4, 32])), reads=[t1b], writes=[qk])
        rec.op("act", lambda e: e.activation(out=vt[:, 0:4, 0:64], in_=psG[3][:, 0:256].rearrange("p (h d) -> p h d", h=4), func=AF.Copy),
               reads=[psG[3]], writes=[vt])
        rec.op("act", lambda e: e.activation(out=vt[:, 4:6, 0:64], in_=psG[3][:, 256:384].rearrange("p (h d) -> p h d", h=2), func=AF.Copy),
               reads=[psG[3]], writes=[vt])
        rec.op("act", lambda e: e.activation(out=vt[:, 10:12, 0:64], in_=psG[3][:, 384:512].rearrange("p (h d) -> p h d", h=2), func=AF.Copy),
               reads=[psG[3]], writes=[vt])
        if DBG["stop"] <= 6:
            return
        rec.op("act", lambda e: e.activation(out=sqt[:, 0:320], in_=psG[4][:, 0:320], func=AF.Square), reads=[psG[4]], writes=[sqt])
        rec.op("dve", lambda e: e.tensor_reduce(out=ssc[:, 0:1], in_=sqt[:, 0:192], axis=AX.X, op=ALU.add), reads=[sqt], writes=[ssc])
        rec.op("dve", lambda e: e.tensor_reduce(out=ssc[:, 1:2], in_=sqt[:, 192:320], axis=AX.X, op=ALU.add), reads=[sqt], writes=[ssc])
        rec.op("act", lambda e: e.activation(out=ssc[:, 0:1], in_=ssc[:, 0:1], func=AF.Sqrt, bias=L.eps[:, 0:1], scale=1.0 / 192.0),
               reads=[ssc, L.eps], writes=[ssc])
        rec.op("act", lambda e: e.activation(out=ssc[:, 1:2], in_=ssc[:, 1:2], func=AF.Sqrt, bias=L.eps[:, 0:1], scale=1.0 / 128.0),
               reads=[ssc, L.eps], writes=[ssc])
        rec.op("dve", lambda e: e.reciprocal(out=rsc[:, :], in_=ssc[:, :]), reads=[ssc], writes=[rsc])
        for (lo, hi, j) in ((0, 192, 0), (192, 320, 1)):
            rec.op("dve", lambda e, lo=lo, hi=hi, j=j: e.scalar_tensor_tensor(out=cn[:, lo:hi], in0=psG[4][:, lo:hi], scalar=rsc[:, j:j + 1],
                                                                             in1=Gct[:, lo:hi], op0=ALU.mult, op1=ALU.mult),
                   reads=[psG[4], rsc, Gct], writes=[cn])
        if DBG["stop"] <= 6.2:
            return
        for j, (lo, hi) in enumerate(((0, 128), (128, 192), (192, 320))):
            rec.op("pe", lambda e, j=j, lo=lo, hi=hi: e.transpose(out=psT[0:hi - lo, j * 128:(j + 1) * 128], in_=cn[:, lo:hi], identity=L.ident_b[:, :]),
                   reads=[cn, L.ident_b], writes=[psT])
        rec.op("dve", lambda e: e.tensor_copy(out=cnT[:, 0, :], in_=psT[:, 0:128]), reads=[psT], writes=[cnT])
        rec.op("dve", lambda e: e.tensor_copy(out=cnT[0:64, 1, :], in_=psT[0:64, 128:256]), reads=[psT], writes=[cnT])
        rec.op("dve", lambda e: e.tensor_copy(out=cnT[:, 2, :], in_=psT[:, 256:384]), reads=[psT], writes=[cnT])
        if DBG["stop"] <= 6.4:
            return
        rec.op("pe", lambda e: e.matmul(psU[0][:, 0:384], lhsT=cnT[:, 0, :], rhs=wuq[:, 0, :], start=True, stop=False), reads=[cnT, wuq], writes=[psU[0]])
        rec.op("pe", lambda e: e.matmul(psU[0][:, 0:384], lhsT=cnT[0:64, 1, :], rhs=wuq[0:64, 1, :], start=False, stop=True), reads=[cnT, wuq], writes=[psU[0]])
        rec.op("pe", lambda e: e.matmul(psU[1][:, 0:512], lhsT=cnT[:, 2, :], rhs=wukv[:, :], start=True, stop=True), reads=[cnT, wukv], writes=[psU[1]])
        if DBG["stop"] <= 6.6:
            return
        qc = psU[0][:, 0:384].rearrange("p (h c) -> p h c", h=4)
        kvc = psU[1][:, 0:512].rearrange("p (h c) -> p h c", h=4)
        qdst = qk[:, 0, 512:1024].rearrange("p (h c) -> p h c", h=4)
        kdst = qk[:, 1, 512:1024].rearrange("p (h c) -> p h c", h=4)
        rec.op("act", lambda e: e.activation(out=qdst[:, :, 0:64], in_=qc[:, :, 0:64], func=AF.Copy), reads=[psU[0]], writes=[qk])
        rec.op("act", lambda e: e.activation(out=kdst[:, :, 0:64], in_=kvc[:, :, 0:64], func=AF.Copy), reads=[psU[1]], writes=[qk])
        rec.op("act", lambda e: e.activation(out=vt[:, 6:10, 0:64], in_=kvc[:, :, 64:128], func=AF.Copy), reads=[psU[1]], writes=[vt])
        if DBG["stop"] <= 6.8:
            return
        rec.op("act", lambda e: e.activation(out=xg[:, 0:128].rearrange("p (h c) -> p h c", h=4), in_=qc[:, :, 64:96], func=AF.Copy), reads=[psU[0]], writes=[xg])
        if DBG["stop"] <= 6.85:
            return
        r = rope_ops(rec, "dve", xg, xg[:, 0:128], 4, 32, tab, c32, s32, t1b, t2b)
        if DBG["stop"] <= 6.9:
            return
        rec.op("dve", lambda e, r=r: e.tensor_copy(out=qdst[:, :, 64:96], in_=r.rearrange("p (h c) -> p h c", h=4)), reads=[t1b], writes=[qk])
        if DBG["stop"] <= 7:
            return
        for a in range(2):
            for (c0, c1) in ((0, 8), (8, 10)):
                for c in range(c0, c1):
                    rec.op("pe", lambda e, a=a, c=c, c0=c0: e.transpose(out=psT[:, (c - c0) * 128:(c - c0 + 1) * 128], in_=qk[:, a, c * 128:(c + 1) * 128],
                                                                        identity=L.ident_b[:, :]), reads=[qk, L.ident_b], writes=[psT])
                eng = "act" if a == 0 else "dve"
                if eng == "act":
                    rec.op("act", lambda e, a=a, c0=c0, c1=c1: e.activation(out=qkt[:, a, c0:c1, col:col + 128],
                                                                          in_=psT[:, 0:(c1 - c0) * 128].rearrange("p (c t) -> p c t", t=128), func=AF.Copy),
                           reads=[psT], writes=[qkt])
                else:
                    rec.op("dve", lambda e, a=a, c0=c0, c1=c1: e.tensor_copy(out=qkt[:, a, c0:c1, col:col + 128],
                                                                           in_=psT[:, 0:(c1 - c0) * 128].rearrange("p (c t) -> p c t", t=128)),
                           reads=[psT], writes=[qkt])
        if DBG["stop"] <= 8:
            return
        rec.dma("sp", vdst_ap, vt[:, :, :], reads=[vt])

    if DBG["stop"] <= 0:
        return
    for blk in range(n_lat_tiles // 4):
        qkt = qkT.next()
        for j in range(4):
            t = blk * 4 + j
            tile(x_src[t * 128:(t + 1) * 128, :], x_src, tab_src[t * 128:(t + 1) * 128, :], 0, qkt, j * 128,
                 outs["v"][:, :, t, :].rearrange("s p d -> p s d"), outs["v"])
        rec.dma("sp", outs["qT"][:, :, blk * 512:(blk + 1) * 512].rearrange("c p t -> p c t"), qkt[:, 0, :, :], reads=[qkt])
        rec.dma("sp", outs["kT"][:, :, blk * 512:(blk + 1) * 512].rearrange("c p t -> p c t"), qkt[:, 1, :, :], reads=[qkt])
    if do_ctx:
        qkt = qkT.next()
        for t in range(2):
            tile(xc_src[t * 128:(t + 1) * 128, :], xc_src, tab_src[TOK + t * 128:TOK + (t + 1) * 128, :], 1, qkt, t * 128,
                 outs["vc"][:, :, t, :].rearrange("s p d -> p s d"), outs["vc"])
        rec.dma("sp", outs["qTc"][:, :, :].rearrange("c p t -> p c t"), qkt[:, 0, :, 0:256], reads=[qkt])
        rec.dma("sp", outs["kTc"][:, :, :].rearrange("c p t -> p c t"), qkt[:, 1, :, 0:256], reads=[qkt])


def _pm(w, kc):
    k, n = w.shape
    return np.ascontiguousarray(w.reshape(kc, 128, n).transpose(1, 0, 2))


def prep_layer(inp, l):
    f = np.float32
    d = {}
    d["w_ada"] = _pm(inp["w_ada"][l], 8)
    d["b_adaT"] = np.ascontiguousarray(inp["b_ada"][l].reshape(48, 128).T)
    d["b_ada"] = np.ascontiguousarray(inp["b_ada"][l])
    d["w_in"] = _pm(inp["w_in"][l][:, _in_perm()], 8)
    wuq = np.zeros((256, 384), f)
    wuq[:192] = inp["mla_w_uq"][l]
    d["w_uq"] = _pm(wuq, 2)
    d["w_ukv"] = np.ascontiguousarray(inp["mla_w_ukv"][l])
    d["gain_g1"] = np.concatenate([np.tile(inp["gqa_q_norm_g"][l], 4), np.tile(inp["gqa_k_norm_g"][l], 2), np.ones(128, f)]).astype(f)
    d["gain_c"] = np.concatenate([inp["mla_q_norm_g"][l], inp["mla_kv_norm_g"][l]]).astype(f)
    return d


def prep_core_common(inp, core):
    b = core // 4
    r = core % 4
    d = {}
    cc = np.stack([inp["c"][b], inp["c_ctx"]], 1)
    d["cT"] = _pm(cc, 8)
    tok = np.arange(r * TOK, (r + 1) * TOK)
    tab = rope_tables(tok)
    ctab = np.zeros((CTX, 192), np.float32)
    ctab[:, 0:32] = 1.0
    ctab[:, 64:128] = 1.0
    d["rope_tab"] = np.ascontiguousarray(np.concatenate([tab, ctab], 0))
    d["c_ident"] = np.eye(128, dtype=np.float32)
    return d


NKT = 2 + 4 * NT
NWIN = NT + 2


def attention(cx, L, st, l, src, need_ctx, n_qb=TOK // 512, kt_limit=None):
    rec = cx.rec
    lam_init = 0.8 - 0.6 * float(np.exp(-0.3 * l))
    lamv_d = cx.din("lamv", [4, 32])
    subg_d = cx.din("subln_g", [64, 1])
    sink_d = cx.din("swa_sink", [4])
    mask_d = cx.din("c_masks", [128, 4, 128])
    lam = rec.sb("lam", [128, 1], F32, st)
    gsub = rec.sb("gsub", [64, 1], F32, st)
    esink = rec.sb("esink", [128, 4], F32, st)
    masks = rec.sb("masks", [128, 4, 128], BF16, st)
    lv = rec.sb("lv", [128, 4, 32], F32, st)
    lp = rec.sb("lp", [128, 2, 32], F32, st)
    ls = rec.sb("ls", [128, 2], F32, st)
    kts = Ring([rec.sb("ktb", [128, NKT * 128], BF16, st) for _ in range(2)])
    vtsr = Ring([rec.sb("vtb", [128, NKT, 128], BF16, st) for _ in range(2)])
    qts = Ring([rec.sb("qtb", [128, TOK], BF16, st) for _ in range(2)])
    qtc = rec.sb("qtc", [128, 10, 256], BF16, st)
    pTs = Ring([rec.sb("pT", [128, 1024], BF16, st) for _ in range(4)])
    zss = Ring([rec.sb("zs", [64, 512], F32, st) for _ in range(2)])
    rzs = Ring([rec.sb("rz", [64, 512], F32, st) for _ in range(2)])
    fa = rec.sb("fa", [64, 512], F32, st)
    fb = rec.sb("fb", [64, 512], F32, st)
    fc = rec.sb("fc", [64, 512], F32, st)
    ots = Ring([rec.sb("ot", [64, 512], BF16, st) for _ in range(2)])
    qmask = [[Ring([rec.sb("qm", [128, 512], BF16, st) for _ in range(2)]) for _ in range(2)] for _ in range(2)]
    for hp in range(2):
        for cp in range(2):
            for b_ in qmask[hp][cp].bufs:
                rec.op("pool", lambda e, b_=b_: e.memset(b_[:, :], 0.0), writes=[b_])
    if "kTwin" not in src:
        oh_d = cx.din("onehot", [128, 8])
        onehot = rec.sb("onehot", [128, 8], F32, st)
        hck = rec.sb("hck", [128, 4, 128], BF16, st)
        hcv = rec.sb("hcv", [128, 4, 128], BF16, st)
        hacc = rec.sb("hacc", [128, 128], F32, st)
        rec.dma("sp", onehot[:, :], oh_d[:, :], writes=[onehot])
    Sr = Ring([rec.ps("S", [128, 1024], F32, st) for _ in range(3)])
    accs = Ring([rec.ps("acc", [128, 512], F32, st) for _ in range(2)])
    dummy = None
    ndummy = 0
    rec.dma("sp", lv[:, :, :].rearrange("p a b -> p (a b)"), lamv_d[:, :].rearrange("a b -> (a b)").partition_broadcast(128), writes=[lv])
    rec.dma("sp", gsub[:, :], subg_d[:, :], writes=[gsub])
    rec.dma("sp", esink[:, :], sink_d[0:4].partition_broadcast(128), writes=[esink])
    rec.dma("pool", masks[:, :, :], mask_d[:, :, :], writes=[masks], cast=True)
    rec.op("dve", lambda e: e.tensor_tensor(out=lp[:, :, :], in0=lv[:, 0:4:2, :], in1=lv[:, 1:4:2, :], op=ALU.mult), reads=[lv], writes=[lp])
    rec.op("dve", lambda e: e.tensor_reduce(out=ls[:, :], in_=lp[:, :, :], axis=AX.X, op=ALU.add), reads=[lp], writes=[ls])
    rec.op("act", lambda e: e.activation(out=ls[:, :], in_=ls[:, :], func=AF.Exp), reads=[ls], writes=[ls])
    rec.op("act", lambda e: e.activation(out=esink[:, :], in_=esink[:, :], func=AF.Exp), reads=[esink], writes=[esink])
    rec.op("dve", lambda e: e.tensor_tensor(out=lam[:, :], in0=ls[:, 0:1], in1=ls[:, 1:2], op=ALU.subtract), reads=[ls], writes=[lam])
    rec.op("dve", lambda e: e.tensor_scalar(out=lam[:, :], in0=lam[:, :], scalar1=lam_init, scalar2=None, op0=ALU.add), reads=[lam], writes=[lam])
    rec.op("dve", lambda e: e.tensor_scalar(out=gsub[:, :], in0=gsub[:, :], scalar1=1.0 - lam_init, scalar2=None, op0=ALU.mult), reads=[gsub], writes=[gsub])
    if need_ctx:
        rec.dma("sp", qtc[:, :, :], src["qTc"][:, :, :].rearrange("c p t -> p c t"), writes=[qtc])

    def mm(out_ap, lhsT, rhs, start, stop, base, reads, writes):
        kw = {}
        if base == 96:
            kw["tile_position"] = (96, 0)
        rec.op("pe", lambda e: e.matmul(out_ap, lhsT=lhsT, rhs=rhs, start=start, stop=stop, skip_group_check=True, **kw), reads=reads, writes=writes)

    def finalize(accl, W, kind, h, dst_ap, sink_h=None):
        rzl = []
        zsl = []
        osl = []
        for a in accl:
            zs = zss.next()
            if sink_h is None:
                rec.op("dve", lambda e, a=a, zs=zs: e.tensor_scalar(out=zs[:, 0:W], in0=a[64:128, 0:W], scalar1=1.0, scalar2=None, op0=ALU.mult),
                       reads=[a], writes=[zs])
            else:
                rec.op("dve", lambda e, a=a, zs=zs: e.tensor_scalar(out=zs[:, 0:W], in0=a[64:128, 0:W], scalar1=esink[0:64, sink_h:sink_h + 1],
                                                                  scalar2=None, op0=ALU.add), reads=[a, esink], writes=[zs])
            zsl.append(zs)
            if kind == "A":
                ob = (fa, fb)[len(osl)]
                rec.op("dve", lambda e, a=a, ob=ob: e.tensor_scalar(out=ob[:, 0:W], in0=a[0:64, 0:W], scalar1=1.0, scalar2=None, op0=ALU.mult),
                       reads=[a], writes=[ob])
                osl.append(ob)
        for zs in zsl:
            rz = rzs.next()
            rec.op("dve", lambda e, zs=zs, rz=rz: e.reciprocal(out=rz[:, 0:W], in_=zs[:, 0:W]), reads=[zs], writes=[rz])
            rzl.append(rz)
        if kind == "A":
            accl = osl
        ot = ots.next()
        if kind != "A":
            a, rz = accl[0], rzl[0]
            rec.op("dve", lambda e: e.tensor_tensor(out=ot[:, 0:W], in0=a[0:64, 0:W], in1=rz[:, 0:W], op=ALU.mult), reads=[a, rz], writes=[ot])
        else:
            a1, a2 = accl
            r1, r2 = rzl
            rec.op("dve", lambda e: e.tensor_tensor(out=fa[:, 0:W], in0=fa[:, 0:W], in1=r1[:, 0:W], op=ALU.mult), reads=[r1], writes=[fa])
            rec.op("dve", lambda e: e.scalar_tensor_tensor(out=fb[:, 0:W], in0=fb[:, 0:W], scalar=lam[0:64, 0:1], in1=r2[:, 0:W],
                                                           op0=ALU.mult, op1=ALU.mult), reads=[r2, lam], writes=[fb])
            rec.op("dve", lambda e: e.tensor_tensor(out=fa[:, 0:W], in0=fa[:, 0:W], in1=fb[:, 0:W], op=ALU.subtract), reads=[fb], writes=[fa])
            rec.op("pool", lambda e: e.tensor_tensor(out=fc[:, 0:W], in0=fa[:, 0:W], in1=fa[:, 0:W], op=ALU.mult), reads=[fa], writes=[fc])
            pm = Sr.next()
            rec.op("pe", lambda e: e.matmul(pm[0:64, 0:W], lhsT=L.ones64[:, :], rhs=fc[:, 0:W], start=True, stop=True), reads=[fc, L.ones64], writes=[pm])
            rec.op("act", lambda e: e.activation(out=fb[:, 0:W], in_=pm[0:64, 0:W], func=AF.Sqrt, bias=L.eps[0:64, 0:1], scale=1.0), reads=[pm, L.eps], writes=[fb])
            rec.op("dve", lambda e: e.reciprocal(out=fc[:, 0:W], in_=fb[:, 0:W]), reads=[fb], writes=[fc])
            rec.op("dve", lambda e: e.scalar_tensor_tensor(out=ot[:, 0:W], in0=fa[:, 0:W], scalar=gsub[:, 0:1], in1=fc[:, 0:W],
                                                           op0=ALU.mult, op1=ALU.mult), reads=[fa, fc, gsub], writes=[ot])
        rec.dma("sp", dst_ap, ot[:, 0:W], reads=[ot])

    def attend(ktb, vtb, q_ap_fn, qbuf, comps, scale, ktiles, W, kind, h, dst_ap, sink_h=None):
        nu = len(comps)
        accl = [accs.next() for _ in range(nu)]
        if nu == 2:
            groups = [[(kt, 0), (kt, 1)] for kt in ktiles]
        else:
            groups = [[(kt, 0) for kt in ktiles[i:i + 2]] for i in range(0, len(ktiles), 2)]
        started = [False] * nu

        def qk(grp):
            S = Sr.next()
            for j, (kt, u) in enumerate(grp):
                base, K = comps[u]
                qap, qb_ = q_ap_fn(u, base, K)
                mm(S[:, j * W:(j + 1) * W], ktb[base:base + K, kt * 128:(kt + 1) * 128], qap, True, True, base, [ktb, qb_], [S])
            return S
        PD = DBG.get("pd", 2)
        Sq = [qk(groups[i]) for i in range(min(PD, len(groups)))]
        for gi, grp in enumerate(groups):
            if gi + PD < len(groups):
                Sq.append(qk(groups[gi + PD]))
            S = Sq.pop(0)
            P = pTs.next()
            n = len(grp) * W
            if DBG["att"] <= 1:
                continue
            rec.op("act", lambda e, S=S, P=P, n=n: e.activation(out=P[:, 0:n], in_=S[:, 0:n], func=AF.Exp, scale=scale), reads=[S], writes=[P])
            if DBG["att"] <= 2:
                continue
            for _ in range(ndummy if W == 512 else 0):
                rec.op("pe", lambda e: e.matmul(dummy[:, 0:128 * DBG.get("dumw", 2)], lhsT=L.ident_b[:, :], rhs=masks[:, 0:DBG.get("dumw", 2), :].rearrange("p a b -> p (a b)"), start=True, stop=True,
                                                skip_group_check=True), reads=[], writes=[])
            for j, (kt, u) in enumerate(grp):
                a = accl[u]
                last = (gi == len(groups) - 1) and (nu == 2 or j == len(grp) - 1)
                mm(a[:, 0:W], vtb[:, kt, :], P[:, j * W:(j + 1) * W], not started[u], last, 0, [vtb, P], [a])
                started[u] = True
        if DBG["att"] >= 4:
            finalize(accl, W, kind, h, dst_ap, sink_h)

    def kall(r, c):
        if "ga" in src:
            b_ = src["ga"][c]
            return b_[r * 128:(r + 1) * 128, :], [b_]
        return src["kTall"][r, c, :, :], []

    def vall(r, slot):
        if "ga" in src:
            b_ = src["ga"][10 + slot]
            return b_[r * 128:(r + 1) * 128, :].rearrange("p (t d) -> p t d", d=128), [b_]
        return src["vall"][r, slot, :, :, :], []

    def load_kv(c, slot):
        ktb = kts.next()
        vtb = vtsr.next()
        rec.dma("sp", ktb[:, 0:256], src["kTc"][c, :, :], writes=[ktb])
        rec.dma("sp", vtb[:, 0:2, :], src["vc"][slot, :, :, :], writes=[vtb])
        for r in range(4):
            ap_, rd = kall(r, c)
            rec.dma("sp", ktb[:, 256 + r * TOK:256 + (r + 1) * TOK], ap_, reads=rd, writes=[ktb])
            ap_, rd = vall(r, slot)
            rec.dma("sp", vtb[:, 2 + r * NT:2 + (r + 1) * NT, :], ap_, reads=rd, writes=[vtb])
        return ktb, vtb

    def load_v(slot):
        vtb = vtsr.next()
        rec.dma("sp", vtb[:, 0:2, :], src["vc"][slot, :, :, :], writes=[vtb])
        for r in range(4):
            ap_, rd = vall(r, slot)
            rec.dma("sp", vtb[:, 2 + r * NT:2 + (r + 1) * NT, :], ap_, reads=rd, writes=[vtb])
        return vtb

    ktiles_all = list(range(NKT)) if kt_limit is None else list(range(kt_limit))
    jobs = []
    for i in range(2):
        jobs.append((i, [(h, [((h % 2) * 64, 32), ((h % 2) * 64 + 32, 32)], h, h // 2, (h % 2) * 64) for h in (2 * i, 2 * i + 1)], 32 ** -0.5, "A"))
    for g in range(2):
        jobs.append((2 + g, [(h, [((h % 2) * 64, 64)], 4 + g, 2 + h // 2, (h % 2) * 64) for h in (2 * g, 2 * g + 1)], 64 ** -0.5, "B"))
    for h in range(4):
        jobs.append((4 + h, [(h, [(0, 96)], 6 + h, 4 + h // 2, (h % 2) * 64)], 96 ** -0.5, "C"))
    if DBG["att"] <= 0:
        return
    hjobs = []
    for (c, heads, scale, kind) in jobs:
        if kind not in DBG["kinds"]:
            continue
        prev_slot = None
        for hi, (h, comps, slot, oc, orow) in enumerate(heads):
            hjobs.append(dict(c=c, h=h, comps=comps, slot=slot, oc=oc, orow=orow, scale=scale, kind=kind,
                              ldk=(hi == 0), ldv=(slot != prev_slot)))
            prev_slot = slot
    state = {"ktb": None, "vtb": None, "qtb": None}

    def issue_loads(j):
        if j["ldk"]:
            qtb = qts.next()
            rec.dma("sp", qtb[:, :], src["qT"][j["c"], :, :], writes=[qtb])
            ktb = kts.next()
            rec.dma("sp", ktb[:, 0:256], src["kTc"][j["c"], :, :], writes=[ktb])
            for r in range(4):
                ap_, rd = kall(r, j["c"])
                rec.dma("sp", ktb[:, 256 + r * TOK:256 + (r + 1) * TOK], ap_, reads=rd, writes=[ktb])
            j["ktb"], j["qtb"] = ktb, qtb
        if j["ldv"]:
            j["vtb"] = load_v(j["slot"])

    if hjobs:
        issue_loads(hjobs[0])
    for ji, j in enumerate(hjobs):
        for k_ in ("ktb", "vtb", "qtb"):
            if k_ in j:
                state[k_] = j[k_]
        ktb, vtb, qtb = state["ktb"], state["vtb"], state["qtb"]
        if ji + 1 < len(hjobs):
            issue_loads(hjobs[ji + 1])
        c, h, comps, oc, orow, scale, kind = j["c"], j["h"], j["comps"], j["oc"], j["orow"], j["scale"], j["kind"]

        def masked_q(src_fn, src_buf, W):
            bl = []
            for cp in range(2):
                mb = qmask[h % 2][cp].next()
                rows = (h % 2) * 64 + cp * 32
                rec.op("pool", lambda e, mb=mb, rows=rows: e.tensor_copy(out=mb[rows:rows + 32, 0:W], in_=src_fn(rows)), reads=[src_buf], writes=[mb])
                bl.append(mb)
            return bl
        for qb in range(n_qb):
            if kind == "A":
                bl = masked_q(lambda rows, qb=qb, qtb=qtb: qtb[rows:rows + 32, qb * 512:(qb + 1) * 512], qtb, 512)
                attend(ktb, vtb, lambda u, base, K, bl=bl: (bl[u][:, 0:512], bl[u]), None, [(0, 128), (0, 128)], scale, ktiles_all, 512,
                       kind, h, src["OT"][oc, orow:orow + 64, qb * 512:(qb + 1) * 512])
            else:
                attend(ktb, vtb, lambda u, base, K, qb=qb, qtb=qtb: (qtb[base:base + K, qb * 512:(qb + 1) * 512], qtb), None, comps, scale, ktiles_all, 512,
                       kind, h, src["OT"][oc, orow:orow + 64, qb * 512:(qb + 1) * 512])
        if need_ctx:
            if kind == "A":
                bl = masked_q(lambda rows, c=c: qtc[rows:rows + 32, c, :], qtc, 256)
                attend(ktb, vtb, lambda u, base, K, bl=bl: (bl[u][:, 0:256], bl[u]), None, [(0, 128), (0, 128)], scale, [0, 1], 256, kind, h,
                       src["OTc"][oc, orow:orow + 64, :])
            else:
                attend(ktb, vtb, lambda u, base, K, c=c: (qtc[base:base + K, c, :], qtc), None, comps, scale, [0, 1], 256, kind, h,
                       src["OTc"][oc, orow:orow + 64, :])
    for g in range(2 if "D" in DBG["kinds"] else 0):
        c = 8 + g
        slot = 10 + g
        qtb = qts.next()
        rec.dma("sp", qtb[:, :], src["qT"][c, :, :], writes=[qtb])
        ktb = kts.next()
        vtb = vtsr.next()
        rec.dma("sp", ktb[:, 0:256], src["kTc"][c, :, :], writes=[ktb])
        rec.dma("sp", vtb[:, 0:2, :], src["vc"][slot, :, :, :], writes=[vtb])
        if "kTwin" in src:
            rec.dma("sp", ktb[:, 256:256 + NWIN * 128], src["kTwin"][g, :, :], writes=[ktb])
            rec.dma("sp", vtb[:, 2:2 + NWIN, :], src["vwin"][g, :, :, :], writes=[vtb])
        else:
            rec.dma("sp", ktb[:, 384:384 + TOK], src["kTown"][c, :, :], writes=[ktb])
            rec.dma("sp", vtb[:, 3:3 + NT, :], src["vown"][slot, :, :, :], writes=[vtb])
            for side in range(2):
                kcol = (TOK - 128) if side == 0 else 0
                vt_i = (NT - 1) if side == 0 else 0
                for r_ in range(4):
                    ap_, rd = kall(r_, c)
                    rec.dma("sp", hck[:, r_, :], ap_[:, kcol:kcol + 128], reads=rd, writes=[hck])
                    ap_, rd = vall(r_, slot)
                    rec.dma("sp", hcv[:, r_, :], ap_[:, vt_i, :], reads=rd, writes=[hcv])
                kd = ktb[:, 256:384] if side == 0 else ktb[:, 384 + TOK:384 + TOK + 128]
                vd = vtb[:, 2, :] if side == 0 else vtb[:, 3 + NT, :]
                for (cand, cb, dst_ap, dst_b) in ((hck, hck, kd, ktb), (hcv, hcv, vd, vtb)):
                    rec.op("dve", lambda e, cand=cand, side=side: e.tensor_scalar(out=hacc[:, :], in0=cand[:, 0, :], scalar1=onehot[:, side * 4:side * 4 + 1],
                                                                               scalar2=None, op0=ALU.mult), reads=[cb, onehot], writes=[hacc])
                    for r_ in range(1, 4):
                        last = r_ == 3
                        rec.op("dve", lambda e, cand=cand, side=side, r_=r_, last=last, dst_ap=dst_ap: e.scalar_tensor_tensor(
                            out=dst_ap if last else hacc[:, :], in0=cand[:, r_, :], scalar=onehot[:, side * 4 + r_:side * 4 + r_ + 1], in1=hacc[:, :],
                            op0=ALU.mult, op1=ALU.add), reads=[cb, onehot, hacc], writes=[dst_b] if last else [hacc])
        for h in (2 * g, 2 * g + 1):
            base = (h % 2) * 64
            def d_qk(j):
                tiles = [0, 1, 2 + j, 3 + j, 4 + j]
                S = Sr.next()
                for i, kt in enumerate(tiles):
                    mm(S[:, i * 128:(i + 1) * 128], ktb[base:base + 64, kt * 128:(kt + 1) * 128], qtb[base:base + 64, j * 128:(j + 1) * 128],
                       True, True, base, [ktb, qtb], [S])
                return S
            for qb in range(n_qb):
                acc = accs.next()
                S_next = d_qk(qb * 4)
                for s in range(4):
                    j = qb * 4 + s
                    tiles = [0, 1, 2 + j, 3 + j, 4 + j]
                    S = S_next
                    if s + 1 < 4:
                        S_next = d_qk(j + 1)
                    P = pTs.next()
                    rec.op("act", lambda e, S=S, P=P: e.activation(out=P[:, 0:640], in_=S[:, 0:640], func=AF.Exp, scale=64 ** -0.5), reads=[S], writes=[P])
                    mp = 2 if j == 0 else 0
                    mn = 3 if j == NT - 1 else 1
                    rec.op("pool", lambda e, P=P, mp=mp: e.tensor_tensor(out=P[:, 256:384], in0=P[:, 256:384], in1=masks[:, mp, :], op=ALU.mult),
                           reads=[masks], writes=[P])
                    rec.op("pool", lambda e, P=P, mn=mn: e.tensor_tensor(out=P[:, 512:640], in0=P[:, 512:640], in1=masks[:, mn, :], op=ALU.mult),
                           reads=[masks], writes=[P])
                    for i, kt in enumerate(tiles):
                        mm(acc[:, s * 128:(s + 1) * 128], vtb[:, kt, :], P[:, i * 128:(i + 1) * 128], i == 0, i == 4, 0, [vtb, P], [acc])
                finalize([acc], 512, "D", h, src["OT"][6 + h // 2, base:base + 64, qb * 512:(qb + 1) * 512], sink_h=h)
            if need_ctx:
                attend(ktb, vtb, lambda u, b_, K, c=c: (qtc[b_:b_ + K, c, :], qtc), None, [(base, 64)], 64 ** -0.5, [0, 1], 256, "D", h,
                       src["OTc"][6 + h // 2, base:base + 64, :], sink_h=h)


def phase3(cx, L, st, x_srcs, wts):
    rec = cx.rec
    wout = rec.sb("wout", [128, 8, 1024], BF16, st)
    rw = rec.sb("rw", [128, 8, 16], F32, st)
    rbias = rec.sb("rbias", [128, 16], F32, st)
    lnt = [rec.sb("lnt", [128, 1024], F32, st) for _ in range(4)]
    pre = wts.get("bf16", False)
    wq = "sp" if pre else "pool"
    for kc in range(8):
        rec.dma(wq, wout[:, kc, :], wts["w_out"][:, kc, :], writes=[wout], cast=not pre)
    rec.dma("sp", rw[:, :, :], wts["router_w"][:, :, :], writes=[rw])
    rec.dma("sp", rbias[:, :], wts["router_bias"][0:16].partition_broadcast(128), writes=[rbias])
    for i in range(4):
        rec.dma("sp", lnt[i][:, :], wts["ln"][i, :].partition_broadcast(128), writes=[lnt[i]])
    x1s = [rec.sb("x1s", [128, 1024], F32, st) for _ in range(4)]
    xts = Ring([rec.sb("xt3", [128, 1024], F32, st) for _ in range(2)])
    u = rec.sb("u", [128, 1024], F32, st)
    tmp = rec.sb("tmp", [128, 1024], F32, st)
    h2Tf = rec.sb("h2Tf", [128, 8, 128], F32, st)
    h2T = rec.sb("h2T", [128, 8, 512], BF16, st)
    otin = rec.sb("otin", [128, 8, 512], BF16, st)
    actT = rec.sb("actT", [128, 16, 2, 512], BF16, st)
    wgus = Ring([rec.sb("wgu", [128, 8, 512], BF16, st) for _ in range(3)])
    wds = Ring([rec.sb("wd", [128, 2, 1024], BF16, st) for _ in range(4)])
    gates = rec.sb("gates", [128, 4, 16], F32, st)
    st6 = rec.sb("st6b", [128, 2, 6], F32, st)
    mv = rec.sb("mvb", [128, 2], F32, st)
    sq1 = rec.sb("sq1b", [128, 1], F32, st)
    rstd = rec.sb("rstdb", [128, 1], F32, st)
    s16 = rec.sb("s16", [128, 16], F32, st)
    sel = rec.sb("sel", [128, 16], F32, st)
    sel2 = rec.sb("sel2", [128, 16], F32, st)
    eq = rec.sb("eq", [128, 16], F32, st)
    m1 = rec.sb("m1", [128, 4], F32, st)
    m2 = rec.sb("m2", [128, 4], F32, st)
    gs = rec.sb("gs", [128, 4], F32, st)
    gm = rec.sb("gm", [128, 1], F32, st)
    sil = Ring([rec.sb("sil", [128, 256], F32, st) for _ in range(2)])
    actb = Ring([rec.sb("actb", [128, 256], BF16, st) for _ in range(2)])
    B = [rec.ps("B", [128, 512], F32, st) for _ in range(8)]
    psTb = rec.buf("psTb", B[7].t[:, :].bitcast(BF16))

    def ln_stats(src):
        for i in range(2):
            rec.op("dve", lambda e, i=i: e.bn_stats(out=st6[:, i, :], in_=src[:, i * 512:(i + 1) * 512]), reads=[src], writes=[st6])
        rec.op("dve", lambda e: e.bn_aggr(out=mv[:, :], in_=st6[:, :, :].rearrange("p a b -> p (a b)")), reads=[st6], writes=[mv])
        rec.op("act", lambda e: e.activation(out=sq1[:, :], in_=mv[:, 1:2], func=AF.Sqrt, bias=L.eps[:, 0:1], scale=1.0), reads=[mv, L.eps], writes=[sq1])
        rec.op("dve", lambda e: e.reciprocal(out=rstd[:, :], in_=sq1[:, :]), reads=[sq1], writes=[rstd])

    def gated_ln(x_in, ybanks, Gt, gt, bt, dst):
        for hh in range(2):
            rec.op("dve", lambda e, hh=hh: e.tensor_tensor(out=tmp[:, hh * 512:(hh + 1) * 512], in0=ybanks[hh][:, :], in1=Gt[:, hh * 512:(hh + 1) * 512],
                                                           op=ALU.mult), reads=[ybanks[hh], Gt], writes=[tmp])
        rec.op("dve", lambda e: e.scalar_tensor_tensor(out=u[:, :], in0=x_in[:, :], scalar=ALPHA, in1=tmp[:, :], op0=ALU.mult, op1=ALU.add),
               reads=[x_in, tmp], writes=[u])
        ln_stats(u)
        rec.op("dve", lambda e: e.tensor_scalar(out=u[:, :], in0=u[:, :], scalar1=mv[:, 0:1], scalar2=rstd[:, 0:1], op0=ALU.subtract, op1=ALU.mult),
               reads=[mv, rstd], writes=[u])
        rec.op("dve", lambda e: e.tensor_tensor(out=u[:, :], in0=u[:, :], in1=gt[:, :], op=ALU.mult), reads=[gt], writes=[u])
        rec.op("dve", lambda e: e.tensor_tensor(out=dst[:, :], in0=u[:, :], in1=bt[:, :], op=ALU.add), reads=[u, bt], writes=[dst])

    for (x_src, OT, x_dst, cond, ntok) in x_srcs:
        nblk = (ntok + 511) // 512
        for blk in range(nblk):
            nt = min(4, (ntok - blk * 512) // 128)
            ncol = nt * 128
            rec.dma("sp", otin[:, :, 0:ncol], OT[:, :, blk * 512:blk * 512 + ncol].rearrange("c p t -> p c t"), writes=[otin])
            for t in range(nt):
                xt = xts.next()
                r0 = blk * 512 + t * 128
                rec.dma("sp", xt[:, :], x_src[r0:r0 + 128, :], writes=[xt])
                for hh in range(2):
                    for kc in range(8):
                        rec.op("pe", lambda e, hh=hh, kc=kc, t=t: e.matmul(B[hh][:, :], lhsT=otin[:, kc, t * 128:(t + 1) * 128],
                                                                       rhs=wout[:, kc, hh * 512:(hh + 1) * 512], start=(kc == 0), stop=(kc == 7)),
                               reads=[otin, wout], writes=[B[hh]])
                x1 = x1s[t]
                gated_ln(xt, B[0:2], L.Gb[0][cond], lnt[0], lnt[1], x1)
                ln_stats(x1)
                rec.op("dve", lambda e, x1=x1: e.tensor_scalar(out=tmp[:, :], in0=x1[:, :], scalar1=mv[:, 0:1], scalar2=rstd[:, 0:1],
                                                               op0=ALU.subtract, op1=ALU.mult), reads=[x1, mv, rstd], writes=[tmp])
                for kc in range(8):
                    bk = B[2 + kc // 4]
                    rec.op("pe", lambda e, kc=kc, bk=bk: e.transpose(out=bk[:, (kc % 4) * 128:(kc % 4 + 1) * 128], in_=tmp[:, kc * 128:(kc + 1) * 128],
                                                                     identity=L.ident_f[:, :]), reads=[tmp, L.ident_f], writes=[bk])
                for kc in range(8):
                    bk = B[2 + kc // 4]
                    rec.op("dve", lambda e, kc=kc, bk=bk, cond=cond: e.tensor_scalar(out=h2Tf[:, kc, :], in0=bk[:, (kc % 4) * 128:(kc % 4 + 1) * 128],
                                                                          scalar1=L.modT[:, 32 + kc, cond:cond + 1], scalar2=L.modT[:, 24 + kc, cond:cond + 1],
                                                                          op0=ALU.mult, op1=ALU.add), reads=[bk, L.modT], writes=[h2Tf])
                rec.op("pool", lambda e, t=t: e.tensor_copy(out=h2T[:, :, t * 128:(t + 1) * 128], in_=h2Tf[:, :, :]), reads=[h2Tf], writes=[h2T])
                for kc in range(8):
                    rec.op("pe", lambda e, kc=kc: e.matmul(B[4][:, 0:16], lhsT=h2Tf[:, kc, :], rhs=rw[:, kc, :], start=(kc == 0), stop=(kc == 7)),
                           reads=[h2Tf, rw], writes=[B[4]])
                rec.op("act", lambda e: e.activation(out=s16[:, :], in_=B[4][:, 0:16], func=AF.Sigmoid), reads=[B[4]], writes=[s16])
                v44 = lambda b_: b_[:, :].rearrange("p (g k) -> p g k", g=4)
                bc4 = lambda b_: b_[:, :].unsqueeze(2).to_broadcast([128, 4, 4])
                rec.op("dve", lambda e: e.tensor_tensor(out=sel[:, :], in0=s16[:, :], in1=rbias[:, :], op=ALU.add), reads=[s16, rbias], writes=[sel])
                rec.op("dve", lambda e: e.tensor_reduce(out=m1[:, :], in_=v44(sel), axis=AX.X, op=ALU.max), reads=[sel], writes=[m1])
                rec.op("dve", lambda e: e.tensor_tensor(out=v44(eq), in0=v44(sel), in1=bc4(m1), op=ALU.is_equal), reads=[sel, m1], writes=[eq])
                rec.op("dve", lambda e: e.scalar_tensor_tensor(out=sel2[:, :], in0=eq[:, :], scalar=-1e9, in1=sel[:, :], op0=ALU.mult, op1=ALU.add),
                       reads=[eq, sel], writes=[sel2])
                rec.op("dve", lambda e: e.tensor_reduce(out=m2[:, :], in_=v44(sel2), axis=AX.X, op=ALU.max), reads=[sel2], writes=[m2])
                rec.op("dve", lambda e: e.tensor_tensor(out=gs[:, :], in0=m1[:, :], in1=m2[:, :], op=ALU.add), reads=[m1, m2], writes=[gs])
                rec.op("dve", lambda e: e.tensor_reduce(out=gm[:, :], in_=gs[:, :], axis=AX.X, op=ALU.max), reads=[gs], writes=[gm])
                rec.op("dve", lambda e: e.tensor_scalar(out=gs[:, :], in0=gs[:, :], scalar1=gm[:, 0:1], scalar2=None, op0=ALU.is_equal), reads=[gm], writes=[gs])
                rec.op("dve", lambda e: e.tensor_tensor(out=v44(eq), in0=v44(sel), in1=bc4(m2), op=ALU.is_ge), reads=[sel, m2], writes=[eq])
                rec.op("dve", lambda e: e.tensor_tensor(out=v44(eq), in0=v44(eq), in1=bc4(gs), op=ALU.mult), reads=[gs], writes=[eq])
                rec.op("dve", lambda e: e.tensor_tensor(out=eq[:, :], in0=eq[:, :], in1=s16[:, :], op=ALU.mult), reads=[s16], writes=[eq])
                rec.op("dve", lambda e: e.tensor_reduce(out=gm[:, :], in_=eq[:, :], axis=AX.X, op=ALU.add), reads=[eq], writes=[gm])
                rec.op("dve", lambda e: e.reciprocal(out=gm[:, :], in_=gm[:, :]), reads=[gm], writes=[gm])
                rec.op("dve", lambda e, t=t: e.tensor_scalar(out=gates[:, t, :], in0=eq[:, :], scalar1=gm[:, 0:1], scalar2=None, op0=ALU.mult),
                       reads=[eq, gm], writes=[gates])
            items = [(ex, t) for ex in range(NE) for t in range(nt)]
            cur_w = [None]

            def mm_part(ex, t):
                if t == 0:
                    cur_w[0] = wgus.next()
                    rec.dma(wq, cur_w[0][:, :, :], wts["wgu"][ex, :, :, :], writes=[cur_w[0]], cast=not pre)
                wgu = cur_w[0]
                bk = B[(ex * nt + t) % 4]
                for kc in range(8):
                    rec.op("pe", lambda e, kc=kc, t=t, bk=bk, wgu=wgu: e.matmul(bk[:, :], lhsT=h2T[:, kc, t * 128:(t + 1) * 128], rhs=wgu[:, kc, :],
                                                                             start=(kc == 0), stop=(kc == 7)), reads=[h2T, wgu], writes=[bk])
                return bk

            def post_part(ex, t, bk):
                sl_ = sil.next()
                ab = actb.next()
                rec.op("act", lambda e, bk=bk, sl_=sl_: e.activation(out=sl_[:, :], in_=bk[:, 0:256], func=AF.Silu), reads=[bk], writes=[sl_])
                rec.op("dve", lambda e, bk=bk, sl_=sl_, ab=ab, t=t, ex=ex: e.scalar_tensor_tensor(out=ab[:, :], in0=sl_[:, :], scalar=gates[:, t, ex:ex + 1],
                                                                                                in1=bk[:, 256:512], op0=ALU.mult, op1=ALU.mult),
                       reads=[sl_, gates, bk], writes=[ab])
                for fc in range(2):
                    rec.op("pe", lambda e, fc=fc, ab=ab: e.transpose(out=psTb[:, fc * 128:(fc + 1) * 128], in_=ab[:, fc * 128:(fc + 1) * 128],
                                                                     identity=L.ident_b[:, :]), reads=[ab, L.ident_b], writes=[psTb, B[7]])
                rec.op("act", lambda e, ex=ex, t=t: e.activation(out=actT[:, ex, :, t * 128:(t + 1) * 128],
                                                                 in_=psTb[:, 0:256].rearrange("p (f c) -> p f c", f=2), func=AF.Copy),
                       reads=[psTb, B[7]], writes=[actT])

            bk_cur = mm_part(*items[0])
            for ii, (ex, t) in enumerate(items):
                bk_next = mm_part(*items[ii + 1]) if ii + 1 < len(items) else None
                post_part(ex, t, bk_cur)
                bk_cur = bk_next
            for ex in range(NE):
                wd = wds.next()
                rec.dma(wq, wd[:, :, :], wts["wd"][ex, :, :, :], writes=[wd], cast=not pre)
                for t in range(nt):
                    for hh in range(2):
                        for fc in range(2):
                            rec.op("pe", lambda e, ex=ex, t=t, hh=hh, fc=fc, wd=wd: e.matmul(
                                B[t * 2 + hh][:, :], lhsT=actT[:, ex, fc, t * 128:(t + 1) * 128], rhs=wd[:, fc, hh * 512:(hh + 1) * 512],
                                start=(ex == 0 and fc == 0), stop=(ex == NE - 1 and fc == 1)), reads=[actT, wd], writes=[B[t * 2 + hh], psTb] if t == 3 and hh == 1 else [B[t * 2 + hh]])
            for t in range(nt):
                r0 = blk * 512 + t * 128
                xo = xts.next()
                gated_ln(x1s[t], B[2 * t:2 * t + 2], L.Gb[1][cond], lnt[2], lnt[3], xo)
                rec.dma("sp", x_dst[r0:r0 + 128, :], xo[:, :], reads=[xo])


def build_A(n_lat_tiles=NT, do_ctx=True):
    cx = Ctx()
    L = NS()
    rec = cx.rec
    setup_consts(cx, L)
    st = ExitStack()
    layer_setup(cx, L, st)
    st.close()
    rec.barrier()
    x_src = cx.din("x_own", [TOK, 1024])
    xc_src = cx.din("xc", [CTX, 1024])
    tab_src = cx.din("rope_tab", [TOK + CTX, 192])
    outs = {"qT": cx.dout("qT", [10, 128, TOK], BF16), "kT": cx.dout("kT", [10, 128, TOK], BF16), "v": cx.dout("v", [12, 128, NT, 128], BF16),
            "qTc": cx.dout("qTc", [10, 128, CTX], BF16), "kTc": cx.dout("kTc", [10, 128, CTX], BF16), "vc": cx.dout("vc", [12, 128, 2, 128], BF16)}
    st = ExitStack()
    phase1(cx, L, st, x_src, xc_src, tab_src, outs, n_lat_tiles=n_lat_tiles, do_ctx=do_ctx)
    rec.wait_all_on("sp")
    rec.emit()
    return cx


def build_B(l, need_ctx):
    cx = Ctx()
    L = NS()
    rec = cx.rec
    setup_consts(cx, L)
    st = ExitStack()
    layer_setup(cx, L, st)
    st.close()
    rec.barrier()
    src = {"qT": cx.din("qT", [10, 128, TOK], BF16), "kTall": cx.din("kTall", [4, 10, 128, TOK], BF16),
           "vall": cx.din("vall", [4, 12, 128, NT, 128], BF16), "kTc": cx.din("kTc", [10, 128, CTX], BF16),
           "vc": cx.din("vc", [12, 128, 2, 128], BF16), "qTc": cx.din("qTc", [10, 128, CTX], BF16),
           "kTwin": cx.din("kTwin", [2, 128, NWIN * 128], BF16), "vwin": cx.din("vwin", [2, 128, NWIN, 128], BF16),
           "OT": (cx.dout if DBG.get("export") else cx.dint)("OT", [8, 128, TOK], BF16),
           "OTc": (cx.dout if DBG.get("export") else cx.dint)("OTc", [8, 128, CTX], BF16)}
    st = ExitStack()
    attention(cx, L, st, l, src, need_ctx)
    st.close()
    rec.barrier()
    wts = {"w_out": cx.din("w_out", [128, 8, 1024]), "wgu": cx.din("wgu", [NE, 128, 8, 512]), "wd": cx.din("wd", [NE, 128, 2, 1024]),
           "router_w": cx.din("router_w", [128, 8, 16]), "router_bias": cx.din("router_bias", [16]), "ln": cx.din("ln", [4, 1024])}
    x_src = cx.din("x_own", [TOK, 1024])
    x_dst = cx.dout("x_next", [TOK, 1024])
    xs = [(x_src, src["OT"], x_dst, 0, TOK)]
    if need_ctx:
        xc_src = cx.din("xc", [CTX, 1024])
        xc_dst = cx.dout("xc_next", [CTX, 1024])
        xs.append((xc_src, src["OTc"], xc_dst, 1, CTX))
    st = ExitStack()
    if DBG.get("p3", True):
        phase3(cx, L, st, xs, wts)
    else:
        rec.dma("sp", x_dst[0:128, :], x_src[0:128, :], key="D_dbg")
    rec.wait_all_on("sp")
    rec.emit()
    return cx


def prep_B_weights(inp, l):
    d = {}
    d["w_out"] = _pm(inp["w_out"][l], 8)
    d["wgu"] = np.ascontiguousarray(np.stack([_pm(np.concatenate([inp["exp_w_gate"][l, e], inp["exp_w_up"][l, e]], 1), 8) for e in range(NE)]))
    d["wd"] = np.ascontiguousarray(np.stack([_pm(inp["exp_w_down"][l, e], 2) for e in range(NE)]))
    d["router_w"] = _pm(inp["router_w"], 8)
    d["router_bias"] = np.ascontiguousarray(inp["router_bias"])
    d["ln"] = np.ascontiguousarray(np.stack([inp["ln1_g"][l], inp["ln1_b"][l], inp["ln2_g"][l], inp["ln2_b"][l]]))
    d["lamv"] = np.ascontiguousarray(np.stack([inp["diff_lambda_q1"][l], inp["diff_lambda_k1"][l], inp["diff_lambda_q2"][l], inp["diff_lambda_k2"][l]]))
    d["subln_g"] = np.ascontiguousarray(inp["diff_subln_g"][l].reshape(64, 1))
    d["swa_sink"] = np.ascontiguousarray(inp["swa_sink"][l])
    return d


def band_masks(r):
    ki = np.arange(128)[:, None]
    qi = np.arange(128)[None, :]
    mprev = (qi <= ki).astype(np.float32)
    mnext = (ki <= qi).astype(np.float32)
    m = np.stack([mprev, mnext, mprev * (0.0 if r == 0 else 1.0), mnext * (0.0 if r == 3 else 1.0)], 1)
    return np.ascontiguousarray(m.astype(np.float32))


KVR = 1280 + 1536


def build_fused():
    cx = Ctx()
    L = NS()
    rec = cx.rec
    nc = cx.nc
    setup_consts(cx, L)
    x_in = cx.din("x_own", [TOK, 1024])
    xc_in = cx.din("xc", [CTX, 1024])
    tab_src = cx.din("rope_tab", [TOK + CTX, 192])
    out = cx.dout("out", [TOK, 1024])
    x1 = cx.dint("x1", [TOK, 1024])
    xc1 = cx.dint("xc1", [CTX, 1024])
    groups = [[0, 1, 2, 3], [4, 5, 6, 7]]
    for l in range(DEPTH):
        cx.sfx = "_l%d" % l
        need_ctx = l < DEPTH - 1
        lst = ExitStack()
        st = ExitStack()
        L.modT = rec.sb("modT", [128, 48, 2], F32, lst)
        L.Gb = [[rec.sb("Gb", [128, 1024], F32, lst) for _ in range(2)] for _ in range(2)]
        wsrc = {"w_out": cx.din("w_out", [128, 8, 1024]), "wgu": cx.din("wgu", [NE, 128, 8, 512]), "wd": cx.din("wd", [NE, 128, 2, 1024])}
        wbf = {"w_out": cx.dint("w_out_bf%d" % l, [128, 8, 1024], BF16), "wgu": cx.dint("wgu_bf%d" % l, [NE, 128, 8, 512], BF16),
               "wd": cx.dint("wd_bf%d" % l, [NE, 128, 2, 1024], BF16)}
        layer_setup(cx, L, st, pst="prealloc")
        st.close()
        rec.barrier()
        rec.release_dma_sems()
        for ex in range(NE):
            rec.dma("pool", wbf["wgu"][ex, :, :, :].rearrange("p a b -> p (a b)"), wsrc["wgu"][ex, :, :, :].rearrange("p a b -> p (a b)"), key="D_wcast", cast=True)
            rec.dma("pool", wbf["wd"][ex, :, :, :].rearrange("p a b -> p (a b)"), wsrc["wd"][ex, :, :, :].rearrange("p a b -> p (a b)"), key="D_wcast", cast=True)
        rec.dma("pool", wbf["w_out"][:, :, :].rearrange("p a b -> p (a b)"), wsrc["w_out"][:, :, :].rearrange("p a b -> p (a b)"), key="D_wcast", cast=True)
        kv_own = nc.dram_tensor("kv_own%d" % l, [KVR, TOK], BF16).ap()
        ga = [Buf("ga", nc.dram_tensor("ga%d_%d" % (l, p_), [4 * 128, TOK], BF16).ap()) for p_ in range(22)]
        qT = cx.dint("qT%d" % l, [10, 128, TOK], BF16)
        qTc = cx.dint("qTc%d" % l, [10, 128, CTX], BF16)
        kTc = cx.dint("kTc%d" % l, [10, 128, CTX], BF16)
        vc = cx.dint("vc%d" % l, [12, 128, 2, 128], BF16)
        OT = cx.dint("OT%d" % l, [8, 128, TOK], BF16)
        OTc = cx.dint("OTc%d" % l, [8, 128, CTX], BF16)
        kT_own = Buf("kTown", kv_own[0:1280, :].rearrange("(c p) t -> c p t", p=128))
        v_own = Buf("vown", kv_own[1280:KVR, :].rearrange("(s p) (t d) -> s p t d", p=128, d=128))
        outs = {"qT": qT, "kT": kT_own, "v": v_own, "qTc": qTc, "kTc": kTc, "vc": vc}
        st = ExitStack()
        phase1(cx, L, st, x_in if l == 0 else x1, xc_in if l == 0 else xc1, tab_src, outs)
        st.close()
        rec.barrier()
        rec.release_dma_sems()
        order = [0, 10, 11, 1, 12, 13, 2, 14, 3, 15, 4, 16, 5, 17, 6, 18, 7, 19, 8, 20, 9, 21]
        for p_ in order:
            rec.collective_piece(lambda e, a=kv_own[p_ * 128:(p_ + 1) * 128, :], b=ga[p_]: e.collective_compute(
                "AllGather", ALU.bypass, replica_groups=groups, ins=[a], outs=[b[:, :]]), ga[p_])
        src = {"qT": qT, "ga": ga, "kTc": kTc, "vc": vc, "qTc": qTc, "kTown": kT_own, "vown": v_own, "OT": OT, "OTc": OTc}
        st = ExitStack()
        attention(cx, L, st, l, src, need_ctx)
        st.close()
        rec.barrier()
        rec.release_dma_sems()
        wts = {"w_out": wbf["w_out"], "wgu": wbf["wgu"], "wd": wbf["wd"], "bf16": True,
               "router_w": cx.din("router_w", [128, 8, 16]), "router_bias": cx.din("router_bias", [16]), "ln": cx.din("ln", [4, 1024])}
        xs = [(x_in if l == 0 else x1, OT, x1 if l == 0 else out, 0, TOK)]
        if need_ctx:
            xs.append((xc_in, OTc, xc1, 1, CTX))
        st = ExitStack()
        phase3(cx, L, st, xs, wts)
        st.close()
        rec.barrier()
        rec.release_dma_sems()
        lst.close()
    rec.wait_all_on("sp")
    rec.emit()
    return cx


def kernel_fused(inp):
    cores = list(range(NCORES))
    lws = [prep_layer(inp, l) for l in range(DEPTH)]
    bws = [prep_B_weights(inp, l) for l in range(DEPTH)]
    shared_w = {"router_w": bws[0]["router_w"], "router_bias": bws[0]["router_bias"]}
    in_maps = []
    for c in cores:
        b, r = c // 4, c % 4
        m = dict(prep_core_common(inp, c))
        m.update(shared_w)
        for l in range(DEPTH):
            for k, v in list(lws[l].items()) + list(bws[l].items()):
                if k not in shared_w:
                    m["%s_l%d" % (k, l)] = v
        m["x_own"] = np.ascontiguousarray(inp["x"][b, r * TOK:(r + 1) * TOK])
        m["xc"] = np.ascontiguousarray(inp["ctx"][b])
        m["c_masks"] = band_masks(r)
        oh = np.zeros((128, 8), np.float32)
        if r > 0:
            oh[:, r - 1] = 1.0
        if r < 3:
            oh[:, 4 + r + 1] = 1.0
        m["onehot"] = oh
        in_maps.append(m)
    prog = _prog("fused", build_fused)
    names = set(prog.dram.keys())
    in_maps = [{k: v for k, v in m.items() if k in names} for m in in_maps]
    res = run_bass_kernel_spmd(prog.nc, in_maps, core_ids=cores).results
    out = np.zeros((BATCH, SEQ, D), np.float32)
    for c in cores:
        out[c // 4, (c % 4) * TOK:(c % 4 + 1) * TOK] = res[c]["out"]
    return out


_PROGS = {}


def _prog(key, fn):
    if key not in _PROGS:
        _PROGS[key] = fn()
    return _PROGS[key]


def kernel(**inputs):
    inp = {k: np.asarray(v) for k, v in inputs.items()}
    return kernel_fused(inp)


def kernel_unfused(**inputs):
    inp = {k: np.asarray(v) for k, v in inputs.items()}
    cores = list(range(NCORES))
    common = [prep_core_common(inp, c) for c in cores]
    x_cur = [np.ascontiguousarray(inp["x"][c // 4, (c % 4) * TOK:(c % 4 + 1) * TOK]) for c in cores]
    xc_cur = [np.ascontiguousarray(inp["ctx"][c // 4]) for c in cores]
    for l in range(DEPTH):
        need_ctx = l < DEPTH - 1
        lw = prep_layer(inp, l)
        pa = _prog("A", build_A)
        in_maps = []
        for c in cores:
            m = dict(lw)
            m.update(common[c])
            m["x_own"] = x_cur[c]
            m["xc"] = xc_cur[c]
            in_maps.append(m)
        ra = run_bass_kernel_spmd(pa.nc, in_maps, core_ids=cores).results
        bw = prep_B_weights(inp, l)
        in_maps = []
        for c in cores:
            b, r = c // 4, c % 4
            grp = [ra[b * 4 + i] for i in range(4)]
            m = {}
            for k in ("w_ada", "b_adaT", "b_ada"):
                m[k] = lw[k]
            m["cT"] = common[c]["cT"]
            m["c_ident"] = common[c]["c_ident"]
            m.update(bw)
            m["qT"] = ra[c]["qT"]
            m["kTc"] = ra[c]["kTc"]
            m["vc"] = ra[c]["vc"]
            m["qTc"] = ra[c]["qTc"]
            m["kTall"] = np.ascontiguousarray(np.stack([g["kT"] for g in grp]))
            m["vall"] = np.ascontiguousarray(np.stack([g["v"] for g in grp]))
            kfull = np.concatenate([g["kT"][8:10] for g in grp], axis=2)
            kpad = np.zeros((2, 128, SEQ + 256), kfull.dtype)
            kpad[:, :, 128:128 + SEQ] = kfull
            m["kTwin"] = np.ascontiguousarray(kpad[:, :, r * TOK:r * TOK + NWIN * 128])
            vfull = np.concatenate([g["v"][10:12] for g in grp], axis=2)
            vpad = np.zeros((2, 128, 4 * NT + 2, 128), vfull.dtype)
            vpad[:, :, 1:1 + 4 * NT] = vfull
            m["vwin"] = np.ascontiguousarray(vpad[:, :, r * NT:r * NT + NWIN])
            m["c_masks"] = band_masks(r)
            m["x_own"] = x_cur[c]
            if need_ctx:
                m["xc"] = xc_cur[c]
            in_maps.append(m)
        pb = _prog(("B", l), lambda: build_B(l, need_ctx))
        rb = run_bass_kernel_spmd(pb.nc, in_maps, core_ids=cores).results
        x_cur = [np.ascontiguousarray(rb[c]["x_next"]) for c in cores]
        if need_ctx:
            xc_cur = [np.ascontiguousarray(rb[c]["xc_next"]) for c in cores]
    out = np.zeros((BATCH, SEQ, D), np.float32)
    for c in cores:
        out[c // 4, (c % 4) * TOK:(c % 4 + 1) * TOK] = x_cur[c]
    return out
```

```python
import numpy as np
from contextlib import ExitStack
import concourse.bass as bass
import concourse.mybir as mybir
from concourse.bass_utils import run_bass_kernel_spmd

F32 = mybir.dt.float32
BF16 = mybir.dt.bfloat16
AF = mybir.ActivationFunctionType
ALU = mybir.AluOpType
AX = mybir.AxisListType

D = 1024
BATCH = 2
SEQ = 16384
DEPTH = 2
CTX = 256
NCORES = 8
TOK = SEQ // 4
NT = TOK // 128
NE = 16
DE = 256
EPS = 1e-6
ALPHA = (2 * DEPTH) ** 0.25
IN_COLS = 2144


class Buf:
    __slots__ = ("name", "t", "w", "r")

    _uid = [0]

    def __init__(self, name, t):
        Buf._uid[0] += 1
        self.name = "%s.%d" % (name, Buf._uid[0])
        self.t = t
        self.w = {}
        self.r = {}

    def __getitem__(self, idx):
        return self.t[idx]


class Rec:
    ENGS = ("pe", "act", "dve", "pool", "sp")

    def __init__(self, nc, stack):
        self.nc = nc
        self.stack = stack
        self.ops = {e: [] for e in self.ENGS}
        self.sems = {}
        self.cnt = {}
        self.seen = {e: {} for e in self.ENGS}
        self.nbuf = 0
        for e in self.ENGS:
            self._sem("E_" + e)

    def _sem(self, key):
        if key not in self.sems:
            if key.startswith("D_H_") and getattr(self, "free", None):
                sem, c0 = self.free.pop()
                self.sems[key] = sem
                self.cnt[key] = c0
            else:
                self.nsem = getattr(self, "nsem", 0) + 1
                self.sems[key] = self.stack.enter_context(self.nc.semaphore("s%d" % self.nsem))
                self.cnt[key] = 0
        return self.sems[key]

    def release_dma_sems(self):
        if not hasattr(self, "free"):
            self.free = []
        for key in [k for k in self.sems if k.startswith("D_")]:
            sem, c = self.sems.pop(key), self.cnt.pop(key)
            if key.startswith("D_H_"):
                self.free.append((sem, c))
            for e in self.ENGS:
                self.seen[e].pop(key, None)

    def collective_piece(self, fn, out_buf):
        key = "E_cc"
        self._sem(key)
        self.cnt[key] += 1
        self.ops["pool"].append(([], fn, self.sems[key], 1))
        out_buf.w = {key: (self.cnt[key], "cc")}
        out_buf.r = {}

    def collective(self, fn):
        self.barrier()
        key = "E_cc"
        self._sem(key)
        self.cnt[key] += 1
        self.ops["pool"].append(([], fn, self.sems[key], 1))
        self.barrier()

    def buf(self, name, t):
        self.nbuf += 1
        return Buf("%s#%d" % (name, self.nbuf), t)

    def sb(self, name, shape, dtype, stack=None):
        st = stack if stack is not None else self.stack
        self.nbuf += 1
        t = st.enter_context(self.nc.sbuf_tensor("%s_%d" % (name, self.nbuf), list(shape), dtype))
        return Buf(name, t)

    def ps(self, name, shape, dtype, stack=None):
        st = stack if stack is not None else self.stack
        self.nbuf += 1
        t = st.enter_context(self.nc.psum_tensor("%s_%d" % (name, self.nbuf), list(shape), dtype))
        return Buf(name, t)

    def _collect(self, eng, reads, writes, is_dma):
        need = {}

        def add(d):
            for k, (v, pe) in d.items():
                if k not in self.sems:
                    continue
                if (not is_dma) and eng == "pe" and pe == "pe" and k == "E_pe":
                    continue
                if need.get(k, 0) < v:
                    need[k] = v
        for b in reads:
            add(b.w)
        for b in writes:
            add(b.w)
            add(b.r)
        waits = []
        seen = self.seen[eng]
        for k, v in need.items():
            if seen.get(k, 0) < v:
                seen[k] = v
                waits.append((self.sems[k], v))
        return waits

    def op(self, eng, fn, reads=(), writes=()):
        waits = self._collect(eng, reads, writes, False)
        key = "E_" + eng
        self.cnt[key] += 1
        tk = (self.cnt[key], eng)
        self.ops[eng].append((waits, fn, self.sems[key], 1))
        for b in reads:
            b.r[key] = tk
        for b in writes:
            b.w = {key: tk}
            b.r = {}

    def dma(self, eng, out_ap, in_ap, reads=(), writes=(), key=None, cast=False):
        if key is None:
            b0 = (list(writes) + list(reads))[0]
            key = b0.name + ("_w" if writes else "_r")
        key = ("D_P_" if eng == "pool" else "D_H_") + key
        self._sem(key)
        waits = self._collect(eng, reads, writes, True)
        self.cnt[key] += 16
        tk = (self.cnt[key], "dma")
        if cast:
            self.ops[eng].append((waits, lambda e, o=out_ap, i=in_ap: e.dma_start(out=o, in_=i, max_dma_last_dim=4096), self.sems[key], 16))
        else:
            self.ops[eng].append((waits, lambda e, o=out_ap, i=in_ap: e.dma_start(out=o, in_=i), self.sems[key], 16))
        for b in reads:
            b.r[key] = tk
        for b in writes:
            b.w = {key: tk}
            b.r = {}

    def barrier(self):
        for e in self.ENGS:
            waits = []
            for k, v in self.cnt.items():
                if v > 0 and self.seen[e].get(k, 0) < v:
                    self.seen[e][k] = v
                    waits.append((self.sems[k], v))
            if waits:
                self.ops[e].append((waits, None, None, 0))

    def wait_all_on(self, eng):
        waits = []
        for k, v in self.cnt.items():
            if v > 0 and self.seen[eng].get(k, 0) < v:
                self.seen[eng][k] = v
                waits.append((self.sems[k], v))
        if waits:
            self.ops[eng].append((waits, None, None, 0))

    def emit(self):
        nc = self.nc
        block = self.stack.enter_context(nc.Block())

        def run(e, ops):
            for waits, fn, sem, inc in ops:
                for s, v in waits:
                    e.wait_ge(s, v)
                if fn is not None:
                    fn(e).then_inc(sem, inc)

        @block.tensor
        def _(e):
            run(e, self.ops["pe"])

        @block.scalar
        def _(e):
            run(e, self.ops["act"])

        @block.vector
        def _(e):
            run(e, self.ops["dve"])

        @block.gpsimd
        def _(e):
            run(e, self.ops["pool"])

        @block.sync
        def _(e):
            run(e, self.ops["sp"])


def _bc(ap, shape):
    return ap.to_broadcast(list(shape))


class Ctx:
    def __init__(self):
        self.nc = bass.Bass("TRN2", target_bir_lowering=False)
        self.stack = ExitStack()
        self.rec = Rec(self.nc, self.stack)
        self.dram = {}

    sfx = ""
    shared = ("c_ident", "cT", "rope_tab", "c_masks", "router_w", "router_bias", "x_own", "xc", "onehot")

    def din(self, name, shape, dtype=F32):
        if name not in self.shared:
            name = name + self.sfx
        if name in self.dram:
            return self.dram[name]
        t = self.nc.dram_tensor(name, list(shape), dtype, kind="ExternalInput")
        b = Buf(name, t.ap())
        self.dram[name] = b
        return b

    def dout(self, name, shape, dtype=F32):
        t = self.nc.dram_tensor(name, list(shape), dtype, kind="ExternalOutput")
        b = Buf(name, t.ap())
        self.dram[name] = b
        return b

    def dint(self, name, shape, dtype=F32):
        t = self.nc.dram_tensor(name, list(shape), dtype)
        b = Buf(name, t.ap())
        self.dram[name] = b
        return b


def _in_perm():
    o = {}
    off = 0
    for name, n in (("Aq", 256), ("Ak", 256), ("Av", 256), ("Bq", 256), ("Bk", 128), ("Bv", 128),
                    ("Ccq", 192), ("Cckv", 128), ("Ckr", 32), ("Dq", 256), ("Dk", 128), ("Dv", 128)):
        o[name] = np.arange(off, off + n)
        off += n
    order = ["Aq", "Ak", "Bq", "Bk", "Dk", "Dq", "Ckr", "Av", "Bv", "Dv", "Ccq", "Cckv"]
    return np.concatenate([o[k] for k in order])


GOFF = (0, 512, 1024, 1312, 1824)
GW = (512, 512, 288, 512, 320)


def rope_tables(tok_idx):
    row = (tok_idx // 64).astype(np.float64)
    col = (tok_idx % 64).astype(np.float64)

    def tab(n):
        inv = 10000.0 ** (-np.arange(n, dtype=np.float32).astype(np.float64) / n)
        inv = (np.float32(10000.0) ** (-(np.arange(n, dtype=np.float32) / np.float32(n)))).astype(np.float32)
        ar = (row.astype(np.float32)[:, None] * inv).astype(np.float32)
        ac = (col.astype(np.float32)[:, None] * inv).astype(np.float32)
        cr, sr, cc, sc = np.cos(ar), np.sin(ar), np.cos(ac), np.sin(ac)
        cos = np.concatenate([cr, cr, cc, cc], 1)
        sin = np.concatenate([-sr, sr, -sc, sc], 1)
        return cos.astype(np.float32), sin.astype(np.float32)
    c32, s32 = tab(8)
    c64, s64 = tab(16)
    return np.ascontiguousarray(np.concatenate([c32, s32, c64, s64], 1), dtype=np.float32)


def setup_consts(cx, L):
    rec, nc = cx.rec, cx.nc
    L.ident_f = rec.sb("identf", [128, 128], F32)
    L.ident_b = rec.sb("identb", [128, 128], BF16)
    L.eps = rec.sb("eps", [128, 1], F32)
    L.ones64 = rec.sb("ones64", [64, 64], F32)
    idn = cx.din("c_ident", [128, 128], F32)
    rec.dma("sp", L.ident_f[:, :], idn[:, :], writes=[L.ident_f])
    rec.op("dve", lambda e: e.tensor_copy(out=L.ident_b[:, :], in_=L.ident_f[:, :]), reads=[L.ident_f], writes=[L.ident_b])
    rec.op("dve", lambda e: e.memset(L.eps[:, :], EPS), writes=[L.eps])
    rec.op("dve", lambda e: e.memset(L.ones64[:, :], 1.0 / 64.0), writes=[L.ones64])


class NS:
    pass


DBG = {"stop": 99, "att": 9, "kinds": "ABCD"}


class Ring:
    def __init__(self, bufs):
        self.bufs = bufs
        self.i = 0

    def next(self):
        b = self.bufs[self.i % len(self.bufs)]
        self.i += 1
        return b


def rope_ops(rec, eng, src_buf, src, nv, R, tab_buf, cos, sin, t1b, t2b):
    h = R // 4
    n = nv * R
    t1 = t1b[:, 0:n]
    t2 = t2b[:, 0:n]
    rec.op(eng, lambda e: e.tensor_tensor(out=t1.rearrange("p (v r) -> p v r", v=nv), in0=src.rearrange("p (v r) -> p v r", v=nv),
                                          in1=cos.unsqueeze(1).to_broadcast([128, nv, R]), op=ALU.mult),
           reads=[src_buf, tab_buf], writes=[t1b])
    s5 = src.rearrange("p (v a b h) -> p v a b h", v=nv, a=2, b=2, h=h)
    t5 = t2.rearrange("p (v a b h) -> p v a b h", v=nv, a=2, b=2, h=h)
    sn = sin.rearrange("p (a b h) -> p a b h", a=2, b=2, h=h)
    for b in (0, 1):
        rec.op(eng, lambda e, b=b: e.tensor_tensor(out=t5[:, :, :, b, :], in0=s5[:, :, :, 1 - b, :],
                                                   in1=sn[:, :, b, :].unsqueeze(1).to_broadcast([128, nv, 2, h]), op=ALU.mult),
               reads=[src_buf, tab_buf], writes=[t2b])
    rec.op(eng, lambda e: e.tensor_tensor(out=t1, in0=t1, in1=t2, op=ALU.add), reads=[t2b], writes=[t1b])
    return t1


def layer_setup(cx, L, st, pst=None):
    rec = cx.rec
    cT = cx.din("cT", [128, 8, 2])
    wada = cx.din("w_ada", [128, 8, 6144])
    badaT = cx.din("b_adaT", [128, 48])
    bada = cx.din("b_ada", [6144])
    if pst != "prealloc":
        L.modT = rec.sb("modT", [128, 48, 2], F32, pst)
        L.Gb = [[rec.sb("Gb", [128, 1024], F32, pst) for _ in range(2)] for _ in range(2)]
    sl0 = rec.sb("sl0", [128, 8, 2], F32, st)
    sl = rec.sb("sl", [128, 8, 2], F32, st)
    slrep = rec.sb("slrep", [128, 8, 2, 128], F32, st)
    bT = rec.sb("bT", [128, 48], F32, st)
    was = Ring([rec.sb("wa", [128, 8, 512], F32, st) for _ in range(2)])
    bbs = Ring([rec.sb("bb", [128, 512], F32, st) for _ in range(2)])
    psA = rec.ps("psA", [128, 512], F32, st)
    psB = Ring([rec.ps("psB", [128, 512], F32, st) for _ in range(2)])
    rec.op("dve", lambda e: e.memset(L.modT[:, :, :], 0.0), writes=[L.modT])
    rec.dma("sp", sl0[:, :, :], cT[:, :, :], writes=[sl0])
    rec.dma("sp", bT[:, :], badaT[:, :], writes=[bT])
    rec.op("act", lambda e: e.activation(out=sl[:, :, :], in_=sl0[:, :, :], func=AF.Silu), reads=[sl0], writes=[sl])
    rec.op("dve", lambda e: e.tensor_copy(out=slrep[:, :, :, :], in_=sl[:, :, :].unsqueeze(3).to_broadcast([128, 8, 2, 128])),
           reads=[sl], writes=[slrep])
    for gi in range(12):
        wa = was.next()
        rec.dma("sp", wa[:, :, :], wada[:, :, gi * 512:(gi + 1) * 512], writes=[wa])
        v = gi // 2
        if v in (2, 5):
            bb = bbs.next()
            rec.dma("pool", bb[:, :], bada[gi * 512:(gi + 1) * 512].partition_broadcast(128), writes=[bb])
            for cond in range(2):
                ps = psB.next()
                for kc in range(8):
                    rec.op("pe", lambda e, kc=kc, cond=cond, ps=ps, wa=wa: e.matmul(ps[:, :], lhsT=slrep[:, kc, cond, :], rhs=wa[:, kc, :],
                                                                                 start=(kc == 0), stop=(kc == 7)),
                           reads=[slrep, wa], writes=[ps])
                gb = L.Gb[0 if v == 2 else 1][cond]
                rec.op("dve", lambda e, ps=ps, gb=gb, bb=bb, gi=gi: e.tensor_tensor(out=gb[:, (gi % 2) * 512:(gi % 2 + 1) * 512], in0=ps[:, :],
                                                                                 in1=bb[:, :], op=ALU.add),
                       reads=[ps, bb], writes=[gb])
        else:
            for jj in range(4):
                for kc in range(8):
                    rec.op("pe", lambda e, kc=kc, jj=jj, wa=wa: e.matmul(psA[:, jj * 2:jj * 2 + 2], lhsT=wa[:, kc, jj * 128:(jj + 1) * 128],
                                                                       rhs=sl[:, kc, :], start=(kc == 0), stop=(kc == 7)),
                           reads=[sl, wa], writes=[psA])
            rec.op("dve", lambda e, gi=gi: e.tensor_tensor(out=L.modT[:, gi * 4:gi * 4 + 4, :],
                                                           in0=psA[:, 0:8].rearrange("p (j c) -> p j c", c=2),
                                                           in1=bT[:, gi * 4:gi * 4 + 4].unsqueeze(2).to_broadcast([128, 4, 2]), op=ALU.add),
                   reads=[psA, bT], writes=[L.modT])
    for lo in (8, 32):
        rec.op("dve", lambda e, lo=lo: e.tensor_scalar(out=L.modT[:, lo:lo + 8, :], in0=L.modT[:, lo:lo + 8, :], scalar1=1.0, scalar2=None,
                                                       op0=ALU.add), reads=[L.modT], writes=[L.modT])


def phase1(cx, L, st, x_src, xc_src, tab_src, outs, n_lat_tiles=NT, do_ctx=True):
    rec = cx.rec
    win_d = cx.din("w_in", [128, 8, IN_COLS])
    wuq_d = cx.din("w_uq", [128, 2, 384])
    wukv_d = cx.din("w_ukv", [128, 512])
    g1_d = cx.din("gain_g1", [512])
    gc_d = cx.din("gain_c", [320])
    win = rec.sb("win", [128, 8, IN_COLS], BF16, st)
    wuq = rec.sb("wuq", [128, 2, 384], BF16, st)
    wukv = rec.sb("wukv", [128, 512], BF16, st)
    for kc in range(8):
        rec.dma("pool", win[:, kc, :], win_d[:, kc, :], writes=[win], cast=True)
    rec.dma("pool", wuq[:, :, :], wuq_d[:, :, :], writes=[wuq], cast=True)
    rec.dma("pool", wukv[:, :], wukv_d[:, :], writes=[wukv], cast=True)
    G1t = rec.sb("G1t", [128, 512], F32, st)
    Gct = rec.sb("Gct", [128, 320], F32, st)
    rec.dma("sp", G1t[:, :], g1_d[0:512].partition_broadcast(128), writes=[G1t])
    rec.dma("sp", Gct[:, :], gc_d[0:320].partition_broadcast(128), writes=[Gct])
    xts = Ring([rec.sb("xt", [128, 1024], F32, st) for _ in range(2)])
    tabs = Ring([rec.sb("tab", [128, 192], F32, st) for _ in range(2)])
    st6 = rec.sb("st6", [128, 2, 6], F32, st)
    mv = rec.sb("mv", [128, 2], F32, st)
    sq1 = rec.sb("sq1", [128, 1], F32, st)
    rstd = rec.sb("rstd", [128, 1], F32, st)
    xn = rec.sb("xn", [128, 1024], BF16, st)
    hT = rec.sb("hT", [128, 8, 128], BF16, st)
    t1b = rec.sb("t1b", [128, 512], F32, st)
    t2b = rec.sb("t2b", [128, 512], F32, st)
    xg = rec.sb("xg", [128, 512], F32, st)
    sqt = rec.sb("sqt", [128, 512], F32, st)
    ss8 = rec.sb("ss8", [128, 8], F32, st)
    rs8 = rec.sb("rs8", [128, 8], F32, st)
    ssc = rec.sb("ssc", [128, 2], F32, st)
    rsc = rec.sb("rsc", [128, 2], F32, st)
    cn = rec.sb("cn", [128, 320], BF16, st)
    cnT = rec.sb("cnT", [128, 3, 128], BF16, st)
    qk = rec.sb("qk", [128, 2, 1280], BF16, st)
    vts = Ring([rec.sb("vt", [128, 12, 128], BF16, st) for _ in range(2)])
    qkT = Ring([rec.sb("qkT", [128, 2, 10, 512], BF16, st) for _ in range(2)])
    psT = rec.ps("psT", [128, 1024], BF16, st)
    psG = [rec.ps("psG", [128, 512], F32, st) for _ in range(5)]
    psU = [rec.ps("psU", [128, 512], F32, st) for _ in range(2)]
    for vt in vts.bufs:
        rec.op("pool", lambda e, vt=vt: e.memset(vt[:, :, 64:128], 1.0), writes=[vt])
    rec.op("pool", lambda e: e.memset(qk[:, :, :], 0.0), writes=[qk])

    def tile(src_ap, src_buf, tab_ap, cond, qkt, col, vdst_ap, vdst_buf):
        xt = xts.next()
        tab = tabs.next()
        vt = vts.next()
        rec.dma("sp", xt[:, :], src_ap, writes=[xt])
        rec.dma("sp", tab[:, :], tab_ap, writes=[tab])
        c32, s32, c64, s64 = tab[:, 0:32], tab[:, 32:64], tab[:, 64:128], tab[:, 128:192]
        for i in range(2):
            rec.op("dve", lambda e, i=i: e.bn_stats(out=st6[:, i, :], in_=xt[:, i * 512:(i + 1) * 512]), reads=[xt], writes=[st6])
        rec.op("dve", lambda e: e.bn_aggr(out=mv[:, :], in_=st6[:, :, :].rearrange("p a b -> p (a b)")), reads=[st6], writes=[mv])
        rec.op("act", lambda e: e.activation(out=sq1[:, :], in_=mv[:, 1:2], func=AF.Sqrt, bias=L.eps[:, 0:1], scale=1.0),
               reads=[mv, L.eps], writes=[sq1])
        rec.op("dve", lambda e: e.reciprocal(out=rstd[:, :], in_=sq1[:, :]), reads=[sq1], writes=[rstd])
        rec.op("dve", lambda e: e.tensor_scalar(out=xn[:, :], in0=xt[:, :], scalar1=mv[:, 0:1], scalar2=rstd[:, 0:1],
                                                op0=ALU.subtract, op1=ALU.mult), reads=[xt, mv, rstd], writes=[xn])
        if DBG["stop"] <= 1:
            return
        for kc in range(8):
            rec.op("pe", lambda e, kc=kc: e.transpose(out=psT[:, kc * 128:(kc + 1) * 128], in_=xn[:, kc * 128:(kc + 1) * 128], identity=L.ident_b[:, :]),
                   reads=[xn, L.ident_b], writes=[psT])
        for kc in range(8):
            rec.op("dve", lambda e, kc=kc: e.tensor_scalar(out=hT[:, kc, :], in0=psT[:, kc * 128:(kc + 1) * 128],
                                                           scalar1=L.modT[:, 8 + kc, cond:cond + 1], scalar2=L.modT[:, kc, cond:cond + 1],
                                                           op0=ALU.mult, op1=ALU.add), reads=[psT, L.modT], writes=[hT])
        if DBG["stop"] <= 2:
            return
        for g in range(5):
            for kc in range(8):
                rec.op("pe", lambda e, g=g, kc=kc: e.matmul(psG[g][:, 0:GW[g]], lhsT=hT[:, kc, :], rhs=win[:, kc, GOFF[g]:GOFF[g] + GW[g]],
                                                            start=(kc == 0), stop=(kc == 7)), reads=[hT, win], writes=[psG[g]])
        if DBG["stop"] <= 3:
            return
        r = rope_ops(rec, "dve", psG[0], psG[0][:, 0:512], 16, 32, tab, c32, s32, t1b, t2b)
        rec.op("act", lambda e, r=r: e.activation(out=qk[:, :, 0:256], in_=r.rearrange("p (a c) -> p a c", a=2), func=AF.Copy),
               reads=[t1b], writes=[qk])
        if DBG["stop"] <= 4:
            return
        rec.op("act", lambda e: e.activation(out=sqt[:, :], in_=psG[1][:, :], func=AF.Square), reads=[psG[1]], writes=[sqt])
        rec.op("dve", lambda e: e.tensor_reduce(out=ss8[:, :], in_=sqt[:, :].rearrange("p (v r) -> p v r", v=8), axis=AX.X, op=ALU.add),
               reads=[sqt], writes=[ss8])
        rec.op("act", lambda e: e.activation(out=ss8[:, :], in_=ss8[:, :], func=AF.Sqrt, bias=L.eps[:, 0:1], scale=1.0 / 64.0),
               reads=[ss8, L.eps], writes=[ss8])
        rec.op("dve", lambda e: e.reciprocal(out=rs8[:, :], in_=ss8[:, :]), reads=[ss8], writes=[rs8])
        rec.op("dve", lambda e: e.memset(rs8[:, 6:8], 1.0), writes=[rs8])
        rec.op("dve", lambda e: e.tensor_tensor(out=xg[:, :], in0=psG[1][:, :], in1=G1t[:, :], op=ALU.mult), reads=[psG[1], G1t], writes=[xg])
        r = rope_ops(rec, "dve", xg, xg[:, 0:512], 8, 64, tab, c64, s64, t1b, t2b)
        r3 = r.rearrange("p (v r) -> p v r", v=8)
        rec.op("dve", lambda e, r3=r3: e.tensor_tensor(out=qk[:, 0, 256:512].rearrange("p (v r) -> p v r", v=4), in0=r3[:, 0:4, :],
                                                in1=rs8[:, 0:4].unsqueeze(2).to_broadcast([128, 4, 64]), op=ALU.mult),
               reads=[t1b, rs8], writes=[qk])
        for (lo, dst0) in ((4, 256), (6, 1024)):
            rec.op("dve", lambda e, lo=lo, dst0=dst0, r3=r3: e.tensor_tensor(
                out=qk[:, 1, dst0:dst0 + 256].rearrange("p (g d r) -> p g d r", g=2, d=2),
                in0=r3[:, lo:lo + 2, :].unsqueeze(2).to_broadcast([128, 2, 2, 64]),
                in1=rs8[:, lo:lo + 2].unsqueeze(2).unsqueeze(3).to_broadcast([128, 2, 2, 64]), op=ALU.mult),
                reads=[t1b, rs8], writes=[qk])
        if DBG["stop"] <= 5:
            return
        r = rope_ops(rec, "dve", psG[2], psG[2][:, 0:256], 4, 64, tab, c64, s64, t1b, t2b)
        rec.op("act", lambda e, r=r: e.activation(out=qk[:, 0, 1024:1280], in_=r, func=AF.Copy), reads=[t1b], writes=[qk])
        r = rope_ops(rec, "dve", psG[2], psG[2][:, 256:288], 1, 32, tab, c32, s32, t1b, t2b)
        rec.op("dve", lambda e, r=r: e.tensor_copy(out=qk[:, 1, 512:1024].rearrange("p (h c) -> p h c", h=4)[:, :, 64:96],
                                              in_=r.unsqueeze(1).to_broadcast([128, 4, 32])), reads=[t1b], writes=[qk])
        rec.op("act", lambda e: e.activation(out=vt[:, 0:4, 0:64], in_=psG[3][:, 0:256].rearrange("p (h d) -> p h d", h=4), func=AF.Copy),
               reads=[psG[3]], writes=[vt])
        rec.op("act", lambda e: e.activation(out=vt[:, 4:6, 0:64], in_=psG[3][:, 256:384].rearrange("p (h d) -> p h d", h=2), func=AF.Copy),
               reads=[psG[3]], writes=[vt])
        rec.op("act", lambda e: e.activation(out=vt[:, 10:12, 0:64], in_=psG[3][:, 384:512].rearrange("p (h d) -> p h d", h=2), func=AF.Copy),
               reads=[psG[3]], writes=[vt])
        if DBG["stop"] <= 6:
            return
        rec.op("act", lambda e: e.activation(out=sqt[:, 0:320], in_=psG[4][:, 0:320], func=AF.Square), reads=[psG[4]], writes=[sqt])
        rec.op("dve", lambda e: e.tensor_reduce(out=ssc[:, 0:1], in_=sqt[:, 0:192], axis=AX.X, op=ALU.add), reads=[sqt], writes=[ssc])
        rec.op("dve", lambda e: e.tensor_reduce(out=ssc[:, 1:2], in_=sqt[:, 192:320], axis=AX.X, op=ALU.add), reads=[sqt], writes=[ssc])
        rec.op("act", lambda e: e.activation(out=ssc[:, 0:1], in_=ssc[:, 0:1], func=AF.Sqrt, bias=L.eps[:, 0:1], scale=1.0 / 192.0),
               reads=[ssc, L.eps], writes=[ssc])
        rec.op("act", lambda e: e.activation(out=ssc[:, 1:2], in_=ssc[:, 1:2], func=AF.Sqrt, bias=L.eps[:, 0:1], scale=1.0 / 128.0),
               reads=[ssc, L.eps], writes=[ssc])
        rec.op("dve", lambda e: e.reciprocal(out=rsc[:, :], in_=ssc[:, :]), reads=[ssc], writes=[rsc])
        for (lo, hi, j) in ((0, 192, 0), (192, 320, 1)):
            rec.op("dve", lambda e, lo=lo, hi=hi, j=j: e.scalar_tensor_tensor(out=cn[:, lo:hi], in0=psG[4][:, lo:hi], scalar=rsc[:, j:j + 1],
                                                                             in1=Gct[:, lo:hi], op0=ALU.mult, op1=ALU.mult),
                   reads=[psG[4], rsc, Gct], writes=[cn])
        if DBG["stop"] <= 6.2:
            return
        for j, (lo, hi) in enumerate(((0, 128), (128, 192), (192, 320))):
            rec.op("pe", lambda e, j=j, lo=lo, hi=hi: e.transpose(out=psT[0:hi - lo, j * 128:(j + 1) * 128], in_=cn[:, lo:hi], identity=L.ident_b[:, :]),
                   reads=[cn, L.ident_b], writes=[psT])
        rec.op("dve", lambda e: e.tensor_copy(out=cnT[:, 0, :], in_=psT[:, 0:128]), reads=[psT], writes=[cnT])
        rec.op("dve", lambda e: e.tensor_copy(out=cnT[0:64, 1, :], in_=psT[0:64, 128:256]), reads=[psT], writes=[cnT])
        rec.op("dve", lambda e: e.tensor_copy(out=cnT[:, 2, :], in_=psT[:, 256:384]), reads=[psT], writes=[cnT])
        if DBG["stop"] <= 6.4:
            return
        rec.op("pe", lambda e: e.matmul(psU[0][:, 0:384], lhsT=cnT[:, 0, :], rhs=wuq[:, 0, :], start=True, stop=False), reads=[cnT, wuq], writes=[psU[0]])
        rec.op("pe", lambda e: e.matmul(psU[0][:, 0:384], lhsT=cnT[0:64, 1, :], rhs=wuq[0:64, 1, :], start=False, stop=True), reads=[cnT, wuq], writes=[psU[0]])
        rec.op("pe", lambda e: e.matmul(psU[1][:, 0:512], lhsT=cnT[:, 2, :], rhs=wukv[:, :], start=True, stop=True), reads=[cnT, wukv], writes=[psU[1]])
        if DBG["stop"] <= 6.6:
            return
        qc = psU[0][:, 0:384].rearrange("p (h c) -> p h c", h=4)
        kvc = psU[1][:, 0:512].rearrange("p (h c) -> p h c", h=4)
        qdst = qk[:, 0, 512:1024].rearrange("p (h c) -> p h c", h=4)
        kdst = qk[:, 1, 512:1024].rearrange("p (h c) -> p h c", h=4)
        rec.op("act", lambda e: e.activation(out=qdst[:, :, 0:64], in_=qc[:, :, 0:64], func=AF.Copy), reads=[psU[0]], writes=[qk])
        rec.op("act", lambda e: e.activation(out=kdst[:, :, 0:64], in_=kvc[:, :, 0:64], func=AF.Copy), reads=[psU[1]], writes=[qk])
        rec.op("act", lambda e: e.activation(out=vt[:, 6:10, 0:64], in_=kvc[:, :, 64:128], func=AF.Copy), reads=[psU[1]], writes=[vt])
        if DBG["stop"] <= 6.8:
            return
        rec.op("act", lambda e: e.activation(out=xg[:, 0:128].rearrange("p (h c) -> p h c", h=4), in_=qc[:, :, 64:96], func=AF.Copy), reads=[psU[0]], writes=[xg])
        if DBG["stop"] <= 6.85:
            return
        r = rope_ops(rec, "dve", xg, xg[:, 0:128], 4, 32, tab, c32, s32, t1b, t2b)
        if DBG["stop"] <= 6.9:
            return
        rec.op("dve", lambda e, r=r: e.tensor_copy(out=qdst[:, :, 64:96], in_=r.rearrange("p (h c) -> p h c", h=4)), reads=[t1b], writes=[qk])
        if DBG["stop"] <= 7:
            return
        for a in range(2):
            for (c0, c1) in ((0, 8), (8, 10)):
                for c in range(c0, c1):
                    rec.op("pe", lambda e, a=a, c=c, c0=c0: e.transpose(out=psT[:, (c - c0) * 128:(c - c0 + 1) * 128], in_=qk[:, a, c * 128:(c + 1) * 128],
                                                                        identity=L.ident_b[:, :]), reads=[qk, L.ident_b], writes=[psT])
                eng = "act" if a == 0 else "dve"
                if eng == "act":
                    rec.op("act", lambda e, a=a, c0=c0, c1=c1: e.activation(out=qkt[:, a, c0:c1, col:col + 128],
                                                                          in_=psT[:, 0:(c1 - c0) * 128].rearrange("p (c t) -> p c t", t=128), func=AF.Copy),
                           reads=[psT], writes=[qkt])
                else:
                    rec.op("dve", lambda e, a=a, c0=c0, c1=c1: e.tensor_copy(out=qkt[:, a, c0:c1, col:col + 128],
                                                                           in_=psT[:, 0:(c1 - c0) * 128].rearrange("p (c t) -> p c t", t=128)),
                           reads=[psT], writes=[qkt])
        if DBG["stop"] <= 8:
            return
        rec.dma("sp", vdst_ap, vt[:, :, :], reads=[vt])

    if DBG["stop"] <= 0:
        return
    for blk in range(n_lat_tiles // 4):
        qkt = qkT.next()
        for j in range(4):
            t = blk * 4 + j
            tile(x_src[t * 128:(t + 1) * 128, :], x_src, tab_src[t * 128:(t + 1) * 128, :], 0, qkt, j * 128,
                 outs["v"][:, :, t, :].rearrange("s p d -> p s d"), outs["v"])
        rec.dma("sp", outs["qT"][:, :, blk * 512:(blk + 1) * 512].rearrange("c p t -> p c t"), qkt[:, 0, :, :], reads=[qkt])
        rec.dma("sp", outs["kT"][:, :, blk * 512:(blk + 1) * 512].rearrange("c p t -> p c t"), qkt[:, 1, :, :], reads=[qkt])
    if do_ctx:
        qkt = qkT.next()
        for t in range(2):
            tile(xc_src[t * 128:(t + 1) * 128, :], xc_src, tab_src[TOK + t * 128:TOK + (t + 1) * 128, :], 1, qkt, t * 128,
                 outs["vc"][:, :, t, :].rearrange("s p d -> p s d"), outs["vc"])
        rec.dma("sp", outs["qTc"][:, :, :].rearrange("c p t -> p c t"), qkt[:, 0, :, 0:256], reads=[qkt])
        rec.dma("sp", outs["kTc"][:, :, :].rearrange("c p t -> p c t"), qkt[:, 1, :, 0:256], reads=[qkt])


def _pm(w, kc):
    k, n = w.shape
    return np.ascontiguousarray(w.reshape(kc, 128, n).transpose(1, 0, 2))


def prep_layer(inp, l):
    f = np.float32
    d = {}
    d["w_ada"] = _pm(inp["w_ada"][l], 8)
    d["b_adaT"] = np.ascontiguousarray(inp["b_ada"][l].reshape(48, 128).T)
    d["b_ada"] = np.ascontiguousarray(inp["b_ada"][l])
    d["w_in"] = _pm(inp["w_in"][l][:, _in_perm()], 8)
    wuq = np.zeros((256, 384), f)
    wuq[:192] = inp["mla_w_uq"][l]
    d["w_uq"] = _pm(wuq, 2)
    d["w_ukv"] = np.ascontiguousarray(inp["mla_w_ukv"][l])
    d["gain_g1"] = np.concatenate([np.tile(inp["gqa_q_norm_g"][l], 4), np.tile(inp["gqa_k_norm_g"][l], 2), np.ones(128, f)]).astype(f)
    d["gain_c"] = np.concatenate([inp["mla_q_norm_g"][l], inp["mla_kv_norm_g"][l]]).astype(f)
    return d


def prep_core_common(inp, core):
    b = core // 4
    r = core % 4
    d = {}
    cc = np.stack([inp["c"][b], inp["c_ctx"]], 1)
    d["cT"] = _pm(cc, 8)
    tok = np.arange(r * TOK, (r + 1) * TOK)
    tab = rope_tables(tok)
    ctab = np.zeros((CTX, 192), np.float32)
    ctab[:, 0:32] = 1.0
    ctab[:, 64:128] = 1.0
    d["rope_tab"] = np.ascontiguousarray(np.concatenate([tab, ctab], 0))
    d["c_ident"] = np.eye(128, dtype=np.float32)
    return d


NKT = 2 + 4 * NT
NWIN = NT + 2


def attention(cx, L, st, l, src, need_ctx, n_qb=TOK // 512, kt_limit=None):
    rec = cx.rec
    lam_init = 0.8 - 0.6 * float(np.exp(-0.3 * l))
    lamv_d = cx.din("lamv", [4, 32])
    subg_d = cx.din("subln_g", [64, 1])
    sink_d = cx.din("swa_sink", [4])
    mask_d = cx.din("c_masks", [128, 4, 128])
    lam = rec.sb("lam", [128, 1], F32, st)
    gsub = rec.sb("gsub", [64, 1], F32, st)
    esink = rec.sb("esink", [128, 4], F32, st)
    masks = rec.sb("masks", [128, 4, 128], BF16, st)
    lv = rec.sb("lv", [128, 4, 32], F32, st)
    lp = rec.sb("lp", [128, 2, 32], F32, st)
    ls = rec.sb("ls", [128, 2], F32, st)
    kts = Ring([rec.sb("ktb", [128, NKT * 128], BF16, st) for _ in range(2)])
    vtsr = Ring([rec.sb("vtb", [128, NKT, 128], BF16, st) for _ in range(2)])
    qts = Ring([rec.sb("qtb", [128, TOK], BF16, st) for _ in range(2)])
    qtc = rec.sb("qtc", [128, 10, 256], BF16, st)
    pTs = Ring([rec.sb("pT", [128, 1024], BF16, st) for _ in range(4)])
    zss = Ring([rec.sb("zs", [64, 512], F32, st) for _ in range(2)])
    rzs = Ring([rec.sb("rz", [64, 512], F32, st) for _ in range(2)])
    fa = rec.sb("fa", [64, 512], F32, st)
    fb = rec.sb("fb", [64, 512], F32, st)
    fc = rec.sb("fc", [64, 512], F32, st)
    ots = Ring([rec.sb("ot", [64, 512], BF16, st) for _ in range(2)])
    qmask = [[Ring([rec.sb("qm", [128, 512], BF16, st) for _ in range(2)]) for _ in range(2)] for _ in range(2)]
    for hp in range(2):
        for cp in range(2):
            for b_ in qmask[hp][cp].bufs:
                rec.op("pool", lambda e, b_=b_: e.memset(b_[:, :], 0.0), writes=[b_])
    if "kTwin" not in src:
        oh_d = cx.din("onehot", [128, 8])
        onehot = rec.sb("onehot", [128, 8], F32, st)
        hck = rec.sb("hck", [128, 4, 128], BF16, st)
        hcv = rec.sb("hcv", [128, 4, 128], BF16, st)
        hacc = rec.sb("hacc", [128, 128], F32, st)
        rec.dma("sp", onehot[:, :], oh_d[:, :], writes=[onehot])
    Sr = Ring([rec.ps("S", [128, 1024], F32, st) for _ in range(3)])
    accs = Ring([rec.ps("acc", [128, 512], F32, st) for _ in range(2)])
    dummy = None
    ndummy = 0
    rec.dma("sp", lv[:, :, :].rearrange("p a b -> p (a b)"), lamv_d[:, :].rearrange("a b -> (a b)").partition_broadcast(128), writes=[lv])
    rec.dma("sp", gsub[:, :], subg_d[:, :], writes=[gsub])
    rec.dma("sp", esink[:, :], sink_d[0:4].partition_broadcast(128), writes=[esink])
    rec.dma("pool", masks[:, :, :], mask_d[:, :, :], writes=[masks], cast=True)
    rec.op("dve", lambda e: e.tensor_tensor(out=lp[:, :, :], in0=lv[:, 0:4:2, :], in1=lv[:, 1:4:2, :], op=ALU.mult), reads=[lv], writes=[lp])
    rec.op("dve", lambda e: e.tensor_reduce(out=ls[:, :], in_=lp[:, :, :], axis=AX.X, op=ALU.add), reads=[lp], writes=[ls])
    rec.op("act", lambda e: e.activation(out=ls[:, :], in_=ls[:, :], func=AF.Exp), reads=[ls], writes=[ls])
    rec.op("act", lambda e: e.activation(out=esink[:, :], in_=esink[:, :], func=AF.Exp), reads=[esink], writes=[esink])
    rec.op("dve", lambda e: e.tensor_tensor(out=lam[:, :], in0=ls[:, 0:1], in1=ls[:, 1:2], op=ALU.subtract), reads=[ls], writes=[lam])
    rec.op("dve", lambda e: e.tensor_scalar(out=lam[:, :], in0=lam[:, :], scalar1=lam_init, scalar2=None, op0=ALU.add), reads=[lam], writes=[lam])
    rec.op("dve", lambda e: e.tensor_scalar(out=gsub[:, :], in0=gsub[:, :], scalar1=1.0 - lam_init, scalar2=None, op0=ALU.mult), reads=[gsub], writes=[gsub])
    if need_ctx:
        rec.dma("sp", qtc[:, :, :], src["qTc"][:, :, :].rearrange("c p t -> p c t"), writes=[qtc])

    def mm(out_ap, lhsT, rhs, start, stop, base, reads, writes):
        kw = {}
        if base == 96:
            kw["tile_position"] = (96, 0)
        rec.op("pe", lambda e: e.matmul(out_ap, lhsT=lhsT, rhs=rhs, start=start, stop=stop, skip_group_check=True, **kw), reads=reads, writes=writes)

    def finalize(accl, W, kind, h, dst_ap, sink_h=None):
        rzl = []
        zsl = []
        osl = []
        for a in accl:
            zs = zss.next()
            if sink_h is None:
                rec.op("dve", lambda e, a=a, zs=zs: e.tensor_scalar(out=zs[:, 0:W], in0=a[64:128, 0:W], scalar1=1.0, scalar2=None, op0=ALU.mult),
                       reads=[a], writes=[zs])
            else:
                rec.op("dve", lambda e, a=a, zs=zs: e.tensor_scalar(out=zs[:, 0:W], in0=a[64:128, 0:W], scalar1=esink[0:64, sink_h:sink_h + 1],
                                                                  scalar2=None, op0=ALU.add), reads=[a, esink], writes=[zs])
            zsl.append(zs)
            if kind == "A":
                ob = (fa, fb)[len(osl)]
                rec.op("dve", lambda e, a=a, ob=ob: e.tensor_scalar(out=ob[:, 0:W], in0=a[0:64, 0:W], scalar1=1.0, scalar2=None, op0=ALU.mult),
                       reads=[a], writes=[ob])
                osl.append(ob)
        for zs in zsl:
            rz = rzs.next()
            rec.op("dve", lambda e, zs=zs, rz=rz: e.reciprocal(out=rz[:, 0:W], in_=zs[:, 0:W]), reads=[zs], writes=[rz])
            rzl.append(rz)
        if kind == "A":
            accl = osl
        ot = ots.next()
        if kind != "A":
            a, rz = accl[0], rzl[0]
            rec.op("dve", lambda e: e.tensor_tensor(out=ot[:, 0:W], in0=a[0:64, 0:W], in1=rz[:, 0:W], op=ALU.mult), reads=[a, rz], writes=[ot])
        else:
            a1, a2 = accl
            r1, r2 = rzl
            rec.op("dve", lambda e: e.tensor_tensor(out=fa[:, 0:W], in0=fa[:, 0:W], in1=r1[:, 0:W], op=ALU.mult), reads=[r1], writes=[fa])
            rec.op("dve", lambda e: e.scalar_tensor_tensor(out=fb[:, 0:W], in0=fb[:, 0:W], scalar=lam[0:64, 0:1], in1=r2[:, 0:W],
                                                           op0=ALU.mult, op1=ALU.mult), reads=[r2, lam], writes=[fb])
            rec.op("dve", lambda e: e.tensor_tensor(out=fa[:, 0:W], in0=fa[:, 0:W], in1=fb[:, 0:W], op=ALU.subtract), reads=[fb], writes=[fa])
            rec.op("pool", lambda e: e.tensor_tensor(out=fc[:, 0:W], in0=fa[:, 0:W], in1=fa[:, 0:W], op=ALU.mult), reads=[fa], writes=[fc])
            pm = Sr.next()
            rec.op("pe", lambda e: e.matmul(pm[0:64, 0:W], lhsT=L.ones64[:, :], rhs=fc[:, 0:W], start=True, stop=True), reads=[fc, L.ones64], writes=[pm])
            rec.op("act", lambda e: e.activation(out=fb[:, 0:W], in_=pm[0:64, 0:W], func=AF.Sqrt, bias=L.eps[0:64, 0:1], scale=1.0), reads=[pm, L.eps], writes=[fb])
            rec.op("dve", lambda e: e.reciprocal(out=fc[:, 0:W], in_=fb[:, 0:W]), reads=[fb], writes=[fc])
            rec.op("dve", lambda e: e.scalar_tensor_tensor(out=ot[:, 0:W], in0=fa[:, 0:W], scalar=gsub[:, 0:1], in1=fc[:, 0:W],
                                                           op0=ALU.mult, op1=ALU.mult), reads=[fa, fc, gsub], writes=[ot])
        rec.dma("sp", dst_ap, ot[:, 0:W], reads=[ot])

    def attend(ktb, vtb, q_ap_fn, qbuf, comps, scale, ktiles, W, kind, h, dst_ap, sink_h=None):
        nu = len(comps)
        accl = [accs.next() for _ in range(nu)]
        if nu == 2:
            groups = [[(kt, 0), (kt, 1)] for kt in ktiles]
        else:
            groups = [[(kt, 0) for kt in ktiles[i:i + 2]] for i in range(0, len(ktiles), 2)]
        started = [False] * nu

        def qk(grp):
            S = Sr.next()
            for j, (kt, u) in enumerate(grp):
                base, K = comps[u]
                qap, qb_ = q_ap_fn(u, base, K)
                mm(S[:, j * W:(j + 1) * W], ktb[base:base + K, kt * 128:(kt + 1) * 128], qap, True, True, base, [ktb, qb_], [S])
            return S
        PD = DBG.get("pd", 2)
        Sq = [qk(groups[i]) for i in range(min(PD, len(groups)))]
        for gi, grp in enumerate(groups):
            if gi + PD < len(groups):
                Sq.append(qk(groups[gi + PD]))
            S = Sq.pop(0)
            P = pTs.next()
            n = len(grp) * W
            if DBG["att"] <= 1:
                continue
            rec.op("act", lambda e, S=S, P=P, n=n: e.activation(out=P[:, 0:n], in_=S[:, 0:n], func=AF.Exp, scale=scale), reads=[S], writes=[P])
            if DBG["att"] <= 2:
                continue
            for _ in range(ndummy if W == 512 else 0):
                rec.op("pe", lambda e: e.matmul(dummy[:, 0:128 * DBG.get("dumw", 2)], lhsT=L.ident_b[:, :], rhs=masks[:, 0:DBG.get("dumw", 2), :].rearrange("p a b -> p (a b)"), start=True, stop=True,
                                                skip_group_check=True), reads=[], writes=[])
            for j, (kt, u) in enumerate(grp):
                a = accl[u]
                last = (gi == len(groups) - 1) and (nu == 2 or j == len(grp) - 1)
                mm(a[:, 0:W], vtb[:, kt, :], P[:, j * W:(j + 1) * W], not started[u], last, 0, [vtb, P], [a])
                started[u] = True
        if DBG["att"] >= 4:
            finalize(accl, W, kind, h, dst_ap, sink_h)

    def kall(r, c):
        if "ga" in src:
            b_ = src["ga"][c]
            return b_[r * 128:(r + 1) * 128, :], [b_]
        return src["kTall"][r, c, :, :], []

    def vall(r, slot):
        if "ga" in src:
            b_ = src["ga"][10 + slot]
            return b_[r * 128:(r + 1) * 128, :].rearrange("p (t d) -> p t d", d=128), [b_]
        return src["vall"][r, slot, :, :, :], []

    def load_kv(c, slot):
        ktb = kts.next()
        vtb = vtsr.next()
        rec.dma("sp", ktb[:, 0:256], src["kTc"][c, :, :], writes=[ktb])
        rec.dma("sp", vtb[:, 0:2, :], src["vc"][slot, :, :, :], writes=[vtb])
        for r in range(4):
            ap_, rd = kall(r, c)
            rec.dma("sp", ktb[:, 256 + r * TOK:256 + (r + 1) * TOK], ap_, reads=rd, writes=[ktb])
            ap_, rd = vall(r, slot)
            rec.dma("sp", vtb[:, 2 + r * NT:2 + (r + 1) * NT, :], ap_, reads=rd, writes=[vtb])
        return ktb, vtb

    def load_v(slot):
        vtb = vtsr.next()
        rec.dma("sp", vtb[:, 0:2, :], src["vc"][slot, :, :, :], writes=[vtb])
        for r in range(4):
            ap_, rd = vall(r, slot)
            rec.dma("sp", vtb[:, 2 + r * NT:2 + (r + 1) * NT, :], ap_, reads=rd, writes=[vtb])
        return vtb

    ktiles_all = list(range(NKT)) if kt_limit is None else list(range(kt_limit))
    jobs = []
    for i in range(2):
        jobs.append((i, [(h, [((h % 2) * 64, 32), ((h % 2) * 64 + 32, 32)], h, h // 2, (h % 2) * 64) for h in (2 * i, 2 * i + 1)], 32 ** -0.5, "A"))
    for g in range(2):
        jobs.append((2 + g, [(h, [((h % 2) * 64, 64)], 4 + g, 2 + h // 2, (h % 2) * 64) for h in (2 * g, 2 * g + 1)], 64 ** -0.5, "B"))
    for h in range(4):
        jobs.append((4 + h, [(h, [(0, 96)], 6 + h, 4 + h // 2, (h % 2) * 64)], 96 ** -0.5, "C"))
    if DBG["att"] <= 0:
        return
    hjobs = []
    for (c, heads, scale, kind) in jobs:
        if kind not in DBG["kinds"]:
            continue
        prev_slot = None
        for hi, (h, comps, slot, oc, orow) in enumerate(heads):
            hjobs.append(dict(c=c, h=h, comps=comps, slot=slot, oc=oc, orow=orow, scale=scale, kind=kind,
                              ldk=(hi == 0), ldv=(slot != prev_slot)))
            prev_slot = slot
    state = {"ktb": None, "vtb": None, "qtb": None}

    def issue_loads(j):
        if j["ldk"]:
            qtb = qts.next()
            rec.dma("sp", qtb[:, :], src["qT"][j["c"], :, :], writes=[qtb])
            ktb = kts.next()
            rec.dma("sp", ktb[:, 0:256], src["kTc"][j["c"], :, :], writes=[ktb])
            for r in range(4):
                ap_, rd = kall(r, j["c"])
                rec.dma("sp", ktb[:, 256 + r * TOK:256 + (r + 1) * TOK], ap_, reads=rd, writes=[ktb])
            j["ktb"], j["qtb"] = ktb, qtb
        if j["ldv"]:
            j["vtb"] = load_v(j["slot"])

    if hjobs:
        issue_loads(hjobs[0])
    for ji, j in enumerate(hjobs):
        for k_ in ("ktb", "vtb", "qtb"):
            if k_ in j:
                state[k_] = j[k_]
        ktb, vtb, qtb = state["ktb"], state["vtb"], state["qtb"]
        if ji + 1 < len(hjobs):
            issue_loads(hjobs[ji + 1])
        c, h, comps, oc, orow, scale, kind = j["c"], j["h"], j["comps"], j["oc"], j["orow"], j["scale"], j["kind"]

        def masked_q(src_fn, src_buf, W):
            bl = []
            for cp in range(2):
                mb = qmask[h % 2][cp].next()
                rows = (h % 2) * 64 + cp * 32
                rec.op("pool", lambda e, mb=mb, rows=rows: e.tensor_copy(out=mb[rows:rows + 32, 0:W], in_=src_fn(rows)), reads=[src_buf], writes=[mb])
                bl.append(mb)
            return bl
        for qb in range(n_qb):
            if kind == "A":
                bl = masked_q(lambda rows, qb=qb, qtb=qtb: qtb[rows:rows + 32, qb * 512:(qb + 1) * 512], qtb, 512)
                attend(ktb, vtb, lambda u, base, K, bl=bl: (bl[u][:, 0:512], bl[u]), None, [(0, 128), (0, 128)], scale, ktiles_all, 512,
                       kind, h, src["OT"][oc, orow:orow + 64, qb * 512:(qb + 1) * 512])
            elif kind == "B":
                mb = qmask[h % 2][0].next()
                r0 = (h % 2) * 64
                rec.op("pool", lambda e, mb=mb, r0=r0, qb=qb, qtb=qtb: e.tensor_copy(out=mb[r0:r0 + 64, 0:512], in_=qtb[r0:r0 + 64, qb * 512:(qb + 1) * 512]),
                       reads=[qtb], writes=[mb])
                attend(ktb, vtb, lambda u, base, K, mb=mb: (mb[:, 0:512], mb), None, [(0, 128)], scale, ktiles_all, 512,
                       kind, h, src["OT"][oc, orow:orow + 64, qb * 512:(qb + 1) * 512])
            else:
                attend(ktb, vtb, lambda u, base, K, qb=qb, qtb=qtb: (qtb[0:128, qb * 512:(qb + 1) * 512], qtb), None, [(0, 128)], scale, ktiles_all, 512,
                       kind, h, src["OT"][oc, orow:orow + 64, qb * 512:(qb + 1) * 512])
        if need_ctx:
            if kind == "A":
                bl = masked_q(lambda rows, c=c: qtc[rows:rows + 32, c, :], qtc, 256)
                attend(ktb, vtb, lambda u, base, K, bl=bl: (bl[u][:, 0:256], bl[u]), None, [(0, 128), (0, 128)], scale, [0, 1], 256, kind, h,
                       src["OTc"][oc, orow:orow + 64, :])
            elif kind == "B":
                mb = qmask[h % 2][0].next()
                r0 = (h % 2) * 64
                rec.op("pool", lambda e, mb=mb, r0=r0, c=c: e.tensor_copy(out=mb[r0:r0 + 64, 0:256], in_=qtc[r0:r0 + 64, c, :]), reads=[qtc], writes=[mb])
                attend(ktb, vtb, lambda u, base, K, mb=mb: (mb[:, 0:256], mb), None, [(0, 128)], scale, [0, 1], 256, kind, h,
                       src["OTc"][oc, orow:orow + 64, :])
            else:
                attend(ktb, vtb, lambda u, base, K, c=c: (qtc[0:128, c, :], qtc), None, [(0, 128)], scale, [0, 1], 256, kind, h,
                       src["OTc"][oc, orow:orow + 64, :])
    for g in range(2 if "D" in DBG["kinds"] else 0):
        c = 8 + g
        slot = 10 + g
        qtb = qts.next()
        rec.dma("sp", qtb[:, :], src["qT"][c, :, :], writes=[qtb])
        ktb = kts.next()
        vtb = vtsr.next()
        rec.dma("sp", ktb[:, 0:256], src["kTc"][c, :, :], writes=[ktb])
        rec.dma("sp", vtb[:, 0:2, :], src["vc"][slot, :, :, :], writes=[vtb])
        if "kTwin" in src:
            rec.dma("sp", ktb[:, 256:256 + NWIN * 128], src["kTwin"][g, :, :], writes=[ktb])
            rec.dma("sp", vtb[:, 2:2 + NWIN, :], src["vwin"][g, :, :, :], writes=[vtb])
        else:
            rec.dma("sp", ktb[:, 384:384 + TOK], src["kTown"][c, :, :], writes=[ktb])
            rec.dma("sp", vtb[:, 3:3 + NT, :], src["vown"][slot, :, :, :], writes=[vtb])
            for side in range(2):
                kcol = (TOK - 128) if side == 0 else 0
                vt_i = (NT - 1) if side == 0 else 0
                for r_ in range(4):
                    ap_, rd = kall(r_, c)
                    rec.dma("sp", hck[:, r_, :], ap_[:, kcol:kcol + 128], reads=rd, writes=[hck])
                    ap_, rd = vall(r_, slot)
                    rec.dma("sp", hcv[:, r_, :], ap_[:, vt_i, :], reads=rd, writes=[hcv])
                kd = ktb[:, 256:384] if side == 0 else ktb[:, 384 + TOK:384 + TOK + 128]
                vd = vtb[:, 2, :] if side == 0 else vtb[:, 3 + NT, :]
                for (cand, cb, dst_ap, dst_b) in ((hck, hck, kd, ktb), (hcv, hcv, vd, vtb)):
                    rec.op("dve", lambda e, cand=cand, side=side: e.tensor_scalar(out=hacc[:, :], in0=cand[:, 0, :], scalar1=onehot[:, side * 4:side * 4 + 1],
                                                                               scalar2=None, op0=ALU.mult), reads=[cb, onehot], writes=[hacc])
                    for r_ in range(1, 4):
                        last = r_ == 3
                        rec.op("dve", lambda e, cand=cand, side=side, r_=r_, last=last, dst_ap=dst_ap: e.scalar_tensor_tensor(
                            out=dst_ap if last else hacc[:, :], in0=cand[:, r_, :], scalar=onehot[:, side * 4 + r_:side * 4 + r_ + 1], in1=hacc[:, :],
                            op0=ALU.mult, op1=ALU.add), reads=[cb, onehot, hacc], writes=[dst_b] if last else [hacc])
        for h in (2 * g, 2 * g + 1):
            base = (h % 2) * 64
            def d_qk(j):
                tiles = [0, 1, 2 + j, 3 + j, 4 + j]
                S = Sr.next()
                for i, kt in enumerate(tiles):
                    mm(S[:, i * 128:(i + 1) * 128], ktb[base:base + 64, kt * 128:(kt + 1) * 128], qtb[base:base + 64, j * 128:(j + 1) * 128],
                       True, True, base, [ktb, qtb], [S])
                return S
            for qb in range(n_qb):
                acc = accs.next()
                S_next = d_qk(qb * 4)
                for s in range(4):
                    j = qb * 4 + s
                    tiles = [0, 1, 2 + j, 3 + j, 4 + j]
                    S = S_next
                    if s + 1 < 4:
                        S_next = d_qk(j + 1)
                    P = pTs.next()
                    rec.op("act", lambda e, S=S, P=P: e.activation(out=P[:, 0:640], in_=S[:, 0:640], func=AF.Exp, scale=64 ** -0.5), reads=[S], writes=[P])
                    mp = 2 if j == 0 else 0
                    mn = 3 if j == NT - 1 else 1
                    rec.op("pool", lambda e, P=P, mp=mp: e.tensor_tensor(out=P[:, 256:384], in0=P[:, 256:384], in1=masks[:, mp, :], op=ALU.mult),
                           reads=[masks], writes=[P])
                    rec.op("pool", lambda e, P=P, mn=mn: e.tensor_tensor(out=P[:, 512:640], in0=P[:, 512:640], in1=masks[:, mn, :], op=ALU.mult),
                           reads=[masks], writes=[P])
                    for i, kt in enumerate(tiles):
                        mm(acc[:, s * 128:(s + 1) * 128], vtb[:, kt, :], P[:, i * 128:(i + 1) * 128], i == 0, i == 4, 0, [vtb, P], [acc])
                finalize([acc], 512, "D", h, src["OT"][6 + h // 2, base:base + 64, qb * 512:(qb + 1) * 512], sink_h=h)
            if need_ctx:
                attend(ktb, vtb, lambda u, b_, K, c=c: (qtc[b_:b_ + K, c, :], qtc), None, [(base, 64)], 64 ** -0.5, [0, 1], 256, "D", h,
                       src["OTc"][6 + h // 2, base:base + 64, :], sink_h=h)


def phase3(cx, L, st, x_srcs, wts):
    rec = cx.rec
    wout = rec.sb("wout", [128, 8, 1024], BF16, st)
    rw = rec.sb("rw", [128, 8, 16], F32, st)
    rbias = rec.sb("rbias", [128, 16], F32, st)
    lnt = [rec.sb("lnt", [128, 1024], F32, st) for _ in range(4)]
    pre = wts.get("bf16", False)
    wq = "sp" if pre else "pool"
    for kc in range(8):
        rec.dma(wq, wout[:, kc, :], wts["w_out"][:, kc, :], writes=[wout], cast=not pre)
    rec.dma("sp", rw[:, :, :], wts["router_w"][:, :, :], writes=[rw])
    rec.dma("sp", rbias[:, :], wts["router_bias"][0:16].partition_broadcast(128), writes=[rbias])
    for i in range(4):
        rec.dma("sp", lnt[i][:, :], wts["ln"][i, :].partition_broadcast(128), writes=[lnt[i]])
    x1s = [rec.sb("x1s", [128, 1024], F32, st) for _ in range(4)]
    xts = Ring([rec.sb("xt3", [128, 1024], F32, st) for _ in range(2)])
    u = rec.sb("u", [128, 1024], F32, st)
    tmp = rec.sb("tmp", [128, 1024], F32, st)
    h2Tf = rec.sb("h2Tf", [128, 8, 128], F32, st)
    h2T = rec.sb("h2T", [128, 8, 512], BF16, st)
    otin = rec.sb("otin", [128, 8, 512], BF16, st)
    actT = rec.sb("actT", [128, 16, 2, 512], BF16, st)
    wgus = Ring([rec.sb("wgu", [128, 8, 512], BF16, st) for _ in range(3)])
    wds = Ring([rec.sb("wd", [128, 2, 1024], BF16, st) for _ in range(4)])
    gates = rec.sb("gates", [128, 4, 16], F32, st)
    st6 = rec.sb("st6b", [128, 2, 6], F32, st)
    mv = rec.sb("mvb", [128, 2], F32, st)
    sq1 = rec.sb("sq1b", [128, 1], F32, st)
    rstd = rec.sb("rstdb", [128, 1], F32, st)
    s16 = rec.sb("s16", [128, 16], F32, st)
    sel = rec.sb("sel", [128, 16], F32, st)
    sel2 = rec.sb("sel2", [128, 16], F32, st)
    eq = rec.sb("eq", [128, 16], F32, st)
    m1 = rec.sb("m1", [128, 4], F32, st)
    m2 = rec.sb("m2", [128, 4], F32, st)
    gs = rec.sb("gs", [128, 4], F32, st)
    gm = rec.sb("gm", [128, 1], F32, st)
    sil = Ring([rec.sb("sil", [128, 256], F32, st) for _ in range(2)])
    actb = Ring([rec.sb("actb", [128, 256], BF16, st) for _ in range(2)])
    B = [rec.ps("B", [128, 512], F32, st) for _ in range(8)]
    psTb = rec.buf("psTb", B[7].t[:, :].bitcast(BF16))

    def ln_stats(src):
        for i in range(2):
            rec.op("dve", lambda e, i=i: e.bn_stats(out=st6[:, i, :], in_=src[:, i * 512:(i + 1) * 512]), reads=[src], writes=[st6])
        rec.op("dve", lambda e: e.bn_aggr(out=mv[:, :], in_=st6[:, :, :].rearrange("p a b -> p (a b)")), reads=[st6], writes=[mv])
        rec.op("act", lambda e: e.activation(out=sq1[:, :], in_=mv[:, 1:2], func=AF.Sqrt, bias=L.eps[:, 0:1], scale=1.0), reads=[mv, L.eps], writes=[sq1])
        rec.op("dve", lambda e: e.reciprocal(out=rstd[:, :], in_=sq1[:, :]), reads=[sq1], writes=[rstd])

    def gated_ln(x_in, ybanks, Gt, gt, bt, dst):
        for hh in range(2):
            rec.op("dve", lambda e, hh=hh: e.tensor_tensor(out=tmp[:, hh * 512:(hh + 1) * 512], in0=ybanks[hh][:, :], in1=Gt[:, hh * 512:(hh + 1) * 512],
                                                           op=ALU.mult), reads=[ybanks[hh], Gt], writes=[tmp])
        rec.op("dve", lambda e: e.scalar_tensor_tensor(out=u[:, :], in0=x_in[:, :], scalar=ALPHA, in1=tmp[:, :], op0=ALU.mult, op1=ALU.add),
               reads=[x_in, tmp], writes=[u])
        ln_stats(u)
        rec.op("dve", lambda e: e.tensor_scalar(out=u[:, :], in0=u[:, :], scalar1=mv[:, 0:1], scalar2=rstd[:, 0:1], op0=ALU.subtract, op1=ALU.mult),
               reads=[mv, rstd], writes=[u])
        rec.op("dve", lambda e: e.tensor_tensor(out=u[:, :], in0=u[:, :], in1=gt[:, :], op=ALU.mult), reads=[gt], writes=[u])
        rec.op("dve", lambda e: e.tensor_tensor(out=dst[:, :], in0=u[:, :], in1=bt[:, :], op=ALU.add), reads=[u, bt], writes=[dst])

    for (x_src, OT, x_dst, cond, ntok) in x_srcs:
        nblk = (ntok + 511) // 512
        for blk in range(nblk):
            nt = min(4, (ntok - blk * 512) // 128)
            ncol = nt * 128
            rec.dma("sp", otin[:, :, 0:ncol], OT[:, :, blk * 512:blk * 512 + ncol].rearrange("c p t -> p c t"), writes=[otin])
            for t in range(nt):
                xt = xts.next()
                r0 = blk * 512 + t * 128
                rec.dma("sp", xt[:, :], x_src[r0:r0 + 128, :], writes=[xt])
                for hh in range(2):
                    for kc in range(8):
                        rec.op("pe", lambda e, hh=hh, kc=kc, t=t: e.matmul(B[hh][:, :], lhsT=otin[:, kc, t * 128:(t + 1) * 128],
                                                                       rhs=wout[:, kc, hh * 512:(hh + 1) * 512], start=(kc == 0), stop=(kc == 7)),
                               reads=[otin, wout], writes=[B[hh]])
                x1 = x1s[t]
                gated_ln(xt, B[0:2], L.Gb[0][cond], lnt[0], lnt[1], x1)
                ln_stats(x1)
                rec.op("dve", lambda e, x1=x1: e.tensor_scalar(out=tmp[:, :], in0=x1[:, :], scalar1=mv[:, 0:1], scalar2=rstd[:, 0:1],
                                                               op0=ALU.subtract, op1=ALU.mult), reads=[x1, mv, rstd], writes=[tmp])
                for kc in range(8):
                    bk = B[2 + kc // 4]
                    rec.op("pe", lambda e, kc=kc, bk=bk: e.transpose(out=bk[:, (kc % 4) * 128:(kc % 4 + 1) * 128], in_=tmp[:, kc * 128:(kc + 1) * 128],
                                                                     identity=L.ident_f[:, :]), reads=[tmp, L.ident_f], writes=[bk])
                for kc in range(8):
                    bk = B[2 + kc // 4]
                    rec.op("dve", lambda e, kc=kc, bk=bk, cond=cond: e.tensor_scalar(out=h2Tf[:, kc, :], in0=bk[:, (kc % 4) * 128:(kc % 4 + 1) * 128],
                                                                          scalar1=L.modT[:, 32 + kc, cond:cond + 1], scalar2=L.modT[:, 24 + kc, cond:cond + 1],
                                                                          op0=ALU.mult, op1=ALU.add), reads=[bk, L.modT], writes=[h2Tf])
                rec.op("pool", lambda e, t=t: e.tensor_copy(out=h2T[:, :, t * 128:(t + 1) * 128], in_=h2Tf[:, :, :]), reads=[h2Tf], writes=[h2T])
                for kc in range(8):
                    rec.op("pe", lambda e, kc=kc: e.matmul(B[4][:, 0:16], lhsT=h2Tf[:, kc, :], rhs=rw[:, kc, :], start=(kc == 0), stop=(kc == 7)),
                           reads=[h2Tf, rw], writes=[B[4]])
                rec.op("act", lambda e: e.activation(out=s16[:, :], in_=B[4][:, 0:16], func=AF.Sigmoid), reads=[B[4]], writes=[s16])
                v44 = lambda b_: b_[:, :].rearrange("p (g k) -> p g k", g=4)
                bc4 = lambda b_: b_[:, :].unsqueeze(2).to_broadcast([128, 4, 4])
                rec.op("dve", lambda e: e.tensor_tensor(out=sel[:, :], in0=s16[:, :], in1=rbias[:, :], op=ALU.add), reads=[s16, rbias], writes=[sel])
                rec.op("dve", lambda e: e.tensor_reduce(out=m1[:, :], in_=v44(sel), axis=AX.X, op=ALU.max), reads=[sel], writes=[m1])
                rec.op("dve", lambda e: e.tensor_tensor(out=v44(eq), in0=v44(sel), in1=bc4(m1), op=ALU.is_equal), reads=[sel, m1], writes=[eq])
                rec.op("dve", lambda e: e.scalar_tensor_tensor(out=sel2[:, :], in0=eq[:, :], scalar=-1e9, in1=sel[:, :], op0=ALU.mult, op1=ALU.add),
                       reads=[eq, sel], writes=[sel2])
                rec.op("dve", lambda e: e.tensor_reduce(out=m2[:, :], in_=v44(sel2), axis=AX.X, op=ALU.max), reads=[sel2], writes=[m2])
                rec.op("dve", lambda e: e.tensor_tensor(out=gs[:, :], in0=m1[:, :], in1=m2[:, :], op=ALU.add), reads=[m1, m2], writes=[gs])
                rec.op("dve", lambda e: e.tensor_reduce(out=gm[:, :], in_=gs[:, :], axis=AX.X, op=ALU.max), reads=[gs], writes=[gm])
                rec.op("dve", lambda e: e.tensor_scalar(out=gs[:, :], in0=gs[:, :], scalar1=gm[:, 0:1], scalar2=None, op0=ALU.is_equal), reads=[gm], writes=[gs])
                rec.op("dve", lambda e: e.tensor_tensor(out=v44(eq), in0=v44(sel), in1=bc4(m2), op=ALU.is_ge), reads=[sel, m2], writes=[eq])
                rec.op("dve", lambda e: e.tensor_tensor(out=v44(eq), in0=v44(eq), in1=bc4(gs), op=ALU.mult), reads=[gs], writes=[eq])
                rec.op("dve", lambda e: e.tensor_tensor(out=eq[:, :], in0=eq[:, :], in1=s16[:, :], op=ALU.mult), reads=[s16], writes=[eq])
                rec.op("dve", lambda e: e.tensor_reduce(out=gm[:, :], in_=eq[:, :], axis=AX.X, op=ALU.add), reads=[eq], writes=[gm])
                rec.op("dve", lambda e: e.reciprocal(out=gm[:, :], in_=gm[:, :]), reads=[gm], writes=[gm])
                rec.op("dve", lambda e, t=t: e.tensor_scalar(out=gates[:, t, :], in0=eq[:, :], scalar1=gm[:, 0:1], scalar2=None, op0=ALU.mult),
                       reads=[eq, gm], writes=[gates])
            items = [(ex, t) for ex in range(NE) for t in range(nt)]
            cur_w = [None]

            def mm_part(ex, t):
                if t == 0:
                    cur_w[0] = wgus.next()
                    rec.dma(wq, cur_w[0][:, :, :], wts["wgu"][ex, :, :, :], writes=[cur_w[0]], cast=not pre)
                wgu = cur_w[0]
                bk = B[(ex * nt + t) % 4]
                for kc in range(8):
                    rec.op("pe", lambda e, kc=kc, t=t, bk=bk, wgu=wgu: e.matmul(bk[:, :], lhsT=h2T[:, kc, t * 128:(t + 1) * 128], rhs=wgu[:, kc, :],
                                                                             start=(kc == 0), stop=(kc == 7)), reads=[h2T, wgu], writes=[bk])
                return bk

            def post_part(ex, t, bk):
                sl_ = sil.next()
                ab = actb.next()
                rec.op("act", lambda e, bk=bk, sl_=sl_: e.activation(out=sl_[:, :], in_=bk[:, 0:256], func=AF.Silu), reads=[bk], writes=[sl_])
                rec.op("dve", lambda e, bk=bk, sl_=sl_, ab=ab, t=t, ex=ex: e.scalar_tensor_tensor(out=ab[:, :], in0=sl_[:, :], scalar=gates[:, t, ex:ex + 1],
                                                                                                in1=bk[:, 256:512], op0=ALU.mult, op1=ALU.mult),
                       reads=[sl_, gates, bk], writes=[ab])
                for fc in range(2):
                    rec.op("pe", lambda e, fc=fc, ab=ab: e.transpose(out=psTb[:, fc * 128:(fc + 1) * 128], in_=ab[:, fc * 128:(fc + 1) * 128],
                                                                     identity=L.ident_b[:, :]), reads=[ab, L.ident_b], writes=[psTb, B[7]])
                rec.op("act", lambda e, ex=ex, t=t: e.activation(out=actT[:, ex, :, t * 128:(t + 1) * 128],
                                                                 in_=psTb[:, 0:256].rearrange("p (f c) -> p f c", f=2), func=AF.Copy),
                       reads=[psTb, B[7]], writes=[actT])

            bk_cur = mm_part(*items[0])
            for ii, (ex, t) in enumerate(items):
                bk_next = mm_part(*items[ii + 1]) if ii + 1 < len(items) else None
                post_part(ex, t, bk_cur)
                bk_cur = bk_next
            for ex in range(NE):
                wd = wds.next()
                rec.dma(wq, wd[:, :, :], wts["wd"][ex, :, :, :], writes=[wd], cast=not pre)
                for t in range(nt):
                    for hh in range(2):
                        for fc in range(2):
                            rec.op("pe", lambda e, ex=ex, t=t, hh=hh, fc=fc, wd=wd: e.matmul(
                                B[t * 2 + hh][:, :], lhsT=actT[:, ex, fc, t * 128:(t + 1) * 128], rhs=wd[:, fc, hh * 512:(hh + 1) * 512],
                                start=(ex == 0 and fc == 0), stop=(ex == NE - 1 and fc == 1)), reads=[actT, wd], writes=[B[t * 2 + hh], psTb] if t == 3 and hh == 1 else [B[t * 2 + hh]])
            for t in range(nt):
                r0 = blk * 512 + t * 128
                xo = xts.next()
                gated_ln(x1s[t], B[2 * t:2 * t + 2], L.Gb[1][cond], lnt[2], lnt[3], xo)
                rec.dma("sp", x_dst[r0:r0 + 128, :], xo[:, :], reads=[xo])


def build_A(n_lat_tiles=NT, do_ctx=True):
    cx = Ctx()
    L = NS()
    rec = cx.rec
    setup_consts(cx, L)
    st = ExitStack()
    layer_setup(cx, L, st)
    st.close()
    rec.barrier()
    x_src = cx.din("x_own", [TOK, 1024])
    xc_src = cx.din("xc", [CTX, 1024])
    tab_src = cx.din("rope_tab", [TOK + CTX, 192])
    outs = {"qT": cx.dout("qT", [10, 128, TOK], BF16), "kT": cx.dout("kT", [10, 128, TOK], BF16), "v": cx.dout("v", [12, 128, NT, 128], BF16),
            "qTc": cx.dout("qTc", [10, 128, CTX], BF16), "kTc": cx.dout("kTc", [10, 128, CTX], BF16), "vc": cx.dout("vc", [12, 128, 2, 128], BF16)}
    st = ExitStack()
    phase1(cx, L, st, x_src, xc_src, tab_src, outs, n_lat_tiles=n_lat_tiles, do_ctx=do_ctx)
    rec.wait_all_on("sp")
    rec.emit()
    return cx


def build_B(l, need_ctx):
    cx = Ctx()
    L = NS()
    rec = cx.rec
    setup_consts(cx, L)
    st = ExitStack()
    layer_setup(cx, L, st)
    st.close()
    rec.barrier()
    src = {"qT": cx.din("qT", [10, 128, TOK], BF16), "kTall": cx.din("kTall", [4, 10, 128, TOK], BF16),
           "vall": cx.din("vall", [4, 12, 128, NT, 128], BF16), "kTc": cx.din("kTc", [10, 128, CTX], BF16),
           "vc": cx.din("vc", [12, 128, 2, 128], BF16), "qTc": cx.din("qTc", [10, 128, CTX], BF16),
           "kTwin": cx.din("kTwin", [2, 128, NWIN * 128], BF16), "vwin": cx.din("vwin", [2, 128, NWIN, 128], BF16),
           "OT": (cx.dout if DBG.get("export") else cx.dint)("OT", [8, 128, TOK], BF16),
           "OTc": (cx.dout if DBG.get("export") else cx.dint)("OTc", [8, 128, CTX], BF16)}
    st = ExitStack()
    attention(cx, L, st, l, src, need_ctx)
    st.close()
    rec.barrier()
    wts = {"w_out": cx.din("w_out", [128, 8, 1024]), "wgu": cx.din("wgu", [NE, 128, 8, 512]), "wd": cx.din("wd", [NE, 128, 2, 1024]),
           "router_w": cx.din("router_w", [128, 8, 16]), "router_bias": cx.din("router_bias", [16]), "ln": cx.din("ln", [4, 1024])}
    x_src = cx.din("x_own", [TOK, 1024])
    x_dst = cx.dout("x_next", [TOK, 1024])
    xs = [(x_src, src["OT"], x_dst, 0, TOK)]
    if need_ctx:
        xc_src = cx.din("xc", [CTX, 1024])
        xc_dst = cx.dout("xc_next", [CTX, 1024])
        xs.append((xc_src, src["OTc"], xc_dst, 1, CTX))
    st = ExitStack()
    if DBG.get("p3", True):
        phase3(cx, L, st, xs, wts)
    else:
        rec.dma("sp", x_dst[0:128, :], x_src[0:128, :], key="D_dbg")
    rec.wait_all_on("sp")
    rec.emit()
    return cx


def prep_B_weights(inp, l):
    d = {}
    d["w_out"] = _pm(inp["w_out"][l], 8)
    d["wgu"] = np.ascontiguousarray(np.stack([_pm(np.concatenate([inp["exp_w_gate"][l, e], inp["exp_w_up"][l, e]], 1), 8) for e in range(NE)]))
    d["wd"] = np.ascontiguousarray(np.stack([_pm(inp["exp_w_down"][l, e], 2) for e in range(NE)]))
    d["router_w"] = _pm(inp["router_w"], 8)
    d["router_bias"] = np.ascontiguousarray(inp["router_bias"])
    d["ln"] = np.ascontiguousarray(np.stack([inp["ln1_g"][l], inp["ln1_b"][l], inp["ln2_g"][l], inp["ln2_b"][l]]))
    d["lamv"] = np.ascontiguousarray(np.stack([inp["diff_lambda_q1"][l], inp["diff_lambda_k1"][l], inp["diff_lambda_q2"][l], inp["diff_lambda_k2"][l]]))
    d["subln_g"] = np.ascontiguousarray(inp["diff_subln_g"][l].reshape(64, 1))
    d["swa_sink"] = np.ascontiguousarray(inp["swa_sink"][l])
    return d


def band_masks(r):
    ki = np.arange(128)[:, None]
    qi = np.arange(128)[None, :]
    mprev = (qi <= ki).astype(np.float32)
    mnext = (ki <= qi).astype(np.float32)
    m = np.stack([mprev, mnext, mprev * (0.0 if r == 0 else 1.0), mnext * (0.0 if r == 3 else 1.0)], 1)
    return np.ascontiguousarray(m.astype(np.float32))


KVR = 1280 + 1536


def build_fused():
    cx = Ctx()
    L = NS()
    rec = cx.rec
    nc = cx.nc
    setup_consts(cx, L)
    x_in = cx.din("x_own", [TOK, 1024])
    xc_in = cx.din("xc", [CTX, 1024])
    tab_src = cx.din("rope_tab", [TOK + CTX, 192])
    out = cx.dout("out", [TOK, 1024])
    x1 = cx.dint("x1", [TOK, 1024])
    xc1 = cx.dint("xc1", [CTX, 1024])
    groups = [[0, 1, 2, 3], [4, 5, 6, 7]]
    for l in range(DEPTH):
        cx.sfx = "_l%d" % l
        need_ctx = l < DEPTH - 1
        lst = ExitStack()
        st = ExitStack()
        L.modT = rec.sb("modT", [128, 48, 2], F32, lst)
        L.Gb = [[rec.sb("Gb", [128, 1024], F32, lst) for _ in range(2)] for _ in range(2)]
        wsrc = {"w_out": cx.din("w_out", [128, 8, 1024]), "wgu": cx.din("wgu", [NE, 128, 8, 512]), "wd": cx.din("wd", [NE, 128, 2, 1024])}
        wbf = {"w_out": cx.dint("w_out_bf%d" % l, [128, 8, 1024], BF16), "wgu": cx.dint("wgu_bf%d" % l, [NE, 128, 8, 512], BF16),
               "wd": cx.dint("wd_bf%d" % l, [NE, 128, 2, 1024], BF16)}
        layer_setup(cx, L, st, pst="prealloc")
        st.close()
        rec.barrier()
        rec.release_dma_sems()
        for ex in range(NE):
            rec.dma("pool", wbf["wgu"][ex, :, :, :].rearrange("p a b -> p (a b)"), wsrc["wgu"][ex, :, :, :].rearrange("p a b -> p (a b)"), key="D_wcast", cast=True)
            rec.dma("pool", wbf["wd"][ex, :, :, :].rearrange("p a b -> p (a b)"), wsrc["wd"][ex, :, :, :].rearrange("p a b -> p (a b)"), key="D_wcast", cast=True)
        rec.dma("pool", wbf["w_out"][:, :, :].rearrange("p a b -> p (a b)"), wsrc["w_out"][:, :, :].rearrange("p a b -> p (a b)"), key="D_wcast", cast=True)
        kv_own = nc.dram_tensor("kv_own%d" % l, [KVR, TOK], BF16).ap()
        ga = [Buf("ga", nc.dram_tensor("ga%d_%d" % (l, p_), [4 * 128, TOK], BF16).ap()) for p_ in range(22)]
        qT = cx.dint("qT%d" % l, [10, 128, TOK], BF16)
        qTc = cx.dint("qTc%d" % l, [10, 128, CTX], BF16)
        kTc = cx.dint("kTc%d" % l, [10, 128, CTX], BF16)
        vc = cx.dint("vc%d" % l, [12, 128, 2, 128], BF16)
        OT = cx.dint("OT%d" % l, [8, 128, TOK], BF16)
        OTc = cx.dint("OTc%d" % l, [8, 128, CTX], BF16)
        kT_own = Buf("kTown", kv_own[0:1280, :].rearrange("(c p) t -> c p t", p=128))
        v_own = Buf("vown", kv_own[1280:KVR, :].rearrange("(s p) (t d) -> s p t d", p=128, d=128))
        outs = {"qT": qT, "kT": kT_own, "v": v_own, "qTc": qTc, "kTc": kTc, "vc": vc}
        st = ExitStack()
        phase1(cx, L, st, x_in if l == 0 else x1, xc_in if l == 0 else xc1, tab_src, outs)
        st.close()
        rec.barrier()
        rec.release_dma_sems()
        order = [0, 10, 11, 1, 12, 13, 2, 14, 3, 15, 4, 16, 5, 17, 6, 18, 7, 19, 8, 20, 9, 21]
        for p_ in order:
            rec.collective_piece(lambda e, a=kv_own[p_ * 128:(p_ + 1) * 128, :], b=ga[p_]: e.collective_compute(
                "AllGather", ALU.bypass, replica_groups=groups, ins=[a], outs=[b[:, :]]), ga[p_])
        src = {"qT": qT, "ga": ga, "kTc": kTc, "vc": vc, "qTc": qTc, "kTown": kT_own, "vown": v_own, "OT": OT, "OTc": OTc}
        st = ExitStack()
        attention(cx, L, st, l, src, need_ctx)
        st.close()
        rec.barrier()
        rec.release_dma_sems()
        wts = {"w_out": wbf["w_out"], "wgu": wbf["wgu"], "wd": wbf["wd"], "bf16": True,
               "router_w": cx.din("router_w", [128, 8, 16]), "router_bias": cx.din("router_bias", [16]), "ln": cx.din("ln", [4, 1024])}
        xs = [(x_in if l == 0 else x1, OT, x1 if l == 0 else out, 0, TOK)]
        if need_ctx:
            xs.append((xc_in, OTc, xc1, 1, CTX))
        st = ExitStack()
        phase3(cx, L, st, xs, wts)
        st.close()
        rec.barrier()
        rec.release_dma_sems()
        lst.close()
    rec.wait_all_on("sp")
    rec.emit()
    return cx


def kernel_fused(inp):
    cores = list(range(NCORES))
    lws = [prep_layer(inp, l) for l in range(DEPTH)]
    bws = [prep_B_weights(inp, l) for l in range(DEPTH)]
    shared_w = {"router_w": bws[0]["router_w"], "router_bias": bws[0]["router_bias"]}
    in_maps = []
    for c in cores:
        b, r = c // 4, c % 4
        m = dict(prep_core_common(inp, c))
        m.update(shared_w)
        for l in range(DEPTH):
            for k, v in list(lws[l].items()) + list(bws[l].items()):
                if k not in shared_w:
                    m["%s_l%d" % (k, l)] = v
        m["x_own"] = np.ascontiguousarray(inp["x"][b, r * TOK:(r + 1) * TOK])
        m["xc"] = np.ascontiguousarray(inp["ctx"][b])
        m["c_masks"] = band_masks(r)
        oh = np.zeros((128, 8), np.float32)
        if r > 0:
            oh[:, r - 1] = 1.0
        if r < 3:
            oh[:, 4 + r + 1] = 1.0
        m["onehot"] = oh
        in_maps.append(m)
    prog = _prog("fused", build_fused)
    names = set(prog.dram.keys())
    in_maps = [{k: v for k, v in m.items() if k in names} for m in in_maps]
    res = run_bass_kernel_spmd(prog.nc, in_maps, core_ids=cores).results
    out = np.zeros((BATCH, SEQ, D), np.float32)
    for c in cores:
        out[c // 4, (c % 4) * TOK:(c % 4 + 1) * TOK] = res[c]["out"]
    return out


_PROGS = {}


def _prog(key, fn):
    if key not in _PROGS:
        _PROGS[key] = fn()
    return _PROGS[key]


def kernel(**inputs):
    inp = {k: np.asarray(v) for k, v in inputs.items()}
    return kernel_fused(inp)


def kernel_unfused(**inputs):
    inp = {k: np.asarray(v) for k, v in inputs.items()}
    cores = list(range(NCORES))
    common = [prep_core_common(inp, c) for c in cores]
    x_cur = [np.ascontiguousarray(inp["x"][c // 4, (c % 4) * TOK:(c % 4 + 1) * TOK]) for c in cores]
    xc_cur = [np.ascontiguousarray(inp["ctx"][c // 4]) for c in cores]
    for l in range(DEPTH):
        need_ctx = l < DEPTH - 1
        lw = prep_layer(inp, l)
        pa = _prog("A", build_A)
        in_maps = []
        for c in cores:
            m = dict(lw)
            m.update(common[c])
            m["x_own"] = x_cur[c]
            m["xc"] = xc_cur[c]
            in_maps.append(m)
        ra = run_bass_kernel_spmd(pa.nc, in_maps, core_ids=cores).results
        bw = prep_B_weights(inp, l)
        in_maps = []
        for c in cores:
            b, r = c // 4, c % 4
            grp = [ra[b * 4 + i] for i in range(4)]
            m = {}
            for k in ("w_ada", "b_adaT", "b_ada"):
                m[k] = lw[k]
            m["cT"] = common[c]["cT"]
            m["c_ident"] = common[c]["c_ident"]
            m.update(bw)
            m["qT"] = ra[c]["qT"]
            m["kTc"] = ra[c]["kTc"]
            m["vc"] = ra[c]["vc"]
            m["qTc"] = ra[c]["qTc"]
            m["kTall"] = np.ascontiguousarray(np.stack([g["kT"] for g in grp]))
            m["vall"] = np.ascontiguousarray(np.stack([g["v"] for g in grp]))
            kfull = np.concatenate([g["kT"][8:10] for g in grp], axis=2)
            kpad = np.zeros((2, 128, SEQ + 256), kfull.dtype)
            kpad[:, :, 128:128 + SEQ] = kfull
            m["kTwin"] = np.ascontiguousarray(kpad[:, :, r * TOK:r * TOK + NWIN * 128])
            vfull = np.concatenate([g["v"][10:12] for g in grp], axis=2)
            vpad = np.zeros((2, 128, 4 * NT + 2, 128), vfull.dtype)
            vpad[:, :, 1:1 + 4 * NT] = vfull
            m["vwin"] = np.ascontiguousarray(vpad[:, :, r * NT:r * NT + NWIN])
            m["c_masks"] = band_masks(r)
            m["x_own"] = x_cur[c]
            if need_ctx:
                m["xc"] = xc_cur[c]
            in_maps.append(m)
        pb = _prog(("B", l), lambda: build_B(l, need_ctx))
        rb = run_bass_kernel_spmd(pb.nc, in_maps, core_ids=cores).results
        x_cur = [np.ascontiguousarray(rb[c]["x_next"]) for c in cores]
        if need_ctx:
            xc_cur = [np.ascontiguousarray(rb[c]["xc_next"]) for c in cores]
    out = np.zeros((BATCH, SEQ, D), np.float32)
    for c in cores:
        out[c // 4, (c % 4) * TOK:(c % 4 + 1) * TOK] = x_cur[c]
    return out
```

```python
import numpy as np
from contextlib import ExitStack
import concourse.bass as bass
import concourse.mybir as mybir
from concourse.bass_utils import run_bass_kernel_spmd

F32 = mybir.dt.float32
BF16 = mybir.dt.bfloat16
AF = mybir.ActivationFunctionType
ALU = mybir.AluOpType
AX = mybir.AxisListType

D = 1024
BATCH = 2
SEQ = 16384
DEPTH = 2
CTX = 256
NCORES = 8
TOK = SEQ // 4
NT = TOK // 128
NE = 16
DE = 256
EPS = 1e-6
ALPHA = (2 * DEPTH) ** 0.25
IN_COLS = 2144


class Buf:
    __slots__ = ("name", "t", "w", "r")

    _uid = [0]

    def __init__(self, name, t):
        Buf._uid[0] += 1
        self.name = "%s.%d" % (name, Buf._uid[0])
        self.t = t
        self.w = {}
        self.r = {}

    def __getitem__(self, idx):
        return self.t[idx]


class Rec:
    ENGS = ("pe", "act", "dve", "pool", "sp")

    def __init__(self, nc, stack):
        self.nc = nc
        self.stack = stack
        self.ops = {e: [] for e in self.ENGS}
        self.sems = {}
        self.cnt = {}
        self.seen = {e: {} for e in self.ENGS}
        self.nbuf = 0
        for e in self.ENGS:
            self._sem("E_" + e)

    def _sem(self, key):
        if key not in self.sems:
            if key.startswith("D_H_") and getattr(self, "free", None):
                sem, c0 = self.free.pop()
                self.sems[key] = sem
                self.cnt[key] = c0
            else:
                self.nsem = getattr(self, "nsem", 0) + 1
                self.sems[key] = self.stack.enter_context(self.nc.semaphore("s%d" % self.nsem))
                self.cnt[key] = 0
        return self.sems[key]

    def release_dma_sems(self):
        if not hasattr(self, "free"):
            self.free = []
        for key in [k for k in self.sems if k.startswith("D_")]:
            sem, c = self.sems.pop(key), self.cnt.pop(key)
            if key.startswith("D_H_"):
                self.free.append((sem, c))
            for e in self.ENGS:
                self.seen[e].pop(key, None)

    def collective_piece(self, fn, out_buf):
        key = "E_cc"
        self._sem(key)
        self.cnt[key] += 1
        self.ops["pool"].append(([], fn, self.sems[key], 1))
        out_buf.w = {key: (self.cnt[key], "cc")}
        out_buf.r = {}

    def collective(self, fn):
        self.barrier()
        key = "E_cc"
        self._sem(key)
        self.cnt[key] += 1
        self.ops["pool"].append(([], fn, self.sems[key], 1))
        self.barrier()

    def buf(self, name, t):
        self.nbuf += 1
        return Buf("%s#%d" % (name, self.nbuf), t)

    def sb(self, name, shape, dtype, stack=None):
        st = stack if stack is not None else self.stack
        self.nbuf += 1
        t = st.enter_context(self.nc.sbuf_tensor("%s_%d" % (name, self.nbuf), list(shape), dtype))
        return Buf(name, t)

    def ps(self, name, shape, dtype, stack=None):
        st = stack if stack is not None else self.stack
        self.nbuf += 1
        t = st.enter_context(self.nc.psum_tensor("%s_%d" % (name, self.nbuf), list(shape), dtype))
        return Buf(name, t)

    def _collect(self, eng, reads, writes, is_dma):
        need = {}

        def add(d):
            for k, (v, pe) in d.items():
                if k not in self.sems:
                    continue
                if (not is_dma) and eng == "pe" and pe == "pe" and k == "E_pe":
                    continue
                if need.get(k, 0) < v:
                    need[k] = v
        for b in reads:
            add(b.w)
        for b in writes:
            add(b.w)
            add(b.r)
        waits = []
        seen = self.seen[eng]
        for k, v in need.items():
            if seen.get(k, 0) < v:
                seen[k] = v
                waits.append((self.sems[k], v))
        return waits

    def op(self, eng, fn, reads=(), writes=()):
        waits = self._collect(eng, reads, writes, False)
        key = "E_" + eng
        self.cnt[key] += 1
        tk = (self.cnt[key], eng)
        self.ops[eng].append((waits, fn, self.sems[key], 1))
        for b in reads:
            b.r[key] = tk
        for b in writes:
            b.w = {key: tk}
            b.r = {}

    def dma(self, eng, out_ap, in_ap, reads=(), writes=(), key=None, cast=False):
        if key is None:
            b0 = (list(writes) + list(reads))[0]
            key = b0.name + ("_w" if writes else "_r")
        key = ("D_P_" if eng == "pool" else "D_H_") + key
        self._sem(key)
        waits = self._collect(eng, reads, writes, True)
        self.cnt[key] += 16
        tk = (self.cnt[key], "dma")
        if cast:
            self.ops[eng].append((waits, lambda e, o=out_ap, i=in_ap: e.dma_start(out=o, in_=i, max_dma_last_dim=4096), self.sems[key], 16))
        else:
            self.ops[eng].append((waits, lambda e, o=out_ap, i=in_ap: e.dma_start(out=o, in_=i), self.sems[key], 16))
        for b in reads:
            b.r[key] = tk
        for b in writes:
            b.w = {key: tk}
            b.r = {}

    def barrier(self):
        for e in self.ENGS:
            waits = []
            for k, v in self.cnt.items():
                if v > 0 and self.seen[e].get(k, 0) < v:
                    self.seen[e][k] = v
                    waits.append((self.sems[k], v))
            if waits:
                self.ops[e].append((waits, None, None, 0))

    def wait_all_on(self, eng):
        waits = []
        for k, v in self.cnt.items():
            if v > 0 and self.seen[eng].get(k, 0) < v:
                self.seen[eng][k] = v
                waits.append((self.sems[k], v))
        if waits:
            self.ops[eng].append((waits, None, None, 0))

    def emit(self):
        nc = self.nc
        block = self.stack.enter_context(nc.Block())

        def run(e, ops):
            for waits, fn, sem, inc in ops:
                for s, v in waits:
                    e.wait_ge(s, v)
                if fn is not None:
                    fn(e).then_inc(sem, inc)

        @block.tensor
        def _(e):
            run(e, self.ops["pe"])

        @block.scalar
        def _(e):
            run(e, self.ops["act"])

        @block.vector
        def _(e):
            run(e, self.ops["dve"])

        @block.gpsimd
        def _(e):
            run(e, self.ops["pool"])

        @block.sync
        def _(e):
            run(e, self.ops["sp"])


def _bc(ap, shape):
    return ap.to_broadcast(list(shape))


class Ctx:
    def __init__(self):
        self.nc = bass.Bass("TRN2", target_bir_lowering=False)
        self.stack = ExitStack()
        self.rec = Rec(self.nc, self.stack)
        self.dram = {}

    sfx = ""
    shared = ("c_ident", "cT", "rope_tab", "c_masks", "router_w", "router_bias", "x_own", "xc", "onehot")

    def din(self, name, shape, dtype=F32):
        if name not in self.shared:
            name = name + self.sfx
        if name in self.dram:
            return self.dram[name]
        t = self.nc.dram_tensor(name, list(shape), dtype, kind="ExternalInput")
        b = Buf(name, t.ap())
        self.dram[name] = b
        return b

    def dout(self, name, shape, dtype=F32):
        t = self.nc.dram_tensor(name, list(shape), dtype, kind="ExternalOutput")
        b = Buf(name, t.ap())
        self.dram[name] = b
        return b

    def dint(self, name, shape, dtype=F32):
        t = self.nc.dram_tensor(name, list(shape), dtype)
        b = Buf(name, t.ap())
        self.dram[name] = b
        return b


def _in_perm():
    o = {}
    off = 0
    for name, n in (("Aq", 256), ("Ak", 256), ("Av", 256), ("Bq", 256), ("Bk", 128), ("Bv", 128),
                    ("Ccq", 192), ("Cckv", 128), ("Ckr", 32), ("Dq", 256), ("Dk", 128), ("Dv", 128)):
        o[name] = np.arange(off, off + n)
        off += n
    order = ["Aq", "Ak", "Bq", "Bk", "Dk", "Dq", "Ckr", "Av", "Bv", "Dv", "Ccq", "Cckv"]
    return np.concatenate([o[k] for k in order])


GOFF = (0, 512, 1024, 1312, 1824)
GW = (512, 512, 288, 512, 320)


def rope_tables(tok_idx):
    row = (tok_idx // 64).astype(np.float64)
    col = (tok_idx % 64).astype(np.float64)

    def tab(n):
        inv = 10000.0 ** (-np.arange(n, dtype=np.float32).astype(np.float64) / n)
        inv = (np.float32(10000.0) ** (-(np.arange(n, dtype=np.float32) / np.float32(n)))).astype(np.float32)
        ar = (row.astype(np.float32)[:, None] * inv).astype(np.float32)
        ac = (col.astype(np.float32)[:, None] * inv).astype(np.float32)
        cr, sr, cc, sc = np.cos(ar), np.sin(ar), np.cos(ac), np.sin(ac)
        cos = np.concatenate([cr, cr, cc, cc], 1)
        sin = np.concatenate([-sr, sr, -sc, sc], 1)
        return cos.astype(np.float32), sin.astype(np.float32)
    c32, s32 = tab(8)
    c64, s64 = tab(16)
    return np.ascontiguousarray(np.concatenate([c32, s32, c64, s64], 1), dtype=np.float32)


def setup_consts(cx, L):
    rec, nc = cx.rec, cx.nc
    L.ident_f = rec.sb("identf", [128, 128], F32)
    L.ident_b = rec.sb("identb", [128, 128], BF16)
    L.eps = rec.sb("eps", [128, 1], F32)
    L.ones64 = rec.sb("ones64", [64, 64], F32)
    idn = cx.din("c_ident", [128, 128], F32)
    rec.dma("sp", L.ident_f[:, :], idn[:, :], writes=[L.ident_f])
    rec.op("dve", lambda e: e.tensor_copy(out=L.ident_b[:, :], in_=L.ident_f[:, :]), reads=[L.ident_f], writes=[L.ident_b])
    rec.op("dve", lambda e: e.memset(L.eps[:, :], EPS), writes=[L.eps])
    rec.op("dve", lambda e: e.memset(L.ones64[:, :], 1.0 / 64.0), writes=[L.ones64])


class NS:
    pass


DBG = {"stop": 99, "att": 9, "kinds": "ABCD"}


class Ring:
    def __init__(self, bufs):
        self.bufs = bufs
        self.i = 0

    def next(self):
        b = self.bufs[self.i % len(self.bufs)]
        self.i += 1
        return b


def rope_ops(rec, eng, src_buf, src, nv, R, tab_buf, cos, sin, t1b, t2b):
    h = R // 4
    n = nv * R
    t1 = t1b[:, 0:n]
    t2 = t2b[:, 0:n]
    rec.op(eng, lambda e: e.tensor_tensor(out=t1.rearrange("p (v r) -> p v r", v=nv), in0=src.rearrange("p (v r) -> p v r", v=nv),
                                          in1=cos.unsqueeze(1).to_broadcast([128, nv, R]), op=ALU.mult),
           reads=[src_buf, tab_buf], writes=[t1b])
    s5 = src.rearrange("p (v a b h) -> p v a b h", v=nv, a=2, b=2, h=h)
    t5 = t2.rearrange("p (v a b h) -> p v a b h", v=nv, a=2, b=2, h=h)
    sn = sin.rearrange("p (a b h) -> p a b h", a=2, b=2, h=h)
    for b in (0, 1):
        rec.op(eng, lambda e, b=b: e.tensor_tensor(out=t5[:, :, :, b, :], in0=s5[:, :, :, 1 - b, :],
                                                   in1=sn[:, :, b, :].unsqueeze(1).to_broadcast([128, nv, 2, h]), op=ALU.mult),
               reads=[src_buf, tab_buf], writes=[t2b])
    rec.op(eng, lambda e: e.tensor_tensor(out=t1, in0=t1, in1=t2, op=ALU.add), reads=[t2b], writes=[t1b])
    return t1


def layer_setup(cx, L, st, pst=None):
    rec = cx.rec
    cT = cx.din("cT", [128, 8, 2])
    wada = cx.din("w_ada", [128, 8, 6144])
    badaT = cx.din("b_adaT", [128, 48])
    bada = cx.din("b_ada", [6144])
    if pst != "prealloc":
        L.modT = rec.sb("modT", [128, 48, 2], F32, pst)
        L.Gb = [[rec.sb("Gb", [128, 1024], F32, pst) for _ in range(2)] for _ in range(2)]
    sl0 = rec.sb("sl0", [128, 8, 2], F32, st)
    sl = rec.sb("sl", [128, 8, 2], F32, st)
    slrep = rec.sb("slrep", [128, 8, 2, 128], F32, st)
    bT = rec.sb("bT", [128, 48], F32, st)
    was = Ring([rec.sb("wa", [128, 8, 512], F32, st) for _ in range(2)])
    bbs = Ring([rec.sb("bb", [128, 512], F32, st) for _ in range(2)])
    psA = rec.ps("psA", [128, 512], F32, st)
    psB = Ring([rec.ps("psB", [128, 512], F32, st) for _ in range(2)])
    rec.op("dve", lambda e: e.memset(L.modT[:, :, :], 0.0), writes=[L.modT])
    rec.dma("sp", sl0[:, :, :], cT[:, :, :], writes=[sl0])
    rec.dma("sp", bT[:, :], badaT[:, :], writes=[bT])
    rec.op("act", lambda e: e.activation(out=sl[:, :, :], in_=sl0[:, :, :], func=AF.Silu), reads=[sl0], writes=[sl])
    rec.op("dve", lambda e: e.tensor_copy(out=slrep[:, :, :, :], in_=sl[:, :, :].unsqueeze(3).to_broadcast([128, 8, 2, 128])),
           reads=[sl], writes=[slrep])
    for gi in range(12):
        wa = was.next()
        rec.dma("sp", wa[:, :, :], wada[:, :, gi * 512:(gi + 1) * 512], writes=[wa])
        v = gi // 2
        if v in (2, 5):
            bb = bbs.next()
            rec.dma("pool", bb[:, :], bada[gi * 512:(gi + 1) * 512].partition_broadcast(128), writes=[bb])
            for cond in range(2):
                ps = psB.next()
                for kc in range(8):
                    rec.op("pe", lambda e, kc=kc, cond=cond, ps=ps, wa=wa: e.matmul(ps[:, :], lhsT=slrep[:, kc, cond, :], rhs=wa[:, kc, :],
                                                                                 start=(kc == 0), stop=(kc == 7)),
                           reads=[slrep, wa], writes=[ps])
                gb = L.Gb[0 if v == 2 else 1][cond]
                rec.op("dve", lambda e, ps=ps, gb=gb, bb=bb, gi=gi: e.tensor_tensor(out=gb[:, (gi % 2) * 512:(gi % 2 + 1) * 512], in0=ps[:, :],
                                                                                 in1=bb[:, :], op=ALU.add),
                       reads=[ps, bb], writes=[gb])
        else:
            for jj in range(4):
                for kc in range(8):
                    rec.op("pe", lambda e, kc=kc, jj=jj, wa=wa: e.matmul(psA[:, jj * 2:jj * 2 + 2], lhsT=wa[:, kc, jj * 128:(jj + 1) * 128],
                                                                       rhs=sl[:, kc, :], start=(kc == 0), stop=(kc == 7)),
                           reads=[sl, wa], writes=[psA])
            rec.op("dve", lambda e, gi=gi: e.tensor_tensor(out=L.modT[:, gi * 4:gi * 4 + 4, :],
                                                           in0=psA[:, 0:8].rearrange("p (j c) -> p j c", c=2),
                                                           in1=bT[:, gi * 4:gi * 4 + 4].unsqueeze(2).to_broadcast([128, 4, 2]), op=ALU.add),
                   reads=[psA, bT], writes=[L.modT])
    for lo in (8, 32):
        rec.op("dve", lambda e, lo=lo: e.tensor_scalar(out=L.modT[:, lo:lo + 8, :], in0=L.modT[:, lo:lo + 8, :], scalar1=1.0, scalar2=None,
                                                       op0=ALU.add), reads=[L.modT], writes=[L.modT])


def phase1(cx, L, st, x_src, xc_src, tab_src, outs, n_lat_tiles=NT, do_ctx=True):
    rec = cx.rec
    win_d = cx.din("w_in", [128, 8, IN_COLS])
    wuq_d = cx.din("w_uq", [128, 2, 384])
    wukv_d = cx.din("w_ukv", [128, 512])
    g1_d = cx.din("gain_g1", [512])
    gc_d = cx.din("gain_c", [320])
    win = rec.sb("win", [128, 8, IN_COLS], BF16, st)
    wuq = rec.sb("wuq", [128, 2, 384], BF16, st)
    wukv = rec.sb("wukv", [128, 512], BF16, st)
    for kc in range(8):
        rec.dma("pool", win[:, kc, :], win_d[:, kc, :], writes=[win], cast=True)
    rec.dma("pool", wuq[:, :, :], wuq_d[:, :, :], writes=[wuq], cast=True)
    rec.dma("pool", wukv[:, :], wukv_d[:, :], writes=[wukv], cast=True)
    G1t = rec.sb("G1t", [128, 512], F32, st)
    Gct = rec.sb("Gct", [128, 320], F32, st)
    rec.dma("sp", G1t[:, :], g1_d[0:512].partition_broadcast(128), writes=[G1t])
    rec.dma("sp", Gct[:, :], gc_d[0:320].partition_broadcast(128), writes=[Gct])
    xts = Ring([rec.sb("xt", [128, 1024], F32, st) for _ in range(2)])
    tabs = Ring([rec.sb("tab", [128, 192], F32, st) for _ in range(2)])
    st6 = rec.sb("st6", [128, 2, 6], F32, st)
    mv = rec.sb("mv", [128, 2], F32, st)
    sq1 = rec.sb("sq1", [128, 1], F32, st)
    rstd = rec.sb("rstd", [128, 1], F32, st)
    xn = rec.sb("xn", [128, 1024], BF16, st)
    hT = rec.sb("hT", [128, 8, 128], BF16, st)
    t1b = rec.sb("t1b", [128, 512], F32, st)
    t2b = rec.sb("t2b", [128, 512], F32, st)
    xg = rec.sb("xg", [128, 512], F32, st)
    sqt = rec.sb("sqt", [128, 512], F32, st)
    ss8 = rec.sb("ss8", [128, 8], F32, st)
    rs8 = rec.sb("rs8", [128, 8], F32, st)
    ssc = rec.sb("ssc", [128, 2], F32, st)
    rsc = rec.sb("rsc", [128, 2], F32, st)
    cn = rec.sb("cn", [128, 320], BF16, st)
    cnT = rec.sb("cnT", [128, 3, 128], BF16, st)
    qk = rec.sb("qk", [128, 2, 1280], BF16, st)
    vts = Ring([rec.sb("vt", [128, 12, 128], BF16, st) for _ in range(2)])
    qkT = Ring([rec.sb("qkT", [128, 2, 10, 512], BF16, st) for _ in range(2)])
    psT = rec.ps("psT", [128, 1024], BF16, st)
    psG = [rec.ps("psG", [128, 512], F32, st) for _ in range(5)]
    psU = [rec.ps("psU", [128, 512], F32, st) for _ in range(2)]
    for vt in vts.bufs:
        rec.op("pool", lambda e, vt=vt: e.memset(vt[:, :, 64:128], 1.0), writes=[vt])
    rec.op("pool", lambda e: e.memset(qk[:, :, :], 0.0), writes=[qk])

    def tile(src_ap, src_buf, tab_ap, cond, qkt, col, vdst_ap, vdst_buf):
        xt = xts.next()
        tab = tabs.next()
        vt = vts.next()
        rec.dma("sp", xt[:, :], src_ap, writes=[xt])
        rec.dma("sp", tab[:, :], tab_ap, writes=[tab])
        c32, s32, c64, s64 = tab[:, 0:32], tab[:, 32:64], tab[:, 64:128], tab[:, 128:192]
        for i in range(2):
            rec.op("dve", lambda e, i=i: e.bn_stats(out=st6[:, i, :], in_=xt[:, i * 512:(i + 1) * 512]), reads=[xt], writes=[st6])
        rec.op("dve", lambda e: e.bn_aggr(out=mv[:, :], in_=st6[:, :, :].rearrange("p a b -> p (a b)")), reads=[st6], writes=[mv])
        rec.op("act", lambda e: e.activation(out=sq1[:, :], in_=mv[:, 1:2], func=AF.Sqrt, bias=L.eps[:, 0:1], scale=1.0),
               reads=[mv, L.eps], writes=[sq1])
        rec.op("dve", lambda e: e.reciprocal(out=rstd[:, :], in_=sq1[:, :]), reads=[sq1], writes=[rstd])
        rec.op("dve", lambda e: e.tensor_scalar(out=xn[:, :], in0=xt[:, :], scalar1=mv[:, 0:1], scalar2=rstd[:, 0:1],
                                                op0=ALU.subtract, op1=ALU.mult), reads=[xt, mv, rstd], writes=[xn])
        if DBG["stop"] <= 1:
            return
        for kc in range(8):
            rec.op("pe", lambda e, kc=kc: e.transpose(out=psT[:, kc * 128:(kc + 1) * 128], in_=xn[:, kc * 128:(kc + 1) * 128], identity=L.ident_b[:, :]),
                   reads=[xn, L.ident_b], writes=[psT])
        for kc in range(8):
            rec.op("dve", lambda e, kc=kc: e.tensor_scalar(out=hT[:, kc, :], in0=psT[:, kc * 128:(kc + 1) * 128],
                                                           scalar1=L.modT[:, 8 + kc, cond:cond + 1], scalar2=L.modT[:, kc, cond:cond + 1],
                                                           op0=ALU.mult, op1=ALU.add), reads=[psT, L.modT], writes=[hT])
        if DBG["stop"] <= 2:
            return
        for g in range(5):
            for kc in range(8):
                rec.op("pe", lambda e, g=g, kc=kc: e.matmul(psG[g][:, 0:GW[g]], lhsT=hT[:, kc, :], rhs=win[:, kc, GOFF[g]:GOFF[g] + GW[g]],
                                                            start=(kc == 0), stop=(kc == 7)), reads=[hT, win], writes=[psG[g]])
        if DBG["stop"] <= 3:
            return
        r = rope_ops(rec, "dve", psG[0], psG[0][:, 0:512], 16, 32, tab, c32, s32, t1b, t2b)
        rec.op("act", lambda e, r=r: e.activation(out=qk[:, :, 0:256], in_=r.rearrange("p (a c) -> p a c", a=2), func=AF.Copy),
               reads=[t1b], writes=[qk])
        if DBG["stop"] <= 4:
            return
        rec.op("act", lambda e: e.activation(out=sqt[:, :], in_=psG[1][:, :], func=AF.Square), reads=[psG[1]], writes=[sqt])
        rec.op("dve", lambda e: e.tensor_reduce(out=ss8[:, :], in_=sqt[:, :].rearrange("p (v r) -> p v r", v=8), axis=AX.X, op=ALU.add),
               reads=[sqt], writes=[ss8])
        rec.op("act", lambda e: e.activation(out=ss8[:, :], in_=ss8[:, :], func=AF.Sqrt, bias=L.eps[:, 0:1], scale=1.0 / 64.0),
               reads=[ss8, L.eps], writes=[ss8])
        rec.op("dve", lambda e: e.reciprocal(out=rs8[:, :], in_=ss8[:, :]), reads=[ss8], writes=[rs8])
        rec.op("dve", lambda e: e.memset(rs8[:, 6:8], 1.0), writes=[rs8])
        rec.op("dve", lambda e: e.tensor_tensor(out=xg[:, :], in0=psG[1][:, :], in1=G1t[:, :], op=ALU.mult), reads=[psG[1], G1t], writes=[xg])
        r = rope_ops(rec, "dve", xg, xg[:, 0:512], 8, 64, tab, c64, s64, t1b, t2b)
        r3 = r.rearrange("p (v r) -> p v r", v=8)
        rec.op("dve", lambda e, r3=r3: e.tensor_tensor(out=qk[:, 0, 256:512].rearrange("p (v r) -> p v r", v=4), in0=r3[:, 0:4, :],
                                                in1=rs8[:, 0:4].unsqueeze(2).to_broadcast([128, 4, 64]), op=ALU.mult),
               reads=[t1b, rs8], writes=[qk])
        for (lo, dst0) in ((4, 256), (6, 1024)):
            rec.op("dve", lambda e, lo=lo, dst0=dst0, r3=r3: e.tensor_tensor(
                out=qk[:, 1, dst0:dst0 + 256].rearrange("p (g d r) -> p g d r", g=2, d=2),
                in0=r3[:, lo:lo + 2, :].unsqueeze(2).to_broadcast([128, 2, 2, 64]),
                in1=rs8[:, lo:lo + 2].unsqueeze(2).unsqueeze(3).to_broadcast([128, 2, 2, 64]), op=ALU.mult),
                reads=[t1b, rs8], writes=[qk])
        if DBG["stop"] <= 5:
            return
        r = rope_ops(rec, "dve", psG[2], psG[2][:, 0:256], 4, 64, tab, c64, s64, t1b, t2b)
        rec.op("act", lambda e, r=r: e.activation(out=qk[:, 0, 1024:1280], in_=r, func=AF.Copy), reads=[t1b], writes=[qk])
        r = rope_ops(rec, "dve", psG[2], psG[2][:, 256:288], 1, 32, tab, c32, s32, t1b, t2b)
        rec.op("dve", lambda e, r=r: e.tensor_copy(out=qk[:, 1, 512:1024].rearrange("p (h c) -> p h c", h=4)[:, :, 64:96],
                                              in_=r.unsqueeze(1).to_broadcast([128, 4, 32])), reads=[t1b], writes=[qk])
        rec.op("act", lambda e: e.activation(out=vt[:, 0:4, 0:64], in_=psG[3][:, 0:256].rearrange("p (h d) -> p h d", h=4), func=AF.Copy),
               reads=[psG[3]], writes=[vt])
        rec.op("act", lambda e: e.activation(out=vt[:, 4:6, 0:64], in_=psG[3][:, 256:384].rearrange("p (h d) -> p h d", h=2), func=AF.Copy),
               reads=[psG[3]], writes=[vt])
        rec.op("act", lambda e: e.activation(out=vt[:, 10:12, 0:64], in_=psG[3][:, 384:512].rearrange("p (h d) -> p h d", h=2), func=AF.Copy),
               reads=[psG[3]], writes=[vt])
        if DBG["stop"] <= 6:
            return
        rec.op("act", lambda e: e.activation(out=sqt[:, 0:320], in_=psG[4][:, 0:320], func=AF.Square), reads=[psG[4]], writes=[sqt])
        rec.op("dve", lambda e: e.tensor_reduce(out=ssc[:, 0:1], in_=sqt[:, 0:192], axis=AX.X, op=ALU.add), reads=[sqt], writes=[ssc])
        rec.op("dve", lambda e: e.tensor_reduce(out=ssc[:, 1:2], in_=sqt[:, 192:320], axis=AX.X, op=ALU.add), reads=[sqt], writes=[ssc])
        rec.op("act", lambda e: e.activation(out=ssc[:, 0:1], in_=ssc[:, 0:1], func=AF.Sqrt, bias=L.eps[:, 0:1], scale=1.0 / 192.0),
               reads=[ssc, L.eps], writes=[ssc])
        rec.op("act", lambda e: e.activation(out=ssc[:, 1:2], in_=ssc[:, 1:2], func=AF.Sqrt, bias=L.eps[:, 0:1], scale=1.0 / 128.0),
               reads=[ssc, L.eps], writes=[ssc])
        rec.op("dve", lambda e: e.reciprocal(out=rsc[:, :], in_=ssc[:, :]), reads=[ssc], writes=[rsc])
        for (lo, hi, j) in ((0, 192, 0), (192, 320, 1)):
            rec.op("dve", lambda e, lo=lo, hi=hi, j=j: e.scalar_tensor_tensor(out=cn[:, lo:hi], in0=psG[4][:, lo:hi], scalar=rsc[:, j:j + 1],
                                                                             in1=Gct[:, lo:hi], op0=ALU.mult, op1=ALU.mult),
                   reads=[psG[4], rsc, Gct], writes=[cn])
        if DBG["stop"] <= 6.2:
            return
        for j, (lo, hi) in enumerate(((0, 128), (128, 192), (192, 320))):
            rec.op("pe", lambda e, j=j, lo=lo, hi=hi: e.transpose(out=psT[0:hi - lo, j * 128:(j + 1) * 128], in_=cn[:, lo:hi], identity=L.ident_b[:, :]),
                   reads=[cn, L.ident_b], writes=[psT])
        rec.op("dve", lambda e: e.tensor_copy(out=cnT[:, 0, :], in_=psT[:, 0:128]), reads=[psT], writes=[cnT])
        rec.op("dve", lambda e: e.tensor_copy(out=cnT[0:64, 1, :], in_=psT[0:64, 128:256]), reads=[psT], writes=[cnT])
        rec.op("dve", lambda e: e.tensor_copy(out=cnT[:, 2, :], in_=psT[:, 256:384]), reads=[psT], writes=[cnT])
        if DBG["stop"] <= 6.4:
            return
        rec.op("pe", lambda e: e.matmul(psU[0][:, 0:384], lhsT=cnT[:, 0, :], rhs=wuq[:, 0, :], start=True, stop=False), reads=[cnT, wuq], writes=[psU[0]])
        rec.op("pe", lambda e: e.matmul(psU[0][:, 0:384], lhsT=cnT[0:64, 1, :], rhs=wuq[0:64, 1, :], start=False, stop=True), reads=[cnT, wuq], writes=[psU[0]])
        rec.op("pe", lambda e: e.matmul(psU[1][:, 0:512], lhsT=cnT[:, 2, :], rhs=wukv[:, :], start=True, stop=True), reads=[cnT, wukv], writes=[psU[1]])
        if DBG["stop"] <= 6.6:
            return
        qc = psU[0][:, 0:384].rearrange("p (h c) -> p h c", h=4)
        kvc = psU[1][:, 0:512].rearrange("p (h c) -> p h c", h=4)
        qdst = qk[:, 0, 512:1024].rearrange("p (h c) -> p h c", h=4)
        kdst = qk[:, 1, 512:1024].rearrange("p (h c) -> p h c", h=4)
        rec.op("act", lambda e: e.activation(out=qdst[:, :, 0:64], in_=qc[:, :, 0:64], func=AF.Copy), reads=[psU[0]], writes=[qk])
        rec.op("act", lambda e: e.activation(out=kdst[:, :, 0:64], in_=kvc[:, :, 0:64], func=AF.Copy), reads=[psU[1]], writes=[qk])
        rec.op("act", lambda e: e.activation(out=vt[:, 6:10, 0:64], in_=kvc[:, :, 64:128], func=AF.Copy), reads=[psU[1]], writes=[vt])
        if DBG["stop"] <= 6.8:
            return
        rec.op("act", lambda e: e.activation(out=xg[:, 0:128].rearrange("p (h c) -> p h c", h=4), in_=qc[:, :, 64:96], func=AF.Copy), reads=[psU[0]], writes=[xg])
        if DBG["stop"] <= 6.85:
            return
        r = rope_ops(rec, "dve", xg, xg[:, 0:128], 4, 32, tab, c32, s32, t1b, t2b)
        if DBG["stop"] <= 6.9:
            return
        rec.op("dve", lambda e, r=r: e.tensor_copy(out=qdst[:, :, 64:96], in_=r.rearrange("p (h c) -> p h c", h=4)), reads=[t1b], writes=[qk])
        if DBG["stop"] <= 7:
            return
        for a in range(2):
            for (c0, c1) in ((0, 8), (8, 10)):
                for c in range(c0, c1):
                    rec.op("pe", lambda e, a=a, c=c, c0=c0: e.transpose(out=psT[:, (c - c0) * 128:(c - c0 + 1) * 128], in_=qk[:, a, c * 128:(c + 1) * 128],
                                                                        identity=L.ident_b[:, :]), reads=[qk, L.ident_b], writes=[psT])
                eng = "act" if a == 0 else "dve"
                if eng == "act":
                    rec.op("act", lambda e, a=a, c0=c0, c1=c1: e.activation(out=qkt[:, a, c0:c1, col:col + 128],
                                                                          in_=psT[:, 0:(c1 - c0) * 128].rearrange("p (c t) -> p c t", t=128), func=AF.Copy),
                           reads=[psT], writes=[qkt])
                else:
                    rec.op("dve", lambda e, a=a, c0=c0, c1=c1: e.tensor_copy(out=qkt[:, a, c0:c1, col:col + 128],
                                                                           in_=psT[:, 0:(c1 - c0) * 128].rearrange("p (c t) -> p c t", t=128)),
                           reads=[psT], writes=[qkt])
        if DBG["stop"] <= 8:
            return
        rec.dma("sp", vdst_ap, vt[:, :, :], reads=[vt])

    if DBG["stop"] <= 0:
        return
    for blk in range(n_lat_tiles // 4):
        qkt = qkT.next()
        for j in range(4):
            t = blk * 4 + j
            tile(x_src[t * 128:(t + 1) * 128, :], x_src, tab_src[t * 128:(t + 1) * 128, :], 0, qkt, j * 128,
                 outs["v"][:, :, t, :].rearrange("s p d -> p s d"), outs["v"])
        rec.dma("sp", outs["qT"][:, :, blk * 512:(blk + 1) * 512].rearrange("c p t -> p c t"), qkt[:, 0, :, :], reads=[qkt])
        rec.dma("sp", outs["kT"][:, :, blk * 512:(blk + 1) * 512].rearrange("c p t -> p c t"), qkt[:, 1, :, :], reads=[qkt])
    if do_ctx:
        qkt = qkT.next()
        for t in range(2):
            tile(xc_src[t * 128:(t + 1) * 128, :], xc_src, tab_src[TOK + t * 128:TOK + (t + 1) * 128, :], 1, qkt, t * 128,
                 outs["vc"][:, :, t, :].rearrange("s p d -> p s d"), outs["vc"])
        rec.dma("sp", outs["qTc"][:, :, :].rearrange("c p t -> p c t"), qkt[:, 0, :, 0:256], reads=[qkt])
        rec.dma("sp", outs["kTc"][:, :, :].rearrange("c p t -> p c t"), qkt[:, 1, :, 0:256], reads=[qkt])


def _pm(w, kc):
    k, n = w.shape
    return np.ascontiguousarray(w.reshape(kc, 128, n).transpose(1, 0, 2))


def prep_layer(inp, l):
    f = np.float32
    d = {}
    d["w_ada"] = _pm(inp["w_ada"][l], 8)
    d["b_adaT"] = np.ascontiguousarray(inp["b_ada"][l].reshape(48, 128).T)
    d["b_ada"] = np.ascontiguousarray(inp["b_ada"][l])
    d["w_in"] = _pm(inp["w_in"][l][:, _in_perm()], 8)
    wuq = np.zeros((256, 384), f)
    wuq[:192] = inp["mla_w_uq"][l]
    d["w_uq"] = _pm(wuq, 2)
    d["w_ukv"] = np.ascontiguousarray(inp["mla_w_ukv"][l])
    d["gain_g1"] = np.concatenate([np.tile(inp["gqa_q_norm_g"][l], 4), np.tile(inp["gqa_k_norm_g"][l], 2), np.ones(128, f)]).astype(f)
    d["gain_c"] = np.concatenate([inp["mla_q_norm_g"][l], inp["mla_kv_norm_g"][l]]).astype(f)
    return d


def prep_core_common(inp, core):
    b = core // 4
    r = core % 4
    d = {}
    cc = np.stack([inp["c"][b], inp["c_ctx"]], 1)
    d["cT"] = _pm(cc, 8)
    tok = np.arange(r * TOK, (r + 1) * TOK)
    tab = rope_tables(tok)
    ctab = np.zeros((CTX, 192), np.float32)
    ctab[:, 0:32] = 1.0
    ctab[:, 64:128] = 1.0
    d["rope_tab"] = np.ascontiguousarray(np.concatenate([tab, ctab], 0))
    d["c_ident"] = np.eye(128, dtype=np.float32)
    return d


NKT = 2 + 4 * NT
NWIN = NT + 2


def attention(cx, L, st, l, src, need_ctx, n_qb=TOK // 512, kt_limit=None):
    rec = cx.rec
    lam_init = 0.8 - 0.6 * float(np.exp(-0.3 * l))
    lamv_d = cx.din("lamv", [4, 32])
    subg_d = cx.din("subln_g", [64, 1])
    sink_d = cx.din("swa_sink", [4])
    mask_d = cx.din("c_masks", [128, 4, 128])
    lam = rec.sb("lam", [128, 1], F32, st)
    gsub = rec.sb("gsub", [64, 1], F32, st)
    esink = rec.sb("esink", [128, 4], F32, st)
    masks = rec.sb("masks", [128, 4, 128], BF16, st)
    lv = rec.sb("lv", [128, 4, 32], F32, st)
    lp = rec.sb("lp", [128, 2, 32], F32, st)
    ls = rec.sb("ls", [128, 2], F32, st)
    kts = Ring([rec.sb("ktb", [128, NKT * 128], BF16, st) for _ in range(2)])
    vtsr = Ring([rec.sb("vtb", [128, NKT, 128], BF16, st) for _ in range(2)])
    qts = Ring([rec.sb("qtb", [128, TOK], BF16, st) for _ in range(2)])
    qtc = rec.sb("qtc", [128, 10, 256], BF16, st)
    pTs = Ring([rec.sb("pT", [128, 1024], BF16, st) for _ in range(4)])
    zss = Ring([rec.sb("zs", [64, 512], F32, st) for _ in range(2)])
    rzs = Ring([rec.sb("rz", [64, 512], F32, st) for _ in range(2)])
    fa = rec.sb("fa", [64, 512], F32, st)
    fb = rec.sb("fb", [64, 512], F32, st)
    fc = rec.sb("fc", [64, 512], F32, st)
    ots = Ring([rec.sb("ot", [64, 512], BF16, st) for _ in range(2)])
    qmask = [[Ring([rec.sb("qm", [128, 512], BF16, st) for _ in range(2)]) for _ in range(2)] for _ in range(2)]
    for hp in range(2):
        for cp in range(2):
            for b_ in qmask[hp][cp].bufs:
                rec.op("pool", lambda e, b_=b_: e.memset(b_[:, :], 0.0), writes=[b_])
    if "kTwin" not in src:
        oh_d = cx.din("onehot", [128, 8])
        onehot = rec.sb("onehot", [128, 8], F32, st)
        hck = rec.sb("hck", [128, 4, 128], BF16, st)
        hcv = rec.sb("hcv", [128, 4, 128], BF16, st)
        hacc = rec.sb("hacc", [128, 128], F32, st)
        rec.dma("sp", onehot[:, :], oh_d[:, :], writes=[onehot])
    Sr = Ring([rec.ps("S", [128, 1024], F32, st) for _ in range(3)])
    accs = Ring([rec.ps("acc", [128, 512], F32, st) for _ in range(2)])
    dummy = None
    ndummy = 0
    rec.dma("sp", lv[:, :, :].rearrange("p a b -> p (a b)"), lamv_d[:, :].rearrange("a b -> (a b)").partition_broadcast(128), writes=[lv])
    rec.dma("sp", gsub[:, :], subg_d[:, :], writes=[gsub])
    rec.dma("sp", esink[:, :], sink_d[0:4].partition_broadcast(128), writes=[esink])
    rec.dma("pool", masks[:, :, :], mask_d[:, :, :], writes=[masks], cast=True)
    rec.op("dve", lambda e: e.tensor_tensor(out=lp[:, :, :], in0=lv[:, 0:4:2, :], in1=lv[:, 1:4:2, :], op=ALU.mult), reads=[lv], writes=[lp])
    rec.op("dve", lambda e: e.tensor_reduce(out=ls[:, :], in_=lp[:, :, :], axis=AX.X, op=ALU.add), reads=[lp], writes=[ls])
    rec.op("act", lambda e: e.activation(out=ls[:, :], in_=ls[:, :], func=AF.Exp), reads=[ls], writes=[ls])
    rec.op("act", lambda e: e.activation(out=esink[:, :], in_=esink[:, :], func=AF.Exp), reads=[esink], writes=[esink])
    rec.op("dve", lambda e: e.tensor_tensor(out=lam[:, :], in0=ls[:, 0:1], in1=ls[:, 1:2], op=ALU.subtract), reads=[ls], writes=[lam])
    rec.op("dve", lambda e: e.tensor_scalar(out=lam[:, :], in0=lam[:, :], scalar1=lam_init, scalar2=None, op0=ALU.add), reads=[lam], writes=[lam])
    rec.op("dve", lambda e: e.tensor_scalar(out=gsub[:, :], in0=gsub[:, :], scalar1=1.0 - lam_init, scalar2=None, op0=ALU.mult), reads=[gsub], writes=[gsub])
    if need_ctx:
        rec.dma("sp", qtc[:, :, :], src["qTc"][:, :, :].rearrange("c p t -> p c t"), writes=[qtc])

    def mm(out_ap, lhsT, rhs, start, stop, base, reads, writes):
        kw = {}
        if base == 96:
            kw["tile_position"] = (96, 0)
        rec.op("pe", lambda e: e.matmul(out_ap, lhsT=lhsT, rhs=rhs, start=start, stop=stop, skip_group_check=True, **kw), reads=reads, writes=writes)

    def finalize(accl, W, kind, h, dst_ap, sink_h=None):
        rzl = []
        zsl = []
        osl = []
        for a in accl:
            zs = zss.next()
            if sink_h is None:
                rec.op("dve", lambda e, a=a, zs=zs: e.tensor_scalar(out=zs[:, 0:W], in0=a[64:128, 0:W], scalar1=1.0, scalar2=None, op0=ALU.mult),
                       reads=[a], writes=[zs])
            else:
                rec.op("dve", lambda e, a=a, zs=zs: e.tensor_scalar(out=zs[:, 0:W], in0=a[64:128, 0:W], scalar1=esink[0:64, sink_h:sink_h + 1],
                                                                  scalar2=None, op0=ALU.add), reads=[a, esink], writes=[zs])
            zsl.append(zs)
            if kind == "A":
                ob = (fa, fb)[len(osl)]
                rec.op("dve", lambda e, a=a, ob=ob: e.tensor_scalar(out=ob[:, 0:W], in0=a[0:64, 0:W], scalar1=1.0, scalar2=None, op0=ALU.mult),
                       reads=[a], writes=[ob])
                osl.append(ob)
        for zs in zsl:
            rz = rzs.next()
            rec.op("dve", lambda e, zs=zs, rz=rz: e.reciprocal(out=rz[:, 0:W], in_=zs[:, 0:W]), reads=[zs], writes=[rz])
            rzl.append(rz)
        if kind == "A":
            accl = osl
        ot = ots.next()
        if kind != "A":
            a, rz = accl[0], rzl[0]
            rec.op("dve", lambda e: e.tensor_tensor(out=ot[:, 0:W], in0=a[0:64, 0:W], in1=rz[:, 0:W], op=ALU.mult), reads=[a, rz], writes=[ot])
        else:
            a1, a2 = accl
            r1, r2 = rzl
            rec.op("dve", lambda e: e.tensor_tensor(out=fa[:, 0:W], in0=fa[:, 0:W], in1=r1[:, 0:W], op=ALU.mult), reads=[r1], writes=[fa])
            rec.op("dve", lambda e: e.scalar_tensor_tensor(out=fb[:, 0:W], in0=fb[:, 0:W], scalar=lam[0:64, 0:1], in1=r2[:, 0:W],
                                                           op0=ALU.mult, op1=ALU.mult), reads=[r2, lam], writes=[fb])
            rec.op("dve", lambda e: e.tensor_tensor(out=fa[:, 0:W], in0=fa[:, 0:W], in1=fb[:, 0:W], op=ALU.subtract), reads=[fb], writes=[fa])
            rec.op("pool", lambda e: e.tensor_tensor(out=fc[:, 0:W], in0=fa[:, 0:W], in1=fa[:, 0:W], op=ALU.mult), reads=[fa], writes=[fc])
            pm = Sr.next()
            rec.op("pe", lambda e: e.matmul(pm[0:64, 0:W], lhsT=L.ones64[:, :], rhs=fc[:, 0:W], start=True, stop=True), reads=[fc, L.ones64], writes=[pm])
            rec.op("act", lambda e: e.activation(out=fb[:, 0:W], in_=pm[0:64, 0:W], func=AF.Sqrt, bias=L.eps[0:64, 0:1], scale=1.0), reads=[pm, L.eps], writes=[fb])
            rec.op("dve", lambda e: e.reciprocal(out=fc[:, 0:W], in_=fb[:, 0:W]), reads=[fb], writes=[fc])
            rec.op("dve", lambda e: e.scalar_tensor_tensor(out=ot[:, 0:W], in0=fa[:, 0:W], scalar=gsub[:, 0:1], in1=fc[:, 0:W],
                                                           op0=ALU.mult, op1=ALU.mult), reads=[fa, fc, gsub], writes=[ot])
        rec.dma("sp", dst_ap, ot[:, 0:W], reads=[ot])

    def attend(ktb, vtb, q_ap_fn, qbuf, comps, scale, ktiles, W, kind, h, dst_ap, sink_h=None):
        nu = len(comps)
        accl = [accs.next() for _ in range(nu)]
        if nu == 2:
            groups = [[(kt, 0), (kt, 1)] for kt in ktiles]
        else:
            groups = [[(kt, 0) for kt in ktiles[i:i + 2]] for i in range(0, len(ktiles), 2)]
        started = [False] * nu

        def qk(grp):
            S = Sr.next()
            for j, (kt, u) in enumerate(grp):
                base, K = comps[u]
                qap, qb_ = q_ap_fn(u, base, K)
                mm(S[:, j * W:(j + 1) * W], ktb[base:base + K, kt * 128:(kt + 1) * 128], qap, True, True, base, [ktb, qb_], [S])
            return S
        PD = DBG.get("pd", 2)
        Sq = [qk(groups[i]) for i in range(min(PD, len(groups)))]
        for gi, grp in enumerate(groups):
            if gi + PD < len(groups):
                Sq.append(qk(groups[gi + PD]))
            S = Sq.pop(0)
            P = pTs.next()
            n = len(grp) * W
            if DBG["att"] <= 1:
                continue
            rec.op("act", lambda e, S=S, P=P, n=n: e.activation(out=P[:, 0:n], in_=S[:, 0:n], func=AF.Exp, scale=scale), reads=[S], writes=[P])
            if DBG["att"] <= 2:
                continue
            for _ in range(ndummy if W == 512 else 0):
                rec.op("pe", lambda e: e.matmul(dummy[:, 0:128 * DBG.get("dumw", 2)], lhsT=L.ident_b[:, :], rhs=masks[:, 0:DBG.get("dumw", 2), :].rearrange("p a b -> p (a b)"), start=True, stop=True,
                                                skip_group_check=True), reads=[], writes=[])
            for j, (kt, u) in enumerate(grp):
                a = accl[u]
                last = (gi == len(groups) - 1) and (nu == 2 or j == len(grp) - 1)
                mm(a[:, 0:W], vtb[:, kt, :], P[:, j * W:(j + 1) * W], not started[u], last, 0, [vtb, P], [a])
                started[u] = True
        if DBG["att"] >= 4:
            finalize(accl, W, kind, h, dst_ap, sink_h)

    def kall(r, c):
        if "ga" in src:
            b_ = src["ga"][c]
            return b_[r * 128:(r + 1) * 128, :], [b_]
        return src["kTall"][r, c, :, :], []

    def vall(r, slot):
        if "ga" in src:
            b_ = src["ga"][10 + slot]
            return b_[r * 128:(r + 1) * 128, :].rearrange("p (t d) -> p t d", d=128), [b_]
        return src["vall"][r, slot, :, :, :], []

    def load_kv(c, slot):
        ktb = kts.next()
        vtb = vtsr.next()
        rec.dma("sp", ktb[:, 0:256], src["kTc"][c, :, :], writes=[ktb])
        rec.dma("sp", vtb[:, 0:2, :], src["vc"][slot, :, :, :], writes=[vtb])
        for r in range(4):
            ap_, rd = kall(r, c)
            rec.dma("sp", ktb[:, 256 + r * TOK:256 + (r + 1) * TOK], ap_, reads=rd, writes=[ktb])
            ap_, rd = vall(r, slot)
            rec.dma("sp", vtb[:, 2 + r * NT:2 + (r + 1) * NT, :], ap_, reads=rd, writes=[vtb])
        return ktb, vtb

    def load_v(slot):
        vtb = vtsr.next()
        rec.dma("sp", vtb[:, 0:2, :], src["vc"][slot, :, :, :], writes=[vtb])
        for r in range(4):
            ap_, rd = vall(r, slot)
            rec.dma("sp", vtb[:, 2 + r * NT:2 + (r + 1) * NT, :], ap_, reads=rd, writes=[vtb])
        return vtb

    ktiles_all = list(range(NKT)) if kt_limit is None else list(range(kt_limit))
    jobs = []
    for i in range(2):
        jobs.append((i, [(h, [((h % 2) * 64, 32), ((h % 2) * 64 + 32, 32)], h, h // 2, (h % 2) * 64) for h in (2 * i, 2 * i + 1)], 32 ** -0.5, "A"))
    for g in range(2):
        jobs.append((2 + g, [(h, [((h % 2) * 64, 64)], 4 + g, 2 + h // 2, (h % 2) * 64) for h in (2 * g, 2 * g + 1)], 64 ** -0.5, "B"))
    for h in range(4):
        jobs.append((4 + h, [(h, [(0, 96)], 6 + h, 4 + h // 2, (h % 2) * 64)], 96 ** -0.5, "C"))
    if DBG["att"] <= 0:
        return
    hjobs = []
    for (c, heads, scale, kind) in jobs:
        if kind not in DBG["kinds"]:
            continue
        prev_slot = None
        for hi, (h, comps, slot, oc, orow) in enumerate(heads):
            hjobs.append(dict(c=c, h=h, comps=comps, slot=slot, oc=oc, orow=orow, scale=scale, kind=kind,
                              ldk=(hi == 0), ldv=(slot != prev_slot)))
            prev_slot = slot
    state = {"ktb": None, "vtb": None, "qtb": None}

    def issue_loads(j):
        if j["ldk"]:
            qtb = qts.next()
            rec.dma("sp", qtb[:, :], src["qT"][j["c"], :, :], writes=[qtb])
            ktb = kts.next()
            rec.dma("sp", ktb[:, 0:256], src["kTc"][j["c"], :, :], writes=[ktb])
            for r in range(4):
                ap_, rd = kall(r, j["c"])
                rec.dma("sp", ktb[:, 256 + r * TOK:256 + (r + 1) * TOK], ap_, reads=rd, writes=[ktb])
            j["ktb"], j["qtb"] = ktb, qtb
        if j["ldv"]:
            j["vtb"] = load_v(j["slot"])

    if hjobs:
        issue_loads(hjobs[0])
    for ji, j in enumerate(hjobs):
        for k_ in ("ktb", "vtb", "qtb"):
            if k_ in j:
                state[k_] = j[k_]
        ktb, vtb, qtb = state["ktb"], state["vtb"], state["qtb"]
        if ji + 1 < len(hjobs):
            issue_loads(hjobs[ji + 1])
        c, h, comps, oc, orow, scale, kind = j["c"], j["h"], j["comps"], j["oc"], j["orow"], j["scale"], j["kind"]

        def masked_q(src_fn, src_buf, W):
            bl = []
            for cp in range(2):
                mb = qmask[h % 2][cp].next()
                rows = (h % 2) * 64 + cp * 32
                rec.op("pool", lambda e, mb=mb, rows=rows: e.tensor_copy(out=mb[rows:rows + 32, 0:W], in_=src_fn(rows)), reads=[src_buf], writes=[mb])
                bl.append(mb)
            return bl
        def prep_q(qb):
            if kind == "A":
                return masked_q(lambda rows, qb=qb, qtb=qtb: qtb[rows:rows + 32, qb * 512:(qb + 1) * 512], qtb, 512)
            if kind == "B":
                mb = qmask[h % 2][0].next()
                r0 = (h % 2) * 64
                rec.op("pool", lambda e, mb=mb, r0=r0, qb=qb, qtb=qtb: e.tensor_copy(out=mb[r0:r0 + 64, 0:512], in_=qtb[r0:r0 + 64, qb * 512:(qb + 1) * 512]),
                       reads=[qtb], writes=[mb])
                return [mb]
            return None
        q_next = prep_q(0) if n_qb > 0 else None
        for qb in range(n_qb):
            bl = q_next
            if qb + 1 < n_qb:
                q_next = prep_q(qb + 1)
            if kind == "A":
                attend(ktb, vtb, lambda u, base, K, bl=bl: (bl[u][:, 0:512], bl[u]), None, [(0, 128), (0, 128)], scale, ktiles_all, 512,
                       kind, h, src["OT"][oc, orow:orow + 64, qb * 512:(qb + 1) * 512])
            elif kind == "B":
                attend(ktb, vtb, lambda u, base, K, bl=bl: (bl[0][:, 0:512], bl[0]), None, [(0, 128)], scale, ktiles_all, 512,
                       kind, h, src["OT"][oc, orow:orow + 64, qb * 512:(qb + 1) * 512])
            else:
                attend(ktb, vtb, lambda u, base, K, qb=qb, qtb=qtb: (qtb[0:128, qb * 512:(qb + 1) * 512], qtb), None, [(0, 128)], scale, ktiles_all, 512,
                       kind, h, src["OT"][oc, orow:orow + 64, qb * 512:(qb + 1) * 512])
        if need_ctx:
            if kind == "A":
                bl = masked_q(lambda rows, c=c: qtc[rows:rows + 32, c, :], qtc, 256)
                attend(ktb, vtb, lambda u, base, K, bl=bl: (bl[u][:, 0:256], bl[u]), None, [(0, 128), (0, 128)], scale, [0, 1], 256, kind, h,
                       src["OTc"][oc, orow:orow + 64, :])
            elif kind == "B":
                mb = qmask[h % 2][0].next()
                r0 = (h % 2) * 64
                rec.op("pool", lambda e, mb=mb, r0=r0, c=c: e.tensor_copy(out=mb[r0:r0 + 64, 0:256], in_=qtc[r0:r0 + 64, c, :]), reads=[qtc], writes=[mb])
                attend(ktb, vtb, lambda u, base, K, mb=mb: (mb[:, 0:256], mb), None, [(0, 128)], scale, [0, 1], 256, kind, h,
                       src["OTc"][oc, orow:orow + 64, :])
            else:
                attend(ktb, vtb, lambda u, base, K, c=c: (qtc[0:128, c, :], qtc), None, [(0, 128)], scale, [0, 1], 256, kind, h,
                       src["OTc"][oc, orow:orow + 64, :])
    for g in range(2 if "D" in DBG["kinds"] else 0):
        c = 8 + g
        slot = 10 + g
        qtb = qts.next()
        rec.dma("sp", qtb[:, :], src["qT"][c, :, :], writes=[qtb])
        ktb = kts.next()
        vtb = vtsr.next()
        rec.dma("sp", ktb[:, 0:256], src["kTc"][c, :, :], writes=[ktb])
        rec.dma("sp", vtb[:, 0:2, :], src["vc"][slot, :, :, :], writes=[vtb])
        if "kTwin" in src:
            rec.dma("sp", ktb[:, 256:256 + NWIN * 128], src["kTwin"][g, :, :], writes=[ktb])
            rec.dma("sp", vtb[:, 2:2 + NWIN, :], src["vwin"][g, :, :, :], writes=[vtb])
        else:
            rec.dma("sp", ktb[:, 384:384 + TOK], src["kTown"][c, :, :], writes=[ktb])
            rec.dma("sp", vtb[:, 3:3 + NT, :], src["vown"][slot, :, :, :], writes=[vtb])
            for side in range(2):
                kcol = (TOK - 128) if side == 0 else 0
                vt_i = (NT - 1) if side == 0 else 0
                for r_ in range(4):
                    ap_, rd = kall(r_, c)
                    rec.dma("sp", hck[:, r_, :], ap_[:, kcol:kcol + 128], reads=rd, writes=[hck])
                    ap_, rd = vall(r_, slot)
                    rec.dma("sp", hcv[:, r_, :], ap_[:, vt_i, :], reads=rd, writes=[hcv])
                kd = ktb[:, 256:384] if side == 0 else ktb[:, 384 + TOK:384 + TOK + 128]
                vd = vtb[:, 2, :] if side == 0 else vtb[:, 3 + NT, :]
                for (cand, cb, dst_ap, dst_b) in ((hck, hck, kd, ktb), (hcv, hcv, vd, vtb)):
                    rec.op("dve", lambda e, cand=cand, side=side: e.tensor_scalar(out=hacc[:, :], in0=cand[:, 0, :], scalar1=onehot[:, side * 4:side * 4 + 1],
                                                                               scalar2=None, op0=ALU.mult), reads=[cb, onehot], writes=[hacc])
                    for r_ in range(1, 4):
                        last = r_ == 3
                        rec.op("dve", lambda e, cand=cand, side=side, r_=r_, last=last, dst_ap=dst_ap: e.scalar_tensor_tensor(
                            out=dst_ap if last else hacc[:, :], in0=cand[:, r_, :], scalar=onehot[:, side * 4 + r_:side * 4 + r_ + 1], in1=hacc[:, :],
                            op0=ALU.mult, op1=ALU.add), reads=[cb, onehot, hacc], writes=[dst_b] if last else [hacc])
        for h in (2 * g, 2 * g + 1):
            base = (h % 2) * 64
            def d_qk(j):
                tiles = [0, 1, 2 + j, 3 + j, 4 + j]
                S = Sr.next()
                for i, kt in enumerate(tiles):
                    mm(S[:, i * 128:(i + 1) * 128], ktb[base:base + 64, kt * 128:(kt + 1) * 128], qtb[base:base + 64, j * 128:(j + 1) * 128],
                       True, True, base, [ktb, qtb], [S])
                return S
            for qb in range(n_qb):
                acc = accs.next()
                S_next = d_qk(qb * 4)
                for s in range(4):
                    j = qb * 4 + s
                    tiles = [0, 1, 2 + j, 3 + j, 4 + j]
                    S = S_next
                    if s + 1 < 4:
                        S_next = d_qk(j + 1)
                    P = pTs.next()
                    rec.op("act", lambda e, S=S, P=P: e.activation(out=P[:, 0:640], in_=S[:, 0:640], func=AF.Exp, scale=64 ** -0.5), reads=[S], writes=[P])
                    mp = 2 if j == 0 else 0
                    mn = 3 if j == NT - 1 else 1
                    rec.op("pool", lambda e, P=P, mp=mp: e.tensor_tensor(out=P[:, 256:384], in0=P[:, 256:384], in1=masks[:, mp, :], op=ALU.mult),
                           reads=[masks], writes=[P])
                    rec.op("pool", lambda e, P=P, mn=mn: e.tensor_tensor(out=P[:, 512:640], in0=P[:, 512:640], in1=masks[:, mn, :], op=ALU.mult),
                           reads=[masks], writes=[P])
                    for i, kt in enumerate(tiles):
                        mm(acc[:, s * 128:(s + 1) * 128], vtb[:, kt, :], P[:, i * 128:(i + 1) * 128], i == 0, i == 4, 0, [vtb, P], [acc])
                finalize([acc], 512, "D", h, src["OT"][6 + h // 2, base:base + 64, qb * 512:(qb + 1) * 512], sink_h=h)
            if need_ctx:
                attend(ktb, vtb, lambda u, b_, K, c=c: (qtc[b_:b_ + K, c, :], qtc), None, [(base, 64)], 64 ** -0.5, [0, 1], 256, "D", h,
                       src["OTc"][6 + h // 2, base:base + 64, :], sink_h=h)


def phase3(cx, L, st, x_srcs, wts):
    rec = cx.rec
    wout = rec.sb("wout", [128, 8, 1024], BF16, st)
    rw = rec.sb("rw", [128, 8, 16], F32, st)
    rbias = rec.sb("rbias", [128, 16], F32, st)
    lnt = [rec.sb("lnt", [128, 1024], F32, st) for _ in range(4)]
    pre = wts.get("bf16", False)
    wq = "sp" if pre else "pool"
    for kc in range(8):
        rec.dma(wq, wout[:, kc, :], wts["w_out"][:, kc, :], writes=[wout], cast=not pre)
    rec.dma("sp", rw[:, :, :], wts["router_w"][:, :, :], writes=[rw])
    rec.dma("sp", rbias[:, :], wts["router_bias"][0:16].partition_broadcast(128), writes=[rbias])
    for i in range(4):
        rec.dma("sp", lnt[i][:, :], wts["ln"][i, :].partition_broadcast(128), writes=[lnt[i]])
    x1s = [rec.sb("x1s", [128, 1024], F32, st) for _ in range(4)]
    xts = Ring([rec.sb("xt3", [128, 1024], F32, st) for _ in range(2)])
    u = rec.sb("u", [128, 1024], F32, st)
    tmp = rec.sb("tmp", [128, 1024], F32, st)
    h2Tf = rec.sb("h2Tf", [128, 8, 128], F32, st)
    h2T = rec.sb("h2T", [128, 8, 512], BF16, st)
    otin = rec.sb("otin", [128, 8, 512], BF16, st)
    actT = rec.sb("actT", [128, 16, 2, 512], BF16, st)
    wgus = Ring([rec.sb("wgu", [128, 8, 512], BF16, st) for _ in range(3)])
    wds = Ring([rec.sb("wd", [128, 2, 1024], BF16, st) for _ in range(4)])
    gates = rec.sb("gates", [128, 4, 16], F32, st)
    st6 = rec.sb("st6b", [128, 2, 6], F32, st)
    mv = rec.sb("mvb", [128, 2], F32, st)
    sq1 = rec.sb("sq1b", [128, 1], F32, st)
    rstd = rec.sb("rstdb", [128, 1], F32, st)
    s16 = rec.sb("s16", [128, 16], F32, st)
    sel = rec.sb("sel", [128, 16], F32, st)
    sel2 = rec.sb("sel2", [128, 16], F32, st)
    eq = rec.sb("eq", [128, 16], F32, st)
    m1 = rec.sb("m1", [128, 4], F32, st)
    m2 = rec.sb("m2", [128, 4], F32, st)
    gs = rec.sb("gs", [128, 4], F32, st)
    gm = rec.sb("gm", [128, 1], F32, st)
    sil = Ring([rec.sb("sil", [128, 256], F32, st) for _ in range(2)])
    actb = Ring([rec.sb("actb", [128, 256], BF16, st) for _ in range(2)])
    B = [rec.ps("B", [128, 512], F32, st) for _ in range(8)]
    psTb = rec.buf("psTb", B[7].t[:, :].bitcast(BF16))

    def ln_stats(src):
        for i in range(2):
            rec.op("dve", lambda e, i=i: e.bn_stats(out=st6[:, i, :], in_=src[:, i * 512:(i + 1) * 512]), reads=[src], writes=[st6])
        rec.op("dve", lambda e: e.bn_aggr(out=mv[:, :], in_=st6[:, :, :].rearrange("p a b -> p (a b)")), reads=[st6], writes=[mv])
        rec.op("act", lambda e: e.activation(out=sq1[:, :], in_=mv[:, 1:2], func=AF.Sqrt, bias=L.eps[:, 0:1], scale=1.0), reads=[mv, L.eps], writes=[sq1])
        rec.op("dve", lambda e: e.reciprocal(out=rstd[:, :], in_=sq1[:, :]), reads=[sq1], writes=[rstd])

    def gated_ln(x_in, ybanks, Gt, gt, bt, dst):
        for hh in range(2):
            rec.op("dve", lambda e, hh=hh: e.tensor_tensor(out=tmp[:, hh * 512:(hh + 1) * 512], in0=ybanks[hh][:, :], in1=Gt[:, hh * 512:(hh + 1) * 512],
                                                           op=ALU.mult), reads=[ybanks[hh], Gt], writes=[tmp])
        rec.op("dve", lambda e: e.scalar_tensor_tensor(out=u[:, :], in0=x_in[:, :], scalar=ALPHA, in1=tmp[:, :], op0=ALU.mult, op1=ALU.add),
               reads=[x_in, tmp], writes=[u])
        ln_stats(u)
        rec.op("dve", lambda e: e.tensor_scalar(out=u[:, :], in0=u[:, :], scalar1=mv[:, 0:1], scalar2=rstd[:, 0:1], op0=ALU.subtract, op1=ALU.mult),
               reads=[mv, rstd], writes=[u])
        rec.op("dve", lambda e: e.tensor_tensor(out=u[:, :], in0=u[:, :], in1=gt[:, :], op=ALU.mult), reads=[gt], writes=[u])
        rec.op("dve", lambda e: e.tensor_tensor(out=dst[:, :], in0=u[:, :], in1=bt[:, :], op=ALU.add), reads=[u, bt], writes=[dst])

    for (x_src, OT, x_dst, cond, ntok) in x_srcs:
        nblk = (ntok + 511) // 512
        for blk in range(nblk):
            nt = min(4, (ntok - blk * 512) // 128)
            ncol = nt * 128
            rec.dma("sp", otin[:, :, 0:ncol], OT[:, :, blk * 512:blk * 512 + ncol].rearrange("c p t -> p c t"), writes=[otin])
            for t in range(nt):
                xt = xts.next()
                r0 = blk * 512 + t * 128
                rec.dma("sp", xt[:, :], x_src[r0:r0 + 128, :], writes=[xt])
                for hh in range(2):
                    for kc in range(8):
                        rec.op("pe", lambda e, hh=hh, kc=kc, t=t: e.matmul(B[hh][:, :], lhsT=otin[:, kc, t * 128:(t + 1) * 128],
                                                                       rhs=wout[:, kc, hh * 512:(hh + 1) * 512], start=(kc == 0), stop=(kc == 7)),
                               reads=[otin, wout], writes=[B[hh]])
                x1 = x1s[t]
                gated_ln(xt, B[0:2], L.Gb[0][cond], lnt[0], lnt[1], x1)
                ln_stats(x1)
                rec.op("dve", lambda e, x1=x1: e.tensor_scalar(out=tmp[:, :], in0=x1[:, :], scalar1=mv[:, 0:1], scalar2=rstd[:, 0:1],
                                                               op0=ALU.subtract, op1=ALU.mult), reads=[x1, mv, rstd], writes=[tmp])
                for kc in range(8):
                    bk = B[2 + kc // 4]
                    rec.op("pe", lambda e, kc=kc, bk=bk: e.transpose(out=bk[:, (kc % 4) * 128:(kc % 4 + 1) * 128], in_=tmp[:, kc * 128:(kc + 1) * 128],
                                                                     identity=L.ident_f[:, :]), reads=[tmp, L.ident_f], writes=[bk])
                for kc in range(8):
                    bk = B[2 + kc // 4]
                    rec.op("dve", lambda e, kc=kc, bk=bk, cond=cond: e.tensor_scalar(out=h2Tf[:, kc, :], in0=bk[:, (kc % 4) * 128:(kc % 4 + 1) * 128],
                                                                          scalar1=L.modT[:, 32 + kc, cond:cond + 1], scalar2=L.modT[:, 24 + kc, cond:cond + 1],
                                                                          op0=ALU.mult, op1=ALU.add), reads=[bk, L.modT], writes=[h2Tf])
                rec.op("pool", lambda e, t=t: e.tensor_copy(out=h2T[:, :, t * 128:(t + 1) * 128], in_=h2Tf[:, :, :]), reads=[h2Tf], writes=[h2T])
                for kc in range(8):
                    rec.op("pe", lambda e, kc=kc: e.matmul(B[4][:, 0:16], lhsT=h2Tf[:, kc, :], rhs=rw[:, kc, :], start=(kc == 0), stop=(kc == 7)),
                           reads=[h2Tf, rw], writes=[B[4]])
                rec.op("act", lambda e: e.activation(out=s16[:, :], in_=B[4][:, 0:16], func=AF.Sigmoid), reads=[B[4]], writes=[s16])
                v44 = lambda b_: b_[:, :].rearrange("p (g k) -> p g k", g=4)
                bc4 = lambda b_: b_[:, :].unsqueeze(2).to_broadcast([128, 4, 4])
                rec.op("dve", lambda e: e.tensor_tensor(out=sel[:, :], in0=s16[:, :], in1=rbias[:, :], op=ALU.add), reads=[s16, rbias], writes=[sel])
                rec.op("dve", lambda e: e.tensor_reduce(out=m1[:, :], in_=v44(sel), axis=AX.X, op=ALU.max), reads=[sel], writes=[m1])
                rec.op("dve", lambda e: e.tensor_tensor(out=v44(eq), in0=v44(sel), in1=bc4(m1), op=ALU.is_equal), reads=[sel, m1], writes=[eq])
                rec.op("dve", lambda e: e.scalar_tensor_tensor(out=sel2[:, :], in0=eq[:, :], scalar=-1e9, in1=sel[:, :], op0=ALU.mult, op1=ALU.add),
                       reads=[eq, sel], writes=[sel2])
                rec.op("dve", lambda e: e.tensor_reduce(out=m2[:, :], in_=v44(sel2), axis=AX.X, op=ALU.max), reads=[sel2], writes=[m2])
                rec.op("dve", lambda e: e.tensor_tensor(out=gs[:, :], in0=m1[:, :], in1=m2[:, :], op=ALU.add), reads=[m1, m2], writes=[gs])
                rec.op("dve", lambda e: e.tensor_reduce(out=gm[:, :], in_=gs[:, :], axis=AX.X, op=ALU.max), reads=[gs], writes=[gm])
                rec.op("dve", lambda e: e.tensor_scalar(out=gs[:, :], in0=gs[:, :], scalar1=gm[:, 0:1], scalar2=None, op0=ALU.is_equal), reads=[gm], writes=[gs])
                rec.op("dve", lambda e: e.tensor_tensor(out=v44(eq), in0=v44(sel), in1=bc4(m2), op=ALU.is_ge), reads=[sel, m2], writes=[eq])
                rec.op("dve", lambda e: e.tensor_tensor(out=v44(eq), in0=v44(eq), in1=bc4(gs), op=ALU.mult), reads=[gs], writes=[eq])
                rec.op("dve", lambda e: e.tensor_tensor(out=eq[:, :], in0=eq[:, :], in1=s16[:, :], op=ALU.mult), reads=[s16], writes=[eq])
                rec.op("dve", lambda e: e.tensor_reduce(out=gm[:, :], in_=eq[:, :], axis=AX.X, op=ALU.add), reads=[eq], writes=[gm])
                rec.op("dve", lambda e: e.reciprocal(out=gm[:, :], in_=gm[:, :]), reads=[gm], writes=[gm])
                rec.op("dve", lambda e, t=t: e.tensor_scalar(out=gates[:, t, :], in0=eq[:, :], scalar1=gm[:, 0:1], scalar2=None, op0=ALU.mult),
                       reads=[eq, gm], writes=[gates])
            items = [(ex, t) for ex in range(NE) for t in range(nt)]
            cur_w = [None]

            def mm_part(ex, t):
                if t == 0:
                    cur_w[0] = wgus.next()
                    rec.dma(wq, cur_w[0][:, :, :], wts["wgu"][ex, :, :, :], writes=[cur_w[0]], cast=not pre)
                wgu = cur_w[0]
                bk = B[(ex * nt + t) % 4]
                for kc in range(8):
                    rec.op("pe", lambda e, kc=kc, t=t, bk=bk, wgu=wgu: e.matmul(bk[:, :], lhsT=h2T[:, kc, t * 128:(t + 1) * 128], rhs=wgu[:, kc, :],
                                                                             start=(kc == 0), stop=(kc == 7)), reads=[h2T, wgu], writes=[bk])
                return bk

            def post_part(ex, t, bk):
                sl_ = sil.next()
                ab = actb.next()
                rec.op("act", lambda e, bk=bk, sl_=sl_: e.activation(out=sl_[:, :], in_=bk[:, 0:256], func=AF.Silu), reads=[bk], writes=[sl_])
                rec.op("dve", lambda e, bk=bk, sl_=sl_, ab=ab, t=t, ex=ex: e.scalar_tensor_tensor(out=ab[:, :], in0=sl_[:, :], scalar=gates[:, t, ex:ex + 1],
                                                                                                in1=bk[:, 256:512], op0=ALU.mult, op1=ALU.mult),
                       reads=[sl_, gates, bk], writes=[ab])
                for fc in range(2):
                    rec.op("pe", lambda e, fc=fc, ab=ab: e.transpose(out=psTb[:, fc * 128:(fc + 1) * 128], in_=ab[:, fc * 128:(fc + 1) * 128],
                                                                     identity=L.ident_b[:, :]), reads=[ab, L.ident_b], writes=[psTb, B[7]])
                rec.op("act", lambda e, ex=ex, t=t: e.activation(out=actT[:, ex, :, t * 128:(t + 1) * 128],
                                                                 in_=psTb[:, 0:256].rearrange("p (f c) -> p f c", f=2), func=AF.Copy),
                       reads=[psTb, B[7]], writes=[actT])

            bk_cur = mm_part(*items[0])
            for ii, (ex, t) in enumerate(items):
                bk_next = mm_part(*items[ii + 1]) if ii + 1 < len(items) else None
                post_part(ex, t, bk_cur)
                bk_cur = bk_next
            for ex in range(NE):
                wd = wds.next()
                rec.dma(wq, wd[:, :, :], wts["wd"][ex, :, :, :], writes=[wd], cast=not pre)
                for t in range(nt):
                    for hh in range(2):
                        for fc in range(2):
                            rec.op("pe", lambda e, ex=ex, t=t, hh=hh, fc=fc, wd=wd: e.matmul(
                                B[t * 2 + hh][:, :], lhsT=actT[:, ex, fc, t * 128:(t + 1) * 128], rhs=wd[:, fc, hh * 512:(hh + 1) * 512],
                                start=(ex == 0 and fc == 0), stop=(ex == NE - 1 and fc == 1)), reads=[actT, wd], writes=[B[t * 2 + hh], psTb] if t == 3 and hh == 1 else [B[t * 2 + hh]])
            for t in range(nt):
                r0 = blk * 512 + t * 128
                xo = xts.next()
                gated_ln(x1s[t], B[2 * t:2 * t + 2], L.Gb[1][cond], lnt[2], lnt[3], xo)
                rec.dma("sp", x_dst[r0:r0 + 128, :], xo[:, :], reads=[xo])


def build_A(n_lat_tiles=NT, do_ctx=True):
    cx = Ctx()
    L = NS()
    rec = cx.rec
    setup_consts(cx, L)
    st = ExitStack()
    layer_setup(cx, L, st)
    st.close()
    rec.barrier()
    x_src = cx.din("x_own", [TOK, 1024])
    xc_src = cx.din("xc", [CTX, 1024])
    tab_src = cx.din("rope_tab", [TOK + CTX, 192])
    outs = {"qT": cx.dout("qT", [10, 128, TOK], BF16), "kT": cx.dout("kT", [10, 128, TOK], BF16), "v": cx.dout("v", [12, 128, NT, 128], BF16),
            "qTc": cx.dout("qTc", [10, 128, CTX], BF16), "kTc": cx.dout("kTc", [10, 128, CTX], BF16), "vc": cx.dout("vc", [12, 128, 2, 128], BF16)}
    st = ExitStack()
    phase1(cx, L, st, x_src, xc_src, tab_src, outs, n_lat_tiles=n_lat_tiles, do_ctx=do_ctx)
    rec.wait_all_on("sp")
    rec.emit()
    return cx


def build_B(l, need_ctx):
    cx = Ctx()
    L = NS()
    rec = cx.rec
    setup_consts(cx, L)
    st = ExitStack()
    layer_setup(cx, L, st)
    st.close()
    rec.barrier()
    src = {"qT": cx.din("qT", [10, 128, TOK], BF16), "kTall": cx.din("kTall", [4, 10, 128, TOK], BF16),
           "vall": cx.din("vall", [4, 12, 128, NT, 128], BF16), "kTc": cx.din("kTc", [10, 128, CTX], BF16),
           "vc": cx.din("vc", [12, 128, 2, 128], BF16), "qTc": cx.din("qTc", [10, 128, CTX], BF16),
           "kTwin": cx.din("kTwin", [2, 128, NWIN * 128], BF16), "vwin": cx.din("vwin", [2, 128, NWIN, 128], BF16),
           "OT": (cx.dout if DBG.get("export") else cx.dint)("OT", [8, 128, TOK], BF16),
           "OTc": (cx.dout if DBG.get("export") else cx.dint)("OTc", [8, 128, CTX], BF16)}
    st = ExitStack()
    attention(cx, L, st, l, src, need_ctx)
    st.close()
    rec.barrier()
    wts = {"w_out": cx.din("w_out", [128, 8, 1024]), "wgu": cx.din("wgu", [NE, 128, 8, 512]), "wd": cx.din("wd", [NE, 128, 2, 1024]),
           "router_w": cx.din("router_w", [128, 8, 16]), "router_bias": cx.din("router_bias", [16]), "ln": cx.din("ln", [4, 1024])}
    x_src = cx.din("x_own", [TOK, 1024])
    x_dst = cx.dout("x_next", [TOK, 1024])
    xs = [(x_src, src["OT"], x_dst, 0, TOK)]
    if need_ctx:
        xc_src = cx.din("xc", [CTX, 1024])
        xc_dst = cx.dout("xc_next", [CTX, 1024])
        xs.append((xc_src, src["OTc"], xc_dst, 1, CTX))
    st = ExitStack()
    if DBG.get("p3", True):
        phase3(cx, L, st, xs, wts)
    else:
        rec.dma("sp", x_dst[0:128, :], x_src[0:128, :], key="D_dbg")
    rec.wait_all_on("sp")
    rec.emit()
    return cx


def prep_B_weights(inp, l):
    d = {}
    d["w_out"] = _pm(inp["w_out"][l], 8)
    d["wgu"] = np.ascontiguousarray(np.stack([_pm(np.concatenate([inp["exp_w_gate"][l, e], inp["exp_w_up"][l, e]], 1), 8) for e in range(NE)]))
    d["wd"] = np.ascontiguousarray(np.stack([_pm(inp["exp_w_down"][l, e], 2) for e in range(NE)]))
    d["router_w"] = _pm(inp["router_w"], 8)
    d["router_bias"] = np.ascontiguousarray(inp["router_bias"])
    d["ln"] = np.ascontiguousarray(np.stack([inp["ln1_g"][l], inp["ln1_b"][l], inp["ln2_g"][l], inp["ln2_b"][l]]))
    d["lamv"] = np.ascontiguousarray(np.stack([inp["diff_lambda_q1"][l], inp["diff_lambda_k1"][l], inp["diff_lambda_q2"][l], inp["diff_lambda_k2"][l]]))
    d["subln_g"] = np.ascontiguousarray(inp["diff_subln_g"][l].reshape(64, 1))
    d["swa_sink"] = np.ascontiguousarray(inp["swa_sink"][l])
    return d


def band_masks(r):
    ki = np.arange(128)[:, None]
    qi = np.arange(128)[None, :]
    mprev = (qi <= ki).astype(np.float32)
    mnext = (ki <= qi).astype(np.float32)
    m = np.stack([mprev, mnext, mprev * (0.0 if r == 0 else 1.0), mnext * (0.0 if r == 3 else 1.0)], 1)
    return np.ascontiguousarray(m.astype(np.float32))


KVR = 1280 + 1536


def build_fused():
    cx = Ctx()
    L = NS()
    rec = cx.rec
    nc = cx.nc
    setup_consts(cx, L)
    x_in = cx.din("x_own", [TOK, 1024])
    xc_in = cx.din("xc", [CTX, 1024])
    tab_src = cx.din("rope_tab", [TOK + CTX, 192])
    out = cx.dout("out", [TOK, 1024])
    x1 = cx.dint("x1", [TOK, 1024])
    xc1 = cx.dint("xc1", [CTX, 1024])
    groups = [[0, 1, 2, 3], [4, 5, 6, 7]]
    for l in range(DEPTH):
        cx.sfx = "_l%d" % l
        need_ctx = l < DEPTH - 1
        lst = ExitStack()
        st = ExitStack()
        L.modT = rec.sb("modT", [128, 48, 2], F32, lst)
        L.Gb = [[rec.sb("Gb", [128, 1024], F32, lst) for _ in range(2)] for _ in range(2)]
        wsrc = {"w_out": cx.din("w_out", [128, 8, 1024]), "wgu": cx.din("wgu", [NE, 128, 8, 512]), "wd": cx.din("wd", [NE, 128, 2, 1024])}
        wbf = {"w_out": cx.dint("w_out_bf%d" % l, [128, 8, 1024], BF16), "wgu": cx.dint("wgu_bf%d" % l, [NE, 128, 8, 512], BF16),
               "wd": cx.dint("wd_bf%d" % l, [NE, 128, 2, 1024], BF16)}
        layer_setup(cx, L, st, pst="prealloc")
        st.close()
        rec.barrier()
        rec.release_dma_sems()
        for ex in range(NE):
            rec.dma("pool", wbf["wgu"][ex, :, :, :].rearrange("p a b -> p (a b)"), wsrc["wgu"][ex, :, :, :].rearrange("p a b -> p (a b)"), key="D_wcast", cast=True)
            rec.dma("pool", wbf["wd"][ex, :, :, :].rearrange("p a b -> p (a b)"), wsrc["wd"][ex, :, :, :].rearrange("p a b -> p (a b)"), key="D_wcast", cast=True)
        rec.dma("pool", wbf["w_out"][:, :, :].rearrange("p a b -> p (a b)"), wsrc["w_out"][:, :, :].rearrange("p a b -> p (a b)"), key="D_wcast", cast=True)
        kv_own = nc.dram_tensor("kv_own%d" % l, [KVR, TOK], BF16).ap()
        ga = [Buf("ga", nc.dram_tensor("ga%d_%d" % (l, p_), [4 * 128, TOK], BF16).ap()) for p_ in range(22)]
        qT = cx.dint("qT%d" % l, [10, 128, TOK], BF16)
        qTc = cx.dint("qTc%d" % l, [10, 128, CTX], BF16)
        kTc = cx.dint("kTc%d" % l, [10, 128, CTX], BF16)
        vc = cx.dint("vc%d" % l, [12, 128, 2, 128], BF16)
        OT = cx.dint("OT%d" % l, [8, 128, TOK], BF16)
        OTc = cx.dint("OTc%d" % l, [8, 128, CTX], BF16)
        kT_own = Buf("kTown", kv_own[0:1280, :].rearrange("(c p) t -> c p t", p=128))
        v_own = Buf("vown", kv_own[1280:KVR, :].rearrange("(s p) (t d) -> s p t d", p=128, d=128))
        outs = {"qT": qT, "kT": kT_own, "v": v_own, "qTc": qTc, "kTc": kTc, "vc": vc}
        st = ExitStack()
        phase1(cx, L, st, x_in if l == 0 else x1, xc_in if l == 0 else xc1, tab_src, outs)
        st.close()
        rec.barrier()
        rec.release_dma_sems()
        order = [0, 10, 11, 1, 12, 13, 2, 14, 3, 15, 4, 16, 5, 17, 6, 18, 7, 19, 8, 20, 9, 21]
        for p_ in order:
            rec.collective_piece(lambda e, a=kv_own[p_ * 128:(p_ + 1) * 128, :], b=ga[p_]: e.collective_compute(
                "AllGather", ALU.bypass, replica_groups=groups, ins=[a], outs=[b[:, :]]), ga[p_])
        src = {"qT": qT, "ga": ga, "kTc": kTc, "vc": vc, "qTc": qTc, "kTown": kT_own, "vown": v_own, "OT": OT, "OTc": OTc}
        st = ExitStack()
        attention(cx, L, st, l, src, need_ctx)
        st.close()
        rec.barrier()
        rec.release_dma_sems()
        wts = {"w_out": wbf["w_out"], "wgu": wbf["wgu"], "wd": wbf["wd"], "bf16": True,
               "router_w": cx.din("router_w", [128, 8, 16]), "router_bias": cx.din("router_bias", [16]), "ln": cx.din("ln", [4, 1024])}
        xs = [(x_in if l == 0 else x1, OT, x1 if l == 0 else out, 0, TOK)]
        if need_ctx:
            xs.append((xc_in, OTc, xc1, 1, CTX))
        st = ExitStack()
        phase3(cx, L, st, xs, wts)
        st.close()
        rec.barrier()
        rec.release_dma_sems()
        lst.close()
    rec.wait_all_on("sp")
    rec.emit()
    return cx


def kernel_fused(inp):
    cores = list(range(NCORES))
    lws = [prep_layer(inp, l) for l in range(DEPTH)]
    bws = [prep_B_weights(inp, l) for l in range(DEPTH)]
    shared_w = {"router_w": bws[0]["router_w"], "router_bias": bws[0]["router_bias"]}
    in_maps = []
    for c in cores:
        b, r = c // 4, c % 4
        m = dict(prep_core_common(inp, c))
        m.update(shared_w)
        for l in range(DEPTH):
            for k, v in list(lws[l].items()) + list(bws[l].items()):
                if k not in shared_w:
                    m["%s_l%d" % (k, l)] = v
        m["x_own"] = np.ascontiguousarray(inp["x"][b, r * TOK:(r + 1) * TOK])
        m["xc"] = np.ascontiguousarray(inp["ctx"][b])
        m["c_masks"] = band_masks(r)
        oh = np.zeros((128, 8), np.float32)
        if r > 0:
            oh[:, r - 1] = 1.0
        if r < 3:
            oh[:, 4 + r + 1] = 1.0
        m["onehot"] = oh
        in_maps.append(m)
    prog = _prog("fused", build_fused)
    names = set(prog.dram.keys())
    in_maps = [{k: v for k, v in m.items() if k in names} for m in in_maps]
    res = run_bass_kernel_spmd(prog.nc, in_maps, core_ids=cores).results
    out = np.zeros((BATCH, SEQ, D), np.float32)
    for c in cores:
        out[c // 4, (c % 4) * TOK:(c % 4 + 1) * TOK] = res[c]["out"]
    return out


_PROGS = {}


def _prog(key, fn):
    if key not in _PROGS:
        _PROGS[key] = fn()
    return _PROGS[key]


def kernel(**inputs):
    inp = {k: np.asarray(v) for k, v in inputs.items()}
    return kernel_fused(inp)


def kernel_unfused(**inputs):
    inp = {k: np.asarray(v) for k, v in inputs.items()}
    cores = list(range(NCORES))
    common = [prep_core_common(inp, c) for c in cores]
    x_cur = [np.ascontiguousarray(inp["x"][c // 4, (c % 4) * TOK:(c % 4 + 1) * TOK]) for c in cores]
    xc_cur = [np.ascontiguousarray(inp["ctx"][c // 4]) for c in cores]
    for l in range(DEPTH):
        need_ctx = l < DEPTH - 1
        lw = prep_layer(inp, l)
        pa = _prog("A", build_A)
        in_maps = []
        for c in cores:
            m = dict(lw)
            m.update(common[c])
            m["x_own"] = x_cur[c]
            m["xc"] = xc_cur[c]
            in_maps.append(m)
        ra = run_bass_kernel_spmd(pa.nc, in_maps, core_ids=cores).results
        bw = prep_B_weights(inp, l)
        in_maps = []
        for c in cores:
            b, r = c // 4, c % 4
            grp = [ra[b * 4 + i] for i in range(4)]
            m = {}
            for k in ("w_ada", "b_adaT", "b_ada"):
                m[k] = lw[k]
            m["cT"] = common[c]["cT"]
            m["c_ident"] = common[c]["c_ident"]
            m.update(bw)
            m["qT"] = ra[c]["qT"]
            m["kTc"] = ra[c]["kTc"]
            m["vc"] = ra[c]["vc"]
            m["qTc"] = ra[c]["qTc"]
            m["kTall"] = np.ascontiguousarray(np.stack([g["kT"] for g in grp]))
            m["vall"] = np.ascontiguousarray(np.stack([g["v"] for g in grp]))
            kfull = np.concatenate([g["kT"][8:10] for g in grp], axis=2)
            kpad = np.zeros((2, 128, SEQ + 256), kfull.dtype)
            kpad[:, :, 128:128 + SEQ] = kfull
            m["kTwin"] = np.ascontiguousarray(kpad[:, :, r * TOK:r * TOK + NWIN * 128])
            vfull = np.concatenate([g["v"][10:12] for g in grp], axis=2)
            vpad = np.zeros((2, 128, 4 * NT + 2, 128), vfull.dtype)
            vpad[:, :, 1:1 + 4 * NT] = vfull
            m["vwin"] = np.ascontiguousarray(vpad[:, :, r * NT:r * NT + NWIN])
            m["c_masks"] = band_masks(r)
            m["x_own"] = x_cur[c]
            if need_ctx:
                m["xc"] = xc_cur[c]
            in_maps.append(m)
        pb = _prog(("B", l), lambda: build_B(l, need_ctx))
        rb = run_bass_kernel_spmd(pb.nc, in_maps, core_ids=cores).results
        x_cur = [np.ascontiguousarray(rb[c]["x_next"]) for c in cores]
        if need_ctx:
            xc_cur = [np.ascontiguousarray(rb[c]["xc_next"]) for c in cores]
    out = np.zeros((BATCH, SEQ, D), np.float32)
    for c in cores:
        out[c // 4, (c % 4) * TOK:(c % 4 + 1) * TOK] = x_cur[c]
    return out
```
